# Optimizing a Trainium2 kernel written in Bass

```python
import math
import jax, jax.numpy as jnp
from jax import lax
import numpy as np

D_MODEL = 1024
BATCH = 8
SEQ = 4096
DEPTH = 1
DEC_BATCH = 128
DEC_SEQ = 4
PAST_LEN = 8192
PAGE_SIZE = 128

HEAD_DIM = 64
N_HEADS_ATTN = 8
N_HEADS_RWKV = 8
C_ATTN = N_HEADS_ATTN * HEAD_DIM
C_RWKV = N_HEADS_RWKV * HEAD_DIM
DILATIONS = ((128, 1), (512, 4), (2048, 16))
MAX_WINDOW = 2048
N_BUCKETS = 32
MAX_DISTANCE = 2048
LORA_DECAY = 32
LORA_ICLR = 32
LORA_GATE = 64
COLS_RWKV = 3 * C_RWKV + LORA_DECAY + LORA_ICLR + LORA_GATE
D_IN = 3 * C_ATTN + COLS_RWKV
PEER_HEADS = 8
PEER_KEYS = 128
PEER_EXPERTS = PEER_KEYS * PEER_KEYS
PEER_QDIM = 256
PEER_HALF = PEER_QDIM // 2
PEER_TOPK = 16
PEER_CHUNK = 256
NORM_EPS = 1e-6
GN_EPS = 64e-5
NEG_INF = -1e30
ATTN_SCALE = HEAD_DIM ** -0.5

kernel_name = 'hymba_dilated_rwkv7_peer_step'


def _rms(x, g):
    x32 = x.astype(jnp.float32)
    return x32 * lax.rsqrt(jnp.mean(x32 * x32, axis=-1, keepdims=True) + NORM_EPS) * g


def _t5_bucket(dist):
    dist = np.asarray(dist, dtype=np.int64)
    max_exact = N_BUCKETS // 2
    safe = np.maximum(dist, 1) / max_exact
    large = max_exact + (np.log(safe) / math.log(MAX_DISTANCE / max_exact) * (N_BUCKETS - max_exact)).astype(np.int64)
    large = np.minimum(large, N_BUCKETS - 1)
    return np.where(dist < max_exact, dist, large).astype(np.int32)


def _branch_prompt(q, k, v, rel_bias, window, dil):
    B, T, H, C = q.shape
    n = window // dil
    L = T + (-T) % window
    G = L // window

    def blocks(a):
        a = jnp.pad(a, ((0, 0), (0, L - T), (0, 0), (0, 0)))
        a = a.reshape(B, L // dil, dil, H, C).transpose(0, 2, 1, 3, 4)
        return a.reshape(B, dil, G, n, H, C)

    def with_prev(a):
        prev = jnp.pad(a[:, :, :-1], ((0, 0), (0, 0), (1, 0), (0, 0), (0, 0), (0, 0)))
        return jnp.concatenate([prev, a], axis=3)

    qb = blocks(q)
    kb = with_prev(blocks(k))
    vb = with_prev(blocks(v))
    qi = np.arange(n)[:, None]
    ki = np.arange(2 * n)[None, :]
    j = n + qi - ki
    band = (j >= 0) & (j <= n)
    pad_key = (np.arange(G) == 0)[:, None, None] & (ki < n)[None]
    mask = band[None] & ~pad_key
    bias = rel_bias[_t5_bucket(np.clip(j, 0, n) * dil)].transpose(2, 0, 1).astype(jnp.float32)
    logits = jnp.einsum('brgqhc,brgshc->brghqs', qb, kb).astype(jnp.float32) * ATTN_SCALE + bias
    logits = jnp.where(mask[None, None, :, None], logits, NEG_INF)
    m = jnp.max(logits, axis=-1, keepdims=True)
    e = jnp.exp(logits - m)
    s = jnp.sum(e, axis=-1)
    o = jnp.einsum('brghqs,brgshc->brgqhc', e, vb.astype(jnp.float32)) / s.transpose(0, 1, 2, 4, 3)[..., None]
    lse = (m[..., 0] + jnp.log(s)).transpose(0, 1, 2, 4, 3)
    o = o.reshape(B, dil, L // dil, H, C).transpose(0, 2, 1, 3, 4).reshape(B, L, H, C)[:, :T]
    lse = lse.reshape(B, dil, L // dil, H).transpose(0, 2, 1, 3).reshape(B, L, H)[:, :T]
    return o, lse


def _branch_sample(q, k_all, v_all, rel_bias, window, dil, lb):
    S = q.shape[1]
    n = window // dil
    jj = np.arange(n + 1)
    idx = lb + np.arange(S)[:, None] - jj[None, :] * dil
    valid = idx >= 0
    idx_c = np.maximum(idx, 0)
    ks = k_all[:, idx_c].astype(jnp.float32)
    vs = v_all[:, idx_c].astype(jnp.float32)
    bias = rel_bias[_t5_bucket(jj * dil)].T.astype(jnp.float32)
    logits = jnp.einsum('bqhc,bqjhc->bhqj', q, ks).astype(jnp.float32) * ATTN_SCALE + bias[None, :, None, :]
    logits = jnp.where(valid[None, None], logits, NEG_INF)
    m = jnp.max(logits, axis=-1, keepdims=True)
    e = jnp.exp(logits - m)
    s = jnp.sum(e, axis=-1)
    o = jnp.einsum('bhqj,bqjhc->bqhc', e, vs) / s.transpose(0, 2, 1)[..., None]
    lse = (m[..., 0] + jnp.log(s)).transpose(0, 2, 1)
    return o, lse


def _merge_branches(outs, lses):
    wts = jax.nn.softmax(jnp.stack(lses), axis=0)
    return jnp.einsum('ibth,ibthc->bthc', wts, jnp.stack(outs))


def _dilated_attn_prompt(q, k, v, rel_bias):
    outs, lses = [], []
    for window, dil in DILATIONS:
        o, lse = _branch_prompt(q, k, v, rel_bias, window, dil)
        outs.append(o)
        lses.append(lse)
    return _merge_branches(outs, lses)


def _dilated_attn_sample(q, k_new, v_new, k_buf, v_buf, rel_bias):
    lb = k_buf.shape[1]
    k_all = jnp.concatenate([k_buf.astype(jnp.float32), k_new.astype(jnp.float32)], axis=1)
    v_all = jnp.concatenate([v_buf.astype(jnp.float32), v_new.astype(jnp.float32)], axis=1)
    outs, lses = [], []
    for window, dil in DILATIONS:
        o, lse = _branch_sample(q, k_all, v_all, rel_bias, window, dil, lb)
        outs.append(o)
        lses.append(lse)
    return _merge_branches(outs, lses)


def _wkv_scan(r, w, k, v, kk, a, s0):
    def step(S, inp):
        r_t, w_t, k_t, v_t, kk_t, a_t = inp
        sa = jnp.einsum('bhvk,bhk->bhv', S, -kk_t)
        S = S * w_t[:, :, None, :] + sa[..., None] * (kk_t * a_t)[:, :, None, :] + v_t[..., None] * k_t[:, :, None, :]
        return S, jnp.einsum('bhvk,bhk->bhv', S, r_t)
    xs = tuple(jnp.moveaxis(t, 1, 0) for t in (r, w, k, v, kk, a))
    s_fin, y = lax.scan(step, s0, xs)
    return jnp.moveaxis(y, 0, 1), s_fin


def _rwkv_mixer(pb, shift0, s0, mu, w0, w_w2, a0, w_a2, w_g2, k_k, k_a, r_k, lnx_g, lnx_b):
    B, T, _ = pb.shape
    pb = pb.astype(jnp.float32)
    prev = jnp.concatenate([shift0[:, None, :].astype(jnp.float32), pb[:, :-1]], axis=1)
    xm = pb + (prev - pb) * mu
    c = C_RWKV
    r, k, v = xm[..., :c], xm[..., c:2 * c], xm[..., 2 * c:3 * c]
    o = 3 * c
    wl = xm[..., o:o + LORA_DECAY]
    al = xm[..., o + LORA_DECAY:o + LORA_DECAY + LORA_ICLR]
    gl = xm[..., o + LORA_DECAY + LORA_ICLR:]
    w_log = -jax.nn.softplus(-(w0 + jnp.tanh(wl) @ w_w2)) - 0.5
    decay = jnp.exp(-jnp.exp(w_log))
    a = jax.nn.sigmoid(a0 + al @ w_a2)
    g = jax.nn.sigmoid(gl) @ w_g2

    def heads(t):
        return t.reshape(B, T, N_HEADS_RWKV, HEAD_DIM)
    kk = heads(k * k_k)
    kk = kk / jnp.maximum(jnp.sqrt(jnp.sum(kk * kk, axis=-1, keepdims=True)), 1e-12)
    k = k * (1.0 + (a - 1.0) * k_a)
    r_h, k_h, v_h = heads(r), heads(k), heads(v)
    y, s_fin = _wkv_scan(r_h, heads(decay), k_h, v_h, kk, heads(a), s0.astype(jnp.float32))
    mean = jnp.mean(y, axis=-1, keepdims=True)
    var = jnp.mean(jnp.square(y - mean), axis=-1, keepdims=True)
    y = ((y - mean) * lax.rsqrt(var + GN_EPS)).reshape(B, T, C_RWKV) * lnx_g + lnx_b
    bonus = jnp.sum(r_h * k_h * r_k, axis=-1, keepdims=True) * v_h
    y = (y + bonus.reshape(B, T, C_RWKV)) * g
    return y, s_fin, pb[:, -1]


def _peer(h, w_pq, sub_keys, expert_u, expert_v):
    n_tok = h.shape[0]
    chunk = min(PEER_CHUNK, n_tok)
    pad = (-n_tok) % chunk
    hc = jnp.pad(h, ((0, pad), (0, 0))).reshape(-1, chunk, D_MODEL)

    def one(hb):
        q = (hb @ w_pq).reshape(chunk, PEER_HEADS, 2, PEER_HALF)
        s = jnp.einsum('nhpc,hpkc->nhpk', q, sub_keys).astype(jnp.float32)
        s1, i1 = lax.top_k(s[:, :, 0], PEER_TOPK)
        s2, i2 = lax.top_k(s[:, :, 1], PEER_TOPK)
        cand = (s1[..., :, None] + s2[..., None, :]).reshape(chunk, PEER_HEADS, PEER_TOPK * PEER_TOPK)
        cid = (i1[..., :, None] * PEER_KEYS + i2[..., None, :]).reshape(chunk, PEER_HEADS, PEER_TOPK * PEER_TOPK)
        top_s, pos = lax.top_k(cand, PEER_TOPK)
        eid = jnp.take_along_axis(cid, pos, axis=-1)
        gate = jax.nn.softmax(top_s, axis=-1)
        act = jax.nn.gelu(jnp.einsum('nd,nhkd->nhk', hb, expert_u[eid]).astype(jnp.float32), approximate=False)
        return jnp.einsum('nhk,nhkd->nd', gate * act, expert_v[eid].astype(jnp.float32))

    return lax.map(one, hc).reshape(-1, D_MODEL)[:n_tok]


def _layer(x, c, k_buf, v_buf, wkv0, shift0, rel_bias, p):
    B, T, _ = x.shape
    mod = jax.nn.silu(c.astype(jnp.float32)) @ p['ada_w'] + p['ada_b']
    sh1, sc1, g1, sh2, sc2, g2 = jnp.split(mod[:, None, :], 6, axis=-1)
    h = _rms(x, p['norm1_g']) * (1.0 + sc1) + sh1
    proj = h @ p['w_in']
    q = _rms(proj[..., :C_ATTN].reshape(B, T, N_HEADS_ATTN, HEAD_DIM), p['q_norm_g'])
    k = _rms(proj[..., C_ATTN:2 * C_ATTN].reshape(B, T, N_HEADS_ATTN, HEAD_DIM), p['k_norm_g'])
    v = proj[..., 2 * C_ATTN:3 * C_ATTN].reshape(B, T, N_HEADS_ATTN, HEAD_DIM).astype(jnp.float32)
    if k_buf is None:
        o_attn = _dilated_attn_prompt(q, k, v, rel_bias)
        keep = min(MAX_WINDOW, T)
        k_rows, v_rows = k[:, T - keep:], v[:, T - keep:]
        wkv0 = jnp.zeros((B, N_HEADS_RWKV, HEAD_DIM, HEAD_DIM), jnp.float32)
        shift0 = jnp.zeros((B, COLS_RWKV), jnp.float32)
    else:
        o_attn = _dilated_attn_sample(q, k, v, k_buf, v_buf, rel_bias)
        k_rows, v_rows = k, v
    y_rwkv, s_fin, shift_new = _rwkv_mixer(
        proj[..., 3 * C_ATTN:], shift0, wkv0, p['mu_shift'], p['w0'], p['w_w2'], p['a0'], p['w_a2'],
        p['w_g2'], p['k_k'], p['k_a'], p['r_k'], p['lnx_g'], p['lnx_b'])
    mix = jnp.concatenate([o_attn.reshape(B, T, C_ATTN), y_rwkv], axis=-1) @ p['w_out']
    x = x + g1 * mix
    h2 = _rms(x, p['norm2_g']) * (1.0 + sc2) + sh2
    ffn = _peer(h2.reshape(B * T, D_MODEL), p['w_peer_q'], p['peer_sub_keys'], p['expert_u'], p['expert_v'])
    x = x + g2 * ffn.reshape(B, T, D_MODEL)
    return x, k_rows, v_rows, s_fin, shift_new


def setup_inputs(seed: int = 0) -> dict:
    key = jax.random.key(seed)
    ks = jax.random.split(key, 32)
    f32 = jnp.float32

    def nrm(i, shape, scale):
        return jax.random.normal(ks[i], shape, f32) * scale

    def unif(i, shape, lo, hi):
        return jax.random.uniform(ks[i], shape, f32, lo, hi)

    win_buf = min(MAX_WINDOW, PAST_LEN)
    d = D_MODEL
    return {
        'x_prompt': nrm(0, (BATCH, SEQ, d), 1.0),
        'x_sample': nrm(1, (DEC_BATCH, DEC_SEQ, d), 1.0),
        'c_prompt': nrm(2, (BATCH, d), 1.0),
        'c_sample': nrm(3, (DEC_BATCH, d), 1.0),
        'cache_k_win': nrm(4, (DEPTH, DEC_BATCH, win_buf, N_HEADS_ATTN, HEAD_DIM), 1.0),
        'cache_v_win': nrm(5, (DEPTH, DEC_BATCH, win_buf, N_HEADS_ATTN, HEAD_DIM), 1.0),
        'state_wkv': nrm(6, (DEPTH, DEC_BATCH, N_HEADS_RWKV, HEAD_DIM, HEAD_DIM), 0.5),
        'state_shift': nrm(7, (DEPTH, DEC_BATCH, COLS_RWKV), 1.0),
        'ada_w': nrm(8, (DEPTH, d, 6 * d), 0.5 * d ** -0.5),
        'ada_b': nrm(9, (DEPTH, 6 * d), 0.02),
        'norm1_g': 1.0 + nrm(10, (DEPTH, d), 0.02),
        'norm2_g': 1.0 + nrm(11, (DEPTH, d), 0.02),
        'w_in': nrm(12, (DEPTH, d, D_IN), d ** -0.5),
        'q_norm_g': 1.0 + nrm(13, (DEPTH, HEAD_DIM), 0.02),
        'k_norm_g': 1.0 + nrm(14, (DEPTH, HEAD_DIM), 0.02),
        'rel_bias': nrm(15, (N_BUCKETS, N_HEADS_ATTN), 0.5),
        'mu_shift': unif(16, (DEPTH, COLS_RWKV), 0.0, 1.0),
        'w0': unif(17, (DEPTH, C_RWKV), -5.0, 1.0),
        'w_w2': nrm(18, (DEPTH, LORA_DECAY, C_RWKV), 0.1),
        'a0': nrm(19, (DEPTH, C_RWKV), 0.5),
        'w_a2': nrm(20, (DEPTH, LORA_ICLR, C_RWKV), 0.1),
        'w_g2': nrm(21, (DEPTH, LORA_GATE, C_RWKV), LORA_GATE ** -0.5),
        'k_k': 0.85 + nrm(22, (DEPTH, C_RWKV), 0.05),
        'k_a': 1.0 + nrm(23, (DEPTH, C_RWKV), 0.05),
        'r_k': nrm(24, (DEPTH, N_HEADS_RWKV, HEAD_DIM), 0.1),
        'lnx_g': 1.0 + nrm(25, (DEPTH, C_RWKV), 0.02),
        'lnx_b': nrm(26, (DEPTH, C_RWKV), 0.02),
        'w_out': nrm(27, (DEPTH, d, d), d ** -0.5),
        'w_peer_q': nrm(28, (DEPTH, d, PEER_HEADS * PEER_QDIM), d ** -0.5),
        'peer_sub_keys': nrm(29, (DEPTH, PEER_HEADS, 2, PEER_KEYS, PEER_HALF), PEER_HALF ** -0.5),
        'expert_u': nrm(30, (DEPTH, PEER_EXPERTS, d), d ** -0.5),
        'expert_v': nrm(31, (DEPTH, PEER_EXPERTS, d), PEER_HEADS ** -0.5),
    }


def reference(x_prompt, x_sample, c_prompt, c_sample, cache_k_win, cache_v_win, state_wkv, state_shift,
              ada_w, ada_b, norm1_g, norm2_g, w_in, q_norm_g, k_norm_g, rel_bias, mu_shift, w0, w_w2, a0,
              w_a2, w_g2, k_k, k_a, r_k, lnx_g, lnx_b, w_out, w_peer_q, peer_sub_keys, expert_u, expert_v):
    xp, xs = x_prompt, x_sample
    kp_l, vp_l, sp_l, hp_l = [], [], [], []
    ks_l, vs_l, ss_l, hs_l = [], [], [], []
    for l in range(DEPTH):
        p = {
            'ada_w': ada_w[l], 'ada_b': ada_b[l], 'norm1_g': norm1_g[l], 'norm2_g': norm2_g[l],
            'w_in': w_in[l], 'q_norm_g': q_norm_g[l], 'k_norm_g': k_norm_g[l], 'mu_shift': mu_shift[l],
            'w0': w0[l], 'w_w2': w_w2[l], 'a0': a0[l], 'w_a2': w_a2[l], 'w_g2': w_g2[l], 'k_k': k_k[l],
            'k_a': k_a[l], 'r_k': r_k[l], 'lnx_g': lnx_g[l], 'lnx_b': lnx_b[l], 'w_out': w_out[l],
            'w_peer_q': w_peer_q[l], 'peer_sub_keys': peer_sub_keys[l], 'expert_u': expert_u[l],
            'expert_v': expert_v[l],
        }
        xp, kp, vp, sp, hp = _layer(xp, c_prompt, None, None, None, None, rel_bias, p)
        xs, kn, vn, sn, hn = _layer(xs, c_sample, cache_k_win[l], cache_v_win[l], state_wkv[l],
                                    state_shift[l], rel_bias, p)
        kp_l.append(kp); vp_l.append(vp); sp_l.append(sp); hp_l.append(hp)
        ks_l.append(kn); vs_l.append(vn); ss_l.append(sn); hs_l.append(hn)
    return (xp, xs, jnp.stack(kp_l), jnp.stack(vp_l), jnp.stack(sp_l), jnp.stack(hp_l),
            jnp.stack(ks_l), jnp.stack(vs_l), jnp.stack(ss_l), jnp.stack(hs_l))
```

```python
import contextlib
import numpy as np
import concourse.bass as bass
import concourse.mybir as mybir
from concourse.bass_utils import run_bass_kernel_spmd

F32 = mybir.dt.float32
BF16 = mybir.dt.bfloat16
I32 = mybir.dt.int32
U32 = mybir.dt.uint32
AF = mybir.ActivationFunctionType
ALU = mybir.AluOpType
AX = mybir.AxisListType

N_DMA_SLOTS = 24
NCORES = 8
D = 1024
T = 4096
NS = 64
NT = T + NS
SB = 16
DIN = 3200
CR = 1664
EPS = 1e-6


class Tok:
    __slots__ = ("lw", "rd", "rd_dma", "name", "excl")

    def __init__(self, name="", excl=False):
        self.excl = excl
        self.lw = None
        self.rd = {}
        self.rd_dma = []
        self.name = name


class Sched:
    ENGS = ("pe", "act", "dve", "pool", "sp")

    def __init__(self, nc):
        self.nc = nc
        self.ins = []
        self.last_by_eng = {}
        self.dmas_since = []

    def barrier(self):
        deps = set(self.last_by_eng.values()) | set(self.dmas_since)
        self.dmas_since = []
        for e in self.ENGS:
            idx = len(self.ins)
            self.ins.append([e, (lambda eh: eh.nop()), set(deps), False, False, 0, 0])
            self.last_by_eng[e] = idx

    def op(self, eng, fn, reads=(), writes=(), dma=False):
        idx = len(self.ins)
        deps = set()
        for t in reads:
            if t.lw is not None:
                deps.add(t.lw)
            if t.excl:
                deps.update(v for kk_, v in t.rd.items() if kk_ != eng)
        for t in writes:
            if t.lw is not None:
                deps.add(t.lw)
            deps.update(t.rd.values())
            deps.update(t.rd_dma)
        for t in reads:
            if dma:
                t.rd_dma.append(idx)
            else:
                t.rd[eng] = idx
        for t in writes:
            t.lw = idx
            t.rd = {}
            t.rd_dma = []
        deps.discard(idx)
        self.ins.append([eng, fn, deps, dma, False, 0, 0])
        if dma:
            self.dmas_since.append(idx)
        else:
            self.last_by_eng[eng] = idx
        return idx

    def pe(self, fn, reads=(), writes=()):
        return self.op("pe", fn, reads, writes)

    def act(self, fn, reads=(), writes=()):
        return self.op("act", fn, reads, writes)

    def dve(self, fn, reads=(), writes=()):
        return self.op("dve", fn, reads, writes)

    def pool(self, fn, reads=(), writes=()):
        return self.op("pool", fn, reads, writes)

    def dma(self, fn, reads=(), writes=(), eng="sp"):
        return self.op(eng, fn, reads, writes, dma=True)

    def emit(self):
        nc = self.nc
        ins = self.ins
        for it in ins:
            for d in it[2]:
                ins[d][4] = True
        last = {}
        for i, it in enumerate(ins):
            if not it[3]:
                last[it[0]] = i
        for i in last.values():
            ins[i][4] = True
        cnt = {e: 0 for e in self.ENGS}
        dma_n = {e: 0 for e in self.ENGS}
        for it in ins:
            e = it[0]
            if it[3]:
                it[4] = True
                it[6] = dma_n[e]
                dma_n[e] += 1
            elif it[4]:
                cnt[e] += 1
                it[5] = cnt[e]
        with contextlib.ExitStack() as st:
            sems = {e: st.enter_context(nc.semaphore("s_" + e)) for e in self.ENGS}
            dsems = {e: [st.enter_context(nc.semaphore("d_%s_%d" % (e, i))) for i in range(N_DMA_SLOTS)]
                     for e in self.ENGS if dma_n[e] > 0}
            block = st.enter_context(nc.Block())
            per_eng = {e: [] for e in self.ENGS}
            for i, it in enumerate(ins):
                per_eng[it[0]].append(i)

            def run(eng_name, eh):
                seen = {e: 0 for e in self.ENGS}
                seen_dma = {}
                for i in per_eng[eng_name]:
                    e, fn, deps, is_dma, sig, c, slot = ins[i]
                    need = {}
                    for d in deps:
                        de, _, _, ddma, _, dc, dslot = ins[d]
                        if ddma:
                            key = (de, dslot % N_DMA_SLOTS)
                            val = 16 * (dslot // N_DMA_SLOTS + 1)
                            if seen_dma.get(key, 0) < val:
                                seen_dma[key] = val
                                need[("d",) + key] = val
                        else:
                            if seen[de] < dc:
                                seen[de] = dc
                                need[("c", de)] = dc
                    if is_dma and slot >= N_DMA_SLOTS:
                        key = (e, slot % N_DMA_SLOTS)
                        val = 16 * (slot // N_DMA_SLOTS)
                        if seen_dma.get(key, 0) < val:
                            seen_dma[key] = val
                            need[("d",) + key] = val
                    for k, v in need.items():
                        if k[0] == "c":
                            eh.wait_ge(sems[k[1]], v)
                        else:
                            eh.wait_ge(dsems[k[1]][k[2]], v)
                    inst = fn(eh)
                    if is_dma:
                        inst.then_inc(dsems[e][slot % N_DMA_SLOTS], 16)
                    elif sig:
                        inst.then_inc(sems[e], 1)
                if eng_name == "sp":
                    for e2 in self.ENGS:
                        if cnt[e2] > 0:
                            eh.wait_ge(sems[e2], cnt[e2])
                        n = dma_n.get(e2, 0)
                        for s in range(min(N_DMA_SLOTS, n)):
                            lastslot = ((n - 1 - s) // N_DMA_SLOTS) * N_DMA_SLOTS + s
                            eh.wait_ge(dsems[e2][s], 16 * (lastslot // N_DMA_SLOTS + 1))

            @block.tensor
            def _(eh):
                run("pe", eh)

            @block.scalar
            def _(eh):
                run("act", eh)

            @block.vector
            def _(eh):
                run("dve", eh)

            @block.gpsimd
            def _(eh):
                run("pool", eh)

            @block.sync
            def _(eh):
                run("sp", eh)


class Ctx:
    pass


def bc(ap, shape):
    return ap.to_broadcast(list(shape))


def build(phase_limit=99, debug=False):
    nc = bass.Bass("TRN2", target_bir_lowering=False)
    S = Sched(nc)
    K = Ctx()
    K.nc, K.S = nc, S

    def din(name, shape, dt=F32):
        return nc.dram_tensor(name, list(shape), dt, kind="ExternalInput").ap()

    def dout(name, shape, dt=F32):
        return nc.dram_tensor(name, list(shape), dt, kind="ExternalOutput").ap()

    I = {}
    I["xT"] = din("xT", [D, NT])
    I["cT"] = din("cT", [128, 8, 17])
    I["ada_w"] = din("ada_w", [D, 6 * D])
    I["ada_bT"] = din("ada_bT", [128, 48])
    I["n1gT"] = din("n1gT", [128, 8])
    I["n2gT"] = din("n2gT", [128, 8])
    I["w_in"] = din("w_in", [D, DIN])
    I["qkg"] = din("qkg", [128, 2])
    I["ident"] = din("ident", [128, 128])
    I["ones"] = din("ones", [128, 128])
    I["blk2"] = din("blk2", [128, 128])
    I["relb"] = din("relb", [32, 8])
    I["ohu"] = din("ohu", [32, 3, 384])
    I["w_out"] = din("w_out", [D, D])
    I["w_pq"] = din("w_pq", [D, 2048])
    I["skT"] = din("skT", [128, 16, 128])
    I["euT"] = din("euT", [D, 16384])
    I["ev"] = din("ev", [16384, D])
    I["iotaR"] = din("iotaR", [128, 128])
    euT_d = nc.dram_tensor("euT_d", [D, 16384], BF16, kind="Internal").ap()
    ev_d = nc.dram_tensor("ev_d", [16384, D], BF16, kind="Internal").ap()
    wpq_d = nc.dram_tensor("wpq_d", [D, 2048], BF16, kind="Internal").ap()
    I["ck_s"] = din("ck_s", [SB, 2048, 512])
    I["cv_s"] = din("cv_s", [SB, 2048, 512])
    I["swkv"] = din("swkv", [128, 4096])
    I["shT"] = din("shT", [128, 3, 4, 16])
    I["shTl"] = din("shTl", [128, 16])
    I["qkrow"] = din("qkrow", [2, 64])
    I["ohs"] = din("ohs", [32, 3, 129])
    rws_d = nc.dram_tensor("rws_d", [SB, 8, 4, 6, 64], F32, kind="Internal").ap()
    ys_d = nc.dram_tensor("ys_d", [SB, 8, 4, 64], F32, kind="Internal").ap()
    reck_d = nc.dram_tensor("reck_d", [SB, 8, 512], F32, kind="Internal").ap()
    recv_d = nc.dram_tensor("recv_d", [SB, 8, 512], F32, kind="Internal").ap()
    I["rwp"] = din("rwp", [128, 8, 4])
    I["mul"] = din("mul", [128, 1])
    I["lw3"] = din("lw3", [128, 512])
    I["w0row"] = din("w0row", [1, 512])
    I["lnx"] = din("lnx", [2, 512])
    I["tri"] = din("tri", [128, 2, 128])
    I["mk1"] = din("mk1", [128, 2, 64])
    I["mk3"] = din("mk3", [128, 64])
    I["id2"] = din("id2", [128, 64])
    zscr = nc.dram_tensor("zscr", [24, 256, 384], F32, kind="Internal").ap()
    mixT_d = nc.dram_tensor("mixT_d", [8, 128, NT], BF16, kind="Internal").ap()
    O = {}
    O["kwp"] = dout("kwp", [2048, 512])
    O["vwp"] = dout("vwp", [2048, 512])
    O["kws"] = dout("kws", [NS, 512])
    O["vws"] = dout("vws", [NS, 512])
    O["shp"] = dout("shp", [1, CR])
    O["shs"] = dout("shs", [SB, CR])
    O["wkvp"] = dout("wkvp", [128, 4, 64])
    O["y"] = dout("y", [NT, D])
    O["wkvs"] = dout("wkvs", [128, 4096])
    if debug:
        O["dbg_hT"] = dout("dbg_hT", [128, 8, NT], BF16)
        O["dbg_mod"] = dout("dbg_mod", [128, 48, 17])
        O["dbg_ebt"] = dout("dbg_ebt", [128, 24, 2, 128], BF16)
        O["dbg_mixT"] = dout("dbg_mixT", [4, 128, T], BF16)
        O["dbg_yr"] = dout("dbg_yr", [T, 512])
        O["dbg_IT"] = dout("dbg_IT", [3, 128, 256])

    st = contextlib.ExitStack()

    uid = [0]

    def sbt(stack, name, shape, dt=F32):
        uid[0] += 1
        return stack.enter_context(nc.sbuf_tensor("s%d_%s" % (uid[0], name), list(shape), dt))

    def sb(name, shape, dt=F32):
        return sbt(st, name, shape, dt)

    banks = [st.enter_context(nc.psum_tensor("bank%d" % i, [128, 512], F32)) for i in range(8)]
    bt = [Tok("bank%d" % i, excl=True) for i in range(8)]

    ident = sb("ident", [128, 128])
    ones = sb("ones", [128, 128])
    blk2 = sb("blk2", [128, 128])
    identb = sb("identb", [128, 128], BF16)
    epsc = sb("epsc", [128, 1])
    t_const = Tok("const")
    S.dma(lambda e: e.dma_start(out=ident[:], in_=I["ident"]), writes=[t_const])
    S.dma(lambda e: e.dma_start(out=ones[:], in_=I["ones"]), writes=[t_const])
    S.dma(lambda e: e.dma_start(out=blk2[:], in_=I["blk2"]), writes=[t_const])
    S.pool(lambda e: e.memset(epsc[:], EPS), writes=[t_const])
    S.dve(lambda e: e.tensor_copy(out=identb[:], in_=ident[:]), reads=[t_const], writes=[t_const])

    cT = sb("cT", [128, 8, 17])
    scT = sb("scT", [128, 8, 17])
    adab = sb("adab", [128, 48])
    n1g = sb("n1g", [128, 8])
    n2g = sb("n2g", [128, 8])
    qkg = sb("qkg", [128, 2])
    modT = sb("modT", [128, 48, 17])
    A1 = sb("A1", [128, 8, 17])
    A2 = sb("A2", [128, 8, 17])
    t_small = Tok("small")
    t_mod = Tok("mod")
    for dst, src in ((cT, "cT"), (adab, "ada_bT"), (n1g, "n1gT"), (n2g, "n2gT"), (qkg, "qkg")):
        S.dma(lambda e, dst=dst, src=src: e.dma_start(out=dst[:], in_=I[src]), writes=[t_small])
    S.act(lambda e: e.activation(out=scT[:], in_=cT[:], func=AF.Silu), reads=[t_small], writes=[t_small])
    adaw_v = I["ada_w"].rearrange("(c p) n -> p c n", p=128)
    with contextlib.ExitStack() as st2:
        wb = [sbt(st2, "adaw", [128, 8, 512]) for i in range(2)]
        wt = [Tok("adaw%d" % i) for i in range(2)]
        for nb in range(12):
            w = wb[nb % 2]
            wtk = wt[nb % 2]
            S.dma(lambda e, w=w, nb=nb: e.dma_start(out=w[:], in_=adaw_v[:, :, nb * 512:(nb + 1) * 512]), writes=[wtk])
            bk = nb % 2
            for oc in range(4):
                for c in range(8):
                    S.pe(lambda e, w=w, oc=oc, c=c, bk=bk: e.matmul(
                        banks[bk][:, oc * 32:oc * 32 + 17], lhsT=w[:, c, oc * 128:(oc + 1) * 128], rhs=scT[:, c, :],
                        start=(c == 0), stop=(c == 7)), reads=[wtk, t_small], writes=[bt[bk]])
            for oc in range(4):
                j = nb * 4 + oc
                S.act(lambda e, oc=oc, j=j, bk=bk: e.activation(
                    out=modT[:, j, :], in_=banks[bk][:, oc * 32:oc * 32 + 17], func=AF.Identity, bias=adab[:, j:j + 1]),
                    reads=[bt[bk], t_small], writes=[t_mod])
    S.barrier()
    S.dve(lambda e: e.tensor_scalar(out=A1[:], in0=modT[:, 8:16, :], scalar1=1.0, scalar2=None, op0=ALU.add),
          reads=[t_mod], writes=[t_mod])
    S.dve(lambda e: e.tensor_tensor(out=A1[:], in0=A1[:], in1=bc(n1g[:].unsqueeze(2), [128, 8, 17]), op=ALU.mult),
          reads=[t_mod, t_small], writes=[t_mod])
    S.dve(lambda e: e.tensor_scalar(out=A2[:], in0=modT[:, 32:40, :], scalar1=1.0, scalar2=None, op0=ALU.add),
          reads=[t_mod], writes=[t_mod])
    S.dve(lambda e: e.tensor_tensor(out=A2[:], in0=A2[:], in1=bc(n2g[:].unsqueeze(2), [128, 8, 17]), op=ALU.mult),
          reads=[t_mod, t_small], writes=[t_mod])
    if debug:
        S.dma(lambda e: e.dma_start(out=O["dbg_mod"], in_=modT[:]), reads=[t_mod])

    stH = contextlib.ExitStack()
    hT = sbt(stH, "hT", [128, 8, NT], BF16)
    groups = [(g * 512, 512) for g in range(8)] + [(T, NS)]
    t_h = [Tok("h%d" % g) for g in range(9)]
    xT_v = I["xT"].rearrange("(c p) n -> p c n", p=128)

    def norm_groups(src_v, dst, Aap, Bidx, t_dst, extra_reads=()):
        with contextlib.ExitStack() as st2:
            xg = [sbt(st2, "xg", [128, 8, 512]) for i in range(2)]
            xgt = [Tok() for i in range(2)]
            sq = sbt(st2, "sq", [128, 8, 512])
            sqt = Tok()
            rs = sbt(st2, "rs", [128, 512])
            rst = Tok()
            for g, (t0, n) in enumerate(groups):
                x_ = xg[g % 2]
                xt_ = xgt[g % 2]
                bk = 2 + g % 2
                S.dma(lambda e, x_=x_, t0=t0, n=n: e.dma_start(out=x_[:, :, 0:n], in_=src_v[:, :, t0:t0 + n]),
                      writes=[xt_], reads=list(extra_reads))
                S.act(lambda e, x_=x_, n=n: e.activation(out=sq[:, :, 0:n], in_=x_[:, :, 0:n], func=AF.Square),
                      reads=[xt_], writes=[sqt])
                for c in range(8):
                    S.pe(lambda e, c=c, n=n, bk=bk: e.matmul(banks[bk][:, 0:n], lhsT=ones[:], rhs=sq[:, c, 0:n],
                                                              start=(c == 0), stop=(c == 7)),
                         reads=[sqt, t_const], writes=[bt[bk]])
                S.act(lambda e, n=n, bk=bk: e.activation(out=rs[:, 0:n], in_=banks[bk][:, 0:n], func=AF.Sqrt,
                                                          scale=1.0 / D, bias=epsc[:]),
                      reads=[bt[bk], t_const], writes=[rst])
                S.dve(lambda e, n=n: e.reciprocal(out=rs[:, 0:n], in_=rs[:, 0:n]), reads=[rst], writes=[rst])
                S.dve(lambda e, x_=x_, n=n: e.tensor_tensor(out=x_[:, :, 0:n], in0=x_[:, :, 0:n],
                                                             in1=bc(rs[:, 0:n].unsqueeze(1), [128, 8, n]), op=ALU.mult),
                      reads=[xt_, rst], writes=[xt_])
                if g < 8:
                    for c in range(8):
                        eng = S.dve if c % 2 == 0 else S.pool
                        eng(lambda e, x_=x_, c=c, t0=t0, n=n: e.tensor_scalar(
                            out=dst[:, c, t0:t0 + n], in0=x_[:, c, 0:n], scalar1=Aap[:, c, 0:1],
                            scalar2=modT[:, Bidx + c, 0:1], op0=ALU.mult, op1=ALU.add),
                            reads=[xt_, t_mod], writes=[t_dst[g]])
                else:
                    xv = x_[:, :, 0:NS].rearrange("p c (b t) -> p c b t", t=4)
                    S.dve(lambda e, xv=xv: e.tensor_tensor(
                        out=xv, in0=xv, in1=bc(Aap[:, :, 1:17].unsqueeze(3), [128, 8, SB, 4]), op=ALU.mult),
                        reads=[xt_, t_mod], writes=[xt_])
                    S.dve(lambda e, xv=xv, t0=t0: e.tensor_tensor(
                        out=dst[:, :, t0:t0 + NS].rearrange("p c (b t) -> p c b t", t=4), in0=xv,
                        in1=bc(modT[:, Bidx:Bidx + 8, 1:17].unsqueeze(3), [128, 8, SB, 4]), op=ALU.add),
                        reads=[xt_, t_mod], writes=[t_dst[g]])

    norm_groups(xT_v, hT, A1, 0, t_h)
    S.barrier()
    if debug:
        S.dma(lambda e: e.dma_start(out=O["dbg_hT"], in_=hT[:]), reads=t_h)


    if phase_limit < 3:
        K.st = st
        S.emit()
        st.close()
        return nc
    stC = contextlib.ExitStack()
    EBT = sbt(stC, "EBT", [128, 24, 2, 128], BF16)
    onesb = sbt(stC, "onesb", [128, 64], BF16)
    t_ebt = Tok("ebt")
    S.dve(lambda e: e.tensor_copy(out=onesb[:], in_=ones[:, 0:64]), reads=[t_const], writes=[t_const])
    with contextlib.ExitStack() as st2:
        relb = sbt(st2, "relb", [32, 8])
        ohu = sbt(st2, "ohu", [32, 3, 384])
        RH = sbt(st2, "RH", [32, 8, 384])
        grep = [sbt(st2, "grep", [128, 384]) for i in range(2)]
        gt = [Tok(), Tok()]
        ebf = [sbt(st2, "ebf", [128, 2, 128]) for i in range(2)]
        et = [Tok(), Tok()]
        t_r = Tok()
        t_rh = Tok()
        S.dma(lambda e: e.dma_start(out=relb[:], in_=I["relb"]), writes=[t_r])
        S.dma(lambda e: e.dma_start(out=ohu[:], in_=I["ohu"]), writes=[t_r])
        S.act(lambda e: e.activation(out=relb[:], in_=relb[:], func=AF.Exp), reads=[t_r], writes=[t_r])
        zt = Tok()
        for br in range(3):
            S.dve(lambda e, br=br: e.tensor_tensor(out=RH[:], in0=bc(relb[:].unsqueeze(2), [32, 8, 384]),
                                                    in1=bc(ohu[:, br, :].unsqueeze(1), [32, 8, 384]), op=ALU.mult),
                  reads=[t_r], writes=[t_rh])
            for h in range(8):
                i = br * 8 + h
                bk = 6 + i % 2
                S.pe(lambda e, h=h, bk=bk: e.matmul(banks[bk][:, 0:384], lhsT=ones[0:32, :], rhs=RH[:, h, :],
                                                     start=True, stop=True), reads=[t_rh, t_const], writes=[bt[bk]])
                g_ = grep[i % 2]
                S.act(lambda e, g_=g_, bk=bk: e.activation(out=g_[:], in_=banks[bk][:, 0:384], func=AF.Copy),
                      reads=[bt[bk]], writes=[gt[i % 2]])
                zi = Tok()
                S.dma(lambda e, g_=g_, i=i: e.dma_start(out=zscr[i, 0:128, :], in_=g_[:]), reads=[gt[i % 2]], writes=[zi])
                S.dma(lambda e, g_=g_, i=i: e.dma_start(out=zscr[i, 128:256, :], in_=g_[:]), reads=[gt[i % 2]], writes=[zi])
                eb_ = ebf[i % 2]
                for part in range(2):
                    src = bass.AP(zscr.tensor, i * 256 * 384 + 255 + part * 128 * 383, [[383, 128], [1, 128]])
                    S.dma(lambda e, eb_=eb_, part=part, src=src: e.dma_start(out=eb_[:, part, :], in_=src),
                          reads=[zi], writes=[et[i % 2]])
                S.dve(lambda e, eb_=eb_, i=i: e.tensor_copy(out=EBT[:, i, :, :], in_=eb_[:]), reads=[et[i % 2]], writes=[t_ebt])
    S.barrier()
    if debug:
        S.dma(lambda e: e.dma_start(out=O["dbg_ebt"], in_=EBT[:]), reads=[t_ebt])

    win_v = I["w_in"].rearrange("(c p) n -> p c n", p=128)

    class _Stop(Exception):
        pass

    def ck(x):
        if phase_limit < x:
            raise _Stop()

    with contextlib.ExitStack() as st2, contextlib.suppress(_Stop):
        ck(3.5)
        wst = [sbt(st2, "wst", [128, 8, 128]) for i in range(2)]
        wstt = [Tok(), Tok()]
        wqkv = sbt(st2, "wqkv", [128, 8, 3, 128], BF16)
        t_w = Tok()
        qT = sbt(st2, "qT", [128, T], BF16)
        kT = sbt(st2, "kT", [128, T], BF16)
        t_q = [Tok() for g in range(8)]
        t_k = [Tok() for g in range(8)]
        V = sbt(st2, "V", [128, 3, 32, 128], BF16)
        t_v = [[Tok() for j in range(32)] for br in range(3)]
        accO = sbt(st2, "accO", [128, 2048])
        accS = sbt(st2, "accS", [128, 2048])
        t_acc = Tok()
        sq = sbt(st2, "sq", [128, 512])
        t_sq = Tok()
        rs = sbt(st2, "rs", [128, 512])
        t_rs = Tok()
        kTf = sbt(st2, "kTf", [128, 512])
        t_kf = Tok()
        ktok = [sbt(st2, "ktok", [128, 4, 128]) for i in range(2)]
        t_kt = [Tok(), Tok()]
        vtok = [sbt(st2, "vtok", [128, 4, 128]) for i in range(2)]
        t_vt = [Tok(), Tok()]
        e0 = [sbt(st2, "e0", [128, 512], BF16) for i in range(2)]
        t_e0 = [Tok(), Tok()]
        ee = [sbt(st2, "ee", [128, 512], BF16) for i in range(2)]
        t_ee = [Tok(), Tok()]
        mixo = sbt(st2, "mixo", [128, 2048], BF16)
        t_mixo = Tok()
        nld = 0
        nblk = 0
        for hp in range(4):
            for wi in range(3):
                w_ = wst[nld % 2]
                wt_ = wstt[nld % 2]
                nld += 1
                col = wi * 512 + hp * 128
                S.dma(lambda e, w_=w_, col=col: e.dma_start(out=w_[:], in_=win_v[:, :, col:col + 128]), writes=[wt_])
                S.pool(lambda e, w_=w_, wi=wi: e.tensor_copy(out=wqkv[:, :, wi, :], in_=w_[:]), reads=[wt_], writes=[t_w])
            for g in range(8):
                for wi, dstT, tks in ((0, qT, t_q), (1, kT, t_k)):
                    bk = 4 + wi
                    for c in range(8):
                        S.pe(lambda e, c=c, wi=wi, g=g, bk=bk: e.matmul(
                            banks[bk][:], lhsT=wqkv[:, c, wi, :], rhs=hT[:, c, g * 512:(g + 1) * 512],
                            start=(c == 0), stop=(c == 7)), reads=[t_w, t_h[g]], writes=[bt[bk]])
                    S.act(lambda e, bk=bk: e.activation(out=sq[:], in_=banks[bk][:], func=AF.Square),
                          reads=[bt[bk]], writes=[t_sq])
                    S.pe(lambda e: e.matmul(banks[6][:], lhsT=blk2[:], rhs=sq[:], start=True, stop=True),
                         reads=[t_sq, t_const], writes=[bt[6]])
                    S.act(lambda e: e.activation(out=rs[:], in_=banks[6][:], func=AF.Sqrt, scale=1.0 / 64, bias=epsc[:]),
                          reads=[bt[6], t_const], writes=[t_rs])
                    S.dve(lambda e: e.reciprocal(out=rs[:], in_=rs[:]), reads=[t_rs], writes=[t_rs])
                    S.dve(lambda e, bk=bk, wi=wi, g=g, dstT=dstT: e.scalar_tensor_tensor(
                        out=dstT[:, g * 512:(g + 1) * 512], in0=banks[bk][:], scalar=qkg[:, wi:wi + 1], in1=rs[:],
                        op0=ALU.mult, op1=ALU.mult), reads=[bt[bk], t_rs, t_small], writes=[tks[g]])
                    if wi == 1 and g >= 4:
                        S.dve(lambda e, bk=bk: e.scalar_tensor_tensor(
                            out=kTf[:], in0=banks[bk][:], scalar=qkg[:, 1:2], in1=rs[:], op0=ALU.mult, op1=ALU.mult),
                            reads=[bt[bk], t_rs, t_small], writes=[t_kf])
                        for j in range(4):
                            S.pe(lambda e, j=j: e.transpose(banks[7][:, j * 128:(j + 1) * 128], kTf[:, j * 128:(j + 1) * 128], ident[:]),
                                 reads=[t_kf, t_const], writes=[bt[7]])
                        kt_ = ktok[g % 2]
                        S.act(lambda e, kt_=kt_: e.activation(out=kt_[:].rearrange("p a b -> p (a b)"), in_=banks[7][:], func=AF.Copy),
                              reads=[bt[7]], writes=[t_kt[g % 2]])
                        r0 = g * 512 - 2048
                        S.dma(lambda e, kt_=kt_, r0=r0, hp=hp: e.dma_start(
                            out=O["kwp"][r0:r0 + 512, hp * 128:(hp + 1) * 128].rearrange("(j p) c -> p j c", p=128), in_=kt_[:]),
                            reads=[t_kt[g % 2]])
            ck(3.6)
            for br, dil in enumerate((1, 4, 16)):
                if br == 1:
                    ck(3.62)
                G = 32 // dil
                for j0 in range(0, 32, 4):
                    bk = 6 + (j0 // 4) % 2
                    for jj in range(4):
                        j = j0 + jj
                        r, g = j // G, j % G
                        start = r + dil * 128 * g
                        tg = sorted(set([(start) // 512, (start + dil * 127) // 512]))
                        for c in range(8):
                            S.pe(lambda e, c=c, jj=jj, start=start, dil=dil, bk=bk: e.matmul(
                                banks[bk][:, jj * 128:(jj + 1) * 128],
                                lhsT=hT[:, c, start:start + dil * 127 + 1:dil], rhs=wqkv[:, c, 2, :],
                                start=(c == 0), stop=(c == 7)), reads=[t_w] + [t_h[x] for x in tg], writes=[bt[bk]])
                    S.act(lambda e, br=br, j0=j0, bk=bk: e.activation(
                        out=V[:, br, j0:j0 + 4, :].rearrange("p a b -> p (a b)"), in_=banks[bk][:], func=AF.Copy),
                        reads=[bt[bk]], writes=[t_v[br][j0 + x] for x in range(4)])
                    if br == 0 and j0 >= 16 :
                        vt_ = vtok[(j0 // 4) % 2]
                        tv_ = t_vt[(j0 // 4) % 2]
                        S.act(lambda e, vt_=vt_, bk=bk: e.activation(out=vt_[:].rearrange("p a b -> p (a b)"), in_=banks[bk][:], func=AF.Copy),
                              reads=[bt[bk]], writes=[tv_])
                        r0 = j0 * 128 - 2048
                        S.dma(lambda e, vt_=vt_, r0=r0, hp=hp: e.dma_start(
                            out=O["vwp"][r0:r0 + 512, hp * 128:(hp + 1) * 128].rearrange("(j p) c -> p j c", p=128), in_=vt_[:]),
                            reads=[tv_])
            ck(3.7)
            for half in range(2):
                for br, dil in enumerate((1, 4, 16)):
                    G = 32 // dil
                    Gh = G // 2
                    for r in range(dil):
                        for g in range(half * Gh, (half + 1) * Gh):
                            j = r * G + g
                            start = r + dil * 128 * g
                            qsl = slice(start, start + dil * 127 + 1, dil)
                            tgq = sorted(set([start // 512, (start + dil * 127) // 512]))
                            parts = [1] if g == 0 else [0, 1]
                            sbk = nblk % 2
                            obk = 2 + nblk % 2
                            ei = nblk % 2
                            nblk += 1
                            for hh in range(2):
                                ps_ = slice(hh * 64, hh * 64 + 64)
                                for part in parts:
                                    if part == 1:
                                        ksl = qsl
                                        tgk = tgq
                                    else:
                                        ps0 = start - dil * 128
                                        ksl = slice(ps0, ps0 + dil * 127 + 1, dil)
                                        tgk = sorted(set([ps0 // 512, (ps0 + dil * 127) // 512]))
                                    S.pe(lambda e, ps_=ps_, ksl=ksl, qsl=qsl, hh=hh, part=part, sbk=sbk: e.matmul(
                                        banks[sbk][:, (hh * 2 + part) * 128:(hh * 2 + part + 1) * 128],
                                        lhsT=kT[ps_, ksl], rhs=qT[ps_, qsl], start=True, stop=True),
                                        reads=[t_k[x] for x in tgk] + [t_q[x] for x in tgq], writes=[bt[sbk]])
                            S.act(lambda e, ei=ei, sbk=sbk: e.activation(out=e0[ei][:], in_=banks[sbk][:], func=AF.Exp, scale=0.125),
                                  reads=[bt[sbk]], writes=[t_e0[ei]])
                            S.pool(lambda e, ei=ei, br=br, hp=hp: e.tensor_tensor(
                                out=ee[ei][:], in0=e0[ei][:],
                                in1=EBT[:, br * 8 + 2 * hp:br * 8 + 2 * hp + 2, :, :].rearrange("p a b c -> p (a b c)"), op=ALU.mult),
                                reads=[t_e0[ei], t_ebt], writes=[t_ee[ei]])
                            for hh in range(2):
                                po = slice(hh * 64, hh * 64 + 64)
                                for pi, part in enumerate(parts):
                                    jj = j if part == 1 else j - 1
                                    esl = slice((hh * 2 + part) * 128, (hh * 2 + part + 1) * 128)
                                    S.pe(lambda e, po=po, br=br, jj=jj, hh=hh, esl=esl, ei=ei, obk=obk, pi=pi, parts=parts: e.matmul(
                                        banks[obk][po, 0:128], lhsT=V[:, br, jj, hh * 64:(hh + 1) * 64], rhs=ee[ei][:, esl],
                                        start=(pi == 0), stop=(pi == len(parts) - 1)),
                                        reads=[t_v[br][jj], t_ee[ei]], writes=[bt[obk]])
                                for pi, part in enumerate(parts):
                                    esl = slice((hh * 2 + part) * 128, (hh * 2 + part + 1) * 128)
                                    S.pe(lambda e, po=po, esl=esl, ei=ei, obk=obk, pi=pi, parts=parts: e.matmul(
                                        banks[obk][po, 128:256], lhsT=onesb[:], rhs=ee[ei][:, esl],
                                        start=(pi == 0), stop=(pi == len(parts) - 1)),
                                        reads=[t_const, t_ee[ei]], writes=[bt[obk]])
                            lo = start - half * 2048
                            asl = slice(lo, lo + dil * 127 + 1, dil)
                            if br == 0:
                                S.dve(lambda e, asl=asl, obk=obk: e.tensor_copy(out=accO[:, asl], in_=banks[obk][:, 0:128]),
                                      reads=[bt[obk]], writes=[t_acc])
                                S.dve(lambda e, asl=asl, obk=obk: e.tensor_copy(out=accS[:, asl], in_=banks[obk][:, 128:256]),
                                      reads=[bt[obk]], writes=[t_acc])
                            else:
                                S.dve(lambda e, asl=asl, obk=obk: e.tensor_tensor(out=accO[:, asl], in0=accO[:, asl], in1=banks[obk][:, 0:128], op=ALU.add),
                                      reads=[bt[obk], t_acc], writes=[t_acc])
                                S.dve(lambda e, asl=asl, obk=obk: e.tensor_tensor(out=accS[:, asl], in0=accS[:, asl], in1=banks[obk][:, 128:256], op=ALU.add),
                                      reads=[bt[obk], t_acc], writes=[t_acc])
                S.dve(lambda e: e.reciprocal(out=accS[:], in_=accS[:]), reads=[t_acc], writes=[t_acc])
                S.dve(lambda e: e.tensor_tensor(out=mixo[:], in0=accO[:], in1=accS[:], op=ALU.mult), reads=[t_acc], writes=[t_mixo, t_acc])
                S.dma(lambda e, hp=hp, half=half: e.dma_start(out=mixT_d[hp, :, half * 2048:(half + 1) * 2048], in_=mixo[:]), reads=[t_mixo])
                if debug:
                    S.dma(lambda e, hp=hp, half=half: e.dma_start(out=O["dbg_mixT"][hp, :, half * 2048:(half + 1) * 2048], in_=mixo[:]), reads=[t_mixo])
                ck(3.8)

    S.barrier()
    stC.close()
    if phase_limit < 4:
        K.st = st
        S.emit()
        st.close()
        return nc
    NEG = -0.6065306597126334
    with contextlib.ExitStack() as st2, contextlib.suppress(_Stop):
        t_rp = Tok("rwparams")
        rwp = sbt(st2, "rwp", [128, 8, 4])
        mul_ = sbt(st2, "mul", [128, 1])
        lw3 = sbt(st2, "lw3", [128, 512])
        w0row = sbt(st2, "w0row", [1, 512])
        lnxg = sbt(st2, "lnxg", [128, 512])
        lnxb = sbt(st2, "lnxb", [128, 512])
        tri = sbt(st2, "tri", [128, 2, 128])
        mk1 = sbt(st2, "mk1", [128, 2, 64])
        mk3 = sbt(st2, "mk3", [128, 64])
        id2 = sbt(st2, "id2", [128, 64])
        omka = sbt(st2, "omka", [128, 4])
        gneps = sbt(st2, "gneps", [128, 1])
        for dst, src in ((rwp, "rwp"), (mul_, "mul"), (lw3, "lw3"), (w0row, "w0row"), (tri, "tri"), (mk1, "mk1"), (mk3, "mk3"), (id2, "id2")):
            S.dma(lambda e, dst=dst, src=src: e.dma_start(out=dst[:], in_=I[src]), writes=[t_rp])
        S.dma(lambda e: e.dma_start(out=lnxg[:], in_=bass.AP(I["lnx"].tensor, 0, [[0, 128], [1, 512]])), writes=[t_rp])
        S.dma(lambda e: e.dma_start(out=lnxb[:], in_=bass.AP(I["lnx"].tensor, 512, [[0, 128], [1, 512]])), writes=[t_rp])
        S.dve(lambda e: e.tensor_scalar(out=omka[:], in0=rwp[:, 4, :], scalar1=-1.0, scalar2=1.0, op0=ALU.mult, op1=ALU.add),
              reads=[t_rp], writes=[t_rp])
        S.pool(lambda e: e.memset(gneps[:], 64e-5), writes=[t_rp])
        wr = sbt(st2, "wr", [128, 8, 1664], BF16)
        t_wr = Tok()
        with contextlib.ExitStack() as st3:
            wst2 = [sbt(st3, "wst2", [128, 8, 416]) for i in range(2)]
            wst2t = [Tok(), Tok()]
            for q4 in range(4):
                w_ = wst2[q4 % 2]
                S.dma(lambda e, w_=w_, q4=q4: e.dma_start(out=w_[:], in_=win_v[:, :, 1536 + q4 * 416:1536 + (q4 + 1) * 416]), writes=[wst2t[q4 % 2]])
                S.pool(lambda e, w_=w_, q4=q4: e.tensor_copy(out=wr[:, :, q4 * 416:(q4 + 1) * 416], in_=w_[:]), reads=[wst2t[q4 % 2]], writes=[t_wr])
        S.barrier()

        def T_(n=""):
            return Tok(n)

        pb = [sbt(st2, "pb", [128, 4, 129]) for x in range(3)]
        pbl = sbt(st2, "pbl", [128, 129])
        t_pb = T_()
        for x in range(3):
            S.pool(lambda e, x=x: e.memset(pb[x][:, :, 0:1], 0.0), writes=[t_pb])
        S.pool(lambda e: e.memset(pbl[:, 0:1], 0.0), writes=[t_pb])
        xm = [sbt(st2, "xm", [128, 4, 128]) for x in range(3)]
        xml = sbt(st2, "xml", [128, 128])
        t_xm = T_()
        twl = sbt(st2, "twl", [128, 128])
        sg_tok = sbt(st2, "sg_tok", [128, 512])
        aT = sbt(st2, "aT", [128, 4, 128])
        g_tok = sbt(st2, "g_tok", [128, 512])
        kk = sbt(st2, "kk", [128, 4, 128])
        sq4 = sbt(st2, "sq4", [128, 4, 128])
        kmod = sbt(st2, "kmod", [128, 4, 128])
        bb = sbt(st2, "bb", [128, 4, 128])
        rk = sbt(st2, "rk", [128, 4, 128])
        dtmp = rk
        bsum = sbt(st2, "bsum", [128, 8])
        ycen = sbt(st2, "ycen", [128, 8, 64])
        ysq = sbt(st2, "ysq", [128, 8, 64])
        gst = sbt(st2, "gst", [128, 8])
        gst2 = sbt(st2, "gst2", [128, 8])
        mixr = sbt(st2, "mixr", [128, 4, 128], BF16)
        st4 = contextlib.ExitStack()
        st2.enter_context(st4)
        Pin = sbt(st4, "Pin", [128, 4, 128])
        Pinv = sbt(st4, "Pinv", [128, 4, 128])
        Pex = sbt(st4, "Pex", [128, 4, 128])
        Phat = sbt(st4, "Phat", [128, 4, 128])
        PCl = sbt(st4, "PCl", [128, 4, 2])
        PC = sbt(st4, "PC", [128, 4, 2])
        AR = sbt(st4, "AR", [128, 4, 2, 2, 64])
        BtT = sbt(st4, "BtT", [128, 4, 128])
        AtT = sbt(st4, "AtT", [128, 4, 128])
        KtT = sbt(st4, "KtT", [128, 4, 128])
        BhT = sbt(st4, "BhT", [128, 4, 128])
        KhT = sbt(st4, "KhT", [128, 4, 128])
        Atok = sbt(st4, "Atok", [128, 512])
        Bhtok = sbt(st4, "Bhtok", [128, 512])
        Khtok = sbt(st4, "Khtok", [128, 512])
        Vtok = sbt(st4, "Vtok", [128, 512])
        NM = sbt(st4, "NM", [128, 8, 2, 64])
        AK = sbt(st4, "AK", [128, 8, 2, 64])
        Aj = [sbt(st4, "Aj", [128, 8, 64])] * 2
        Nj = [sbt(st4, "Nj", [128, 8, 64])] * 2
        Tj = [sbt(st4, "Tj", [128, 8, 64])] * 2
        Z = sbt(st4, "Z", [128, 8, 128])
        AV = sbt(st4, "AV", [128, 8, 128])
        McT = sbt(st4, "McT", [128, 4, 2, 64])
        dPC = sbt(st4, "dPC", [128, 4, 2, 64])
        RpT = sbt(st4, "RpT", [128, 4, 2, 64])
        Hs = sbt(st4, "Hs", [128, 4, 64])
        t_H = T_()
        S.pool(lambda e: e.memset(Hs[:], 0.0), writes=[t_H])
        (t_lora, t_sg, t_a, t_g, t_kk, t_km, t_b, t_rk, t_bs, t_P, t_AR, t_BK, t_BKh, t_tok, t_NM, t_AK, t_A0, t_Z, t_AV,
         t_Mc, t_Rp, t_y, t_mixr) = [T_() for i in range(23)]
        t_Aj = [T_()] * 2
        t_Nj = [T_()] * 2
        t_Tj = [T_()] * 2
        B = banks

        def v4(ap):
            return ap.rearrange("p q (c t) -> p q c t", c=2)

        for sbi in range(32):
            t0 = sbi * 128
            hg = t_h[t0 // 512]
            for x in range(3):
                bk = x
                for p in range(4):
                    for c in range(8):
                        S.pe(lambda e, x=x, p=p, c=c, bk=bk, t0=t0: e.matmul(
                            B[bk][:, p * 128:(p + 1) * 128], lhsT=wr[:, c, x * 512 + p * 128:x * 512 + (p + 1) * 128],
                            rhs=hT[:, c, t0:t0 + 128], start=(c == 0), stop=(c == 7)), reads=[t_wr, hg], writes=[bt[bk]])
                S.act(lambda e, x=x, bk=bk: e.activation(out=pb[x][:, :, 1:129], in_=B[bk][:].rearrange("p (q t) -> p q t", q=4), func=AF.Copy),
                      reads=[bt[bk], t_xm], writes=[t_pb])
            for c in range(8):
                S.pe(lambda e, c=c, t0=t0: e.matmul(B[3][:, 0:128], lhsT=wr[:, c, 1536:1664], rhs=hT[:, c, t0:t0 + 128],
                                                     start=(c == 0), stop=(c == 7)), reads=[t_wr, hg], writes=[bt[3]])
            S.act(lambda e: e.activation(out=pbl[:, 1:129], in_=B[3][:, 0:128], func=AF.Copy), reads=[bt[3], t_xm], writes=[t_pb])
            if sbi == 31:
                for x in range(3):
                    S.dma(lambda e, x=x: e.dma_start(
                        out=bass.AP(O["shp"].tensor, x * 512, [[1, 128], [128, 4], [1, 1]]), in_=pb[x][:, :, 128:129], allow_slow_non_contiguous=True), reads=[t_pb])
                S.dma(lambda e: e.dma_start(out=bass.AP(O["shp"].tensor, 1536, [[1, 128], [1, 1]]), in_=pbl[:, 128:129], allow_slow_non_contiguous=True), reads=[t_pb])
            for x in range(3):
                S.dve(lambda e, x=x: e.tensor_tensor(out=dtmp[:], in0=pb[x][:, :, 0:128], in1=pb[x][:, :, 1:129], op=ALU.subtract),
                      reads=[t_pb], writes=[t_xm, t_rk])
                S.dve(lambda e, x=x: e.tensor_tensor(out=dtmp[:], in0=dtmp[:], in1=bc(rwp[:, x, :].unsqueeze(2), [128, 4, 128]), op=ALU.mult),
                      reads=[t_xm, t_rp, t_rk], writes=[t_xm, t_rk])
                S.dve(lambda e, x=x: e.tensor_tensor(out=xm[x][:], in0=dtmp[:], in1=pb[x][:, :, 1:129], op=ALU.add),
                      reads=[t_xm, t_pb, t_rk], writes=[t_xm])
            S.dve(lambda e: e.tensor_tensor(out=xml[:], in0=pbl[:, 0:128], in1=pbl[:, 1:129], op=ALU.subtract), reads=[t_pb], writes=[t_lora])
            S.dve(lambda e: e.scalar_tensor_tensor(out=xml[:], in0=xml[:], scalar=mul_[:, 0:1], in1=pbl[:, 1:129], op0=ALU.mult, op1=ALU.add),
                  reads=[t_lora, t_pb, t_rp], writes=[t_lora])
            for x in range(3):
                S.pool(lambda e, x=x: e.tensor_copy(out=pb[x][:, :, 0:1], in_=pb[x][:, :, 128:129]), reads=[t_pb, t_xm], writes=[t_pb])
            S.pool(lambda e: e.tensor_copy(out=pbl[:, 0:1], in_=pbl[:, 128:129]), reads=[t_pb, t_lora], writes=[t_pb])
            S.act(lambda e: e.activation(out=twl[0:32, :], in_=xml[0:32, :], func=AF.Tanh), reads=[t_lora], writes=[t_sg])
            S.act(lambda e: e.activation(out=twl[64:128, :], in_=xml[64:128, :], func=AF.Sigmoid), reads=[t_lora], writes=[t_sg])
            S.pe(lambda e: e.matmul(B[4][:], lhsT=twl[0:32, :], rhs=lw3[0:32, :], start=True, stop=False), reads=[t_sg, t_rp], writes=[bt[4]])
            S.pe(lambda e: e.matmul(B[4][:], lhsT=ones[0:1, :], rhs=w0row[0:1, :], start=False, stop=True), reads=[t_const, t_rp], writes=[bt[4]])
            S.act(lambda e: e.activation(out=sg_tok[:], in_=B[4][:], func=AF.Sigmoid), reads=[bt[4]], writes=[t_sg])
            for p in range(4):
                S.pe(lambda e, p=p: e.matmul(B[5][:, p * 128:(p + 1) * 128], lhsT=lw3[32:64, p * 128:(p + 1) * 128], rhs=xml[32:64, :],
                                              start=True, stop=True), reads=[t_lora, t_rp], writes=[bt[5]])
            S.dve(lambda e: e.tensor_tensor(out=aT[:], in0=B[5][:].rearrange("p (q t) -> p q t", q=4),
                                            in1=bc(rwp[:, 6, :].unsqueeze(2), [128, 4, 128]), op=ALU.add), reads=[bt[5], t_rp], writes=[t_a])
            S.act(lambda e: e.activation(out=aT[:], in_=aT[:], func=AF.Sigmoid), reads=[t_a], writes=[t_a])
            S.pe(lambda e: e.matmul(B[6][:], lhsT=twl[64:128, :], rhs=lw3[64:128, :], start=True, stop=True), reads=[t_sg, t_rp], writes=[bt[6]])
            S.act(lambda e: e.activation(out=g_tok[:], in_=B[6][:], func=AF.Copy), reads=[bt[6], t_y], writes=[t_g])
            S.dve(lambda e: e.tensor_tensor(out=kk[:], in0=xm[1][:], in1=bc(rwp[:, 3, :].unsqueeze(2), [128, 4, 128]), op=ALU.mult),
                  reads=[t_xm, t_rp], writes=[t_kk])
            S.act(lambda e: e.activation(out=sq4[:], in_=kk[:], func=AF.Square), reads=[t_kk], writes=[t_kk])
            S.pe(lambda e: e.matmul(B[7][:], lhsT=blk2[:], rhs=sq4[:].rearrange("p q t -> p (q t)"), start=True, stop=True),
                 reads=[t_kk, t_const], writes=[bt[7]])
            S.act(lambda e: e.activation(out=sq4[:].rearrange("p q t -> p (q t)"), in_=B[7][:], func=AF.Sqrt), reads=[bt[7]], writes=[t_kk])
            S.dve(lambda e: e.tensor_scalar(out=sq4[:], in0=sq4[:], scalar1=1e-12, scalar2=None, op0=ALU.max), reads=[t_kk], writes=[t_kk])
            S.dve(lambda e: e.reciprocal(out=sq4[:], in_=sq4[:]), reads=[t_kk], writes=[t_kk])
            S.dve(lambda e: e.tensor_tensor(out=kk[:], in0=kk[:], in1=sq4[:], op=ALU.mult), reads=[t_kk], writes=[t_kk])
            S.dve(lambda e: e.tensor_tensor(out=kmod[:], in0=aT[:], in1=bc(rwp[:, 4, :].unsqueeze(2), [128, 4, 128]), op=ALU.mult),
                  reads=[t_a, t_rp], writes=[t_km])
            S.dve(lambda e: e.tensor_tensor(out=kmod[:], in0=kmod[:], in1=bc(omka[:].unsqueeze(2), [128, 4, 128]), op=ALU.add),
                  reads=[t_km, t_rp], writes=[t_km])
            S.dve(lambda e: e.tensor_tensor(out=kmod[:], in0=kmod[:], in1=xm[1][:], op=ALU.mult), reads=[t_km, t_xm], writes=[t_km])
            S.dve(lambda e: e.tensor_tensor(out=bb[:], in0=kk[:], in1=aT[:], op=ALU.mult), reads=[t_kk, t_a], writes=[t_b])
            S.pool(lambda e: e.tensor_tensor(out=rk[:], in0=xm[0][:], in1=kmod[:], op=ALU.mult), reads=[t_xm, t_km], writes=[t_rk])
            S.pool(lambda e: e.tensor_tensor(out=rk[:], in0=rk[:], in1=bc(rwp[:, 5, :].unsqueeze(2), [128, 4, 128]), op=ALU.mult),
                   reads=[t_rk, t_rp], writes=[t_rk])
            for h in range(8):
                p, hh = h // 2, h % 2
                fp = slice(hh * 64, hh * 64 + 64)
                S.pe(lambda e, p=p, fp=fp, h=h: e.matmul(B[6][:, 256 + h:256 + h + 1], lhsT=rk[fp, p, :], rhs=ones[fp, 0:1], start=True, stop=True),
                     reads=[t_rk, t_const], writes=[bt[6]])
            S.act(lambda e: e.activation(out=bsum[:], in_=B[6][:, 256:264], func=AF.Copy), reads=[bt[6], t_y], writes=[t_bs])
            for p in range(4):
                S.pe(lambda e, p=p: e.matmul(B[0][:, p * 128:(p + 1) * 128], lhsT=sg_tok[:, p * 128:(p + 1) * 128], rhs=tri[:, 0, :],
                                              start=True, stop=True), reads=[t_sg, t_rp], writes=[bt[0]])
            for p in range(4):
                S.pe(lambda e, p=p: e.matmul(B[1][:, p * 128:(p + 1) * 128], lhsT=sg_tok[:, p * 128:(p + 1) * 128], rhs=tri[:, 1, :],
                                              start=True, stop=True), reads=[t_sg, t_rp], writes=[bt[1]])
            lp = B[0][:].rearrange("p (q t) -> p q t", q=4)
            S.act(lambda e: e.activation(out=Pin[:], in_=lp, func=AF.Exp), reads=[bt[0]], writes=[t_P])
            S.act(lambda e: e.activation(out=Pinv[:], in_=lp, func=AF.Exp, scale=-1.0), reads=[bt[0]], writes=[t_P])
            S.act(lambda e: e.activation(out=Pex[:], in_=B[1][:].rearrange("p (q t) -> p q t", q=4), func=AF.Exp), reads=[bt[1]], writes=[t_P])
            lp4 = B[0][:].rearrange("p (q c t) -> p q c t", q=4, c=2)
            S.act(lambda e: e.activation(out=PCl[:], in_=lp4[:, :, :, 63], func=AF.Copy), reads=[bt[0]], writes=[t_P])
            S.act(lambda e: e.activation(out=v4(Phat[:]), in_=lp4, func=AF.Copy), reads=[bt[0]], writes=[t_P])
            S.dve(lambda e: e.tensor_tensor(out=v4(Phat[:]), in0=bc(PCl[:].unsqueeze(3), [128, 4, 2, 64]), in1=v4(Phat[:]), op=ALU.subtract),
                  reads=[t_P], writes=[t_P])
            S.act(lambda e: e.activation(out=Phat[:], in_=Phat[:], func=AF.Exp), reads=[t_P], writes=[t_P])
            S.act(lambda e: e.activation(out=PC[:], in_=PCl[:], func=AF.Exp), reads=[t_P], writes=[t_P])
            S.dve(lambda e: e.scalar_tensor_tensor(out=AR[:, :, :, 0, :], in0=v4(kk[:]), scalar=-1.0, in1=v4(Pex[:]), op0=ALU.mult, op1=ALU.mult),
                  reads=[t_kk, t_P], writes=[t_AR])
            S.dve(lambda e: e.tensor_tensor(out=AR[:, :, :, 1, :], in0=v4(xm[0][:]), in1=v4(Pin[:]), op=ALU.mult), reads=[t_xm, t_P], writes=[t_AR])
            S.pool(lambda e: e.tensor_tensor(out=BtT[:], in0=bb[:], in1=Pinv[:], op=ALU.mult), reads=[t_b, t_P], writes=[t_BK])
            S.pool(lambda e: e.tensor_tensor(out=KtT[:], in0=kmod[:], in1=Pinv[:], op=ALU.mult), reads=[t_km, t_P], writes=[t_BK])
            S.pool(lambda e: e.tensor_tensor(out=BhT[:], in0=bb[:], in1=Phat[:], op=ALU.mult), reads=[t_b, t_P], writes=[t_BKh])
            S.pool(lambda e: e.tensor_tensor(out=KhT[:], in0=kmod[:], in1=Phat[:], op=ALU.mult), reads=[t_km, t_P], writes=[t_BKh])
            S.pool(lambda e: e.tensor_copy(out=v4(AtT[:]), in_=AR[:, :, :, 0, :]), reads=[t_AR], writes=[t_BKh])
            for src_fn, dst, rd, bk in ((lambda p: AtT[:, p, :], Atok, [t_BKh], 2), (lambda p: BhT[:, p, :], Bhtok, [t_BKh], 3),
                                        (lambda p: KhT[:, p, :], Khtok, [t_BKh], 4), (lambda p: xm[2][:, p, :], Vtok, [t_xm], 5)):
                for p in range(4):
                    S.pe(lambda e, p=p, src_fn=src_fn, bk=bk: e.transpose(B[bk][:, p * 128:(p + 1) * 128], src_fn(p), ident[:]),
                         reads=rd + [t_const], writes=[bt[bk]])
                S.act(lambda e, dst=dst, bk=bk: e.activation(out=dst[:], in_=B[bk][:], func=AF.Copy), reads=[bt[bk], t_y, t_AV, t_Z], writes=[t_tok])
            for ch in range(2):
                tp = slice(ch * 64, ch * 64 + 64)
                for h in range(8):
                    p, hh = h // 2, h % 2
                    fp = slice(hh * 64, hh * 64 + 64)
                    csl = slice(ch * 64, ch * 64 + 64)
                    arr = AR[fp, p, ch, :, :].rearrange("p a t -> p (a t)")
                    S.pe(lambda e, tp=tp, fp=fp, p=p, h=h, csl=csl, arr=arr: e.matmul(
                        B[0][tp, (h % 4) * 128:(h % 4 + 1) * 128] if h < 4 else B[1][tp, (h % 4) * 128:(h % 4 + 1) * 128],
                        lhsT=BtT[fp, p, csl], rhs=arr, start=True, stop=True), reads=[t_BK, t_AR], writes=[bt[0 if h < 4 else 1]])
                    S.pe(lambda e, tp=tp, fp=fp, p=p, h=h, csl=csl, arr=arr: e.matmul(
                        B[2][tp, (h % 4) * 128:(h % 4 + 1) * 128] if h < 4 else B[3][tp, (h % 4) * 128:(h % 4 + 1) * 128],
                        lhsT=KtT[fp, p, csl], rhs=arr, start=True, stop=True), reads=[t_BK, t_AR], writes=[bt[2 if h < 4 else 3]])
                    S.pe(lambda e, tp=tp, fp=fp, p=p, h=h, csl=csl, ch=ch: e.matmul(
                        B[4][tp, h * 64:(h + 1) * 64], lhsT=AR[fp, p, ch, 0, :], rhs=BtT[fp, p, csl], start=True, stop=True),
                        reads=[t_BK, t_AR], writes=[bt[4]])
            mk1b = bc(mk1[:].unsqueeze(1), [128, 4, 2, 64])
            for hf in range(2):
                S.dve(lambda e, hf=hf: e.tensor_tensor(out=NM[:, hf * 4:(hf + 1) * 4, :, :], in0=B[hf][:].rearrange("p (h a t) -> p h a t", h=4, a=2),
                                                        in1=mk1b, op=ALU.mult), reads=[bt[hf], t_rp], writes=[t_NM])
                S.dve(lambda e, hf=hf: e.tensor_tensor(out=AK[:, hf * 4:(hf + 1) * 4, :, :], in0=B[2 + hf][:].rearrange("p (h a t) -> p h a t", h=4, a=2),
                                                        in1=mk1b, op=ALU.mult), reads=[bt[2 + hf], t_rp], writes=[t_AK])
            S.dve(lambda e: e.tensor_tensor(out=Aj[0][:], in0=B[4][:].rearrange("p (h t) -> p h t", h=8), in1=bc(mk3[:].unsqueeze(1), [128, 8, 64]), op=ALU.mult),
                  reads=[bt[4], t_rp], writes=[t_Aj[0]])
            S.pool(lambda e: e.tensor_copy(out=Nj[0][:], in_=NM[:, :, 0, :]), reads=[t_NM], writes=[t_Nj[0]])
            S.pool(lambda e: e.tensor_tensor(out=Tj[0][:], in0=NM[:, :, 0, :], in1=bc(id2[:].unsqueeze(1), [128, 8, 64]), op=ALU.add),
                   reads=[t_NM, t_rp], writes=[t_Tj[0]])
            for lv in range(1, 6):
                a_o = Aj[0]
                n_o = Nj[0]
                t_o = Tj[0]
                ta, tn, tt = t_Aj[0], t_Nj[0], t_Tj[0]
                for ch in range(2):
                    tp = slice(ch * 64, ch * 64 + 64)
                    for h in range(8):
                        S.pe(lambda e, tp=tp, h=h: e.matmul(B[5][tp, h * 64:(h + 1) * 64], lhsT=n_o[tp, h, :], rhs=a_o[tp, h, :],
                                                             start=True, stop=True), reads=[ta, tn], writes=[bt[5]])
                if lv < 5:
                    for ch in range(2):
                        tp = slice(ch * 64, ch * 64 + 64)
                        for h in range(8):
                            S.pe(lambda e, tp=tp, h=h: e.matmul(B[6][tp, h * 64:(h + 1) * 64], lhsT=a_o[tp, h, :], rhs=n_o[tp, h, :],
                                                                 start=True, stop=True), reads=[ta, tn], writes=[bt[6]])
                S.act(lambda e: e.activation(out=a_o[:].rearrange("p h t -> p (h t)"), in_=B[5][:], func=AF.Copy), reads=[bt[5]], writes=[ta])
                if lv < 5:
                    S.act(lambda e: e.activation(out=n_o[:].rearrange("p h t -> p (h t)"), in_=B[6][:], func=AF.Copy), reads=[bt[6]], writes=[tn])
                for ch in range(2):
                    tp = slice(ch * 64, ch * 64 + 64)
                    for h in range(8):
                        S.pe(lambda e, tp=tp, h=h: e.matmul(B[7][tp, h * 64:(h + 1) * 64], lhsT=a_o[tp, h, :], rhs=t_o[tp, h, :],
                                                             start=True, stop=True), reads=[ta, tt], writes=[bt[7]])
                S.dve(lambda e: e.tensor_tensor(out=t_o[:], in0=B[7][:].rearrange("p (h t) -> p h t", h=8), in1=t_o[:], op=ALU.add),
                      reads=[bt[7], tt], writes=[tt])
            TT = Tj[5 % 2]
            t_TT = t_Tj[5 % 2]
            for ch in range(2):
                tp = slice(ch * 64, ch * 64 + 64)
                for h in range(8):
                    S.pe(lambda e, tp=tp, h=h: e.matmul(B[4][tp, h * 64:(h + 1) * 64], lhsT=AK[tp, h, 0, :], rhs=Vtok[tp, h * 64:(h + 1) * 64],
                                                         start=True, stop=True), reads=[t_AK, t_tok], writes=[bt[4]])
            S.act(lambda e: e.activation(out=Z[:, :, 64:128], in_=B[4][:].rearrange("p (h t) -> p h t", h=8), func=AF.Copy), reads=[bt[4]], writes=[t_Z])
            S.pool(lambda e: e.tensor_copy(out=Z[:, :, 0:64], in_=Atok[:].rearrange("p (h t) -> p h t", h=8)), reads=[t_tok], writes=[t_Z])
            for ch in range(2):
                tp = slice(ch * 64, ch * 64 + 64)
                for h in range(8):
                    S.pe(lambda e, tp=tp, h=h: e.matmul(B[h // 4][tp, (h % 4) * 128:(h % 4 + 1) * 128], lhsT=TT[tp, h, :], rhs=Z[tp, h, :],
                                                         start=True, stop=True), reads=[t_TT, t_Z], writes=[bt[h // 4]])
            for hf in range(2):
                S.act(lambda e, hf=hf: e.activation(out=AV[:, hf * 4:(hf + 1) * 4, :].rearrange("p h t -> p (h t)"), in_=B[hf][:], func=AF.Copy),
                      reads=[bt[hf]], writes=[t_AV])
            for ch in range(2):
                tp = slice(ch * 64, ch * 64 + 64)
                for h in range(8):
                    p, hh = h // 2, h % 2
                    fp = slice(hh * 64, hh * 64 + 64)
                    col = (p * 2 + ch) * 64
                    S.pe(lambda e, tp=tp, fp=fp, h=h, col=col: e.matmul(B[2][fp, col:col + 64], lhsT=AV[tp, h, 0:64], rhs=Bhtok[tp, h * 64:(h + 1) * 64],
                                                                         start=True, stop=True), reads=[t_AV, t_tok], writes=[bt[2]])
                    S.pe(lambda e, tp=tp, fp=fp, h=h, col=col: e.matmul(B[3][fp, col:col + 64], lhsT=AV[tp, h, 0:64], rhs=NM[tp, h, 1, :],
                                                                         start=True, stop=True), reads=[t_AV, t_NM], writes=[bt[3]])
            S.dve(lambda e: e.tensor_tensor(out=dPC[:], in0=bc(PC[:].unsqueeze(3), [128, 4, 2, 64]),
                                            in1=bc(id2[:].unsqueeze(1).unsqueeze(1), [128, 4, 2, 64]), op=ALU.mult), reads=[t_P, t_rp], writes=[t_Mc])
            S.dve(lambda e: e.tensor_tensor(out=McT[:], in0=B[2][:].rearrange("p (q c t) -> p q c t", q=4, c=2), in1=dPC[:], op=ALU.add),
                  reads=[bt[2], t_Mc], writes=[t_Mc])
            S.dve(lambda e: e.tensor_tensor(out=RpT[:], in0=B[3][:].rearrange("p (q c t) -> p q c t", q=4, c=2), in1=AR[:, :, :, 1, :], op=ALU.add),
                  reads=[bt[3], t_AR], writes=[t_Rp])
            for ch in range(2):
                tp = slice(ch * 64, ch * 64 + 64)
                for h in range(8):
                    p, hh = h // 2, h % 2
                    fp = slice(hh * 64, hh * 64 + 64)
                    S.pe(lambda e, tp=tp, h=h: e.matmul(B[6][tp, h * 64:(h + 1) * 64], lhsT=NM[tp, h, 1, :], rhs=AV[tp, h, 64:128], start=True, stop=False),
                         reads=[t_NM, t_AV], writes=[bt[6]])
                    S.pe(lambda e, tp=tp, h=h: e.matmul(B[6][tp, h * 64:(h + 1) * 64], lhsT=AK[tp, h, 1, :], rhs=Vtok[tp, h * 64:(h + 1) * 64], start=False, stop=False),
                         reads=[t_AK, t_tok], writes=[bt[6]])
                    S.pe(lambda e, tp=tp, fp=fp, p=p, h=h, ch=ch: e.matmul(B[6][tp, h * 64:(h + 1) * 64], lhsT=RpT[fp, p, ch, :], rhs=Hs[fp, p, :], start=False, stop=True),
                         reads=[t_Rp, t_H], writes=[bt[6]])
                for h in range(8):
                    p, hh = h // 2, h % 2
                    fp = slice(hh * 64, hh * 64 + 64)
                    S.pe(lambda e, tp=tp, fp=fp, p=p, h=h: e.matmul(B[5][fp, p * 64:(p + 1) * 64], lhsT=Bhtok[tp, h * 64:(h + 1) * 64], rhs=AV[tp, h, 64:128], start=True, stop=False),
                         reads=[t_tok, t_AV], writes=[bt[5]])
                    S.pe(lambda e, tp=tp, fp=fp, p=p, h=h: e.matmul(B[5][fp, p * 64:(p + 1) * 64], lhsT=Khtok[tp, h * 64:(h + 1) * 64], rhs=Vtok[tp, h * 64:(h + 1) * 64], start=False, stop=False),
                         reads=[t_tok], writes=[bt[5]])
                    S.pe(lambda e, fp=fp, p=p, ch=ch: e.matmul(B[5][fp, p * 64:(p + 1) * 64], lhsT=McT[fp, p, ch, :], rhs=Hs[fp, p, :], start=False, stop=True),
                         reads=[t_Mc, t_H], writes=[bt[5]])
                S.act(lambda e: e.activation(out=Hs[:].rearrange("p q v -> p (q v)"), in_=B[5][:, 0:256], func=AF.Copy), reads=[bt[5]], writes=[t_H])
            yps = B[6][:].rearrange("p (h v) -> p h v", h=8)
            S.dve(lambda e: e.tensor_reduce(out=gst[:], in_=yps, axis=AX.X, op=ALU.add), reads=[bt[6]], writes=[t_y])
            S.dve(lambda e: e.tensor_scalar(out=gst[:], in0=gst[:], scalar1=1.0 / 64, scalar2=None, op0=ALU.mult), reads=[t_y], writes=[t_y])
            S.dve(lambda e: e.tensor_tensor(out=ycen[:], in0=yps, in1=bc(gst[:].unsqueeze(2), [128, 8, 64]), op=ALU.subtract), reads=[t_y, bt[6]], writes=[t_y])
            S.act(lambda e: e.activation(out=ysq[:], in_=ycen[:], func=AF.Square), reads=[t_y], writes=[t_y])
            S.dve(lambda e: e.tensor_reduce(out=gst2[:], in_=ysq[:], axis=AX.X, op=ALU.add), reads=[t_y], writes=[t_y])
            S.act(lambda e: e.activation(out=gst2[:], in_=gst2[:], func=AF.Sqrt, scale=1.0 / 64, bias=gneps[:]), reads=[t_y, t_rp], writes=[t_y])
            S.dve(lambda e: e.reciprocal(out=gst2[:], in_=gst2[:]), reads=[t_y], writes=[t_y])
            S.dve(lambda e: e.tensor_tensor(out=ycen[:], in0=ycen[:], in1=bc(gst2[:].unsqueeze(2), [128, 8, 64]), op=ALU.mult), reads=[t_y], writes=[t_y])
            yc2 = ycen[:].rearrange("p h v -> p (h v)")
            S.dve(lambda e: e.tensor_tensor(out=yc2, in0=yc2, in1=lnxg[:], op=ALU.mult), reads=[t_y, t_rp], writes=[t_y])
            S.dve(lambda e: e.tensor_tensor(out=yc2, in0=yc2, in1=lnxb[:], op=ALU.add), reads=[t_y, t_rp], writes=[t_y])
            S.pool(lambda e: e.tensor_tensor(out=ysq[:], in0=Vtok[:].rearrange("p (h v) -> p h v", h=8), in1=bc(bsum[:].unsqueeze(2), [128, 8, 64]), op=ALU.mult),
                   reads=[t_tok, t_bs, t_y], writes=[t_y])
            S.dve(lambda e: e.tensor_tensor(out=ycen[:], in0=ycen[:], in1=ysq[:], op=ALU.add), reads=[t_y], writes=[t_y])
            S.dve(lambda e: e.tensor_tensor(out=yc2, in0=yc2, in1=g_tok[:], op=ALU.mult), reads=[t_y, t_g], writes=[t_y])
            if debug:
                S.dma(lambda e, t0=t0: e.dma_start(out=O["dbg_yr"][t0:t0 + 128, :], in_=yc2), reads=[t_y])
            for p in range(4):
                S.pe(lambda e, p=p: e.transpose(B[7][:, p * 128:(p + 1) * 128], ycen[:, 2 * p:2 * p + 2, :].rearrange("p h v -> p (h v)"), ident[:]),
                     reads=[t_y, t_const], writes=[bt[7]])
            S.act(lambda e: e.activation(out=mixr[:].rearrange("p q t -> p (q t)"), in_=B[7][:], func=AF.Copy), reads=[bt[7]], writes=[t_mixr])
            S.dma(lambda e, t0=t0: e.dma_start(out=mixT_d[4:8, :, t0:t0 + 128].rearrange("q p t -> p q t"), in_=mixr[:]), reads=[t_mixr])
            ck(4.0 + 0.01 * (sbi + 1))
        S.dma(lambda e: e.dma_start(out=O["wkvp"], in_=Hs[:]), reads=[t_H])

        S.barrier()
        st4.close()
        ck(4.95)
        NSS = 64
        hs_perm = lambda c: hT[:, c, T:T + NS].rearrange("p (b t) -> p t b", t=4)
        for x in range(3):
            S.dma(lambda e, x=x: e.dma_start(out=pb[x][:, :, 0:16], in_=I["shT"][:, x, :, :]), writes=[t_pb])
        S.dma(lambda e: e.dma_start(out=pbl[:, 0:16], in_=I["shTl"]), writes=[t_pb])
        for x in range(3):
            bk = x
            for p in range(4):
                for c in range(8):
                    S.pe(lambda e, x=x, p=p, c=c, bk=bk: e.matmul(
                        B[bk][:, p * 128:p * 128 + NSS], lhsT=wr[:, c, x * 512 + p * 128:x * 512 + (p + 1) * 128],
                        rhs=hs_perm(c), start=(c == 0), stop=(c == 7)), reads=[t_wr, t_h[8]], writes=[bt[bk]])
            S.act(lambda e, x=x, bk=bk: e.activation(out=pb[x][:, :, 16:80], in_=B[bk][:].rearrange("p (q t) -> p q t", q=4)[:, :, 0:NSS], func=AF.Copy),
                  reads=[bt[bk], t_xm], writes=[t_pb])
        for c in range(8):
            S.pe(lambda e, c=c: e.matmul(B[3][:, 0:NSS], lhsT=wr[:, c, 1536:1664], rhs=hs_perm(c), start=(c == 0), stop=(c == 7)),
                 reads=[t_wr, t_h[8]], writes=[bt[3]])
        S.act(lambda e: e.activation(out=pbl[:, 16:80], in_=B[3][:, 0:NSS], func=AF.Copy), reads=[bt[3], t_xm], writes=[t_pb])
        shrow = sbt(st2, "shrow", [16, 1664])
        t_shrow = T_()
        for q4 in range(4):
            for c in range(8):
                S.pe(lambda e, q4=q4, c=c: e.matmul(B[4][0:16, 0:416], lhsT=hT[:, c, T + 3:T + NS:4], rhs=wr[:, c, q4 * 416:(q4 + 1) * 416],
                                                    start=(c == 0), stop=(c == 7)), reads=[t_wr, t_h[8]], writes=[bt[4]])
            S.act(lambda e, q4=q4: e.activation(out=shrow[:, q4 * 416:(q4 + 1) * 416], in_=B[4][0:16, 0:416], func=AF.Copy), reads=[bt[4]], writes=[t_shrow])
        S.dma(lambda e: e.dma_start(out=O["shs"], in_=shrow[:]), reads=[t_shrow])
        n_ = NSS
        for x in range(3):
            S.dve(lambda e, x=x: e.tensor_tensor(out=dtmp[:, :, 0:n_], in0=pb[x][:, :, 0:n_], in1=pb[x][:, :, 16:16 + n_], op=ALU.subtract),
                  reads=[t_pb], writes=[t_xm, t_rk])
            S.dve(lambda e, x=x: e.tensor_tensor(out=dtmp[:, :, 0:n_], in0=dtmp[:, :, 0:n_], in1=bc(rwp[:, x, :].unsqueeze(2), [128, 4, n_]), op=ALU.mult),
                  reads=[t_xm, t_rp, t_rk], writes=[t_xm, t_rk])
            S.dve(lambda e, x=x: e.tensor_tensor(out=xm[x][:, :, 0:n_], in0=dtmp[:, :, 0:n_], in1=pb[x][:, :, 16:16 + n_], op=ALU.add),
                  reads=[t_xm, t_pb, t_rk], writes=[t_xm])
        S.dve(lambda e: e.tensor_tensor(out=xml[:, 0:n_], in0=pbl[:, 0:n_], in1=pbl[:, 16:16 + n_], op=ALU.subtract), reads=[t_pb], writes=[t_lora])
        S.dve(lambda e: e.scalar_tensor_tensor(out=xml[:, 0:n_], in0=xml[:, 0:n_], scalar=mul_[:, 0:1], in1=pbl[:, 16:16 + n_], op0=ALU.mult, op1=ALU.add),
              reads=[t_lora, t_pb, t_rp], writes=[t_lora])
        S.act(lambda e: e.activation(out=twl[0:32, 0:n_], in_=xml[0:32, 0:n_], func=AF.Tanh), reads=[t_lora], writes=[t_sg])
        S.act(lambda e: e.activation(out=twl[64:128, 0:n_], in_=xml[64:128, 0:n_], func=AF.Sigmoid), reads=[t_lora], writes=[t_sg])
        S.pe(lambda e: e.matmul(B[4][0:n_, :], lhsT=twl[0:32, 0:n_], rhs=lw3[0:32, :], start=True, stop=False), reads=[t_sg, t_rp], writes=[bt[4]])
        S.pe(lambda e: e.matmul(B[4][0:n_, :], lhsT=ones[0:1, 0:n_], rhs=w0row[0:1, :], start=False, stop=True), reads=[t_const, t_rp], writes=[bt[4]])
        S.act(lambda e: e.activation(out=sg_tok[0:n_, :], in_=B[4][0:n_, :], func=AF.Sigmoid), reads=[bt[4]], writes=[t_sg])
        for p in range(4):
            S.pe(lambda e, p=p: e.matmul(B[5][:, p * 128:p * 128 + n_], lhsT=lw3[32:64, p * 128:(p + 1) * 128], rhs=xml[32:64, 0:n_],
                                          start=True, stop=True), reads=[t_lora, t_rp], writes=[bt[5]])
        S.dve(lambda e: e.tensor_tensor(out=aT[:, :, 0:n_], in0=B[5][:].rearrange("p (q t) -> p q t", q=4)[:, :, 0:n_],
                                        in1=bc(rwp[:, 6, :].unsqueeze(2), [128, 4, n_]), op=ALU.add), reads=[bt[5], t_rp], writes=[t_a])
        S.act(lambda e: e.activation(out=aT[:, :, 0:n_], in_=aT[:, :, 0:n_], func=AF.Sigmoid), reads=[t_a], writes=[t_a])
        S.pe(lambda e: e.matmul(B[6][0:n_, :], lhsT=twl[64:128, 0:n_], rhs=lw3[64:128, :], start=True, stop=True), reads=[t_sg, t_rp], writes=[bt[6]])
        S.act(lambda e: e.activation(out=g_tok[0:n_, :], in_=B[6][0:n_, :], func=AF.Copy), reads=[bt[6], t_y], writes=[t_g])
        S.dve(lambda e: e.tensor_tensor(out=kk[:, :, 0:n_], in0=xm[1][:, :, 0:n_], in1=bc(rwp[:, 3, :].unsqueeze(2), [128, 4, n_]), op=ALU.mult),
              reads=[t_xm, t_rp], writes=[t_kk])
        S.act(lambda e: e.activation(out=sq4[:, :, 0:n_], in_=kk[:, :, 0:n_], func=AF.Square), reads=[t_kk], writes=[t_kk])
        for p in range(4):
            S.pe(lambda e, p=p: e.matmul(B[7][:, p * 128:p * 128 + n_], lhsT=blk2[:], rhs=sq4[:, p, 0:n_], start=True, stop=True),
                 reads=[t_kk, t_const], writes=[bt[7]])
        S.act(lambda e: e.activation(out=sq4[:, :, 0:n_], in_=B[7][:].rearrange("p (q t) -> p q t", q=4)[:, :, 0:n_], func=AF.Sqrt), reads=[bt[7]], writes=[t_kk])
        S.dve(lambda e: e.tensor_scalar(out=sq4[:, :, 0:n_], in0=sq4[:, :, 0:n_], scalar1=1e-12, scalar2=None, op0=ALU.max), reads=[t_kk], writes=[t_kk])
        S.dve(lambda e: e.reciprocal(out=sq4[:, :, 0:n_], in_=sq4[:, :, 0:n_]), reads=[t_kk], writes=[t_kk])
        S.dve(lambda e: e.tensor_tensor(out=kk[:, :, 0:n_], in0=kk[:, :, 0:n_], in1=sq4[:, :, 0:n_], op=ALU.mult), reads=[t_kk], writes=[t_kk])
        S.dve(lambda e: e.tensor_tensor(out=kmod[:, :, 0:n_], in0=aT[:, :, 0:n_], in1=bc(rwp[:, 4, :].unsqueeze(2), [128, 4, n_]), op=ALU.mult),
              reads=[t_a, t_rp], writes=[t_km])
        S.dve(lambda e: e.tensor_tensor(out=kmod[:, :, 0:n_], in0=kmod[:, :, 0:n_], in1=bc(omka[:].unsqueeze(2), [128, 4, n_]), op=ALU.add),
              reads=[t_km, t_rp], writes=[t_km])
        S.dve(lambda e: e.tensor_tensor(out=kmod[:, :, 0:n_], in0=kmod[:, :, 0:n_], in1=xm[1][:, :, 0:n_], op=ALU.mult), reads=[t_km, t_xm], writes=[t_km])
        S.dve(lambda e: e.tensor_tensor(out=bb[:, :, 0:n_], in0=kk[:, :, 0:n_], in1=aT[:, :, 0:n_], op=ALU.mult), reads=[t_kk, t_a], writes=[t_b])
        S.pool(lambda e: e.tensor_tensor(out=rk[:, :, 0:n_], in0=xm[0][:, :, 0:n_], in1=kmod[:, :, 0:n_], op=ALU.mult), reads=[t_xm, t_km], writes=[t_rk])
        S.pool(lambda e: e.tensor_tensor(out=rk[:, :, 0:n_], in0=rk[:, :, 0:n_], in1=bc(rwp[:, 5, :].unsqueeze(2), [128, 4, n_]), op=ALU.mult),
               reads=[t_rk, t_rp], writes=[t_rk])
        for h in range(8):
            p, hh = h // 2, h % 2
            fp = slice(hh * 64, hh * 64 + 64)
            S.pe(lambda e, p=p, fp=fp, h=h: e.matmul(B[6][0:n_, 256 + h:256 + h + 1], lhsT=rk[fp, p, 0:n_], rhs=ones[fp, 0:1], start=True, stop=True),
                 reads=[t_rk, t_const], writes=[bt[6]])
        S.act(lambda e: e.activation(out=bsum[0:n_, :], in_=B[6][0:n_, 256:264], func=AF.Copy), reads=[bt[6], t_y], writes=[t_bs])
        tok6 = sbt(st2, "tok6", [64, 6, 512])
        t_tok6 = T_()
        S.act(lambda e: e.activation(out=tok6[:, 1, :], in_=sg_tok[0:n_, :], func=AF.Exp, scale=NEG), reads=[t_sg], writes=[t_tok6])
        for xi, (srcT, rd) in enumerate(((xm[0], t_xm), (None, None), (kmod, t_km), (xm[2], t_xm), (kk, t_kk), (bb, t_b))):
            if srcT is None:
                continue
            bk = 2 + xi % 2
            for p in range(4):
                S.pe(lambda e, p=p, srcT=srcT, bk=bk: e.transpose(B[bk][0:n_, p * 128:(p + 1) * 128], srcT[:, p, 0:n_], ident[:]),
                     reads=[rd, t_const], writes=[bt[bk]])
            S.act(lambda e, xi=xi, bk=bk: e.activation(out=tok6[:, xi, :], in_=B[bk][0:n_, :], func=AF.Copy), reads=[bt[bk]], writes=[t_tok6])
        t_rwsd = T_()
        t_rwsd_l = [T_() for i in range(24)]
        for t in range(4):
            for xi in range(6):
                S.dma(lambda e, t=t, xi=xi: e.dma_start(out=rws_d[:, :, t, xi, :], in_=tok6[16 * t:16 * t + 16, xi, :].rearrange("p (h c) -> p h c", h=8)),
                      reads=[t_tok6], writes=[t_rwsd_l[t * 6 + xi]])
        X6 = sbt(st2, "X6", [128, 4, 6, 64])
        St = sbt(st2, "St", [128, 64, 64])
        tmpS = sbt(st2, "tmpS", [128, 64, 64])
        sp_ = sbt(st2, "sp", [128, 64])
        ys = sbt(st2, "ys", [128, 4, 64])
        t_X6, t_St, t_tmpS, t_sp, t_ys = [T_() for i in range(5)]
        S.dma(lambda e: e.dma_start(out=X6[:].rearrange("p t x c -> p (t x c)"), in_=rws_d.rearrange("b h t x c -> (b h) (t x c)")), reads=t_rwsd_l, writes=[t_X6])
        S.dma(lambda e: e.dma_start(out=St[:].rearrange("p v k -> p (v k)"), in_=I["swkv"]), writes=[t_St])
        for t in range(4):
            def bk_(xi, t=t):
                return bc(X6[:, t, xi, :].unsqueeze(1), [128, 64, 64])
            def bv_(ap):
                return bc(ap.unsqueeze(2), [128, 64, 64])
            S.dve(lambda e, t=t: e.tensor_tensor(out=tmpS[:], in0=St[:], in1=bk_(4, t), op=ALU.mult), reads=[t_St, t_X6], writes=[t_tmpS])
            S.dve(lambda e: e.tensor_reduce(out=sp_[:], in_=tmpS[:], axis=AX.X, op=ALU.add), reads=[t_tmpS], writes=[t_sp])
            S.dve(lambda e, t=t: e.tensor_tensor(out=St[:], in0=St[:], in1=bk_(1, t), op=ALU.mult), reads=[t_St, t_X6, t_tmpS], writes=[t_St])
            S.pool(lambda e, t=t: e.tensor_tensor(out=tmpS[:], in0=bv_(sp_[:]), in1=bk_(5, t), op=ALU.mult), reads=[t_sp, t_X6], writes=[t_tmpS])
            S.dve(lambda e: e.tensor_tensor(out=St[:], in0=St[:], in1=tmpS[:], op=ALU.subtract), reads=[t_St, t_tmpS], writes=[t_St])
            S.pool(lambda e, t=t: e.tensor_tensor(out=tmpS[:], in0=bv_(X6[:, t, 3, :]), in1=bk_(2, t), op=ALU.mult), reads=[t_X6, t_St], writes=[t_tmpS])
            S.dve(lambda e: e.tensor_tensor(out=St[:], in0=St[:], in1=tmpS[:], op=ALU.add), reads=[t_St, t_tmpS], writes=[t_St])
            S.dve(lambda e, t=t: e.tensor_tensor(out=tmpS[:], in0=St[:], in1=bk_(0, t), op=ALU.mult), reads=[t_St, t_X6], writes=[t_tmpS])
            S.dve(lambda e, t=t: e.tensor_reduce(out=ys[:, t, :], in_=tmpS[:], axis=AX.X, op=ALU.add), reads=[t_tmpS], writes=[t_ys])
        S.dma(lambda e: e.dma_start(out=O["wkvs"], in_=St[:].rearrange("p v k -> p (v k)")), reads=[t_St])
        t_ysd = T_()
        S.dma(lambda e: e.dma_start(out=ys_d.rearrange("b h t v -> (b h) (t v)"), in_=ys[:].rearrange("p t v -> p (t v)")), reads=[t_ys], writes=[t_ysd])
        ysr = sbt(st2, "ysr", [64, 8, 64])
        t_ysr = T_()
        for t in range(4):
            S.dma(lambda e, t=t: e.dma_start(out=ysr[16 * t:16 * t + 16, :, :], in_=ys_d[:, :, t, :]), reads=[t_ysd], writes=[t_ysr])
        yps_s = ysr[:]
        Y0 = slice(0, 64)
        S.dve(lambda e: e.tensor_reduce(out=gst[Y0], in_=yps_s, axis=AX.X, op=ALU.add), reads=[t_ysr], writes=[t_y])
        S.dve(lambda e: e.tensor_scalar(out=gst[Y0], in0=gst[Y0], scalar1=1.0 / 64, scalar2=None, op0=ALU.mult), reads=[t_y], writes=[t_y])
        S.dve(lambda e: e.tensor_tensor(out=ycen[Y0], in0=yps_s, in1=bc(gst[Y0].unsqueeze(2), [64, 8, 64]), op=ALU.subtract), reads=[t_y, t_ysr], writes=[t_y])
        S.act(lambda e: e.activation(out=ysq[Y0], in_=ycen[Y0], func=AF.Square), reads=[t_y], writes=[t_y])
        S.dve(lambda e: e.tensor_reduce(out=gst2[Y0], in_=ysq[Y0], axis=AX.X, op=ALU.add), reads=[t_y], writes=[t_y])
        S.act(lambda e: e.activation(out=gst2[Y0], in_=gst2[Y0], func=AF.Sqrt, scale=1.0 / 64, bias=gneps[Y0]), reads=[t_y, t_rp], writes=[t_y])
        S.dve(lambda e: e.reciprocal(out=gst2[Y0], in_=gst2[Y0]), reads=[t_y], writes=[t_y])
        S.dve(lambda e: e.tensor_tensor(out=ycen[Y0], in0=ycen[Y0], in1=bc(gst2[Y0].unsqueeze(2), [64, 8, 64]), op=ALU.mult), reads=[t_y], writes=[t_y])
        yc2_s = ycen[Y0].rearrange("p h v -> p (h v)")
        S.dve(lambda e: e.tensor_tensor(out=yc2_s, in0=yc2_s, in1=lnxg[Y0], op=ALU.mult), reads=[t_y, t_rp], writes=[t_y])
        S.dve(lambda e: e.tensor_tensor(out=yc2_s, in0=yc2_s, in1=lnxb[Y0], op=ALU.add), reads=[t_y, t_rp], writes=[t_y])
        S.pool(lambda e: e.tensor_tensor(out=ysq[Y0], in0=tok6[:, 3, :].rearrange("p (h v) -> p h v", h=8), in1=bc(bsum[Y0].unsqueeze(2), [64, 8, 64]), op=ALU.mult),
               reads=[t_tok6, t_bs, t_y], writes=[t_y])
        S.dve(lambda e: e.tensor_tensor(out=ycen[Y0], in0=ycen[Y0], in1=ysq[Y0], op=ALU.add), reads=[t_y], writes=[t_y])
        S.dve(lambda e: e.tensor_tensor(out=yc2_s, in0=yc2_s, in1=g_tok[Y0], op=ALU.mult), reads=[t_y, t_g], writes=[t_y])
        for p in range(4):
            S.pe(lambda e, p=p: e.transpose(B[7][:, p * 128:p * 128 + 64], ycen[Y0, 2 * p:2 * p + 2, :].rearrange("p h v -> p (h v)"), ident[0:64, 0:64]),
                 reads=[t_y, t_const], writes=[bt[7]])
        S.act(lambda e: e.activation(out=mixr[:, :, 0:64].rearrange("p q (b t) -> p q t b", t=4),
                                     in_=B[7][:].rearrange("p (q x) -> p q x", q=4)[:, :, 0:64].rearrange("p q (t b) -> p q t b", t=4), func=AF.Copy),
              reads=[bt[7]], writes=[t_mixr])
        S.dma(lambda e: e.dma_start(out=mixT_d[4:8, :, T:T + NS].rearrange("q p t -> p q t"), in_=mixr[:, :, 0:64]), reads=[t_mixr])


    S.barrier()
    with contextlib.ExitStack() as st2, contextlib.suppress(_Stop):
        ck(5.0)
        wq3 = sbt(st2, "wq3", [128, 8, 1536], BF16)
        t_wq3 = Tok()
        with contextlib.ExitStack() as st3:
            wst3 = [sbt(st3, "wst3", [128, 8, 512]) for i in range(2)]
            wst3t = [Tok(), Tok()]
            for q3 in range(3):
                w_ = wst3[q3 % 2]
                S.dma(lambda e, w_=w_, q3=q3: e.dma_start(out=w_[:], in_=win_v[:, :, q3 * 512:(q3 + 1) * 512]), writes=[wst3t[q3 % 2]])
                S.pool(lambda e, w_=w_, q3=q3: e.tensor_copy(out=wq3[:, :, q3 * 512:(q3 + 1) * 512], in_=w_[:]), reads=[wst3t[q3 % 2]], writes=[t_wq3])
        S.barrier()
        B = banks
        NQ = 64
        hsp = sbt(st2, "hsp", [128, 8, 64], BF16)
        t_hsp = Tok()
        S.pool(lambda e: e.tensor_copy(out=hsp[:].rearrange("p c (s b) -> p c s b", s=4), in_=hT[:, :, T:T + NS].rearrange("p c (b s) -> p c s b", s=4)),
               reads=[t_h[8]], writes=[t_hsp])
        hs_sb = lambda c: hsp[:, c, :]
        qkvs = sbt(st2, "qkvs", [64, 3, 8, 64])
        sqs = sbt(st2, "sqs", [64, 8, 64])
        ssn = sbt(st2, "ssn", [64, 8])
        gqk = sbt(st2, "gqk", [64, 2, 64])
        t_qkv, t_sqs, t_gqk = Tok(), Tok(), Tok()
        for wi in range(2):
            S.dma(lambda e, wi=wi: e.dma_start(out=gqk[:, wi, :], in_=bass.AP(I["qkrow"].tensor, wi * 64, [[0, 64], [1, 64]])), writes=[t_gqk])
        for wi in range(3):
            for c in range(8):
                S.pe(lambda e, wi=wi, c=c: e.matmul(B[wi][0:NQ, :], lhsT=hs_sb(c), rhs=wq3[:, c, wi * 512:(wi + 1) * 512], start=(c == 0), stop=(c == 7)),
                     reads=[t_wq3, t_hsp], writes=[bt[wi]])
            pv = B[wi][0:NQ, :].rearrange("p (h c) -> p h c", h=8)
            if wi == 2:
                S.act(lambda e, pv=pv: e.activation(out=qkvs[:, 2, :, :], in_=pv, func=AF.Copy), reads=[bt[wi]], writes=[t_qkv])
            else:
                S.act(lambda e, pv=pv: e.activation(out=sqs[:], in_=pv, func=AF.Square), reads=[bt[wi]], writes=[t_sqs])
                S.dve(lambda e: e.tensor_reduce(out=ssn[:], in_=sqs[:], axis=AX.X, op=ALU.add), reads=[t_sqs], writes=[t_sqs])
                S.act(lambda e: e.activation(out=ssn[:], in_=ssn[:], func=AF.Sqrt, scale=1.0 / 64, bias=epsc[0:64]), reads=[t_sqs, t_const], writes=[t_sqs])
                S.dve(lambda e: e.reciprocal(out=ssn[:], in_=ssn[:]), reads=[t_sqs], writes=[t_sqs])
                S.dve(lambda e, pv=pv, wi=wi: e.tensor_tensor(out=qkvs[:, wi, :, :], in0=pv, in1=bc(ssn[:].unsqueeze(2), [64, 8, 64]), op=ALU.mult),
                      reads=[bt[wi], t_sqs], writes=[t_qkv])
                S.dve(lambda e, wi=wi: e.tensor_tensor(out=qkvs[:, wi, :, :], in0=qkvs[:, wi, :, :], in1=bc(gqk[:, wi, :].unsqueeze(1), [64, 8, 64]), op=ALU.mult),
                      reads=[t_qkv, t_gqk], writes=[t_qkv])
        ck(5.1)
        t_rec = Tok()
        for wi, oname, recd, cname in ((1, "kws", reck_d, "ck_s"), (2, "vws", recv_d, "cv_s")):
            for s_ in range(4):
                S.dma(lambda e, wi=wi, oname=oname, s_=s_: e.dma_start(out=O[oname].rearrange("(b s) c -> s b c", s=4)[s_],
                                                                        in_=qkvs[16 * s_:16 * s_ + 16, wi, :, :].rearrange("p h c -> p (h c)")), reads=[t_qkv])
                S.dma(lambda e, wi=wi, recd=recd, s_=s_: e.dma_start(out=recd[:, 4 + s_, :], in_=qkvs[16 * s_:16 * s_ + 16, wi, :, :].rearrange("p h c -> p (h c)")),
                      reads=[t_qkv], writes=[t_rec])
            S.dma(lambda e, recd=recd, cname=cname: e.dma_start(out=recd[:, 0:4, :], in_=I[cname][:, 2044:2048, :]), writes=[t_rec])
        ck(5.2)
        btab = sbt(st2, "btab", [64, 3, 8, 129])
        t_btab = Tok()
        with contextlib.ExitStack() as st3:
            relr = sbt(st3, "relr", [32, 8])
            ohs = sbt(st3, "ohs", [32, 3, 129])
            RHs = sbt(st3, "RHs", [32, 8, 129])
            t_r2, t_rh2 = Tok(), Tok()
            S.dma(lambda e: e.dma_start(out=relr[:], in_=I["relb"]), writes=[t_r2])
            S.dma(lambda e: e.dma_start(out=ohs[:], in_=I["ohs"]), writes=[t_r2])
            for br in range(3):
                S.dve(lambda e, br=br: e.tensor_tensor(out=RHs[:], in0=bc(relr[:].unsqueeze(2), [32, 8, 129]), in1=bc(ohs[:, br, :].unsqueeze(1), [32, 8, 129]), op=ALU.mult),
                      reads=[t_r2], writes=[t_rh2])
                for h in range(8):
                    bk = 3 + h % 2
                    S.pe(lambda e, h=h, bk=bk: e.matmul(B[bk][0:64, 0:129], lhsT=ones[0:32, 0:64], rhs=RHs[:, h, :], start=True, stop=True),
                         reads=[t_rh2, t_const], writes=[bt[bk]])
                    S.act(lambda e, h=h, bk=bk, br=br: e.activation(out=btab[:, br, h, :], in_=B[bk][0:64, 0:129], func=AF.Copy), reads=[bt[bk]], writes=[t_btab])
        S.barrier()
        ck(5.3)
        Kt = [sbt(st2, "Kt", [64, 129, 64])] * 2
        Vt = [sbt(st2, "Vt", [64, 129, 64])] * 2
        t_Kt = [[Tok() for i in range(24)]] * 2
        t_Vt = [[Tok() for i in range(24)]] * 2
        lg = sbt(st2, "lg", [64, 129])
        zz = sbt(st2, "zz", [64, 1])
        ov = sbt(st2, "ov", [64, 64])
        Oacc = sbt(st2, "Oacc", [64, 8, 64])
        Zacc = sbt(st2, "Zacc", [64, 8])
        t_lg, t_zz, t_ov, t_acc2 = Tok(), Tok(), Tok(), Tok()
        S.pool(lambda e: e.memset(Oacc[:], 0.0), writes=[t_acc2])
        S.pool(lambda e: e.memset(Zacc[:], 0.0), writes=[t_acc2])
        it = 0
        for br, dil in enumerate((1, 4, 16)):
            ncache = 125 if dil == 1 else 128
            for h in range(8):
                bi = it % 2
                it += 1
                for tl, tk, cname, recd in ((Kt[bi], t_Kt[bi], "ck_s", reck_d), (Vt[bi], t_Vt[bi], "cv_s", recv_d)):
                    for s_ in range(4):
                        ps_ = slice(16 * s_, 16 * s_ + 16)
                        for j0 in range(0, ncache, 32):
                            j1 = min(ncache, j0 + 32)
                            src = bass.AP(I[cname].tensor, (2048 + s_ - 128 * dil + j0 * dil) * 512 + h * 64, [[2048 * 512, 16], [dil * 512, j1 - j0], [1, 64]])
                            S.dma(lambda e, tl=tl, ps_=ps_, src=src, j0=j0, j1=j1: e.dma_start(out=tl[ps_, j0:j1, :], in_=src), writes=[tk[8 + 4 * s_ + j0 // 32]])
                        r0 = (1 + s_) if dil == 1 else (4 + s_)
                        src2 = bass.AP(recd.tensor, r0 * 512 + h * 64, [[8 * 512, 16], [512, 129 - ncache], [1, 64]])
                        S.dma(lambda e, tl=tl, ps_=ps_, src2=src2, ncache=ncache: e.dma_start(out=tl[ps_, ncache:129, :], in_=src2), reads=[t_rec], writes=[tk[2 * s_ + 1]])
                K_, V_ = Kt[bi], Vt[bi]
                S.dve(lambda e, K_=K_, h=h: e.tensor_tensor(out=K_[:], in0=K_[:], in1=bc(qkvs[:, 0, h, :].unsqueeze(1), [64, 129, 64]), op=ALU.mult),
                      reads=t_Kt[bi] + [t_qkv], writes=t_Kt[bi])
                S.dve(lambda e, K_=K_: e.tensor_reduce(out=lg[:], in_=K_[:], axis=AX.X, op=ALU.add), reads=t_Kt[bi], writes=[t_lg])
                S.dve(lambda e, br=br, h=h: e.scalar_tensor_tensor(out=lg[:], in0=lg[:], scalar=0.125, in1=btab[:, br, h, :], op0=ALU.mult, op1=ALU.add),
                      reads=[t_lg, t_btab], writes=[t_lg])
                S.act(lambda e: e.activation(out=lg[:], in_=lg[:], func=AF.Exp), reads=[t_lg], writes=[t_lg])
                S.dve(lambda e: e.tensor_reduce(out=zz[:], in_=lg[:], axis=AX.X, op=ALU.add), reads=[t_lg], writes=[t_zz])
                S.dve(lambda e, h=h: e.tensor_tensor(out=Zacc[:, h:h + 1], in0=Zacc[:, h:h + 1], in1=zz[:], op=ALU.add), reads=[t_zz, t_acc2], writes=[t_acc2])
                S.pool(lambda e, V_=V_: e.tensor_tensor(out=V_[:], in0=V_[:], in1=bc(lg[:].unsqueeze(2), [64, 129, 64]), op=ALU.mult),
                       reads=t_Vt[bi] + [t_lg], writes=t_Vt[bi])
                S.dve(lambda e, V_=V_: e.tensor_reduce(out=ov[:], in_=V_[:].rearrange("p j c -> p c j"), axis=AX.X, op=ALU.add), reads=t_Vt[bi], writes=[t_ov])
                S.dve(lambda e, h=h: e.tensor_tensor(out=Oacc[:, h, :], in0=Oacc[:, h, :], in1=ov[:], op=ALU.add), reads=[t_ov, t_acc2], writes=[t_acc2])
                ck(5.4 + 0.001 * it)
        S.dve(lambda e: e.reciprocal(out=Zacc[:], in_=Zacc[:]), reads=[t_acc2], writes=[t_acc2])
        S.dve(lambda e: e.tensor_tensor(out=Oacc[:], in0=Oacc[:], in1=bc(Zacc[:].unsqueeze(2), [64, 8, 64]), op=ALU.mult), reads=[t_acc2], writes=[t_acc2])
        mixs = sbt(st2, "mixs", [128, 4, 64], BF16)
        t_mixs = Tok()
        for p in range(4):
            S.pe(lambda e, p=p: e.transpose(B[5][:, p * 128:p * 128 + 64], Oacc[:, 2 * p:2 * p + 2, :].rearrange("p h c -> p (h c)"), ident[0:64, 0:64]),
                 reads=[t_acc2, t_const], writes=[bt[5]])
        S.act(lambda e: e.activation(out=mixs[:].rearrange("p q (b s) -> p q s b", s=4),
                                     in_=B[5][:].rearrange("p (q x) -> p q x", q=4)[:, :, 0:64].rearrange("p q (s b) -> p q s b", s=4), func=AF.Copy),
              reads=[bt[5]], writes=[t_mixs])
        S.dma(lambda e: e.dma_start(out=mixT_d[0:4, :, T:T + NS].rearrange("q p t -> p q t"), in_=mixs[:]), reads=[t_mixs])
    S.barrier()
    stH.close()
    if phase_limit < 6:
        K.st = st
        S.emit()
        st.close()
        return nc
    with contextlib.ExitStack() as st2:
        cst = [sbt(st2, "cst", [128, 4096]) for i in range(3)]
        cbf = [sbt(st2, "cbf", [128, 4096], BF16) for i in range(3)]
        tcs = [Tok() for i in range(3)]
        tcb = [Tok() for i in range(3)]
        nb = 0
        for src, dstd, nblk in ((I["euT"], euT_d, 32), (I["ev"], ev_d, 32), (I["w_pq"], wpq_d, 4)):
            sv = src.rearrange("a b -> (a b)").rearrange("(n p f) -> n p f", p=128, f=4096)
            dv = dstd.rearrange("a b -> (a b)").rearrange("(n p f) -> n p f", p=128, f=4096)
            for b_ in range(nblk):
                i = nb % 3
                nb += 1
                S.dma(lambda e, i=i, sv=sv, b_=b_: e.dma_start(out=cst[i][:], in_=sv[b_]), writes=[tcs[i]])
                if i == 0:
                    S.dve(lambda e, i=i: e.tensor_copy(out=cbf[i][:], in_=cst[i][:]), reads=[tcs[i]], writes=[tcb[i]])
                elif i == 1:
                    S.act(lambda e, i=i: e.activation(out=cbf[i][:], in_=cst[i][:], func=AF.Copy), reads=[tcs[i]], writes=[tcb[i]])
                else:
                    S.pool(lambda e, i=i: e.tensor_copy(out=cbf[i][:], in_=cst[i][:]), reads=[tcs[i]], writes=[tcb[i]])
                S.dma(lambda e, i=i, dv=dv, b_=b_: e.dma_start(out=dv[b_], in_=cbf[i][:]), reads=[tcb[i]])
    S.barrier()

    with contextlib.ExitStack() as st2, contextlib.suppress(_Stop):
        t_pc = Tok("peerconst")
        wout = sbt(st2, "wout", [128, 8, 1024], BF16)
        skT = sbt(st2, "skT", [128, 16, 128], BF16)
        iotaR = sbt(st2, "iotaR", [128, 128])
        with contextlib.ExitStack() as st3:
            wo_st = sbt(st3, "wo_st", [128, 8, 1024])
            sk_st = sbt(st3, "sk_st", [128, 16, 128])
            S.dma(lambda e: e.dma_start(out=wo_st[:], in_=I["w_out"].rearrange("(c p) n -> p c n", p=128)), writes=[t_pc])
            S.dma(lambda e: e.dma_start(out=sk_st[:], in_=I["skT"]), writes=[t_pc])
            S.dma(lambda e: e.dma_start(out=iotaR[:], in_=I["iotaR"]), writes=[t_pc])
            S.dve(lambda e: e.tensor_copy(out=wout[:], in_=wo_st[:]), reads=[t_pc], writes=[t_pc])
            S.dve(lambda e: e.tensor_copy(out=skT[:], in_=sk_st[:]), reads=[t_pc], writes=[t_pc])
        S.barrier()
        GS = 256
        x1g = sbt(st2, "x1g", [128, 8, GS])
        tmpg = sbt(st2, "tmpg", [128, 8, GS])
        h2g = sbt(st2, "h2g", [128, 8, GS], BF16)
        mixg = sbt(st2, "mixg", [128, 8, GS], BF16)
        rsg = sbt(st2, "rsg", [128, GS])
        qpT = sbt(st2, "qpT", [128, 16, GS], BF16)
        wpqb = sbt(st2, "wpqb", [128, 8, 512], BF16)
        s_sb = sbt(st2, "s_sb", [128, 16, 128])
        s2 = sbt(st2, "s2", [128, 256])
        vals = sbt(st2, "vals", [128, 16, 16])
        idxu = sbt(st2, "idxu", [128, 16, 16], U32)
        idxf = sbt(st2, "idxf", [128, 16, 16])
        cand = sbt(st2, "cand", [128, 8, 256])
        ts_ = sbt(st2, "ts", [128, 8, 16])
        posu = sbt(st2, "posu", [128, 8, 16], U32)
        au = sbt(st2, "au", [128, 8, 16], U32)
        bu = sbt(st2, "bu", [128, 8, 16], U32)
        a_f = sbt(st2, "a_f", [128, 8, 16])
        b_f = sbt(st2, "b_f", [128, 8, 16])
        eq = sbt(st2, "eq", [128, 8, 16, 16])
        I1 = sbt(st2, "I1", [128, 8, 16])
        I2 = sbt(st2, "I2", [128, 8, 16])
        gt_ = sbt(st2, "gt", [128, 8, 16])
        zs = sbt(st2, "zs", [128, 8])
        I1T = sbt(st2, "I1T", [128, GS])
        I2T = sbt(st2, "I2T", [128, GS])
        gT = sbt(st2, "gT", [128, GS])
        A4 = [sbt(st2, "A4", [128, 4, 128], BF16) for i in range(2)]
        B4 = [sbt(st2, "B4", [128, 4, 128], BF16) for i in range(2)]
        WT = sbt(st2, "WT", [128, 128, GS], BF16)
        eub = [sbt(st2, "eub", [128, 8, 256], BF16) for i in range(2)]
        evb = [sbt(st2, "evb", [128, 2, 1024], BF16) for i in range(2)]
        gU = [sbt(st2, "gU", [128, GS], BF16) for i in range(2)]
        Wg = [sbt(st2, "Wg", [128, GS], BF16) for i in range(2)]
        ytk = [sbt(st2, "ytk", [128, 1024]) for i in range(2)]
        (t_x1, t_tmp, t_h2, t_mixg, t_rsg, t_qp, t_wpq, t_s, t_s2, t_vals, t_cand, t_ts, t_ab, t_eq, t_I, t_g, t_IT, t_WT) = [Tok() for i in range(18)]
        t_A4 = [Tok(), Tok()]
        t_B4 = [Tok(), Tok()]
        t_eub = [Tok(), Tok()]
        t_evb = [Tok(), Tok()]
        t_gU = [Tok(), Tok()]
        t_Wg = [Tok(), Tok()]
        t_ytk = [Tok(), Tok()]
        B = banks
        euv = euT_d.rearrange("(c p) e -> p c e", p=128)
        evv = ev_d.rearrange("(k p) d -> p k d", p=128)
        wpqv = wpq_d.rearrange("(c p) n -> p c n", p=128)
        iota16 = iotaR[:, 0:16]
        pgroups = [(g * GS, GS) for g in range(T // GS)] + [(T, NS)]
        nyt = 0
        for gi, (t0, n) in enumerate(pgroups):
            samp = (gi == len(pgroups) - 1)
            S.dma(lambda e, t0=t0, n=n: e.dma_start(out=mixg[:, :, 0:n], in_=mixT_d[:, :, t0:t0 + n].rearrange("j p t -> p j t")), writes=[t_mixg])
            S.dma(lambda e, t0=t0, n=n: e.dma_start(out=x1g[:, :, 0:n], in_=xT_v[:, :, t0:t0 + n]), writes=[t_x1])
            for dc in range(8):
                bk = dc % 2
                for j in range(8):
                    S.pe(lambda e, dc=dc, j=j, bk=bk, n=n: e.matmul(B[bk][:, 0:n], lhsT=wout[:, j, dc * 128:(dc + 1) * 128], rhs=mixg[:, j, 0:n],
                                                                     start=(j == 0), stop=(j == 7)), reads=[t_pc, t_mixg], writes=[bt[bk]])
                if not samp:
                    S.dve(lambda e, dc=dc, bk=bk, n=n: e.scalar_tensor_tensor(out=x1g[:, dc, 0:n], in0=B[bk][:, 0:n], scalar=modT[:, 16 + dc, 0:1],
                                                                               in1=x1g[:, dc, 0:n], op0=ALU.mult, op1=ALU.add),
                          reads=[bt[bk], t_mod, t_x1], writes=[t_x1])
                else:
                    S.dve(lambda e, dc=dc, bk=bk: e.tensor_tensor(out=tmpg[:, dc, 0:NS].rearrange("p (b t) -> p b t", t=4),
                                                                  in0=B[bk][:, 0:NS].rearrange("p (b t) -> p b t", t=4),
                                                                  in1=bc(modT[:, 16 + dc, 1:17].unsqueeze(2), [128, SB, 4]), op=ALU.mult),
                          reads=[bt[bk], t_mod], writes=[t_tmp])
                    S.dve(lambda e, dc=dc: e.tensor_tensor(out=x1g[:, dc, 0:NS], in0=x1g[:, dc, 0:NS], in1=tmpg[:, dc, 0:NS], op=ALU.add),
                          reads=[t_tmp, t_x1], writes=[t_x1])
            S.act(lambda e, n=n: e.activation(out=tmpg[:, :, 0:n], in_=x1g[:, :, 0:n], func=AF.Square), reads=[t_x1], writes=[t_tmp])
            for c in range(8):
                S.pe(lambda e, c=c, n=n: e.matmul(B[2][:, 0:n], lhsT=ones[:], rhs=tmpg[:, c, 0:n], start=(c == 0), stop=(c == 7)),
                     reads=[t_tmp, t_const], writes=[bt[2]])
            S.act(lambda e, n=n: e.activation(out=rsg[:, 0:n], in_=B[2][:, 0:n], func=AF.Sqrt, scale=1.0 / D, bias=epsc[:]),
                  reads=[bt[2], t_const], writes=[t_rsg])
            S.dve(lambda e, n=n: e.reciprocal(out=rsg[:, 0:n], in_=rsg[:, 0:n]), reads=[t_rsg], writes=[t_rsg])
            S.dve(lambda e, n=n: e.tensor_tensor(out=tmpg[:, :, 0:n], in0=x1g[:, :, 0:n], in1=bc(rsg[:, 0:n].unsqueeze(1), [128, 8, n]), op=ALU.mult),
                  reads=[t_x1, t_rsg, t_tmp], writes=[t_tmp])
            if not samp:
                for c in range(8):
                    eng = S.dve if c % 2 == 0 else S.pool
                    eng(lambda e, c=c, n=n: e.tensor_scalar(out=h2g[:, c, 0:n], in0=tmpg[:, c, 0:n], scalar1=A2[:, c, 0:1], scalar2=modT[:, 24 + c, 0:1],
                                                            op0=ALU.mult, op1=ALU.add), reads=[t_tmp, t_mod], writes=[t_h2])
            else:
                tv = tmpg[:, :, 0:NS].rearrange("p c (b t) -> p c b t", t=4)
                S.dve(lambda e, tv=tv: e.tensor_tensor(out=tv, in0=tv, in1=bc(A2[:, :, 1:17].unsqueeze(3), [128, 8, SB, 4]), op=ALU.mult),
                      reads=[t_tmp, t_mod], writes=[t_tmp])
                S.dve(lambda e, tv=tv: e.tensor_tensor(out=h2g[:, :, 0:NS].rearrange("p c (b t) -> p c b t", t=4), in0=tv,
                                                       in1=bc(modT[:, 24:32, 1:17].unsqueeze(3), [128, 8, SB, 4]), op=ALU.add),
                      reads=[t_tmp, t_mod], writes=[t_h2])
            ck(6.1)
            for jb in range(4):
                S.dma(lambda e, jb=jb: e.dma_start(out=wpqb[:], in_=wpqv[:, :, jb * 512:(jb + 1) * 512]), writes=[t_wpq])
                for j in range(4):
                    for c in range(8):
                        S.pe(lambda e, j=j, c=c, n=n: e.matmul(B[j][:, 0:n], lhsT=wpqb[:, c, j * 128:(j + 1) * 128], rhs=h2g[:, c, 0:n],
                                                               start=(c == 0), stop=(c == 7)), reads=[t_wpq, t_h2], writes=[bt[j]])
                for j in range(4):
                    S.act(lambda e, j=j, jb=jb, n=n: e.activation(out=qpT[:, jb * 4 + j, 0:n], in_=B[j][:, 0:n], func=AF.Copy), reads=[bt[j]], writes=[t_qp])
            for tt0 in range(0, n, 128):
                m = min(128, n - tt0)
                for j in range(16):
                    bk = 4 + j // 4
                    S.pe(lambda e, j=j, bk=bk, tt0=tt0, m=m: e.matmul(B[bk][0:m, (j % 4) * 128:(j % 4 + 1) * 128], lhsT=qpT[:, j, tt0:tt0 + m], rhs=skT[:, j, :],
                                                                      start=True, stop=True), reads=[t_qp, t_pc], writes=[bt[bk]])
                for q in range(4):
                    S.act(lambda e, q=q, m=m: e.activation(out=s_sb[0:m, q * 4:(q + 1) * 4, :].rearrange("p a k -> p (a k)"), in_=B[4 + q][0:m, :], func=AF.Copy),
                          reads=[bt[4 + q]], writes=[t_s])
                for j in range(16):
                    S.dve(lambda e, j=j, m=m: e.max(out=vals[0:m, j, 0:8], in_=s_sb[0:m, j, :]), reads=[t_s], writes=[t_vals])
                    S.dve(lambda e, j=j, m=m: e.match_replace(out=s2[0:m, 0:128], in_to_replace=vals[0:m, j, 0:8], in_values=s_sb[0:m, j, :], imm_value=-1e30),
                          reads=[t_s, t_vals], writes=[t_s2])
                    S.dve(lambda e, j=j, m=m: e.max(out=vals[0:m, j, 8:16], in_=s2[0:m, 0:128]), reads=[t_s2], writes=[t_vals])
                    S.dve(lambda e, j=j, m=m: e.max_index(out=idxu[0:m, j, 0:8], in_max=vals[0:m, j, 0:8], in_values=s_sb[0:m, j, :]), reads=[t_s, t_vals], writes=[t_vals])
                    S.dve(lambda e, j=j, m=m: e.max_index(out=idxu[0:m, j, 8:16], in_max=vals[0:m, j, 8:16], in_values=s_sb[0:m, j, :]), reads=[t_s, t_vals], writes=[t_vals])
                S.dve(lambda e, m=m: e.tensor_copy(out=idxf[0:m], in_=idxu[0:m]), reads=[t_vals], writes=[t_vals])
                v2 = vals[0:m].rearrange("p (h two) k -> p h two k", two=2)
                i2v = idxf[0:m].rearrange("p (h two) k -> p h two k", two=2)
                S.dve(lambda e, m=m, v2=v2: e.tensor_tensor(out=cand[0:m].rearrange("p h (a b) -> p h a b", a=16),
                                                            in0=bc(v2[:, :, 0, :].unsqueeze(3), [m, 8, 16, 16]),
                                                            in1=bc(v2[:, :, 1, :].unsqueeze(2), [m, 8, 16, 16]), op=ALU.add), reads=[t_vals], writes=[t_cand])
                for h in range(8):
                    S.dve(lambda e, h=h, m=m: e.max(out=ts_[0:m, h, 0:8], in_=cand[0:m, h, :]), reads=[t_cand], writes=[t_ts])
                    S.dve(lambda e, h=h, m=m: e.match_replace(out=s2[0:m, :], in_to_replace=ts_[0:m, h, 0:8], in_values=cand[0:m, h, :], imm_value=-1e30),
                          reads=[t_cand, t_ts], writes=[t_s2])
                    S.dve(lambda e, h=h, m=m: e.max(out=ts_[0:m, h, 8:16], in_=s2[0:m, :]), reads=[t_s2], writes=[t_ts])
                    S.dve(lambda e, h=h, m=m: e.max_index(out=posu[0:m, h, 0:8], in_max=ts_[0:m, h, 0:8], in_values=cand[0:m, h, :]), reads=[t_cand, t_ts], writes=[t_ts])
                    S.dve(lambda e, h=h, m=m: e.max_index(out=posu[0:m, h, 8:16], in_max=ts_[0:m, h, 8:16], in_values=cand[0:m, h, :]), reads=[t_cand, t_ts], writes=[t_ts])
                S.dve(lambda e, m=m: e.tensor_single_scalar(out=au[0:m], in_=posu[0:m], scalar=4, op=ALU.logical_shift_right), reads=[t_ts], writes=[t_ab])
                S.dve(lambda e, m=m: e.tensor_single_scalar(out=bu[0:m], in_=posu[0:m], scalar=15, op=ALU.bitwise_and), reads=[t_ts], writes=[t_ab])
                S.dve(lambda e, m=m: e.tensor_copy(out=a_f[0:m], in_=au[0:m]), reads=[t_ab], writes=[t_ab])
                S.dve(lambda e, m=m: e.tensor_copy(out=b_f[0:m], in_=bu[0:m]), reads=[t_ab], writes=[t_ab])
                for which, sel, dstI in ((0, a_f, I1), (1, b_f, I2)):
                    S.dve(lambda e, m=m, sel=sel: e.tensor_tensor(out=eq[0:m], in0=bc(iota16[0:m].unsqueeze(1).unsqueeze(1), [m, 8, 16, 16]),
                                                                  in1=bc(sel[0:m].unsqueeze(3), [m, 8, 16, 16]), op=ALU.is_equal),
                          reads=[t_ab, t_pc], writes=[t_eq])
                    S.dve(lambda e, m=m, which=which, i2v=i2v: e.tensor_tensor(out=eq[0:m], in0=eq[0:m], in1=bc(i2v[:, :, which, :].unsqueeze(2), [m, 8, 16, 16]), op=ALU.mult),
                          reads=[t_eq, t_vals], writes=[t_eq])
                    S.dve(lambda e, m=m, dstI=dstI: e.tensor_reduce(out=dstI[0:m], in_=eq[0:m], axis=AX.X, op=ALU.add), reads=[t_eq], writes=[t_I])
                S.dve(lambda e, m=m: e.tensor_tensor(out=gt_[0:m], in0=ts_[0:m], in1=bc(ts_[0:m, :, 0:1], [m, 8, 16]), op=ALU.subtract), reads=[t_ts], writes=[t_g])
                S.act(lambda e, m=m: e.activation(out=gt_[0:m], in_=gt_[0:m], func=AF.Exp), reads=[t_g], writes=[t_g])
                S.dve(lambda e, m=m: e.tensor_reduce(out=zs[0:m], in_=gt_[0:m], axis=AX.X, op=ALU.add), reads=[t_g], writes=[t_g])
                S.dve(lambda e, m=m: e.reciprocal(out=zs[0:m], in_=zs[0:m]), reads=[t_g], writes=[t_g])
                S.dve(lambda e, m=m: e.tensor_tensor(out=gt_[0:m], in0=gt_[0:m], in1=bc(zs[0:m].unsqueeze(2), [m, 8, 16]), op=ALU.mult), reads=[t_g], writes=[t_g])
                for srcI, dstT, rd in ((I1, I1T, t_I), (I2, I2T, t_I), (gt_, gT, t_g)):
                    S.pe(lambda e, srcI=srcI, m=m: e.transpose(B[0][:, 0:m], srcI[0:m].rearrange("p h k -> p (h k)"), ident[0:m, 0:m]),
                         reads=[rd, t_const], writes=[bt[0]])
                    S.act(lambda e, dstT=dstT, tt0=tt0, m=m: e.activation(out=dstT[:, tt0:tt0 + m], in_=B[0][:, 0:m], func=AF.Copy), reads=[bt[0]], writes=[t_IT])
            if debug and gi == 0:
                for qq, tl in enumerate((I1T, I2T, gT)):
                    S.dma(lambda e, qq=qq, tl=tl: e.dma_start(out=O["dbg_IT"][qq], in_=tl[:]), reads=[t_IT])
            ck(6.2)
            for n0 in range(0, n, 4):
                bi = (n0 // 4) % 2
                for q in range(4):
                    nn = n0 + q
                    S.dve(lambda e, bi=bi, q=q, nn=nn: e.tensor_scalar(out=A4[bi][:, q, :], in0=iotaR[:], scalar1=I1T[:, nn:nn + 1], scalar2=gT[:, nn:nn + 1],
                                                                        op0=ALU.is_equal, op1=ALU.mult), reads=[t_IT, t_pc], writes=[t_A4[bi]])
                    S.pool(lambda e, bi=bi, q=q, nn=nn: e.tensor_scalar(out=B4[bi][:, q, :], in0=iotaR[:], scalar1=I2T[:, nn:nn + 1], scalar2=None,
                                                                         op0=ALU.is_equal), reads=[t_IT, t_pc], writes=[t_B4[bi]])
                bk = 6 + bi
                for q in range(4):
                    S.pe(lambda e, bi=bi, q=q, bk=bk: e.matmul(B[bk][:, q * 128:(q + 1) * 128], lhsT=B4[bi][:, q, :], rhs=A4[bi][:, q, :], start=True, stop=True),
                         reads=[t_A4[bi], t_B4[bi]], writes=[bt[bk]])
                S.act(lambda e, bk=bk, n0=n0: e.activation(out=WT[:, :, n0:n0 + 4].rearrange("p i n -> p n i"),
                                                           in_=B[bk][:].rearrange("p (n i) -> p n i", n=4), func=AF.Copy), reads=[bt[bk]], writes=[t_WT])
            ck(6.3)
            for blk in range(64):
                bi = blk % 2
                S.dma(lambda e, bi=bi, blk=blk: e.dma_start(out=eub[bi][:], in_=euv[:, :, blk * 256:(blk + 1) * 256]), writes=[t_eub[bi]])
                S.dma(lambda e, bi=bi, blk=blk: e.dma_start(out=evb[bi][:], in_=evv[:, blk * 2:(blk + 1) * 2, :]), writes=[t_evb[bi]])
                for k2 in range(2):
                    i1 = blk * 2 + k2
                    ui = i1 % 2
                    ubk = 4 + ui
                    for c in range(8):
                        S.pe(lambda e, bi=bi, k2=k2, c=c, ubk=ubk, n=n: e.matmul(B[ubk][:, 0:n], lhsT=eub[bi][:, c, k2 * 128:(k2 + 1) * 128], rhs=h2g[:, c, 0:n],
                                                                                 start=(c == 0), stop=(c == 7)), reads=[t_eub[bi], t_h2], writes=[bt[ubk]])
                    S.act(lambda e, ui=ui, ubk=ubk, n=n: e.activation(out=gU[ui][:, 0:n], in_=B[ubk][:, 0:n], func=AF.Gelu), reads=[bt[ubk]], writes=[t_gU[ui]])
                    eng = S.dve if ui == 0 else S.pool
                    eng(lambda e, ui=ui, i1=i1, n=n: e.tensor_tensor(out=Wg[ui][:, 0:n], in0=gU[ui][:, 0:n], in1=WT[:, i1, 0:n], op=ALU.mult),
                        reads=[t_gU[ui], t_WT], writes=[t_Wg[ui]])
                    for dc in range(8):
                        abk = dc // 2
                        S.pe(lambda e, bi=bi, k2=k2, dc=dc, abk=abk, ui=ui, i1=i1, n=n: e.matmul(
                            B[abk][:, (dc % 2) * 256:(dc % 2) * 256 + n], lhsT=evb[bi][:, k2, dc * 128:(dc + 1) * 128], rhs=Wg[ui][:, 0:n],
                            start=(i1 == 0), stop=(i1 == 127)), reads=[t_evb[bi], t_Wg[ui]], writes=[bt[abk]])
            for dc in range(8):
                abk = dc // 2
                src = B[abk][:, (dc % 2) * 256:(dc % 2) * 256 + n]
                if not samp:
                    S.dve(lambda e, dc=dc, src=src, n=n: e.scalar_tensor_tensor(out=x1g[:, dc, 0:n], in0=src, scalar=modT[:, 40 + dc, 0:1], in1=x1g[:, dc, 0:n],
                                                                                 op0=ALU.mult, op1=ALU.add), reads=[bt[abk], t_mod, t_x1], writes=[t_x1])
                else:
                    S.dve(lambda e, dc=dc, src=src: e.tensor_tensor(out=tmpg[:, dc, 0:NS].rearrange("p (b t) -> p b t", t=4),
                                                                    in0=src.rearrange("p (b t) -> p b t", t=4),
                                                                    in1=bc(modT[:, 40 + dc, 1:17].unsqueeze(2), [128, SB, 4]), op=ALU.mult),
                          reads=[bt[abk], t_mod, t_tmp], writes=[t_tmp])
                    S.dve(lambda e, dc=dc: e.tensor_tensor(out=x1g[:, dc, 0:NS], in0=x1g[:, dc, 0:NS], in1=tmpg[:, dc, 0:NS], op=ALU.add),
                          reads=[t_tmp, t_x1], writes=[t_x1])
            for tt0 in range(0, n, 128):
                m = min(128, n - tt0)
                yi = nyt % 2
                nyt += 1
                for dc in range(8):
                    bk = 4 + dc // 4
                    S.pe(lambda e, dc=dc, bk=bk, tt0=tt0, m=m: e.transpose(B[bk][0:m, (dc % 4) * 128:(dc % 4 + 1) * 128], x1g[:, dc, tt0:tt0 + m], ident[:]),
                         reads=[t_x1, t_const], writes=[bt[bk]])
                for hf in range(2):
                    S.act(lambda e, hf=hf, yi=yi, m=m: e.activation(out=ytk[yi][0:m, hf * 512:(hf + 1) * 512], in_=B[4 + hf][0:m, :], func=AF.Copy),
                          reads=[bt[4 + hf]], writes=[t_ytk[yi]])
                S.dma(lambda e, yi=yi, t0=t0, tt0=tt0, m=m: e.dma_start(out=O["y"][t0 + tt0:t0 + tt0 + m, :], in_=ytk[yi][0:m, :]), reads=[t_ytk[yi]])
            ck(6.5 + 0.01 * gi)
    K.st = st
    S.emit()
    st.close()
    return nc


def host_prep(inp, core):
    f = np.float32
    xp = np.asarray(inp["x_prompt"], f)[core]
    xs = np.asarray(inp["x_sample"], f)[core * SB:(core + 1) * SB].reshape(NS, D)
    xT = np.ascontiguousarray(np.concatenate([xp, xs], 0).T)
    cvec = np.concatenate([np.asarray(inp["c_prompt"], f)[core:core + 1],
                           np.asarray(inp["c_sample"], f)[core * SB:(core + 1) * SB]], 0)
    cT = np.ascontiguousarray(cvec.reshape(17, 8, 128).transpose(2, 1, 0))
    m = {}
    m["xT"] = xT
    m["cT"] = cT
    bsl = slice(core * SB, (core + 1) * SB)
    m["ck_s"] = np.ascontiguousarray(np.asarray(inp["cache_k_win"], f)[0, bsl].reshape(SB, 2048, 512))
    m["cv_s"] = np.ascontiguousarray(np.asarray(inp["cache_v_win"], f)[0, bsl].reshape(SB, 2048, 512))
    m["swkv"] = np.ascontiguousarray(np.asarray(inp["state_wkv"], f)[0, bsl].reshape(128, 4096))
    sh = np.asarray(inp["state_shift"], f)[0, bsl]
    m["shT"] = np.ascontiguousarray(sh[:, 0:1536].reshape(SB, 3, 4, 128).transpose(3, 1, 2, 0))
    m["shTl"] = np.ascontiguousarray(sh[:, 1536:1664].T)
    return m


def _t5_bucket(dist):
    import math
    dist = np.asarray(dist, dtype=np.int64)
    max_exact = 16
    safe = np.maximum(dist, 1) / max_exact
    large = max_exact + (np.log(safe) / math.log(2048 / max_exact) * (32 - max_exact)).astype(np.int64)
    large = np.minimum(large, 31)
    return np.where(dist < max_exact, dist, large).astype(np.int32)


def _ohu_table():
    t = np.zeros((32, 3, 384), np.float32)
    for br, dil in enumerate((1, 4, 16)):
        j = np.arange(129)
        b = _t5_bucket(j * dil)
        t[b, br, j + 127] = 1.0
    return t


def host_shared(inp):
    f = np.float32
    m = {}
    m["ada_w"] = np.ascontiguousarray(np.asarray(inp["ada_w"], f)[0])
    m["ada_bT"] = np.ascontiguousarray(np.asarray(inp["ada_b"], f)[0].reshape(48, 128).T)
    m["n1gT"] = np.ascontiguousarray(np.asarray(inp["norm1_g"], f)[0].reshape(8, 128).T)
    m["n2gT"] = np.ascontiguousarray(np.asarray(inp["norm2_g"], f)[0].reshape(8, 128).T)
    m["w_in"] = np.ascontiguousarray(np.asarray(inp["w_in"], f)[0])
    qg = np.asarray(inp["q_norm_g"], f)[0]
    kg = np.asarray(inp["k_norm_g"], f)[0]
    m["qkg"] = np.ascontiguousarray(np.stack([np.tile(qg, 2), np.tile(kg, 2)], 1))
    m["ident"] = np.eye(128, dtype=f)
    m["ones"] = np.ones((128, 128), f)
    b2 = np.zeros((128, 128), f)
    b2[:64, :64] = 1
    b2[64:, 64:] = 1
    m["blk2"] = b2
    m["relb"] = np.ascontiguousarray(np.asarray(inp["rel_bias"], f))
    m["ohu"] = _ohu_table()

    def pf(v):
        return np.asarray(v, f).reshape(4, 128).T
    mu = np.asarray(inp["mu_shift"], f)[0]
    m["rwp"] = np.ascontiguousarray(np.stack([pf(mu[0:512]), pf(mu[512:1024]), pf(mu[1024:1536]), pf(inp["k_k"][0]), pf(inp["k_a"][0]),
                                              pf(np.asarray(inp["r_k"], f)[0].reshape(512)), pf(inp["a0"][0]), pf(inp["w0"][0])], 1))
    m["mul"] = np.ascontiguousarray(mu[1536:1664].reshape(128, 1))
    m["lw3"] = np.ascontiguousarray(np.concatenate([np.asarray(inp["w_w2"], f)[0], np.asarray(inp["w_a2"], f)[0], np.asarray(inp["w_g2"], f)[0]], 0))
    m["w0row"] = np.ascontiguousarray(np.asarray(inp["w0"], f)[0].reshape(1, 512))
    m["lnx"] = np.ascontiguousarray(np.stack([np.asarray(inp["lnx_g"], f)[0], np.asarray(inp["lnx_b"], f)[0]], 0))
    m["w_out"] = np.ascontiguousarray(np.asarray(inp["w_out"], f)[0])
    m["w_pq"] = np.ascontiguousarray(np.asarray(inp["w_peer_q"], f)[0])
    sk = np.asarray(inp["peer_sub_keys"], f)[0]
    m["skT"] = np.ascontiguousarray(sk.reshape(16, 128, 128).transpose(2, 0, 1))
    m["euT"] = np.ascontiguousarray(np.asarray(inp["expert_u"], f)[0].T)
    m["ev"] = np.ascontiguousarray(np.asarray(inp["expert_v"], f)[0])
    m["qkrow"] = np.ascontiguousarray(np.stack([qg, kg], 0))
    t_ = np.zeros((32, 3, 129), f)
    for br_, dil_ in enumerate((1, 4, 16)):
        jp = np.arange(129)
        t_[_t5_bucket((128 - jp) * dil_), br_, jp] = 1.0
    m["ohs"] = t_
    m["iotaR"] = np.ascontiguousarray(np.tile(np.arange(128, dtype=f)[None, :], (128, 1)))
    NEG = -0.6065306597126334
    i = np.arange(128)[:, None]
    t = np.arange(128)[None, :]
    same = (i // 64) == (t // 64)
    m["tri"] = np.ascontiguousarray(np.stack([np.where(same & (i <= t), NEG, 0.0), np.where(same & (i < t), NEG, 0.0)], 1).astype(f))
    ii = (np.arange(128) % 64)[:, None]
    tt = np.arange(64)[None, :]
    m["mk1"] = np.ascontiguousarray(np.stack([(tt > ii), (tt >= ii)], 1).astype(f))
    m["mk3"] = np.ascontiguousarray((tt < ii).astype(f))
    m["id2"] = np.ascontiguousarray((tt == ii).astype(f))
    return m


def kernel(**inp):
    nc = build()
    shared = host_shared(inp)
    in_maps = []
    for c in range(NCORES):
        m = dict(shared)
        m.update(host_prep(inp, c))
        in_maps.append(m)
    res = run_bass_kernel_spmd(nc, in_maps, core_ids=list(range(NCORES)))
    R = res.results
    f = np.float32
    y_p = np.stack([R[c]["y"][0:T] for c in range(NCORES)], 0).astype(f)
    y_s = np.concatenate([R[c]["y"][T:NT].reshape(SB, 4, D) for c in range(NCORES)], 0).astype(f)
    kwp = np.stack([R[c]["kwp"].reshape(2048, 8, 64) for c in range(NCORES)], 0)[None].astype(f)
    vwp = np.stack([R[c]["vwp"].reshape(2048, 8, 64) for c in range(NCORES)], 0)[None].astype(f)
    wkvp = np.stack([R[c]["wkvp"].reshape(2, 64, 4, 64).transpose(2, 0, 3, 1).reshape(8, 64, 64) for c in range(NCORES)], 0)[None].astype(f)
    shp = np.stack([R[c]["shp"].reshape(CR) for c in range(NCORES)], 0)[None].astype(f)
    kws = np.concatenate([R[c]["kws"].reshape(SB, 4, 8, 64) for c in range(NCORES)], 0)[None].astype(f)
    vws = np.concatenate([R[c]["vws"].reshape(SB, 4, 8, 64) for c in range(NCORES)], 0)[None].astype(f)
    wkvs = np.concatenate([R[c]["wkvs"].reshape(SB, 8, 64, 64) for c in range(NCORES)], 0)[None].astype(f)
    shs = np.concatenate([R[c]["shs"].reshape(SB, CR) for c in range(NCORES)], 0)[None].astype(f)
    return (y_p, y_s, kwp, vwp, wkvp, shp, kws, vws, wkvs, shs)
```

```python
import contextlib
import numpy as np
import concourse.bass as bass
import concourse.mybir as mybir
from concourse.bass_utils import run_bass_kernel_spmd

F32 = mybir.dt.float32
BF16 = mybir.dt.bfloat16
I32 = mybir.dt.int32
U32 = mybir.dt.uint32
AF = mybir.ActivationFunctionType
ALU = mybir.AluOpType
AX = mybir.AxisListType

N_DMA_SLOTS = 24
PE_NOSYNC = True
NCORES = 8
D = 1024
T = 4096
NS = 64
NT = T + NS
SB = 16
DIN = 3200
CR = 1664
EPS = 1e-6


class Tok:
    __slots__ = ("lw", "rd", "rd_dma", "name", "excl")

    def __init__(self, name="", excl=False):
        self.excl = excl
        self.lw = None
        self.rd = {}
        self.rd_dma = []
        self.name = name


class Sched:
    ENGS = ("pe", "act", "dve", "pool", "sp")

    def __init__(self, nc):
        self.nc = nc
        self.ins = []
        self.last_by_eng = {}
        self.dmas_since = []
        self.nosync = False

    def barrier(self):
        deps = set(self.last_by_eng.values()) | set(self.dmas_since)
        self.dmas_since = []
        for e in self.ENGS:
            idx = len(self.ins)
            self.ins.append([e, (lambda eh: eh.nop()), set(deps), False, False, 0, 0, False])
            self.last_by_eng[e] = idx

    def op(self, eng, fn, reads=(), writes=(), dma=False, strided=False):
        idx = len(self.ins)
        deps = set()
        for t in reads:
            if t.lw is not None:
                deps.add(t.lw)
            if t.excl:
                deps.update(v for kk_, v in t.rd.items() if kk_ != eng)
        for t in writes:
            if t.lw is not None:
                deps.add(t.lw)
            deps.update(t.rd.values())
            deps.update(t.rd_dma)
        for t in reads:
            if dma:
                t.rd_dma.append(idx)
            else:
                t.rd[eng] = idx
        for t in writes:
            t.lw = idx
            t.rd = {}
            t.rd_dma = []
        deps.discard(idx)
        self.ins.append([eng, fn, deps, dma, False, 0, 0, strided or (not self.nosync)])
        if dma:
            self.dmas_since.append(idx)
        else:
            self.last_by_eng[eng] = idx
        return idx

    def pe(self, fn, reads=(), writes=(), strided=False):
        return self.op("pe", fn, reads, writes, strided=strided)

    def act(self, fn, reads=(), writes=()):
        return self.op("act", fn, reads, writes)

    def dve(self, fn, reads=(), writes=()):
        return self.op("dve", fn, reads, writes)

    def pool(self, fn, reads=(), writes=()):
        return self.op("pool", fn, reads, writes)

    def dma(self, fn, reads=(), writes=(), eng="sp"):
        return self.op(eng, fn, reads, writes, dma=True)

    def emit(self):
        nc = self.nc
        ins = self.ins
        for i_, it in enumerate(ins):
            if PE_NOSYNC and it[0] == "pe" and not it[3] and not it[7]:
                it[2] = set(d for d in it[2] if not (ins[d][0] == "pe" and not ins[d][3]))
            for d in it[2]:
                ins[d][4] = True
        last = {}
        for i, it in enumerate(ins):
            if not it[3]:
                last[it[0]] = i
        for i in last.values():
            ins[i][4] = True
        cnt = {e: 0 for e in self.ENGS}
        dma_n = {e: 0 for e in self.ENGS}
        for it in ins:
            e = it[0]
            if it[3]:
                it[4] = True
                it[6] = dma_n[e]
                dma_n[e] += 1
            elif it[4]:
                cnt[e] += 1
                it[5] = cnt[e]
        with contextlib.ExitStack() as st:
            sems = {e: st.enter_context(nc.semaphore("s_" + e)) for e in self.ENGS}
            dsems = {e: [st.enter_context(nc.semaphore("d_%s_%d" % (e, i))) for i in range(N_DMA_SLOTS)]
                     for e in self.ENGS if dma_n[e] > 0}
            block = st.enter_context(nc.Block())
            per_eng = {e: [] for e in self.ENGS}
            for i, it in enumerate(ins):
                per_eng[it[0]].append(i)

            def run(eng_name, eh):
                seen = {e: 0 for e in self.ENGS}
                seen_dma = {}
                for i in per_eng[eng_name]:
                    e, fn, deps, is_dma, sig, c, slot = ins[i][:7]
                    need = {}
                    for d in deps:
                        de, _, _, ddma, _, dc, dslot = ins[d][:7]
                        if ddma:
                            key = (de, dslot % N_DMA_SLOTS)
                            val = 16 * (dslot // N_DMA_SLOTS + 1)
                            if seen_dma.get(key, 0) < val:
                                seen_dma[key] = val
                                need[("d",) + key] = val
                        else:
                            if seen[de] < dc:
                                seen[de] = dc
                                need[("c", de)] = dc
                    if is_dma and slot >= N_DMA_SLOTS:
                        key = (e, slot % N_DMA_SLOTS)
                        val = 16 * (slot // N_DMA_SLOTS)
                        if seen_dma.get(key, 0) < val:
                            seen_dma[key] = val
                            need[("d",) + key] = val
                    for k, v in need.items():
                        if k[0] == "c":
                            eh.wait_ge(sems[k[1]], v)
                        else:
                            eh.wait_ge(dsems[k[1]][k[2]], v)
                    inst = fn(eh)
                    if is_dma:
                        inst.then_inc(dsems[e][slot % N_DMA_SLOTS], 16)
                    elif sig:
                        inst.then_inc(sems[e], 1)
                if eng_name == "sp":
                    for e2 in self.ENGS:
                        if cnt[e2] > 0:
                            eh.wait_ge(sems[e2], cnt[e2])
                        n = dma_n.get(e2, 0)
                        for s in range(min(N_DMA_SLOTS, n)):
                            lastslot = ((n - 1 - s) // N_DMA_SLOTS) * N_DMA_SLOTS + s
                            eh.wait_ge(dsems[e2][s], 16 * (lastslot // N_DMA_SLOTS + 1))

            @block.tensor
            def _(eh):
                run("pe", eh)

            @block.scalar
            def _(eh):
                run("act", eh)

            @block.vector
            def _(eh):
                run("dve", eh)

            @block.gpsimd
            def _(eh):
                run("pool", eh)

            @block.sync
            def _(eh):
                run("sp", eh)


class Ctx:
    pass


def bc(ap, shape):
    return ap.to_broadcast(list(shape))


def build(phase_limit=99, debug=False):
    nc = bass.Bass("TRN2", target_bir_lowering=False)
    S = Sched(nc)
    K = Ctx()
    K.nc, K.S = nc, S

    def din(name, shape, dt=F32):
        return nc.dram_tensor(name, list(shape), dt, kind="ExternalInput").ap()

    def dout(name, shape, dt=F32):
        return nc.dram_tensor(name, list(shape), dt, kind="ExternalOutput").ap()

    I = {}
    I["xT"] = din("xT", [D, NT])
    I["cT"] = din("cT", [128, 8, 17])
    I["ada_w"] = din("ada_w", [D, 6 * D])
    I["ada_bT"] = din("ada_bT", [128, 48])
    I["n1gT"] = din("n1gT", [128, 8])
    I["n2gT"] = din("n2gT", [128, 8])
    I["w_in"] = din("w_in", [D, DIN])
    I["qkg"] = din("qkg", [128, 2])
    I["ident"] = din("ident", [128, 128])
    I["ones"] = din("ones", [128, 128])
    I["blk2"] = din("blk2", [128, 128])
    I["relb"] = din("relb", [32, 8])
    I["ohu"] = din("ohu", [32, 3, 384])
    I["w_out"] = din("w_out", [D, D])
    I["w_pq"] = din("w_pq", [D, 2048])
    I["skT"] = din("skT", [128, 16, 128])
    I["euT"] = din("euT", [D, 16384])
    I["ev"] = din("ev", [16384, D])
    I["iotaR"] = din("iotaR", [128, 128])
    euT_d = nc.dram_tensor("euT_d", [64, 128, 8, 256], BF16, kind="Internal").ap()
    ev_d = nc.dram_tensor("ev_d", [16384, D], BF16, kind="Internal").ap()
    wpq_d = nc.dram_tensor("wpq_d", [D, 2048], BF16, kind="Internal").ap()
    I["ck_s"] = din("ck_s", [SB, 2048, 512])
    I["cv_s"] = din("cv_s", [SB, 2048, 512])
    I["swkv"] = din("swkv", [128, 4096])
    I["shT"] = din("shT", [128, 3, 4, 16])
    I["shTl"] = din("shTl", [128, 16])
    I["qkrow"] = din("qkrow", [2, 64])
    I["ohs"] = din("ohs", [32, 3, 129])
    rws_d = nc.dram_tensor("rws_d", [SB, 8, 4, 6, 64], F32, kind="Internal").ap()
    ys_d = nc.dram_tensor("ys_d", [SB, 8, 4, 64], F32, kind="Internal").ap()
    reck_d = nc.dram_tensor("reck_d", [SB, 8, 512], F32, kind="Internal").ap()
    recv_d = nc.dram_tensor("recv_d", [SB, 8, 512], F32, kind="Internal").ap()
    I["rwp"] = din("rwp", [128, 8, 4])
    I["mul"] = din("mul", [128, 1])
    I["lw3"] = din("lw3", [128, 512])
    I["w0row"] = din("w0row", [1, 512])
    I["lnx"] = din("lnx", [2, 512])
    I["tri"] = din("tri", [128, 2, 128])
    I["mk1"] = din("mk1", [128, 2, 64])
    I["mk3"] = din("mk3", [128, 64])
    I["id2"] = din("id2", [128, 64])
    zscr = nc.dram_tensor("zscr", [24, 256, 384], F32, kind="Internal").ap()
    mixT_d = nc.dram_tensor("mixT_d", [8, 128, NT], BF16, kind="Internal").ap()
    O = {}
    O["kwp"] = dout("kwp", [2048, 512])
    O["vwp"] = dout("vwp", [2048, 512])
    O["kws"] = dout("kws", [NS, 512])
    O["vws"] = dout("vws", [NS, 512])
    O["shp"] = dout("shp", [1, CR])
    O["shs"] = dout("shs", [SB, CR])
    O["wkvp"] = dout("wkvp", [128, 4, 64])
    O["y"] = dout("y", [NT, D])
    O["wkvs"] = dout("wkvs", [128, 4096])
    if debug:
        O["dbg_hT"] = dout("dbg_hT", [128, 8, NT], BF16)
        O["dbg_mod"] = dout("dbg_mod", [128, 48, 17])
        O["dbg_ebt"] = dout("dbg_ebt", [128, 24, 2, 128], BF16)
        O["dbg_mixT"] = dout("dbg_mixT", [4, 128, T], BF16)
        O["dbg_yr"] = dout("dbg_yr", [T, 512])
        O["dbg_IT"] = dout("dbg_IT", [3, 128, 256])

    st = contextlib.ExitStack()

    uid = [0]

    def sbt(stack, name, shape, dt=F32):
        uid[0] += 1
        return stack.enter_context(nc.sbuf_tensor("s%d_%s" % (uid[0], name), list(shape), dt))

    def sb(name, shape, dt=F32):
        return sbt(st, name, shape, dt)

    banks = [st.enter_context(nc.psum_tensor("bank%d" % i, [128, 512], F32)) for i in range(8)]
    bt = [Tok("bank%d" % i, excl=True) for i in range(8)]

    ident = sb("ident", [128, 128])
    ones = sb("ones", [128, 128])
    blk2 = sb("blk2", [128, 128])
    identb = sb("identb", [128, 128], BF16)
    epsc = sb("epsc", [128, 1])
    t_const = Tok("const")
    S.dma(lambda e: e.dma_start(out=ident[:], in_=I["ident"]), writes=[t_const])
    S.dma(lambda e: e.dma_start(out=ones[:], in_=I["ones"]), writes=[t_const])
    S.dma(lambda e: e.dma_start(out=blk2[:], in_=I["blk2"]), writes=[t_const])
    S.pool(lambda e: e.memset(epsc[:], EPS), writes=[t_const])
    S.dve(lambda e: e.tensor_copy(out=identb[:], in_=ident[:]), reads=[t_const], writes=[t_const])

    S.nosync = False
    cT = sb("cT", [128, 8, 17])
    scT = sb("scT", [128, 8, 17])
    adab = sb("adab", [128, 48])
    n1g = sb("n1g", [128, 8])
    n2g = sb("n2g", [128, 8])
    qkg = sb("qkg", [128, 2])
    modT = sb("modT", [128, 48, 17])
    A1 = sb("A1", [128, 8, 17])
    A2 = sb("A2", [128, 8, 17])
    t_small = Tok("small")
    t_mod = Tok("mod")
    for dst, src in ((cT, "cT"), (adab, "ada_bT"), (n1g, "n1gT"), (n2g, "n2gT"), (qkg, "qkg")):
        S.dma(lambda e, dst=dst, src=src: e.dma_start(out=dst[:], in_=I[src]), writes=[t_small])
    S.act(lambda e: e.activation(out=scT[:], in_=cT[:], func=AF.Silu), reads=[t_small], writes=[t_small])
    adaw_v = I["ada_w"].rearrange("(c p) n -> p c n", p=128)
    with contextlib.ExitStack() as st2:
        wb = [sbt(st2, "adaw", [128, 8, 512]) for i in range(2)]
        wt = [Tok("adaw%d" % i) for i in range(2)]
        for nb in range(12):
            w = wb[nb % 2]
            wtk = wt[nb % 2]
            S.dma(lambda e, w=w, nb=nb: e.dma_start(out=w[:], in_=adaw_v[:, :, nb * 512:(nb + 1) * 512]), writes=[wtk])
            bk = nb % 2
            for oc in range(4):
                for c in range(8):
                    S.pe(lambda e, w=w, oc=oc, c=c, bk=bk: e.matmul(
                        banks[bk][:, oc * 32:oc * 32 + 17], lhsT=w[:, c, oc * 128:(oc + 1) * 128], rhs=scT[:, c, :],
                        start=(c == 0), stop=(c == 7)), reads=[wtk, t_small], writes=[bt[bk]])
            for oc in range(4):
                j = nb * 4 + oc
                S.act(lambda e, oc=oc, j=j, bk=bk: e.activation(
                    out=modT[:, j, :], in_=banks[bk][:, oc * 32:oc * 32 + 17], func=AF.Identity, bias=adab[:, j:j + 1]),
                    reads=[bt[bk], t_small], writes=[t_mod])
    S.barrier()
    S.dve(lambda e: e.tensor_scalar(out=A1[:], in0=modT[:, 8:16, :], scalar1=1.0, scalar2=None, op0=ALU.add),
          reads=[t_mod], writes=[t_mod])
    S.dve(lambda e: e.tensor_tensor(out=A1[:], in0=A1[:], in1=bc(n1g[:].unsqueeze(2), [128, 8, 17]), op=ALU.mult),
          reads=[t_mod, t_small], writes=[t_mod])
    S.dve(lambda e: e.tensor_scalar(out=A2[:], in0=modT[:, 32:40, :], scalar1=1.0, scalar2=None, op0=ALU.add),
          reads=[t_mod], writes=[t_mod])
    S.dve(lambda e: e.tensor_tensor(out=A2[:], in0=A2[:], in1=bc(n2g[:].unsqueeze(2), [128, 8, 17]), op=ALU.mult),
          reads=[t_mod, t_small], writes=[t_mod])
    if debug:
        S.dma(lambda e: e.dma_start(out=O["dbg_mod"], in_=modT[:]), reads=[t_mod])

    stH = contextlib.ExitStack()
    stC_holder = []
    hT = sbt(stH, "hT", [128, 8, NT], BF16)
    groups = [(g * 512, 512) for g in range(8)] + [(T, NS)]
    t_h = [Tok("h%d" % g) for g in range(9)]
    xT_v = I["xT"].rearrange("(c p) n -> p c n", p=128)

    def norm_groups(src_v, dst, Aap, Bidx, t_dst, extra_reads=()):
        with contextlib.ExitStack() as st2:
            xg = [sbt(st2, "xg", [128, 8, 512]) for i in range(2)]
            xgt = [Tok() for i in range(2)]
            sq = sbt(st2, "sq", [128, 8, 512])
            sqt = Tok()
            rs = sbt(st2, "rs", [128, 512])
            rst = Tok()
            for g, (t0, n) in enumerate(groups):
                x_ = xg[g % 2]
                xt_ = xgt[g % 2]
                bk = 2 + g % 2
                S.dma(lambda e, x_=x_, t0=t0, n=n: e.dma_start(out=x_[:, :, 0:n], in_=src_v[:, :, t0:t0 + n]),
                      writes=[xt_], reads=list(extra_reads))
                S.act(lambda e, x_=x_, n=n: e.activation(out=sq[:, :, 0:n], in_=x_[:, :, 0:n], func=AF.Square),
                      reads=[xt_], writes=[sqt])
                for c in range(8):
                    S.pe(lambda e, c=c, n=n, bk=bk: e.matmul(banks[bk][:, 0:n], lhsT=ones[:], rhs=sq[:, c, 0:n],
                                                              start=(c == 0), stop=(c == 7)),
                         reads=[sqt, t_const], writes=[bt[bk]])
                S.act(lambda e, n=n, bk=bk: e.activation(out=rs[:, 0:n], in_=banks[bk][:, 0:n], func=AF.Sqrt,
                                                          scale=1.0 / D, bias=epsc[:]),
                      reads=[bt[bk], t_const], writes=[rst])
                S.dve(lambda e, n=n: e.reciprocal(out=rs[:, 0:n], in_=rs[:, 0:n]), reads=[rst], writes=[rst])
                S.dve(lambda e, x_=x_, n=n: e.tensor_tensor(out=x_[:, :, 0:n], in0=x_[:, :, 0:n],
                                                             in1=bc(rs[:, 0:n].unsqueeze(1), [128, 8, n]), op=ALU.mult),
                      reads=[xt_, rst], writes=[xt_])
                if g < 8:
                    for c in range(8):
                        eng = S.dve if c % 2 == 0 else S.pool
                        eng(lambda e, x_=x_, c=c, t0=t0, n=n: e.tensor_scalar(
                            out=dst[:, c, t0:t0 + n], in0=x_[:, c, 0:n], scalar1=Aap[:, c, 0:1],
                            scalar2=modT[:, Bidx + c, 0:1], op0=ALU.mult, op1=ALU.add),
                            reads=[xt_, t_mod], writes=[t_dst[g]])
                else:
                    xv = x_[:, :, 0:NS].rearrange("p c (b t) -> p c b t", t=4)
                    S.dve(lambda e, xv=xv: e.tensor_tensor(
                        out=xv, in0=xv, in1=bc(Aap[:, :, 1:17].unsqueeze(3), [128, 8, SB, 4]), op=ALU.mult),
                        reads=[xt_, t_mod], writes=[xt_])
                    S.dve(lambda e, xv=xv, t0=t0: e.tensor_tensor(
                        out=dst[:, :, t0:t0 + NS].rearrange("p c (b t) -> p c b t", t=4), in0=xv,
                        in1=bc(modT[:, Bidx:Bidx + 8, 1:17].unsqueeze(3), [128, 8, SB, 4]), op=ALU.add),
                        reads=[xt_, t_mod], writes=[t_dst[g]])

    norm_groups(xT_v, hT, A1, 0, t_h)
    S.barrier()
    if debug:
        S.dma(lambda e: e.dma_start(out=O["dbg_hT"], in_=hT[:]), reads=t_h)


    if phase_limit < 3:
        S.emit()
        for sk_ in (stC_holder + [stH, st]):
            sk_.close()
        return nc
    S.nosync = False
    stC = contextlib.ExitStack()
    stC_holder.append(stC)
    EBT = sbt(stC, "EBT", [128, 24, 2, 128], BF16)
    onesb = sbt(stC, "onesb", [128, 64], BF16)
    t_ebt = Tok("ebt")
    g0_cst = [sbt(stC, "cst", [128, 1024]) for i in range(3)]
    g0_cbf = [sbt(stC, "cbf", [128, 1024], BF16) for i in range(3)]
    g0_tcs = [Tok() for i in range(3)]
    g0_tcb = [Tok() for i in range(3)]
    g0_blocks = []
    euv_src = I["euT"].rearrange("(c p) e -> p c e", p=128)
    for b_ in range(128):
        g0_blocks.append((lambda i, b_=b_: (g0_cst[i][:].rearrange("p (c e) -> p c e", c=8), euv_src[:, :, b_ * 128:(b_ + 1) * 128]),
                          lambda i, b_=b_: (euT_d[b_ // 2][:, :, (b_ % 2) * 128:(b_ % 2 + 1) * 128], g0_cbf[i][:].rearrange("p (c e) -> p c e", c=8))))
    for src_, dstd_, nblk_ in ((I["ev"], ev_d, 128), (I["w_pq"], wpq_d, 16)):
        sv_ = src_.rearrange("a b -> (a b)").rearrange("(n p f) -> n p f", p=128, f=1024)
        dv_ = dstd_.rearrange("a b -> (a b)").rearrange("(n p f) -> n p f", p=128, f=1024)
        for b_ in range(nblk_):
            g0_blocks.append((lambda i, b_=b_, sv_=sv_: (g0_cst[i][:], sv_[b_]), lambda i, b_=b_, dv_=dv_: (dv_[b_], g0_cbf[i][:])))
    g0_tick = [0]

    def g0_step():
        t = g0_tick[0]
        g0_tick[0] += 1
        nb_ = len(g0_blocks)
        if t - 2 >= 0 and t - 2 < nb_:
            k = t - 2
            i = k % 3
            o_, i_ = g0_blocks[k][1](i)
            S.dma(lambda e, o_=o_, i_=i_: e.dma_start(out=o_, in_=i_), reads=[g0_tcb[i]])
        if t < nb_:
            i = t % 3
            o_, i_ = g0_blocks[t][0](i)
            S.dma(lambda e, o_=o_, i_=i_: e.dma_start(out=o_, in_=i_), writes=[g0_tcs[i]])
        if t - 1 >= 0 and t - 1 < nb_:
            i = (t - 1) % 3
            S.pool(lambda e, i=i: e.tensor_copy(out=g0_cbf[i][:], in_=g0_cst[i][:]), reads=[g0_tcs[i]], writes=[g0_tcb[i]])
    S.dve(lambda e: e.tensor_copy(out=onesb[:], in_=ones[:, 0:64]), reads=[t_const], writes=[t_const])
    with contextlib.ExitStack() as st2:
        relb = sbt(st2, "relb", [32, 8])
        ohu = sbt(st2, "ohu", [32, 3, 384])
        RH = sbt(st2, "RH", [32, 8, 384])
        grep = [sbt(st2, "grep", [128, 384]) for i in range(2)]
        gt = [Tok(), Tok()]
        ebf = [sbt(st2, "ebf", [128, 2, 128]) for i in range(2)]
        et = [Tok(), Tok()]
        t_r = Tok()
        t_rh = Tok()
        S.dma(lambda e: e.dma_start(out=relb[:], in_=I["relb"]), writes=[t_r])
        S.dma(lambda e: e.dma_start(out=ohu[:], in_=I["ohu"]), writes=[t_r])
        S.act(lambda e: e.activation(out=relb[:], in_=relb[:], func=AF.Exp), reads=[t_r], writes=[t_r])
        zt = Tok()
        for br in range(3):
            S.dve(lambda e, br=br: e.tensor_tensor(out=RH[:], in0=bc(relb[:].unsqueeze(2), [32, 8, 384]),
                                                    in1=bc(ohu[:, br, :].unsqueeze(1), [32, 8, 384]), op=ALU.mult),
                  reads=[t_r], writes=[t_rh])
            for h in range(8):
                i = br * 8 + h
                bk = 6 + i % 2
                S.pe(lambda e, h=h, bk=bk: e.matmul(banks[bk][:, 0:384], lhsT=ones[0:32, :], rhs=RH[:, h, :],
                                                     start=True, stop=True), reads=[t_rh, t_const], writes=[bt[bk]])
                g_ = grep[i % 2]
                S.act(lambda e, g_=g_, bk=bk: e.activation(out=g_[:], in_=banks[bk][:, 0:384], func=AF.Copy),
                      reads=[bt[bk]], writes=[gt[i % 2]])
                zi = Tok()
                S.dma(lambda e, g_=g_, i=i: e.dma_start(out=zscr[i, 0:128, :], in_=g_[:]), reads=[gt[i % 2]], writes=[zi])
                S.dma(lambda e, g_=g_, i=i: e.dma_start(out=zscr[i, 128:256, :], in_=g_[:]), reads=[gt[i % 2]], writes=[zi])
                eb_ = ebf[i % 2]
                for part in range(2):
                    src = bass.AP(zscr.tensor, i * 256 * 384 + 255 + part * 128 * 383, [[383, 128], [1, 128]])
                    S.dma(lambda e, eb_=eb_, part=part, src=src: e.dma_start(out=eb_[:, part, :], in_=src),
                          reads=[zi], writes=[et[i % 2]])
                S.dve(lambda e, eb_=eb_, i=i: e.tensor_copy(out=EBT[:, i, :, :], in_=eb_[:]), reads=[et[i % 2]], writes=[t_ebt])
    S.barrier()
    if debug:
        S.dma(lambda e: e.dma_start(out=O["dbg_ebt"], in_=EBT[:]), reads=[t_ebt])

    win_v = I["w_in"].rearrange("(c p) n -> p c n", p=128)

    class _Stop(Exception):
        pass

    def ck(x):
        if phase_limit < x:
            raise _Stop()

    with contextlib.ExitStack() as st2, contextlib.suppress(_Stop):
        ck(3.5)
        wst = [sbt(st2, "wst", [128, 8, 128]) for i in range(2)]
        wstt = [Tok(), Tok()]
        wqkv = sbt(st2, "wqkv", [128, 8, 3, 128], BF16)
        t_w = Tok()
        qT = sbt(st2, "qT", [128, T], BF16)
        kT = sbt(st2, "kT", [128, T], BF16)
        t_q = [Tok() for g in range(8)]
        t_k = [Tok() for g in range(8)]
        V = sbt(st2, "V", [128, 3, 32, 128], BF16)
        t_v = [[Tok() for j in range(32)] for br in range(3)]
        accO = sbt(st2, "accO", [128, 2048])
        accS = sbt(st2, "accS", [128, 2048])
        t_acc = Tok()
        sq = sbt(st2, "sq", [128, 512])
        t_sq = Tok()
        rs = sbt(st2, "rs", [128, 512])
        t_rs = Tok()
        kTf = sbt(st2, "kTf", [128, 512])
        t_kf = Tok()
        ktok = [sbt(st2, "ktok", [128, 4, 128]) for i in range(2)]
        t_kt = [Tok(), Tok()]
        vtok = [sbt(st2, "vtok", [128, 4, 128]) for i in range(2)]
        t_vt = [Tok(), Tok()]
        e0 = [sbt(st2, "e0", [128, 512], BF16) for i in range(3)]
        t_e0 = [Tok(), Tok(), Tok()]
        ee = [sbt(st2, "ee", [128, 512], BF16) for i in range(3)]
        t_ee = [Tok(), Tok(), Tok()]
        mixo = sbt(st2, "mixo", [128, 2048], BF16)
        t_mixo = Tok()
        nld = 0
        nblk = 0
        for hp in range(4):
            for wi in range(3):
                w_ = wst[nld % 2]
                wt_ = wstt[nld % 2]
                nld += 1
                col = wi * 512 + hp * 128
                S.dma(lambda e, w_=w_, col=col: e.dma_start(out=w_[:], in_=win_v[:, :, col:col + 128]), writes=[wt_])
                S.pool(lambda e, w_=w_, wi=wi: e.tensor_copy(out=wqkv[:, :, wi, :], in_=w_[:]), reads=[wt_], writes=[t_w])
            for g in range(8):
                for wi, dstT, tks in ((0, qT, t_q), (1, kT, t_k)):
                    bk = 4 + wi
                    for c in range(8):
                        S.pe(lambda e, c=c, wi=wi, g=g, bk=bk: e.matmul(
                            banks[bk][:], lhsT=wqkv[:, c, wi, :], rhs=hT[:, c, g * 512:(g + 1) * 512],
                            start=(c == 0), stop=(c == 7)), reads=[t_w, t_h[g]], writes=[bt[bk]])
                    S.act(lambda e, bk=bk: e.activation(out=sq[:], in_=banks[bk][:], func=AF.Square),
                          reads=[bt[bk]], writes=[t_sq])
                    S.pe(lambda e: e.matmul(banks[6][:], lhsT=blk2[:], rhs=sq[:], start=True, stop=True),
                         reads=[t_sq, t_const], writes=[bt[6]])
                    S.act(lambda e: e.activation(out=rs[:], in_=banks[6][:], func=AF.Sqrt, scale=1.0 / 64, bias=epsc[:]),
                          reads=[bt[6], t_const], writes=[t_rs])
                    S.dve(lambda e: e.reciprocal(out=rs[:], in_=rs[:]), reads=[t_rs], writes=[t_rs])
                    S.dve(lambda e, bk=bk, wi=wi, g=g, dstT=dstT: e.scalar_tensor_tensor(
                        out=dstT[:, g * 512:(g + 1) * 512], in0=banks[bk][:], scalar=qkg[:, wi:wi + 1], in1=rs[:],
                        op0=ALU.mult, op1=ALU.mult), reads=[bt[bk], t_rs, t_small], writes=[tks[g]])
                    if wi == 1 and g >= 4:
                        S.dve(lambda e, bk=bk: e.scalar_tensor_tensor(
                            out=kTf[:], in0=banks[bk][:], scalar=qkg[:, 1:2], in1=rs[:], op0=ALU.mult, op1=ALU.mult),
                            reads=[bt[bk], t_rs, t_small], writes=[t_kf])
                        for j in range(4):
                            S.pe(lambda e, j=j: e.transpose(banks[7][:, j * 128:(j + 1) * 128], kTf[:, j * 128:(j + 1) * 128], ident[:]),
                                 reads=[t_kf, t_const], writes=[bt[7]])
                        kt_ = ktok[g % 2]
                        S.act(lambda e, kt_=kt_: e.activation(out=kt_[:].rearrange("p a b -> p (a b)"), in_=banks[7][:], func=AF.Copy),
                              reads=[bt[7]], writes=[t_kt[g % 2]])
                        r0 = g * 512 - 2048
                        S.dma(lambda e, kt_=kt_, r0=r0, hp=hp: e.dma_start(
                            out=O["kwp"][r0:r0 + 512, hp * 128:(hp + 1) * 128].rearrange("(j p) c -> p j c", p=128), in_=kt_[:]),
                            reads=[t_kt[g % 2]])
            ck(3.6)
            for br, dil in enumerate((1, 4, 16)):
                if br == 1:
                    ck(3.62)
                G = 32 // dil
                for j0 in range(0, 32, 4):
                    bk = 6 + (j0 // 4) % 2
                    for jj in range(4):
                        j = j0 + jj
                        r, g = j // G, j % G
                        start = r + dil * 128 * g
                        tg = sorted(set([(start) // 512, (start + dil * 127) // 512]))
                        for c in range(8):
                            S.pe(lambda e, c=c, jj=jj, start=start, dil=dil, bk=bk: e.matmul(
                                banks[bk][:, jj * 128:(jj + 1) * 128],
                                lhsT=hT[:, c, start:start + dil * 127 + 1:dil], rhs=wqkv[:, c, 2, :],
                                start=(c == 0), stop=(c == 7)), reads=[t_w] + [t_h[x] for x in tg], writes=[bt[bk]], strided=(dil > 1))
                    S.act(lambda e, br=br, j0=j0, bk=bk: e.activation(
                        out=V[:, br, j0:j0 + 4, :].rearrange("p a b -> p (a b)"), in_=banks[bk][:], func=AF.Copy),
                        reads=[bt[bk]], writes=[t_v[br][j0 + x] for x in range(4)])
                    if br == 0 and j0 >= 16 :
                        vt_ = vtok[(j0 // 4) % 2]
                        tv_ = t_vt[(j0 // 4) % 2]
                        S.act(lambda e, vt_=vt_, bk=bk: e.activation(out=vt_[:].rearrange("p a b -> p (a b)"), in_=banks[bk][:], func=AF.Copy),
                              reads=[bt[bk]], writes=[tv_])
                        r0 = j0 * 128 - 2048
                        S.dma(lambda e, vt_=vt_, r0=r0, hp=hp: e.dma_start(
                            out=O["vwp"][r0:r0 + 512, hp * 128:(hp + 1) * 128].rearrange("(j p) c -> p j c", p=128), in_=vt_[:]),
                            reads=[tv_])
            ck(3.7)
            for half in range(2):
                for br, dil in enumerate((1, 4, 16)):
                    G = 32 // dil
                    Gh = G // 2
                    for r in range(dil):
                        for g in range(half * Gh, (half + 1) * Gh):
                            j = r * G + g
                            start = r + dil * 128 * g
                            qsl = slice(start, start + dil * 127 + 1, dil)
                            tgq = sorted(set([start // 512, (start + dil * 127) // 512]))
                            parts = [1] if g == 0 else [0, 1]
                            g0_step()
                            sbk = nblk % 3
                            obk = 3 + nblk % 3
                            ei = nblk % 3
                            nblk += 1
                            for hh in range(2):
                                ps_ = slice(hh * 64, hh * 64 + 64)
                                for part in parts:
                                    if part == 1:
                                        ksl = qsl
                                        tgk = tgq
                                    else:
                                        ps0 = start - dil * 128
                                        ksl = slice(ps0, ps0 + dil * 127 + 1, dil)
                                        tgk = sorted(set([ps0 // 512, (ps0 + dil * 127) // 512]))
                                    S.pe(lambda e, ps_=ps_, ksl=ksl, qsl=qsl, hh=hh, part=part, sbk=sbk: e.matmul(
                                        banks[sbk][:, (hh * 2 + part) * 128:(hh * 2 + part + 1) * 128],
                                        lhsT=kT[ps_, ksl], rhs=qT[ps_, qsl], start=True, stop=True),
                                        reads=[t_k[x] for x in tgk] + [t_q[x] for x in tgq], writes=[bt[sbk]], strided=(dil > 1))
                            S.act(lambda e, ei=ei, sbk=sbk: e.activation(out=e0[ei][:], in_=banks[sbk][:], func=AF.Exp, scale=0.125),
                                  reads=[bt[sbk]], writes=[t_e0[ei]])
                            S.dve(lambda e, ei=ei, br=br, hp=hp: e.tensor_tensor(
                                out=ee[ei][:], in0=e0[ei][:],
                                in1=EBT[:, br * 8 + 2 * hp:br * 8 + 2 * hp + 2, :, :].rearrange("p a b c -> p (a b c)"), op=ALU.mult),
                                reads=[t_e0[ei], t_ebt], writes=[t_ee[ei]])
                            for hh in range(2):
                                po = slice(hh * 64, hh * 64 + 64)
                                for pi, part in enumerate(parts):
                                    jj = j if part == 1 else j - 1
                                    esl = slice((hh * 2 + part) * 128, (hh * 2 + part + 1) * 128)
                                    S.pe(lambda e, po=po, br=br, jj=jj, hh=hh, esl=esl, ei=ei, obk=obk, pi=pi, parts=parts: e.matmul(
                                        banks[obk][po, 0:128], lhsT=V[:, br, jj, hh * 64:(hh + 1) * 64], rhs=ee[ei][:, esl],
                                        start=(pi == 0), stop=(pi == len(parts) - 1)),
                                        reads=[t_v[br][jj], t_ee[ei]], writes=[bt[obk]])
                                for pi, part in enumerate(parts):
                                    esl = slice((hh * 2 + part) * 128, (hh * 2 + part + 1) * 128)
                                    S.pe(lambda e, po=po, esl=esl, ei=ei, obk=obk, pi=pi, parts=parts: e.matmul(
                                        banks[obk][po, 128:256], lhsT=onesb[:], rhs=ee[ei][:, esl],
                                        start=(pi == 0), stop=(pi == len(parts) - 1)),
                                        reads=[t_const, t_ee[ei]], writes=[bt[obk]])
                            lo = start - half * 2048
                            asl = slice(lo, lo + dil * 127 + 1, dil)
                            if br == 0:
                                S.dve(lambda e, asl=asl, obk=obk: e.tensor_copy(out=accO[:, asl], in_=banks[obk][:, 0:128]),
                                      reads=[bt[obk]], writes=[t_acc])
                                S.dve(lambda e, asl=asl, obk=obk: e.tensor_copy(out=accS[:, asl], in_=banks[obk][:, 128:256]),
                                      reads=[bt[obk]], writes=[t_acc])
                            else:
                                S.dve(lambda e, asl=asl, obk=obk: e.tensor_tensor(out=accO[:, asl], in0=accO[:, asl], in1=banks[obk][:, 0:128], op=ALU.add),
                                      reads=[bt[obk], t_acc], writes=[t_acc])
                                S.dve(lambda e, asl=asl, obk=obk: e.tensor_tensor(out=accS[:, asl], in0=accS[:, asl], in1=banks[obk][:, 128:256], op=ALU.add),
                                      reads=[bt[obk], t_acc], writes=[t_acc])
                S.dve(lambda e: e.reciprocal(out=accS[:], in_=accS[:]), reads=[t_acc], writes=[t_acc])
                S.dve(lambda e: e.tensor_tensor(out=mixo[:], in0=accO[:], in1=accS[:], op=ALU.mult), reads=[t_acc], writes=[t_mixo, t_acc])
                S.dma(lambda e, hp=hp, half=half: e.dma_start(out=mixT_d[hp, :, half * 2048:(half + 1) * 2048], in_=mixo[:]), reads=[t_mixo])
                if debug:
                    S.dma(lambda e, hp=hp, half=half: e.dma_start(out=O["dbg_mixT"][hp, :, half * 2048:(half + 1) * 2048], in_=mixo[:]), reads=[t_mixo])
                ck(3.8)

    while g0_tick[0] < len(g0_blocks) + 2:
        g0_step()
    S.barrier()
    stC.close()
    if phase_limit < 4:
        S.emit()
        for sk_ in (stC_holder + [stH, st]):
            sk_.close()
        return nc
    S.nosync = False
    NEG = -0.6065306597126334
    with contextlib.ExitStack() as st2, contextlib.suppress(_Stop):
        t_rp = Tok("rwparams")
        rwp = sbt(st2, "rwp", [128, 8, 4])
        mul_ = sbt(st2, "mul", [128, 1])
        lw3 = sbt(st2, "lw3", [128, 512])
        w0row = sbt(st2, "w0row", [1, 512])
        lnxg = sbt(st2, "lnxg", [128, 512])
        lnxb = sbt(st2, "lnxb", [128, 512])
        tri = sbt(st2, "tri", [128, 2, 128])
        mk1 = sbt(st2, "mk1", [128, 2, 64])
        mk3 = sbt(st2, "mk3", [128, 64])
        id2 = sbt(st2, "id2", [128, 64])
        omka = sbt(st2, "omka", [128, 4])
        gneps = sbt(st2, "gneps", [128, 1])
        for dst, src in ((rwp, "rwp"), (mul_, "mul"), (lw3, "lw3"), (w0row, "w0row"), (tri, "tri"), (mk1, "mk1"), (mk3, "mk3"), (id2, "id2")):
            S.dma(lambda e, dst=dst, src=src: e.dma_start(out=dst[:], in_=I[src]), writes=[t_rp])
        S.dma(lambda e: e.dma_start(out=lnxg[:], in_=bass.AP(I["lnx"].tensor, 0, [[0, 128], [1, 512]])), writes=[t_rp])
        S.dma(lambda e: e.dma_start(out=lnxb[:], in_=bass.AP(I["lnx"].tensor, 512, [[0, 128], [1, 512]])), writes=[t_rp])
        S.dve(lambda e: e.tensor_scalar(out=omka[:], in0=rwp[:, 4, :], scalar1=-1.0, scalar2=1.0, op0=ALU.mult, op1=ALU.add),
              reads=[t_rp], writes=[t_rp])
        S.pool(lambda e: e.memset(gneps[:], 64e-5), writes=[t_rp])
        wr = sbt(st2, "wr", [128, 8, 1664], BF16)
        t_wr = Tok()
        with contextlib.ExitStack() as st3:
            wst2 = [sbt(st3, "wst2", [128, 8, 416]) for i in range(2)]
            wst2t = [Tok(), Tok()]
            for q4 in range(4):
                w_ = wst2[q4 % 2]
                S.dma(lambda e, w_=w_, q4=q4: e.dma_start(out=w_[:], in_=win_v[:, :, 1536 + q4 * 416:1536 + (q4 + 1) * 416]), writes=[wst2t[q4 % 2]])
                S.pool(lambda e, w_=w_, q4=q4: e.tensor_copy(out=wr[:, :, q4 * 416:(q4 + 1) * 416], in_=w_[:]), reads=[wst2t[q4 % 2]], writes=[t_wr])
        S.barrier()

        def T_(n=""):
            return Tok(n)

        pb = [sbt(st2, "pb", [128, 4, 129]) for x in range(3)]
        pbl = sbt(st2, "pbl", [128, 129])
        t_pb = T_()
        for x in range(3):
            S.pool(lambda e, x=x: e.memset(pb[x][:, :, 0:1], 0.0), writes=[t_pb])
        S.pool(lambda e: e.memset(pbl[:, 0:1], 0.0), writes=[t_pb])
        xm = [sbt(st2, "xm", [128, 4, 128]) for x in range(3)]
        xml = sbt(st2, "xml", [128, 128])
        t_xm = T_()
        twl = sbt(st2, "twl", [128, 128])
        sg_tok = sbt(st2, "sg_tok", [128, 512])
        aT = sbt(st2, "aT", [128, 4, 128])
        g_tok = sbt(st2, "g_tok", [128, 512])
        kk = sbt(st2, "kk", [128, 4, 128])
        sq4 = sbt(st2, "sq4", [128, 4, 128])
        kmod = sbt(st2, "kmod", [128, 4, 128])
        bb = sbt(st2, "bb", [128, 4, 128])
        rk = sbt(st2, "rk", [128, 4, 128])
        dtmp = rk
        bsum = sbt(st2, "bsum", [128, 8])
        ycen = sbt(st2, "ycen", [128, 8, 64])
        ysq = sbt(st2, "ysq", [128, 8, 64])
        gst = sbt(st2, "gst", [128, 8])
        gst2 = sbt(st2, "gst2", [128, 8])
        mixr = sbt(st2, "mixr", [128, 4, 128], BF16)
        st4 = contextlib.ExitStack()
        st2.enter_context(st4)
        Pin = sbt(st4, "Pin", [128, 4, 128])
        Pinv = sbt(st4, "Pinv", [128, 4, 128])
        Pex = sbt(st4, "Pex", [128, 4, 128])
        Phat = sbt(st4, "Phat", [128, 4, 128])
        PCl = sbt(st4, "PCl", [128, 4, 2])
        PC = sbt(st4, "PC", [128, 4, 2])
        AR = sbt(st4, "AR", [128, 4, 2, 2, 64])
        BtT = sbt(st4, "BtT", [128, 4, 128])
        AtT = sbt(st4, "AtT", [128, 4, 128])
        KtT = sbt(st4, "KtT", [128, 4, 128])
        BhT = sbt(st4, "BhT", [128, 4, 128])
        KhT = sbt(st4, "KhT", [128, 4, 128])
        Atok = sbt(st4, "Atok", [128, 512])
        Bhtok = sbt(st4, "Bhtok", [128, 512])
        Khtok = sbt(st4, "Khtok", [128, 512])
        Vtok = sbt(st4, "Vtok", [128, 512])
        NM = sbt(st4, "NM", [128, 8, 2, 64])
        AK = sbt(st4, "AK", [128, 8, 2, 64])
        Aj = [sbt(st4, "Aj", [128, 8, 64])] * 2
        Nj = [sbt(st4, "Nj", [128, 8, 64])] * 2
        Tj = [sbt(st4, "Tj", [128, 8, 64])] * 2
        Z = sbt(st4, "Z", [128, 8, 128])
        AV = sbt(st4, "AV", [128, 8, 128])
        McT = sbt(st4, "McT", [128, 4, 2, 64])
        dPC = sbt(st4, "dPC", [128, 4, 2, 64])
        RpT = sbt(st4, "RpT", [128, 4, 2, 64])
        Hs = sbt(st4, "Hs", [128, 4, 64])
        t_H = T_()
        S.pool(lambda e: e.memset(Hs[:], 0.0), writes=[t_H])
        (t_lora, t_sg, t_a, t_g, t_kk, t_km, t_b, t_rk, t_bs, t_P, t_AR, t_BK, t_BKh, t_tok, t_NM, t_AK, t_A0, t_Z, t_AV,
         t_Mc, t_Rp, t_y, t_mixr) = [T_() for i in range(23)]
        t_Aj = [T_()] * 2
        t_Nj = [T_()] * 2
        t_Tj = [T_()] * 2
        B = banks

        def v4(ap):
            return ap.rearrange("p q (c t) -> p q c t", c=2)

        for sbi in range(32):
            t0 = sbi * 128
            hg = t_h[t0 // 512]
            for x in range(3):
                bk = x
                for p in range(4):
                    for c in range(8):
                        S.pe(lambda e, x=x, p=p, c=c, bk=bk, t0=t0: e.matmul(
                            B[bk][:, p * 128:(p + 1) * 128], lhsT=wr[:, c, x * 512 + p * 128:x * 512 + (p + 1) * 128],
                            rhs=hT[:, c, t0:t0 + 128], start=(c == 0), stop=(c == 7)), reads=[t_wr, hg], writes=[bt[bk]])
                S.act(lambda e, x=x, bk=bk: e.activation(out=pb[x][:, :, 1:129], in_=B[bk][:].rearrange("p (q t) -> p q t", q=4), func=AF.Copy),
                      reads=[bt[bk], t_xm], writes=[t_pb])
            for c in range(8):
                S.pe(lambda e, c=c, t0=t0: e.matmul(B[3][:, 0:128], lhsT=wr[:, c, 1536:1664], rhs=hT[:, c, t0:t0 + 128],
                                                     start=(c == 0), stop=(c == 7)), reads=[t_wr, hg], writes=[bt[3]])
            S.act(lambda e: e.activation(out=pbl[:, 1:129], in_=B[3][:, 0:128], func=AF.Copy), reads=[bt[3], t_xm], writes=[t_pb])
            if sbi == 31:
                for x in range(3):
                    S.dma(lambda e, x=x: e.dma_start(
                        out=bass.AP(O["shp"].tensor, x * 512, [[1, 128], [128, 4], [1, 1]]), in_=pb[x][:, :, 128:129], allow_slow_non_contiguous=True), reads=[t_pb])
                S.dma(lambda e: e.dma_start(out=bass.AP(O["shp"].tensor, 1536, [[1, 128], [1, 1]]), in_=pbl[:, 128:129], allow_slow_non_contiguous=True), reads=[t_pb])
            for x in range(3):
                S.dve(lambda e, x=x: e.tensor_tensor(out=dtmp[:], in0=pb[x][:, :, 0:128], in1=pb[x][:, :, 1:129], op=ALU.subtract),
                      reads=[t_pb], writes=[t_xm, t_rk])
                S.dve(lambda e, x=x: e.tensor_tensor(out=dtmp[:], in0=dtmp[:], in1=bc(rwp[:, x, :].unsqueeze(2), [128, 4, 128]), op=ALU.mult),
                      reads=[t_xm, t_rp, t_rk], writes=[t_xm, t_rk])
                S.dve(lambda e, x=x: e.tensor_tensor(out=xm[x][:], in0=dtmp[:], in1=pb[x][:, :, 1:129], op=ALU.add),
                      reads=[t_xm, t_pb, t_rk], writes=[t_xm])
            S.dve(lambda e: e.tensor_tensor(out=xml[:], in0=pbl[:, 0:128], in1=pbl[:, 1:129], op=ALU.subtract), reads=[t_pb], writes=[t_lora])
            S.dve(lambda e: e.scalar_tensor_tensor(out=xml[:], in0=xml[:], scalar=mul_[:, 0:1], in1=pbl[:, 1:129], op0=ALU.mult, op1=ALU.add),
                  reads=[t_lora, t_pb, t_rp], writes=[t_lora])
            for x in range(3):
                S.pool(lambda e, x=x: e.tensor_copy(out=pb[x][:, :, 0:1], in_=pb[x][:, :, 128:129]), reads=[t_pb, t_xm], writes=[t_pb])
            S.pool(lambda e: e.tensor_copy(out=pbl[:, 0:1], in_=pbl[:, 128:129]), reads=[t_pb, t_lora], writes=[t_pb])
            S.act(lambda e: e.activation(out=twl[0:32, :], in_=xml[0:32, :], func=AF.Tanh), reads=[t_lora], writes=[t_sg])
            S.act(lambda e: e.activation(out=twl[64:128, :], in_=xml[64:128, :], func=AF.Sigmoid), reads=[t_lora], writes=[t_sg])
            S.pe(lambda e: e.matmul(B[4][:], lhsT=twl[0:32, :], rhs=lw3[0:32, :], start=True, stop=False), reads=[t_sg, t_rp], writes=[bt[4]])
            S.pe(lambda e: e.matmul(B[4][:], lhsT=ones[0:1, :], rhs=w0row[0:1, :], start=False, stop=True), reads=[t_const, t_rp], writes=[bt[4]])
            S.act(lambda e: e.activation(out=sg_tok[:], in_=B[4][:], func=AF.Sigmoid), reads=[bt[4]], writes=[t_sg])
            for p in range(4):
                S.pe(lambda e, p=p: e.matmul(B[5][:, p * 128:(p + 1) * 128], lhsT=lw3[32:64, p * 128:(p + 1) * 128], rhs=xml[32:64, :],
                                              start=True, stop=True), reads=[t_lora, t_rp], writes=[bt[5]])
            S.dve(lambda e: e.tensor_tensor(out=aT[:], in0=B[5][:].rearrange("p (q t) -> p q t", q=4),
                                            in1=bc(rwp[:, 6, :].unsqueeze(2), [128, 4, 128]), op=ALU.add), reads=[bt[5], t_rp], writes=[t_a])
            S.act(lambda e: e.activation(out=aT[:], in_=aT[:], func=AF.Sigmoid), reads=[t_a], writes=[t_a])
            S.pe(lambda e: e.matmul(B[6][:], lhsT=twl[64:128, :], rhs=lw3[64:128, :], start=True, stop=True), reads=[t_sg, t_rp], writes=[bt[6]])
            S.act(lambda e: e.activation(out=g_tok[:], in_=B[6][:], func=AF.Copy), reads=[bt[6], t_y], writes=[t_g])
            S.dve(lambda e: e.tensor_tensor(out=kk[:], in0=xm[1][:], in1=bc(rwp[:, 3, :].unsqueeze(2), [128, 4, 128]), op=ALU.mult),
                  reads=[t_xm, t_rp], writes=[t_kk])
            S.act(lambda e: e.activation(out=sq4[:], in_=kk[:], func=AF.Square), reads=[t_kk], writes=[t_kk])
            S.pe(lambda e: e.matmul(B[7][:], lhsT=blk2[:], rhs=sq4[:].rearrange("p q t -> p (q t)"), start=True, stop=True),
                 reads=[t_kk, t_const], writes=[bt[7]])
            S.act(lambda e: e.activation(out=sq4[:].rearrange("p q t -> p (q t)"), in_=B[7][:], func=AF.Sqrt), reads=[bt[7]], writes=[t_kk])
            S.dve(lambda e: e.tensor_scalar(out=sq4[:], in0=sq4[:], scalar1=1e-12, scalar2=None, op0=ALU.max), reads=[t_kk], writes=[t_kk])
            S.dve(lambda e: e.reciprocal(out=sq4[:], in_=sq4[:]), reads=[t_kk], writes=[t_kk])
            S.dve(lambda e: e.tensor_tensor(out=kk[:], in0=kk[:], in1=sq4[:], op=ALU.mult), reads=[t_kk], writes=[t_kk])
            S.dve(lambda e: e.tensor_tensor(out=kmod[:], in0=aT[:], in1=bc(rwp[:, 4, :].unsqueeze(2), [128, 4, 128]), op=ALU.mult),
                  reads=[t_a, t_rp], writes=[t_km])
            S.dve(lambda e: e.tensor_tensor(out=kmod[:], in0=kmod[:], in1=bc(omka[:].unsqueeze(2), [128, 4, 128]), op=ALU.add),
                  reads=[t_km, t_rp], writes=[t_km])
            S.dve(lambda e: e.tensor_tensor(out=kmod[:], in0=kmod[:], in1=xm[1][:], op=ALU.mult), reads=[t_km, t_xm], writes=[t_km])
            S.dve(lambda e: e.tensor_tensor(out=bb[:], in0=kk[:], in1=aT[:], op=ALU.mult), reads=[t_kk, t_a], writes=[t_b])
            S.pool(lambda e: e.tensor_tensor(out=rk[:], in0=xm[0][:], in1=kmod[:], op=ALU.mult), reads=[t_xm, t_km], writes=[t_rk])
            S.pool(lambda e: e.tensor_tensor(out=rk[:], in0=rk[:], in1=bc(rwp[:, 5, :].unsqueeze(2), [128, 4, 128]), op=ALU.mult),
                   reads=[t_rk, t_rp], writes=[t_rk])
            for h in range(8):
                p, hh = h // 2, h % 2
                fp = slice(hh * 64, hh * 64 + 64)
                S.pe(lambda e, p=p, fp=fp, h=h: e.matmul(B[6][:, 256 + h:256 + h + 1], lhsT=rk[fp, p, :], rhs=ones[fp, 0:1], start=True, stop=True),
                     reads=[t_rk, t_const], writes=[bt[6]])
            S.act(lambda e: e.activation(out=bsum[:], in_=B[6][:, 256:264], func=AF.Copy), reads=[bt[6], t_y], writes=[t_bs])
            for p in range(4):
                S.pe(lambda e, p=p: e.matmul(B[0][:, p * 128:(p + 1) * 128], lhsT=sg_tok[:, p * 128:(p + 1) * 128], rhs=tri[:, 0, :],
                                              start=True, stop=True), reads=[t_sg, t_rp], writes=[bt[0]])
            for p in range(4):
                S.pe(lambda e, p=p: e.matmul(B[1][:, p * 128:(p + 1) * 128], lhsT=sg_tok[:, p * 128:(p + 1) * 128], rhs=tri[:, 1, :],
                                              start=True, stop=True), reads=[t_sg, t_rp], writes=[bt[1]])
            lp = B[0][:].rearrange("p (q t) -> p q t", q=4)
            S.act(lambda e: e.activation(out=Pin[:], in_=lp, func=AF.Exp), reads=[bt[0]], writes=[t_P])
            S.act(lambda e: e.activation(out=Pinv[:], in_=lp, func=AF.Exp, scale=-1.0), reads=[bt[0]], writes=[t_P])
            S.act(lambda e: e.activation(out=Pex[:], in_=B[1][:].rearrange("p (q t) -> p q t", q=4), func=AF.Exp), reads=[bt[1]], writes=[t_P])
            lp4 = B[0][:].rearrange("p (q c t) -> p q c t", q=4, c=2)
            S.act(lambda e: e.activation(out=PCl[:], in_=lp4[:, :, :, 63], func=AF.Copy), reads=[bt[0]], writes=[t_P])
            S.act(lambda e: e.activation(out=v4(Phat[:]), in_=lp4, func=AF.Copy), reads=[bt[0]], writes=[t_P])
            S.dve(lambda e: e.tensor_tensor(out=v4(Phat[:]), in0=bc(PCl[:].unsqueeze(3), [128, 4, 2, 64]), in1=v4(Phat[:]), op=ALU.subtract),
                  reads=[t_P], writes=[t_P])
            S.act(lambda e: e.activation(out=Phat[:], in_=Phat[:], func=AF.Exp), reads=[t_P], writes=[t_P])
            S.act(lambda e: e.activation(out=PC[:], in_=PCl[:], func=AF.Exp), reads=[t_P], writes=[t_P])
            S.dve(lambda e: e.scalar_tensor_tensor(out=AR[:, :, :, 0, :], in0=v4(kk[:]), scalar=-1.0, in1=v4(Pex[:]), op0=ALU.mult, op1=ALU.mult),
                  reads=[t_kk, t_P], writes=[t_AR])
            S.dve(lambda e: e.tensor_tensor(out=AR[:, :, :, 1, :], in0=v4(xm[0][:]), in1=v4(Pin[:]), op=ALU.mult), reads=[t_xm, t_P], writes=[t_AR])
            S.pool(lambda e: e.tensor_tensor(out=BtT[:], in0=bb[:], in1=Pinv[:], op=ALU.mult), reads=[t_b, t_P], writes=[t_BK])
            S.pool(lambda e: e.tensor_tensor(out=KtT[:], in0=kmod[:], in1=Pinv[:], op=ALU.mult), reads=[t_km, t_P], writes=[t_BK])
            S.pool(lambda e: e.tensor_tensor(out=BhT[:], in0=bb[:], in1=Phat[:], op=ALU.mult), reads=[t_b, t_P], writes=[t_BKh])
            S.pool(lambda e: e.tensor_tensor(out=KhT[:], in0=kmod[:], in1=Phat[:], op=ALU.mult), reads=[t_km, t_P], writes=[t_BKh])
            S.pool(lambda e: e.tensor_copy(out=v4(AtT[:]), in_=AR[:, :, :, 0, :]), reads=[t_AR], writes=[t_BKh])
            for src_fn, dst, rd, bk in ((lambda p: AtT[:, p, :], Atok, [t_BKh], 2), (lambda p: BhT[:, p, :], Bhtok, [t_BKh], 3),
                                        (lambda p: KhT[:, p, :], Khtok, [t_BKh], 4), (lambda p: xm[2][:, p, :], Vtok, [t_xm], 5)):
                for p in range(4):
                    S.pe(lambda e, p=p, src_fn=src_fn, bk=bk: e.transpose(B[bk][:, p * 128:(p + 1) * 128], src_fn(p), ident[:]),
                         reads=rd + [t_const], writes=[bt[bk]])
                S.act(lambda e, dst=dst, bk=bk: e.activation(out=dst[:], in_=B[bk][:], func=AF.Copy), reads=[bt[bk], t_y, t_AV, t_Z], writes=[t_tok])
            for ch in range(2):
                tp = slice(ch * 64, ch * 64 + 64)
                for h in range(8):
                    p, hh = h // 2, h % 2
                    fp = slice(hh * 64, hh * 64 + 64)
                    csl = slice(ch * 64, ch * 64 + 64)
                    arr = AR[fp, p, ch, :, :].rearrange("p a t -> p (a t)")
                    S.pe(lambda e, tp=tp, fp=fp, p=p, h=h, csl=csl, arr=arr: e.matmul(
                        B[0][tp, (h % 4) * 128:(h % 4 + 1) * 128] if h < 4 else B[1][tp, (h % 4) * 128:(h % 4 + 1) * 128],
                        lhsT=BtT[fp, p, csl], rhs=arr, start=True, stop=True), reads=[t_BK, t_AR], writes=[bt[0 if h < 4 else 1]])
                    S.pe(lambda e, tp=tp, fp=fp, p=p, h=h, csl=csl, arr=arr: e.matmul(
                        B[2][tp, (h % 4) * 128:(h % 4 + 1) * 128] if h < 4 else B[3][tp, (h % 4) * 128:(h % 4 + 1) * 128],
                        lhsT=KtT[fp, p, csl], rhs=arr, start=True, stop=True), reads=[t_BK, t_AR], writes=[bt[2 if h < 4 else 3]])
                    S.pe(lambda e, tp=tp, fp=fp, p=p, h=h, csl=csl, ch=ch: e.matmul(
                        B[4][tp, h * 64:(h + 1) * 64], lhsT=AR[fp, p, ch, 0, :], rhs=BtT[fp, p, csl], start=True, stop=True),
                        reads=[t_BK, t_AR], writes=[bt[4]])
            mk1b = bc(mk1[:].unsqueeze(1), [128, 4, 2, 64])
            for hf in range(2):
                S.dve(lambda e, hf=hf: e.tensor_tensor(out=NM[:, hf * 4:(hf + 1) * 4, :, :], in0=B[hf][:].rearrange("p (h a t) -> p h a t", h=4, a=2),
                                                        in1=mk1b, op=ALU.mult), reads=[bt[hf], t_rp], writes=[t_NM])
                S.dve(lambda e, hf=hf: e.tensor_tensor(out=AK[:, hf * 4:(hf + 1) * 4, :, :], in0=B[2 + hf][:].rearrange("p (h a t) -> p h a t", h=4, a=2),
                                                        in1=mk1b, op=ALU.mult), reads=[bt[2 + hf], t_rp], writes=[t_AK])
            S.dve(lambda e: e.tensor_tensor(out=Aj[0][:], in0=B[4][:].rearrange("p (h t) -> p h t", h=8), in1=bc(mk3[:].unsqueeze(1), [128, 8, 64]), op=ALU.mult),
                  reads=[bt[4], t_rp], writes=[t_Aj[0]])
            S.pool(lambda e: e.tensor_copy(out=Nj[0][:], in_=NM[:, :, 0, :]), reads=[t_NM], writes=[t_Nj[0]])
            S.pool(lambda e: e.tensor_tensor(out=Tj[0][:], in0=NM[:, :, 0, :], in1=bc(id2[:].unsqueeze(1), [128, 8, 64]), op=ALU.add),
                   reads=[t_NM, t_rp], writes=[t_Tj[0]])
            for lv in range(1, 6):
                a_o = Aj[0]
                n_o = Nj[0]
                t_o = Tj[0]
                ta, tn, tt = t_Aj[0], t_Nj[0], t_Tj[0]
                for ch in range(2):
                    tp = slice(ch * 64, ch * 64 + 64)
                    for h in range(8):
                        S.pe(lambda e, tp=tp, h=h: e.matmul(B[5][tp, h * 64:(h + 1) * 64], lhsT=n_o[tp, h, :], rhs=a_o[tp, h, :],
                                                             start=True, stop=True), reads=[ta, tn], writes=[bt[5]])
                if lv < 5:
                    for ch in range(2):
                        tp = slice(ch * 64, ch * 64 + 64)
                        for h in range(8):
                            S.pe(lambda e, tp=tp, h=h: e.matmul(B[6][tp, h * 64:(h + 1) * 64], lhsT=a_o[tp, h, :], rhs=n_o[tp, h, :],
                                                                 start=True, stop=True), reads=[ta, tn], writes=[bt[6]])
                S.act(lambda e: e.activation(out=a_o[:].rearrange("p h t -> p (h t)"), in_=B[5][:], func=AF.Copy), reads=[bt[5]], writes=[ta])
                if lv < 5:
                    S.act(lambda e: e.activation(out=n_o[:].rearrange("p h t -> p (h t)"), in_=B[6][:], func=AF.Copy), reads=[bt[6]], writes=[tn])
                for ch in range(2):
                    tp = slice(ch * 64, ch * 64 + 64)
                    for h in range(8):
                        S.pe(lambda e, tp=tp, h=h: e.matmul(B[7][tp, h * 64:(h + 1) * 64], lhsT=a_o[tp, h, :], rhs=t_o[tp, h, :],
                                                             start=True, stop=True), reads=[ta, tt], writes=[bt[7]])
                S.dve(lambda e: e.tensor_tensor(out=t_o[:], in0=B[7][:].rearrange("p (h t) -> p h t", h=8), in1=t_o[:], op=ALU.add),
                      reads=[bt[7], tt], writes=[tt])
            TT = Tj[5 % 2]
            t_TT = t_Tj[5 % 2]
            for ch in range(2):
                tp = slice(ch * 64, ch * 64 + 64)
                for h in range(8):
                    S.pe(lambda e, tp=tp, h=h: e.matmul(B[4][tp, h * 64:(h + 1) * 64], lhsT=AK[tp, h, 0, :], rhs=Vtok[tp, h * 64:(h + 1) * 64],
                                                         start=True, stop=True), reads=[t_AK, t_tok], writes=[bt[4]])
            S.act(lambda e: e.activation(out=Z[:, :, 64:128], in_=B[4][:].rearrange("p (h t) -> p h t", h=8), func=AF.Copy), reads=[bt[4]], writes=[t_Z])
            S.pool(lambda e: e.tensor_copy(out=Z[:, :, 0:64], in_=Atok[:].rearrange("p (h t) -> p h t", h=8)), reads=[t_tok], writes=[t_Z])
            for ch in range(2):
                tp = slice(ch * 64, ch * 64 + 64)
                for h in range(8):
                    S.pe(lambda e, tp=tp, h=h: e.matmul(B[h // 4][tp, (h % 4) * 128:(h % 4 + 1) * 128], lhsT=TT[tp, h, :], rhs=Z[tp, h, :],
                                                         start=True, stop=True), reads=[t_TT, t_Z], writes=[bt[h // 4]])
            for hf in range(2):
                S.act(lambda e, hf=hf: e.activation(out=AV[:, hf * 4:(hf + 1) * 4, :].rearrange("p h t -> p (h t)"), in_=B[hf][:], func=AF.Copy),
                      reads=[bt[hf]], writes=[t_AV])
            for ch in range(2):
                tp = slice(ch * 64, ch * 64 + 64)
                for h in range(8):
                    p, hh = h // 2, h % 2
                    fp = slice(hh * 64, hh * 64 + 64)
                    col = (p * 2 + ch) * 64
                    S.pe(lambda e, tp=tp, fp=fp, h=h, col=col: e.matmul(B[2][fp, col:col + 64], lhsT=AV[tp, h, 0:64], rhs=Bhtok[tp, h * 64:(h + 1) * 64],
                                                                         start=True, stop=True), reads=[t_AV, t_tok], writes=[bt[2]])
                    S.pe(lambda e, tp=tp, fp=fp, h=h, col=col: e.matmul(B[3][fp, col:col + 64], lhsT=AV[tp, h, 0:64], rhs=NM[tp, h, 1, :],
                                                                         start=True, stop=True), reads=[t_AV, t_NM], writes=[bt[3]])
            S.dve(lambda e: e.tensor_tensor(out=dPC[:], in0=bc(PC[:].unsqueeze(3), [128, 4, 2, 64]),
                                            in1=bc(id2[:].unsqueeze(1).unsqueeze(1), [128, 4, 2, 64]), op=ALU.mult), reads=[t_P, t_rp], writes=[t_Mc])
            S.dve(lambda e: e.tensor_tensor(out=McT[:], in0=B[2][:].rearrange("p (q c t) -> p q c t", q=4, c=2), in1=dPC[:], op=ALU.add),
                  reads=[bt[2], t_Mc], writes=[t_Mc])
            S.dve(lambda e: e.tensor_tensor(out=RpT[:], in0=B[3][:].rearrange("p (q c t) -> p q c t", q=4, c=2), in1=AR[:, :, :, 1, :], op=ALU.add),
                  reads=[bt[3], t_AR], writes=[t_Rp])
            for ch in range(2):
                tp = slice(ch * 64, ch * 64 + 64)
                for h in range(8):
                    p, hh = h // 2, h % 2
                    fp = slice(hh * 64, hh * 64 + 64)
                    S.pe(lambda e, tp=tp, h=h: e.matmul(B[6][tp, h * 64:(h + 1) * 64], lhsT=NM[tp, h, 1, :], rhs=AV[tp, h, 64:128], start=True, stop=False),
                         reads=[t_NM, t_AV], writes=[bt[6]])
                    S.pe(lambda e, tp=tp, h=h: e.matmul(B[6][tp, h * 64:(h + 1) * 64], lhsT=AK[tp, h, 1, :], rhs=Vtok[tp, h * 64:(h + 1) * 64], start=False, stop=False),
                         reads=[t_AK, t_tok], writes=[bt[6]])
                    S.pe(lambda e, tp=tp, fp=fp, p=p, h=h, ch=ch: e.matmul(B[6][tp, h * 64:(h + 1) * 64], lhsT=RpT[fp, p, ch, :], rhs=Hs[fp, p, :], start=False, stop=True),
                         reads=[t_Rp, t_H], writes=[bt[6]])
                for h in range(8):
                    p, hh = h // 2, h % 2
                    fp = slice(hh * 64, hh * 64 + 64)
                    S.pe(lambda e, tp=tp, fp=fp, p=p, h=h: e.matmul(B[5][fp, p * 64:(p + 1) * 64], lhsT=Bhtok[tp, h * 64:(h + 1) * 64], rhs=AV[tp, h, 64:128], start=True, stop=False),
                         reads=[t_tok, t_AV], writes=[bt[5]])
                    S.pe(lambda e, tp=tp, fp=fp, p=p, h=h: e.matmul(B[5][fp, p * 64:(p + 1) * 64], lhsT=Khtok[tp, h * 64:(h + 1) * 64], rhs=Vtok[tp, h * 64:(h + 1) * 64], start=False, stop=False),
                         reads=[t_tok], writes=[bt[5]])
                    S.pe(lambda e, fp=fp, p=p, ch=ch: e.matmul(B[5][fp, p * 64:(p + 1) * 64], lhsT=McT[fp, p, ch, :], rhs=Hs[fp, p, :], start=False, stop=True),
                         reads=[t_Mc, t_H], writes=[bt[5]])
                S.act(lambda e: e.activation(out=Hs[:].rearrange("p q v -> p (q v)"), in_=B[5][:, 0:256], func=AF.Copy), reads=[bt[5]], writes=[t_H])
            yps = B[6][:].rearrange("p (h v) -> p h v", h=8)
            S.dve(lambda e: e.tensor_reduce(out=gst[:], in_=yps, axis=AX.X, op=ALU.add), reads=[bt[6]], writes=[t_y])
            S.dve(lambda e: e.tensor_scalar(out=gst[:], in0=gst[:], scalar1=1.0 / 64, scalar2=None, op0=ALU.mult), reads=[t_y], writes=[t_y])
            S.dve(lambda e: e.tensor_tensor(out=ycen[:], in0=yps, in1=bc(gst[:].unsqueeze(2), [128, 8, 64]), op=ALU.subtract), reads=[t_y, bt[6]], writes=[t_y])
            S.act(lambda e: e.activation(out=ysq[:], in_=ycen[:], func=AF.Square), reads=[t_y], writes=[t_y])
            S.dve(lambda e: e.tensor_reduce(out=gst2[:], in_=ysq[:], axis=AX.X, op=ALU.add), reads=[t_y], writes=[t_y])
            S.act(lambda e: e.activation(out=gst2[:], in_=gst2[:], func=AF.Sqrt, scale=1.0 / 64, bias=gneps[:]), reads=[t_y, t_rp], writes=[t_y])
            S.dve(lambda e: e.reciprocal(out=gst2[:], in_=gst2[:]), reads=[t_y], writes=[t_y])
            S.dve(lambda e: e.tensor_tensor(out=ycen[:], in0=ycen[:], in1=bc(gst2[:].unsqueeze(2), [128, 8, 64]), op=ALU.mult), reads=[t_y], writes=[t_y])
            yc2 = ycen[:].rearrange("p h v -> p (h v)")
            S.dve(lambda e: e.tensor_tensor(out=yc2, in0=yc2, in1=lnxg[:], op=ALU.mult), reads=[t_y, t_rp], writes=[t_y])
            S.dve(lambda e: e.tensor_tensor(out=yc2, in0=yc2, in1=lnxb[:], op=ALU.add), reads=[t_y, t_rp], writes=[t_y])
            S.pool(lambda e: e.tensor_tensor(out=ysq[:], in0=Vtok[:].rearrange("p (h v) -> p h v", h=8), in1=bc(bsum[:].unsqueeze(2), [128, 8, 64]), op=ALU.mult),
                   reads=[t_tok, t_bs, t_y], writes=[t_y])
            S.dve(lambda e: e.tensor_tensor(out=ycen[:], in0=ycen[:], in1=ysq[:], op=ALU.add), reads=[t_y], writes=[t_y])
            S.dve(lambda e: e.tensor_tensor(out=yc2, in0=yc2, in1=g_tok[:], op=ALU.mult), reads=[t_y, t_g], writes=[t_y])
            if debug:
                S.dma(lambda e, t0=t0: e.dma_start(out=O["dbg_yr"][t0:t0 + 128, :], in_=yc2), reads=[t_y])
            for p in range(4):
                S.pe(lambda e, p=p: e.transpose(B[7][:, p * 128:(p + 1) * 128], ycen[:, 2 * p:2 * p + 2, :].rearrange("p h v -> p (h v)"), ident[:]),
                     reads=[t_y, t_const], writes=[bt[7]])
            S.act(lambda e: e.activation(out=mixr[:].rearrange("p q t -> p (q t)"), in_=B[7][:], func=AF.Copy), reads=[bt[7]], writes=[t_mixr])
            S.dma(lambda e, t0=t0: e.dma_start(out=mixT_d[4:8, :, t0:t0 + 128].rearrange("q p t -> p q t"), in_=mixr[:]), reads=[t_mixr])
            ck(4.0 + 0.01 * (sbi + 1))
        S.dma(lambda e: e.dma_start(out=O["wkvp"], in_=Hs[:]), reads=[t_H])

        S.barrier()
        st4.close()
        ck(4.95)
        NSS = 64
        hs_perm = lambda c: hT[:, c, T:T + NS].rearrange("p (b t) -> p t b", t=4)
        for x in range(3):
            S.dma(lambda e, x=x: e.dma_start(out=pb[x][:, :, 0:16], in_=I["shT"][:, x, :, :]), writes=[t_pb])
        S.dma(lambda e: e.dma_start(out=pbl[:, 0:16], in_=I["shTl"]), writes=[t_pb])
        for x in range(3):
            bk = x
            for p in range(4):
                for c in range(8):
                    S.pe(lambda e, x=x, p=p, c=c, bk=bk: e.matmul(
                        B[bk][:, p * 128:p * 128 + NSS], lhsT=wr[:, c, x * 512 + p * 128:x * 512 + (p + 1) * 128],
                        rhs=hs_perm(c), start=(c == 0), stop=(c == 7)), reads=[t_wr, t_h[8]], writes=[bt[bk]])
            S.act(lambda e, x=x, bk=bk: e.activation(out=pb[x][:, :, 16:80], in_=B[bk][:].rearrange("p (q t) -> p q t", q=4)[:, :, 0:NSS], func=AF.Copy),
                  reads=[bt[bk], t_xm], writes=[t_pb])
        for c in range(8):
            S.pe(lambda e, c=c: e.matmul(B[3][:, 0:NSS], lhsT=wr[:, c, 1536:1664], rhs=hs_perm(c), start=(c == 0), stop=(c == 7)),
                 reads=[t_wr, t_h[8]], writes=[bt[3]])
        S.act(lambda e: e.activation(out=pbl[:, 16:80], in_=B[3][:, 0:NSS], func=AF.Copy), reads=[bt[3], t_xm], writes=[t_pb])
        shrow = sbt(st2, "shrow", [16, 1664])
        t_shrow = T_()
        hl = sbt(st2, "hl", [128, 8, 16], BF16)
        t_hl = T_()
        S.pool(lambda e: e.tensor_copy(out=hl[:], in_=hT[:, :, T + 3:T + NS:4]), reads=[t_h[8]], writes=[t_hl])
        for q4 in range(4):
            for c in range(8):
                S.pe(lambda e, q4=q4, c=c: e.matmul(B[4][0:16, 0:416], lhsT=hl[:, c, :], rhs=wr[:, c, q4 * 416:(q4 + 1) * 416],
                                                    start=(c == 0), stop=(c == 7)), reads=[t_wr, t_hl], writes=[bt[4]])
            S.act(lambda e, q4=q4: e.activation(out=shrow[:, q4 * 416:(q4 + 1) * 416], in_=B[4][0:16, 0:416], func=AF.Copy), reads=[bt[4]], writes=[t_shrow])
        S.dma(lambda e: e.dma_start(out=O["shs"], in_=shrow[:]), reads=[t_shrow])
        n_ = NSS
        for x in range(3):
            S.dve(lambda e, x=x: e.tensor_tensor(out=dtmp[:, :, 0:n_], in0=pb[x][:, :, 0:n_], in1=pb[x][:, :, 16:16 + n_], op=ALU.subtract),
                  reads=[t_pb], writes=[t_xm, t_rk])
            S.dve(lambda e, x=x: e.tensor_tensor(out=dtmp[:, :, 0:n_], in0=dtmp[:, :, 0:n_], in1=bc(rwp[:, x, :].unsqueeze(2), [128, 4, n_]), op=ALU.mult),
                  reads=[t_xm, t_rp, t_rk], writes=[t_xm, t_rk])
            S.dve(lambda e, x=x: e.tensor_tensor(out=xm[x][:, :, 0:n_], in0=dtmp[:, :, 0:n_], in1=pb[x][:, :, 16:16 + n_], op=ALU.add),
                  reads=[t_xm, t_pb, t_rk], writes=[t_xm])
        S.dve(lambda e: e.tensor_tensor(out=xml[:, 0:n_], in0=pbl[:, 0:n_], in1=pbl[:, 16:16 + n_], op=ALU.subtract), reads=[t_pb], writes=[t_lora])
        S.dve(lambda e: e.scalar_tensor_tensor(out=xml[:, 0:n_], in0=xml[:, 0:n_], scalar=mul_[:, 0:1], in1=pbl[:, 16:16 + n_], op0=ALU.mult, op1=ALU.add),
              reads=[t_lora, t_pb, t_rp], writes=[t_lora])
        S.act(lambda e: e.activation(out=twl[0:32, 0:n_], in_=xml[0:32, 0:n_], func=AF.Tanh), reads=[t_lora], writes=[t_sg])
        S.act(lambda e: e.activation(out=twl[64:128, 0:n_], in_=xml[64:128, 0:n_], func=AF.Sigmoid), reads=[t_lora], writes=[t_sg])
        S.pe(lambda e: e.matmul(B[4][0:n_, :], lhsT=twl[0:32, 0:n_], rhs=lw3[0:32, :], start=True, stop=False), reads=[t_sg, t_rp], writes=[bt[4]])
        S.pe(lambda e: e.matmul(B[4][0:n_, :], lhsT=ones[0:1, 0:n_], rhs=w0row[0:1, :], start=False, stop=True), reads=[t_const, t_rp], writes=[bt[4]])
        S.act(lambda e: e.activation(out=sg_tok[0:n_, :], in_=B[4][0:n_, :], func=AF.Sigmoid), reads=[bt[4]], writes=[t_sg])
        for p in range(4):
            S.pe(lambda e, p=p: e.matmul(B[5][:, p * 128:p * 128 + n_], lhsT=lw3[32:64, p * 128:(p + 1) * 128], rhs=xml[32:64, 0:n_],
                                          start=True, stop=True), reads=[t_lora, t_rp], writes=[bt[5]])
        S.dve(lambda e: e.tensor_tensor(out=aT[:, :, 0:n_], in0=B[5][:].rearrange("p (q t) -> p q t", q=4)[:, :, 0:n_],
                                        in1=bc(rwp[:, 6, :].unsqueeze(2), [128, 4, n_]), op=ALU.add), reads=[bt[5], t_rp], writes=[t_a])
        S.act(lambda e: e.activation(out=aT[:, :, 0:n_], in_=aT[:, :, 0:n_], func=AF.Sigmoid), reads=[t_a], writes=[t_a])
        S.pe(lambda e: e.matmul(B[6][0:n_, :], lhsT=twl[64:128, 0:n_], rhs=lw3[64:128, :], start=True, stop=True), reads=[t_sg, t_rp], writes=[bt[6]])
        S.act(lambda e: e.activation(out=g_tok[0:n_, :], in_=B[6][0:n_, :], func=AF.Copy), reads=[bt[6], t_y], writes=[t_g])
        S.dve(lambda e: e.tensor_tensor(out=kk[:, :, 0:n_], in0=xm[1][:, :, 0:n_], in1=bc(rwp[:, 3, :].unsqueeze(2), [128, 4, n_]), op=ALU.mult),
              reads=[t_xm, t_rp], writes=[t_kk])
        S.act(lambda e: e.activation(out=sq4[:, :, 0:n_], in_=kk[:, :, 0:n_], func=AF.Square), reads=[t_kk], writes=[t_kk])
        for p in range(4):
            S.pe(lambda e, p=p: e.matmul(B[7][:, p * 128:p * 128 + n_], lhsT=blk2[:], rhs=sq4[:, p, 0:n_], start=True, stop=True),
                 reads=[t_kk, t_const], writes=[bt[7]])
        S.act(lambda e: e.activation(out=sq4[:, :, 0:n_], in_=B[7][:].rearrange("p (q t) -> p q t", q=4)[:, :, 0:n_], func=AF.Sqrt), reads=[bt[7]], writes=[t_kk])
        S.dve(lambda e: e.tensor_scalar(out=sq4[:, :, 0:n_], in0=sq4[:, :, 0:n_], scalar1=1e-12, scalar2=None, op0=ALU.max), reads=[t_kk], writes=[t_kk])
        S.dve(lambda e: e.reciprocal(out=sq4[:, :, 0:n_], in_=sq4[:, :, 0:n_]), reads=[t_kk], writes=[t_kk])
        S.dve(lambda e: e.tensor_tensor(out=kk[:, :, 0:n_], in0=kk[:, :, 0:n_], in1=sq4[:, :, 0:n_], op=ALU.mult), reads=[t_kk], writes=[t_kk])
        S.dve(lambda e: e.tensor_tensor(out=kmod[:, :, 0:n_], in0=aT[:, :, 0:n_], in1=bc(rwp[:, 4, :].unsqueeze(2), [128, 4, n_]), op=ALU.mult),
              reads=[t_a, t_rp], writes=[t_km])
        S.dve(lambda e: e.tensor_tensor(out=kmod[:, :, 0:n_], in0=kmod[:, :, 0:n_], in1=bc(omka[:].unsqueeze(2), [128, 4, n_]), op=ALU.add),
              reads=[t_km, t_rp], writes=[t_km])
        S.dve(lambda e: e.tensor_tensor(out=kmod[:, :, 0:n_], in0=kmod[:, :, 0:n_], in1=xm[1][:, :, 0:n_], op=ALU.mult), reads=[t_km, t_xm], writes=[t_km])
        S.dve(lambda e: e.tensor_tensor(out=bb[:, :, 0:n_], in0=kk[:, :, 0:n_], in1=aT[:, :, 0:n_], op=ALU.mult), reads=[t_kk, t_a], writes=[t_b])
        S.pool(lambda e: e.tensor_tensor(out=rk[:, :, 0:n_], in0=xm[0][:, :, 0:n_], in1=kmod[:, :, 0:n_], op=ALU.mult), reads=[t_xm, t_km], writes=[t_rk])
        S.pool(lambda e: e.tensor_tensor(out=rk[:, :, 0:n_], in0=rk[:, :, 0:n_], in1=bc(rwp[:, 5, :].unsqueeze(2), [128, 4, n_]), op=ALU.mult),
               reads=[t_rk, t_rp], writes=[t_rk])
        for h in range(8):
            p, hh = h // 2, h % 2
            fp = slice(hh * 64, hh * 64 + 64)
            S.pe(lambda e, p=p, fp=fp, h=h: e.matmul(B[6][0:n_, 256 + h:256 + h + 1], lhsT=rk[fp, p, 0:n_], rhs=ones[fp, 0:1], start=True, stop=True),
                 reads=[t_rk, t_const], writes=[bt[6]])
        S.act(lambda e: e.activation(out=bsum[0:n_, :], in_=B[6][0:n_, 256:264], func=AF.Copy), reads=[bt[6], t_y], writes=[t_bs])
        tok6 = sbt(st2, "tok6", [64, 6, 512])
        t_tok6 = T_()
        S.act(lambda e: e.activation(out=tok6[:, 1, :], in_=sg_tok[0:n_, :], func=AF.Exp, scale=NEG), reads=[t_sg], writes=[t_tok6])
        for xi, (srcT, rd) in enumerate(((xm[0], t_xm), (None, None), (kmod, t_km), (xm[2], t_xm), (kk, t_kk), (bb, t_b))):
            if srcT is None:
                continue
            bk = 2 + xi % 2
            for p in range(4):
                S.pe(lambda e, p=p, srcT=srcT, bk=bk: e.transpose(B[bk][0:n_, p * 128:(p + 1) * 128], srcT[:, p, 0:n_], ident[:]),
                     reads=[rd, t_const], writes=[bt[bk]])
            S.act(lambda e, xi=xi, bk=bk: e.activation(out=tok6[:, xi, :], in_=B[bk][0:n_, :], func=AF.Copy), reads=[bt[bk]], writes=[t_tok6])
        t_rwsd = T_()
        t_rwsd_l = [T_() for i in range(24)]
        for t in range(4):
            for xi in range(6):
                S.dma(lambda e, t=t, xi=xi: e.dma_start(out=rws_d[:, :, t, xi, :], in_=tok6[16 * t:16 * t + 16, xi, :].rearrange("p (h c) -> p h c", h=8)),
                      reads=[t_tok6], writes=[t_rwsd_l[t * 6 + xi]])
        X6 = sbt(st2, "X6", [128, 4, 6, 64])
        St = sbt(st2, "St", [128, 64, 64])
        tmpS = sbt(st2, "tmpS", [128, 64, 64])
        sp_ = sbt(st2, "sp", [128, 64])
        ys = sbt(st2, "ys", [128, 4, 64])
        t_X6, t_St, t_tmpS, t_sp, t_ys = [T_() for i in range(5)]
        S.dma(lambda e: e.dma_start(out=X6[:].rearrange("p t x c -> p (t x c)"), in_=rws_d.rearrange("b h t x c -> (b h) (t x c)")), reads=t_rwsd_l, writes=[t_X6])
        S.dma(lambda e: e.dma_start(out=St[:].rearrange("p v k -> p (v k)"), in_=I["swkv"]), writes=[t_St])
        for t in range(4):
            def bk_(xi, t=t):
                return bc(X6[:, t, xi, :].unsqueeze(1), [128, 64, 64])
            def bv_(ap):
                return bc(ap.unsqueeze(2), [128, 64, 64])
            S.dve(lambda e, t=t: e.tensor_tensor(out=tmpS[:], in0=St[:], in1=bk_(4, t), op=ALU.mult), reads=[t_St, t_X6], writes=[t_tmpS])
            S.dve(lambda e: e.tensor_reduce(out=sp_[:], in_=tmpS[:], axis=AX.X, op=ALU.add), reads=[t_tmpS], writes=[t_sp])
            S.dve(lambda e, t=t: e.tensor_tensor(out=St[:], in0=St[:], in1=bk_(1, t), op=ALU.mult), reads=[t_St, t_X6, t_tmpS], writes=[t_St])
            S.pool(lambda e, t=t: e.tensor_tensor(out=tmpS[:], in0=bv_(sp_[:]), in1=bk_(5, t), op=ALU.mult), reads=[t_sp, t_X6], writes=[t_tmpS])
            S.dve(lambda e: e.tensor_tensor(out=St[:], in0=St[:], in1=tmpS[:], op=ALU.subtract), reads=[t_St, t_tmpS], writes=[t_St])
            S.pool(lambda e, t=t: e.tensor_tensor(out=tmpS[:], in0=bv_(X6[:, t, 3, :]), in1=bk_(2, t), op=ALU.mult), reads=[t_X6, t_St], writes=[t_tmpS])
            S.dve(lambda e: e.tensor_tensor(out=St[:], in0=St[:], in1=tmpS[:], op=ALU.add), reads=[t_St, t_tmpS], writes=[t_St])
            S.dve(lambda e, t=t: e.tensor_tensor(out=tmpS[:], in0=St[:], in1=bk_(0, t), op=ALU.mult), reads=[t_St, t_X6], writes=[t_tmpS])
            S.dve(lambda e, t=t: e.tensor_reduce(out=ys[:, t, :], in_=tmpS[:], axis=AX.X, op=ALU.add), reads=[t_tmpS], writes=[t_ys])
        S.dma(lambda e: e.dma_start(out=O["wkvs"], in_=St[:].rearrange("p v k -> p (v k)")), reads=[t_St])
        t_ysd = T_()
        S.dma(lambda e: e.dma_start(out=ys_d.rearrange("b h t v -> (b h) (t v)"), in_=ys[:].rearrange("p t v -> p (t v)")), reads=[t_ys], writes=[t_ysd])
        ysr = sbt(st2, "ysr", [64, 8, 64])
        t_ysr = T_()
        for t in range(4):
            S.dma(lambda e, t=t: e.dma_start(out=ysr[16 * t:16 * t + 16, :, :], in_=ys_d[:, :, t, :]), reads=[t_ysd], writes=[t_ysr])
        yps_s = ysr[:]
        Y0 = slice(0, 64)
        S.dve(lambda e: e.tensor_reduce(out=gst[Y0], in_=yps_s, axis=AX.X, op=ALU.add), reads=[t_ysr], writes=[t_y])
        S.dve(lambda e: e.tensor_scalar(out=gst[Y0], in0=gst[Y0], scalar1=1.0 / 64, scalar2=None, op0=ALU.mult), reads=[t_y], writes=[t_y])
        S.dve(lambda e: e.tensor_tensor(out=ycen[Y0], in0=yps_s, in1=bc(gst[Y0].unsqueeze(2), [64, 8, 64]), op=ALU.subtract), reads=[t_y, t_ysr], writes=[t_y])
        S.act(lambda e: e.activation(out=ysq[Y0], in_=ycen[Y0], func=AF.Square), reads=[t_y], writes=[t_y])
        S.dve(lambda e: e.tensor_reduce(out=gst2[Y0], in_=ysq[Y0], axis=AX.X, op=ALU.add), reads=[t_y], writes=[t_y])
        S.act(lambda e: e.activation(out=gst2[Y0], in_=gst2[Y0], func=AF.Sqrt, scale=1.0 / 64, bias=gneps[Y0]), reads=[t_y, t_rp], writes=[t_y])
        S.dve(lambda e: e.reciprocal(out=gst2[Y0], in_=gst2[Y0]), reads=[t_y], writes=[t_y])
        S.dve(lambda e: e.tensor_tensor(out=ycen[Y0], in0=ycen[Y0], in1=bc(gst2[Y0].unsqueeze(2), [64, 8, 64]), op=ALU.mult), reads=[t_y], writes=[t_y])
        yc2_s = ycen[Y0].rearrange("p h v -> p (h v)")
        S.dve(lambda e: e.tensor_tensor(out=yc2_s, in0=yc2_s, in1=lnxg[Y0], op=ALU.mult), reads=[t_y, t_rp], writes=[t_y])
        S.dve(lambda e: e.tensor_tensor(out=yc2_s, in0=yc2_s, in1=lnxb[Y0], op=ALU.add), reads=[t_y, t_rp], writes=[t_y])
        S.pool(lambda e: e.tensor_tensor(out=ysq[Y0], in0=tok6[:, 3, :].rearrange("p (h v) -> p h v", h=8), in1=bc(bsum[Y0].unsqueeze(2), [64, 8, 64]), op=ALU.mult),
               reads=[t_tok6, t_bs, t_y], writes=[t_y])
        S.dve(lambda e: e.tensor_tensor(out=ycen[Y0], in0=ycen[Y0], in1=ysq[Y0], op=ALU.add), reads=[t_y], writes=[t_y])
        S.dve(lambda e: e.tensor_tensor(out=yc2_s, in0=yc2_s, in1=g_tok[Y0], op=ALU.mult), reads=[t_y, t_g], writes=[t_y])
        for p in range(4):
            S.pe(lambda e, p=p: e.transpose(B[7][:, p * 128:p * 128 + 64], ycen[Y0, 2 * p:2 * p + 2, :].rearrange("p h v -> p (h v)"), ident[0:64, 0:64]),
                 reads=[t_y, t_const], writes=[bt[7]])
        S.act(lambda e: e.activation(out=mixr[:, :, 0:64].rearrange("p q (b t) -> p q t b", t=4),
                                     in_=B[7][:].rearrange("p (q x) -> p q x", q=4)[:, :, 0:64].rearrange("p q (t b) -> p q t b", t=4), func=AF.Copy),
              reads=[bt[7]], writes=[t_mixr])
        S.dma(lambda e: e.dma_start(out=mixT_d[4:8, :, T:T + NS].rearrange("q p t -> p q t"), in_=mixr[:, :, 0:64]), reads=[t_mixr])


    S.barrier()
    with contextlib.ExitStack() as st2, contextlib.suppress(_Stop):
        ck(5.0)
        wq3 = sbt(st2, "wq3", [128, 8, 1536], BF16)
        t_wq3 = Tok()
        with contextlib.ExitStack() as st3:
            wst3 = [sbt(st3, "wst3", [128, 8, 512]) for i in range(2)]
            wst3t = [Tok(), Tok()]
            for q3 in range(3):
                w_ = wst3[q3 % 2]
                S.dma(lambda e, w_=w_, q3=q3: e.dma_start(out=w_[:], in_=win_v[:, :, q3 * 512:(q3 + 1) * 512]), writes=[wst3t[q3 % 2]])
                S.pool(lambda e, w_=w_, q3=q3: e.tensor_copy(out=wq3[:, :, q3 * 512:(q3 + 1) * 512], in_=w_[:]), reads=[wst3t[q3 % 2]], writes=[t_wq3])
        S.barrier()
        B = banks
        NQ = 64
        hsp = sbt(st2, "hsp", [128, 8, 64], BF16)
        t_hsp = Tok()
        S.pool(lambda e: e.tensor_copy(out=hsp[:].rearrange("p c (s b) -> p c s b", s=4), in_=hT[:, :, T:T + NS].rearrange("p c (b s) -> p c s b", s=4)),
               reads=[t_h[8]], writes=[t_hsp])
        hs_sb = lambda c: hsp[:, c, :]
        qkvs = sbt(st2, "qkvs", [64, 3, 8, 64])
        sqs = sbt(st2, "sqs", [64, 8, 64])
        ssn = sbt(st2, "ssn", [64, 8])
        gqk = sbt(st2, "gqk", [64, 2, 64])
        t_qkv, t_sqs, t_gqk = Tok(), Tok(), Tok()
        for wi in range(2):
            S.dma(lambda e, wi=wi: e.dma_start(out=gqk[:, wi, :], in_=bass.AP(I["qkrow"].tensor, wi * 64, [[0, 64], [1, 64]])), writes=[t_gqk])
        for wi in range(3):
            for c in range(8):
                S.pe(lambda e, wi=wi, c=c: e.matmul(B[wi][0:NQ, :], lhsT=hs_sb(c), rhs=wq3[:, c, wi * 512:(wi + 1) * 512], start=(c == 0), stop=(c == 7)),
                     reads=[t_wq3, t_hsp], writes=[bt[wi]])
            pv = B[wi][0:NQ, :].rearrange("p (h c) -> p h c", h=8)
            if wi == 2:
                S.act(lambda e, pv=pv: e.activation(out=qkvs[:, 2, :, :], in_=pv, func=AF.Copy), reads=[bt[wi]], writes=[t_qkv])
            else:
                S.act(lambda e, pv=pv: e.activation(out=sqs[:], in_=pv, func=AF.Square), reads=[bt[wi]], writes=[t_sqs])
                S.dve(lambda e: e.tensor_reduce(out=ssn[:], in_=sqs[:], axis=AX.X, op=ALU.add), reads=[t_sqs], writes=[t_sqs])
                S.act(lambda e: e.activation(out=ssn[:], in_=ssn[:], func=AF.Sqrt, scale=1.0 / 64, bias=epsc[0:64]), reads=[t_sqs, t_const], writes=[t_sqs])
                S.dve(lambda e: e.reciprocal(out=ssn[:], in_=ssn[:]), reads=[t_sqs], writes=[t_sqs])
                S.dve(lambda e, pv=pv, wi=wi: e.tensor_tensor(out=qkvs[:, wi, :, :], in0=pv, in1=bc(ssn[:].unsqueeze(2), [64, 8, 64]), op=ALU.mult),
                      reads=[bt[wi], t_sqs], writes=[t_qkv])
                S.dve(lambda e, wi=wi: e.tensor_tensor(out=qkvs[:, wi, :, :], in0=qkvs[:, wi, :, :], in1=bc(gqk[:, wi, :].unsqueeze(1), [64, 8, 64]), op=ALU.mult),
                      reads=[t_qkv, t_gqk], writes=[t_qkv])
        ck(5.1)
        t_rec = Tok()
        for wi, oname, recd, cname in ((1, "kws", reck_d, "ck_s"), (2, "vws", recv_d, "cv_s")):
            for s_ in range(4):
                S.dma(lambda e, wi=wi, oname=oname, s_=s_: e.dma_start(out=O[oname].rearrange("(b s) c -> s b c", s=4)[s_],
                                                                        in_=qkvs[16 * s_:16 * s_ + 16, wi, :, :].rearrange("p h c -> p (h c)")), reads=[t_qkv])
                S.dma(lambda e, wi=wi, recd=recd, s_=s_: e.dma_start(out=recd[:, 4 + s_, :], in_=qkvs[16 * s_:16 * s_ + 16, wi, :, :].rearrange("p h c -> p (h c)")),
                      reads=[t_qkv], writes=[t_rec])
            S.dma(lambda e, recd=recd, cname=cname: e.dma_start(out=recd[:, 0:4, :], in_=I[cname][:, 2044:2048, :]), writes=[t_rec])
        ck(5.2)
        btab = sbt(st2, "btab", [64, 3, 8, 129])
        t_btab = Tok()
        with contextlib.ExitStack() as st3:
            relr = sbt(st3, "relr", [32, 8])
            ohs = sbt(st3, "ohs", [32, 3, 129])
            RHs = sbt(st3, "RHs", [32, 8, 129])
            t_r2, t_rh2 = Tok(), Tok()
            S.dma(lambda e: e.dma_start(out=relr[:], in_=I["relb"]), writes=[t_r2])
            S.dma(lambda e: e.dma_start(out=ohs[:], in_=I["ohs"]), writes=[t_r2])
            for br in range(3):
                S.dve(lambda e, br=br: e.tensor_tensor(out=RHs[:], in0=bc(relr[:].unsqueeze(2), [32, 8, 129]), in1=bc(ohs[:, br, :].unsqueeze(1), [32, 8, 129]), op=ALU.mult),
                      reads=[t_r2], writes=[t_rh2])
                for h in range(8):
                    bk = 3 + h % 2
                    S.pe(lambda e, h=h, bk=bk: e.matmul(B[bk][0:64, 0:129], lhsT=ones[0:32, 0:64], rhs=RHs[:, h, :], start=True, stop=True),
                         reads=[t_rh2, t_const], writes=[bt[bk]])
                    S.act(lambda e, h=h, bk=bk, br=br: e.activation(out=btab[:, br, h, :], in_=B[bk][0:64, 0:129], func=AF.Copy), reads=[bt[bk]], writes=[t_btab])
        S.barrier()
        ck(5.3)
        Kt = [sbt(st2, "Kt", [64, 129, 64])] * 2
        Vt = [sbt(st2, "Vt", [64, 129, 64])] * 2
        t_Kt = [[Tok() for i in range(24)]] * 2
        t_Vt = [[Tok() for i in range(24)]] * 2
        lg = sbt(st2, "lg", [64, 129])
        zz = sbt(st2, "zz", [64, 1])
        ov = sbt(st2, "ov", [64, 64])
        Oacc = sbt(st2, "Oacc", [64, 8, 64])
        Zacc = sbt(st2, "Zacc", [64, 8])
        t_lg, t_zz, t_ov, t_acc2 = Tok(), Tok(), Tok(), Tok()
        S.pool(lambda e: e.memset(Oacc[:], 0.0), writes=[t_acc2])
        S.pool(lambda e: e.memset(Zacc[:], 0.0), writes=[t_acc2])
        it = 0
        for br, dil in enumerate((1, 4, 16)):
            ncache = 125 if dil == 1 else 128
            for h in range(8):
                bi = it % 2
                it += 1
                for tl, tk, cname, recd in ((Kt[bi], t_Kt[bi], "ck_s", reck_d), (Vt[bi], t_Vt[bi], "cv_s", recv_d)):
                    for s_ in range(4):
                        ps_ = slice(16 * s_, 16 * s_ + 16)
                        for j0 in range(0, ncache, 32):
                            j1 = min(ncache, j0 + 32)
                            src = bass.AP(I[cname].tensor, (2048 + s_ - 128 * dil + j0 * dil) * 512 + h * 64, [[2048 * 512, 16], [dil * 512, j1 - j0], [1, 64]])
                            S.dma(lambda e, tl=tl, ps_=ps_, src=src, j0=j0, j1=j1: e.dma_start(out=tl[ps_, j0:j1, :], in_=src), writes=[tk[8 + 4 * s_ + j0 // 32]])
                        r0 = (1 + s_) if dil == 1 else (4 + s_)
                        src2 = bass.AP(recd.tensor, r0 * 512 + h * 64, [[8 * 512, 16], [512, 129 - ncache], [1, 64]])
                        S.dma(lambda e, tl=tl, ps_=ps_, src2=src2, ncache=ncache: e.dma_start(out=tl[ps_, ncache:129, :], in_=src2), reads=[t_rec], writes=[tk[2 * s_ + 1]])
                K_, V_ = Kt[bi], Vt[bi]
                S.dve(lambda e, K_=K_, h=h: e.tensor_tensor(out=K_[:], in0=K_[:], in1=bc(qkvs[:, 0, h, :].unsqueeze(1), [64, 129, 64]), op=ALU.mult),
                      reads=t_Kt[bi] + [t_qkv], writes=t_Kt[bi])
                S.dve(lambda e, K_=K_: e.tensor_reduce(out=lg[:], in_=K_[:], axis=AX.X, op=ALU.add), reads=t_Kt[bi], writes=[t_lg])
                S.dve(lambda e, br=br, h=h: e.scalar_tensor_tensor(out=lg[:], in0=lg[:], scalar=0.125, in1=btab[:, br, h, :], op0=ALU.mult, op1=ALU.add),
                      reads=[t_lg, t_btab], writes=[t_lg])
                S.act(lambda e: e.activation(out=lg[:], in_=lg[:], func=AF.Exp), reads=[t_lg], writes=[t_lg])
                S.dve(lambda e: e.tensor_reduce(out=zz[:], in_=lg[:], axis=AX.X, op=ALU.add), reads=[t_lg], writes=[t_zz])
                S.dve(lambda e, h=h: e.tensor_tensor(out=Zacc[:, h:h + 1], in0=Zacc[:, h:h + 1], in1=zz[:], op=ALU.add), reads=[t_zz, t_acc2], writes=[t_acc2])
                S.pool(lambda e, V_=V_: e.tensor_tensor(out=V_[:], in0=V_[:], in1=bc(lg[:].unsqueeze(2), [64, 129, 64]), op=ALU.mult),
                       reads=t_Vt[bi] + [t_lg], writes=t_Vt[bi])
                S.dve(lambda e, V_=V_: e.tensor_reduce(out=ov[:], in_=V_[:].rearrange("p j c -> p c j"), axis=AX.X, op=ALU.add), reads=t_Vt[bi], writes=[t_ov])
                S.dve(lambda e, h=h: e.tensor_tensor(out=Oacc[:, h, :], in0=Oacc[:, h, :], in1=ov[:], op=ALU.add), reads=[t_ov, t_acc2], writes=[t_acc2])
                ck(5.4 + 0.001 * it)
        S.dve(lambda e: e.reciprocal(out=Zacc[:], in_=Zacc[:]), reads=[t_acc2], writes=[t_acc2])
        S.dve(lambda e: e.tensor_tensor(out=Oacc[:], in0=Oacc[:], in1=bc(Zacc[:].unsqueeze(2), [64, 8, 64]), op=ALU.mult), reads=[t_acc2], writes=[t_acc2])
        mixs = sbt(st2, "mixs", [128, 4, 64], BF16)
        t_mixs = Tok()
        for p in range(4):
            S.pe(lambda e, p=p: e.transpose(B[5][:, p * 128:p * 128 + 64], Oacc[:, 2 * p:2 * p + 2, :].rearrange("p h c -> p (h c)"), ident[0:64, 0:64]),
                 reads=[t_acc2, t_const], writes=[bt[5]])
        S.act(lambda e: e.activation(out=mixs[:].rearrange("p q (b s) -> p q s b", s=4),
                                     in_=B[5][:].rearrange("p (q x) -> p q x", q=4)[:, :, 0:64].rearrange("p q (s b) -> p q s b", s=4), func=AF.Copy),
              reads=[bt[5]], writes=[t_mixs])
        S.dma(lambda e: e.dma_start(out=mixT_d[0:4, :, T:T + NS].rearrange("q p t -> p q t"), in_=mixs[:]), reads=[t_mixs])
    S.barrier()
    stH.close()
    if phase_limit < 6:
        S.emit()
        for sk_ in (stC_holder + [stH, st]):
            sk_.close()
        return nc
    S.nosync = True
    with contextlib.ExitStack() as st2, contextlib.suppress(_Stop):
        t_pc = Tok("peerconst")
        wout = sbt(st2, "wout", [128, 8, 1024], BF16)
        skT = sbt(st2, "skT", [128, 16, 128], BF16)
        iotaR = sbt(st2, "iotaR", [128, 128])
        with contextlib.ExitStack() as st3:
            wo_st = sbt(st3, "wo_st", [128, 8, 1024])
            sk_st = sbt(st3, "sk_st", [128, 16, 128])
            S.dma(lambda e: e.dma_start(out=wo_st[:], in_=I["w_out"].rearrange("(c p) n -> p c n", p=128)), writes=[t_pc])
            S.dma(lambda e: e.dma_start(out=sk_st[:], in_=I["skT"]), writes=[t_pc])
            S.dma(lambda e: e.dma_start(out=iotaR[:], in_=I["iotaR"]), writes=[t_pc])
            S.dve(lambda e: e.tensor_copy(out=wout[:], in_=wo_st[:]), reads=[t_pc], writes=[t_pc])
            S.dve(lambda e: e.tensor_copy(out=skT[:], in_=sk_st[:]), reads=[t_pc], writes=[t_pc])
        S.barrier()
        GS = 256
        x1g = sbt(st2, "x1g", [128, 8, GS])
        tmpg = sbt(st2, "tmpg", [128, 8, GS])
        h2g = sbt(st2, "h2g", [128, 8, GS], BF16)
        mixg = sbt(st2, "mixg", [128, 8, GS], BF16)
        rsg = sbt(st2, "rsg", [128, GS])
        qpT = sbt(st2, "qpT", [128, 16, GS], BF16)
        wpqb = sbt(st2, "wpqb", [128, 8, 512], BF16)
        s_sb = sbt(st2, "s_sb", [128, 16, 128])
        s2 = sbt(st2, "s2", [128, 256])
        vals = sbt(st2, "vals", [128, 16, 16])
        idxu = sbt(st2, "idxu", [128, 16, 16], U32)
        idxf = sbt(st2, "idxf", [128, 16, 16])
        cand = sbt(st2, "cand", [128, 8, 256])
        ts_ = sbt(st2, "ts", [128, 8, 16])
        posu = sbt(st2, "posu", [128, 8, 16], U32)
        au = sbt(st2, "au", [128, 8, 16], U32)
        bu = sbt(st2, "bu", [128, 8, 16], U32)
        a_f = sbt(st2, "a_f", [128, 8, 16])
        b_f = sbt(st2, "b_f", [128, 8, 16])
        eq = sbt(st2, "eq", [128, 8, 16, 16])
        I1 = sbt(st2, "I1", [128, 8, 16])
        I2 = sbt(st2, "I2", [128, 8, 16])
        gt_ = sbt(st2, "gt", [128, 8, 16])
        zs = sbt(st2, "zs", [128, 8])
        I1T = sbt(st2, "I1T", [128, GS])
        I2T = sbt(st2, "I2T", [128, GS])
        gT = sbt(st2, "gT", [128, GS])
        A4 = [sbt(st2, "A4", [128, 4, 128], BF16) for i in range(2)]
        B4 = [sbt(st2, "B4", [128, 4, 128], BF16) for i in range(2)]
        WT = sbt(st2, "WT", [128, 128, GS], BF16)
        eub = [sbt(st2, "eub", [128, 8, 256], BF16) for i in range(2)]
        evb = [sbt(st2, "evb", [128, 2, 1024], BF16) for i in range(2)]
        gU = [sbt(st2, "gU", [128, GS], BF16) for i in range(2)]
        Wg = [sbt(st2, "Wg", [128, GS], BF16) for i in range(2)]
        ytk = [sbt(st2, "ytk", [128, 1024]) for i in range(2)]
        (t_x1, t_tmp, t_h2, t_mixg, t_rsg, t_qp, t_wpq, t_s, t_s2, t_vals, t_cand, t_ts, t_ab, t_eq, t_I, t_g, t_IT, t_WT) = [Tok() for i in range(18)]
        t_A4 = [Tok(), Tok()]
        t_B4 = [Tok(), Tok()]
        t_eub = [Tok(), Tok()]
        t_evb = [Tok(), Tok()]
        t_gU = [Tok(), Tok()]
        t_Wg = [Tok(), Tok()]
        t_ytk = [Tok(), Tok()]
        B = banks
        evv = ev_d.rearrange("(k p) d -> p k d", p=128)
        wpqv = wpq_d.rearrange("(c p) n -> p c n", p=128)
        iota16 = iotaR[:, 0:16]
        pgroups = [(g * GS, GS) for g in range(T // GS)] + [(T, NS)]
        nyt = 0
        for gi, (t0, n) in enumerate(pgroups):
            samp = (gi == len(pgroups) - 1)
            S.dma(lambda e, t0=t0, n=n: e.dma_start(out=mixg[:, :, 0:n], in_=mixT_d[:, :, t0:t0 + n].rearrange("j p t -> p j t")), writes=[t_mixg])
            S.dma(lambda e, t0=t0, n=n: e.dma_start(out=x1g[:, :, 0:n], in_=xT_v[:, :, t0:t0 + n]), writes=[t_x1])
            for dc in range(8):
                bk = dc % 2
                for j in range(8):
                    S.pe(lambda e, dc=dc, j=j, bk=bk, n=n: e.matmul(B[bk][:, 0:n], lhsT=wout[:, j, dc * 128:(dc + 1) * 128], rhs=mixg[:, j, 0:n],
                                                                     start=(j == 0), stop=(j == 7)), reads=[t_pc, t_mixg], writes=[bt[bk]])
                if not samp:
                    S.dve(lambda e, dc=dc, bk=bk, n=n: e.scalar_tensor_tensor(out=x1g[:, dc, 0:n], in0=B[bk][:, 0:n], scalar=modT[:, 16 + dc, 0:1],
                                                                               in1=x1g[:, dc, 0:n], op0=ALU.mult, op1=ALU.add),
                          reads=[bt[bk], t_mod, t_x1], writes=[t_x1])
                else:
                    S.dve(lambda e, dc=dc, bk=bk: e.tensor_tensor(out=tmpg[:, dc, 0:NS].rearrange("p (b t) -> p b t", t=4),
                                                                  in0=B[bk][:, 0:NS].rearrange("p (b t) -> p b t", t=4),
                                                                  in1=bc(modT[:, 16 + dc, 1:17].unsqueeze(2), [128, SB, 4]), op=ALU.mult),
                          reads=[bt[bk], t_mod], writes=[t_tmp])
                    S.dve(lambda e, dc=dc: e.tensor_tensor(out=x1g[:, dc, 0:NS], in0=x1g[:, dc, 0:NS], in1=tmpg[:, dc, 0:NS], op=ALU.add),
                          reads=[t_tmp, t_x1], writes=[t_x1])
            S.act(lambda e, n=n: e.activation(out=tmpg[:, :, 0:n], in_=x1g[:, :, 0:n], func=AF.Square), reads=[t_x1], writes=[t_tmp])
            for c in range(8):
                S.pe(lambda e, c=c, n=n: e.matmul(B[2][:, 0:n], lhsT=ones[:], rhs=tmpg[:, c, 0:n], start=(c == 0), stop=(c == 7)),
                     reads=[t_tmp, t_const], writes=[bt[2]])
            S.act(lambda e, n=n: e.activation(out=rsg[:, 0:n], in_=B[2][:, 0:n], func=AF.Sqrt, scale=1.0 / D, bias=epsc[:]),
                  reads=[bt[2], t_const], writes=[t_rsg])
            S.dve(lambda e, n=n: e.reciprocal(out=rsg[:, 0:n], in_=rsg[:, 0:n]), reads=[t_rsg], writes=[t_rsg])
            S.dve(lambda e, n=n: e.tensor_tensor(out=tmpg[:, :, 0:n], in0=x1g[:, :, 0:n], in1=bc(rsg[:, 0:n].unsqueeze(1), [128, 8, n]), op=ALU.mult),
                  reads=[t_x1, t_rsg, t_tmp], writes=[t_tmp])
            if not samp:
                for c in range(8):
                    eng = S.dve if c % 2 == 0 else S.pool
                    eng(lambda e, c=c, n=n: e.tensor_scalar(out=h2g[:, c, 0:n], in0=tmpg[:, c, 0:n], scalar1=A2[:, c, 0:1], scalar2=modT[:, 24 + c, 0:1],
                                                            op0=ALU.mult, op1=ALU.add), reads=[t_tmp, t_mod], writes=[t_h2])
            else:
                tv = tmpg[:, :, 0:NS].rearrange("p c (b t) -> p c b t", t=4)
                S.dve(lambda e, tv=tv: e.tensor_tensor(out=tv, in0=tv, in1=bc(A2[:, :, 1:17].unsqueeze(3), [128, 8, SB, 4]), op=ALU.mult),
                      reads=[t_tmp, t_mod], writes=[t_tmp])
                S.dve(lambda e, tv=tv: e.tensor_tensor(out=h2g[:, :, 0:NS].rearrange("p c (b t) -> p c b t", t=4), in0=tv,
                                                       in1=bc(modT[:, 24:32, 1:17].unsqueeze(3), [128, 8, SB, 4]), op=ALU.add),
                      reads=[t_tmp, t_mod], writes=[t_h2])
            ck(6.1)
            for jb in range(4):
                S.dma(lambda e, jb=jb: e.dma_start(out=wpqb[:], in_=wpqv[:, :, jb * 512:(jb + 1) * 512]), writes=[t_wpq])
                for j in range(4):
                    for c in range(8):
                        S.pe(lambda e, j=j, c=c, n=n: e.matmul(B[j][:, 0:n], lhsT=wpqb[:, c, j * 128:(j + 1) * 128], rhs=h2g[:, c, 0:n],
                                                               start=(c == 0), stop=(c == 7)), reads=[t_wpq, t_h2], writes=[bt[j]])
                for j in range(4):
                    S.act(lambda e, j=j, jb=jb, n=n: e.activation(out=qpT[:, jb * 4 + j, 0:n], in_=B[j][:, 0:n], func=AF.Copy), reads=[bt[j]], writes=[t_qp])
            for tt0 in range(0, n, 128):
                m = min(128, n - tt0)
                for j in range(16):
                    bk = 4 + j // 4
                    S.pe(lambda e, j=j, bk=bk, tt0=tt0, m=m: e.matmul(B[bk][0:m, (j % 4) * 128:(j % 4 + 1) * 128], lhsT=qpT[:, j, tt0:tt0 + m], rhs=skT[:, j, :],
                                                                      start=True, stop=True), reads=[t_qp, t_pc], writes=[bt[bk]])
                for q in range(4):
                    S.act(lambda e, q=q, m=m: e.activation(out=s_sb[0:m, q * 4:(q + 1) * 4, :].rearrange("p a k -> p (a k)"), in_=B[4 + q][0:m, :], func=AF.Copy),
                          reads=[bt[4 + q]], writes=[t_s])
                for j in range(16):
                    S.dve(lambda e, j=j, m=m: e.max(out=vals[0:m, j, 0:8], in_=s_sb[0:m, j, :]), reads=[t_s], writes=[t_vals])
                    S.dve(lambda e, j=j, m=m: e.match_replace(out=s2[0:m, 0:128], in_to_replace=vals[0:m, j, 0:8], in_values=s_sb[0:m, j, :], imm_value=-1e30),
                          reads=[t_s, t_vals], writes=[t_s2])
                    S.dve(lambda e, j=j, m=m: e.max(out=vals[0:m, j, 8:16], in_=s2[0:m, 0:128]), reads=[t_s2], writes=[t_vals])
                    S.dve(lambda e, j=j, m=m: e.max_index(out=idxu[0:m, j, 0:8], in_max=vals[0:m, j, 0:8], in_values=s_sb[0:m, j, :]), reads=[t_s, t_vals], writes=[t_vals])
                    S.dve(lambda e, j=j, m=m: e.max_index(out=idxu[0:m, j, 8:16], in_max=vals[0:m, j, 8:16], in_values=s_sb[0:m, j, :]), reads=[t_s, t_vals], writes=[t_vals])
                S.dve(lambda e, m=m: e.tensor_copy(out=idxf[0:m], in_=idxu[0:m]), reads=[t_vals], writes=[t_vals])
                v2 = vals[0:m].rearrange("p (h two) k -> p h two k", two=2)
                i2v = idxf[0:m].rearrange("p (h two) k -> p h two k", two=2)
                S.dve(lambda e, m=m, v2=v2: e.tensor_tensor(out=cand[0:m].rearrange("p h (a b) -> p h a b", a=16),
                                                            in0=bc(v2[:, :, 0, :].unsqueeze(3), [m, 8, 16, 16]),
                                                            in1=bc(v2[:, :, 1, :].unsqueeze(2), [m, 8, 16, 16]), op=ALU.add), reads=[t_vals], writes=[t_cand])
                for h in range(8):
                    S.dve(lambda e, h=h, m=m: e.max(out=ts_[0:m, h, 0:8], in_=cand[0:m, h, :]), reads=[t_cand], writes=[t_ts])
                    S.dve(lambda e, h=h, m=m: e.match_replace(out=s2[0:m, :], in_to_replace=ts_[0:m, h, 0:8], in_values=cand[0:m, h, :], imm_value=-1e30),
                          reads=[t_cand, t_ts], writes=[t_s2])
                    S.dve(lambda e, h=h, m=m: e.max(out=ts_[0:m, h, 8:16], in_=s2[0:m, :]), reads=[t_s2], writes=[t_ts])
                    S.dve(lambda e, h=h, m=m: e.max_index(out=posu[0:m, h, 0:8], in_max=ts_[0:m, h, 0:8], in_values=cand[0:m, h, :]), reads=[t_cand, t_ts], writes=[t_ts])
                    S.dve(lambda e, h=h, m=m: e.max_index(out=posu[0:m, h, 8:16], in_max=ts_[0:m, h, 8:16], in_values=cand[0:m, h, :]), reads=[t_cand, t_ts], writes=[t_ts])
                S.dve(lambda e, m=m: e.tensor_single_scalar(out=au[0:m], in_=posu[0:m], scalar=4, op=ALU.logical_shift_right), reads=[t_ts], writes=[t_ab])
                S.dve(lambda e, m=m: e.tensor_single_scalar(out=bu[0:m], in_=posu[0:m], scalar=15, op=ALU.bitwise_and), reads=[t_ts], writes=[t_ab])
                S.dve(lambda e, m=m: e.tensor_copy(out=a_f[0:m], in_=au[0:m]), reads=[t_ab], writes=[t_ab])
                S.dve(lambda e, m=m: e.tensor_copy(out=b_f[0:m], in_=bu[0:m]), reads=[t_ab], writes=[t_ab])
                for which, sel, dstI in ((0, a_f, I1), (1, b_f, I2)):
                    S.dve(lambda e, m=m, sel=sel: e.tensor_tensor(out=eq[0:m], in0=bc(iota16[0:m].unsqueeze(1).unsqueeze(1), [m, 8, 16, 16]),
                                                                  in1=bc(sel[0:m].unsqueeze(3), [m, 8, 16, 16]), op=ALU.is_equal),
                          reads=[t_ab, t_pc], writes=[t_eq])
                    S.dve(lambda e, m=m, which=which, i2v=i2v: e.tensor_tensor(out=eq[0:m], in0=eq[0:m], in1=bc(i2v[:, :, which, :].unsqueeze(2), [m, 8, 16, 16]), op=ALU.mult),
                          reads=[t_eq, t_vals], writes=[t_eq])
                    S.dve(lambda e, m=m, dstI=dstI: e.tensor_reduce(out=dstI[0:m], in_=eq[0:m], axis=AX.X, op=ALU.add), reads=[t_eq], writes=[t_I])
                S.dve(lambda e, m=m: e.tensor_tensor(out=gt_[0:m], in0=ts_[0:m], in1=bc(ts_[0:m, :, 0:1], [m, 8, 16]), op=ALU.subtract), reads=[t_ts], writes=[t_g])
                S.act(lambda e, m=m: e.activation(out=gt_[0:m], in_=gt_[0:m], func=AF.Exp), reads=[t_g], writes=[t_g])
                S.dve(lambda e, m=m: e.tensor_reduce(out=zs[0:m], in_=gt_[0:m], axis=AX.X, op=ALU.add), reads=[t_g], writes=[t_g])
                S.dve(lambda e, m=m: e.reciprocal(out=zs[0:m], in_=zs[0:m]), reads=[t_g], writes=[t_g])
                S.dve(lambda e, m=m: e.tensor_tensor(out=gt_[0:m], in0=gt_[0:m], in1=bc(zs[0:m].unsqueeze(2), [m, 8, 16]), op=ALU.mult), reads=[t_g], writes=[t_g])
                for srcI, dstT, rd in ((I1, I1T, t_I), (I2, I2T, t_I), (gt_, gT, t_g)):
                    S.pe(lambda e, srcI=srcI, m=m: e.transpose(B[0][:, 0:m], srcI[0:m].rearrange("p h k -> p (h k)"), ident[0:m, 0:m]),
                         reads=[rd, t_const], writes=[bt[0]])
                    S.act(lambda e, dstT=dstT, tt0=tt0, m=m: e.activation(out=dstT[:, tt0:tt0 + m], in_=B[0][:, 0:m], func=AF.Copy), reads=[bt[0]], writes=[t_IT])
            if debug and gi == 0:
                for qq, tl in enumerate((I1T, I2T, gT)):
                    S.dma(lambda e, qq=qq, tl=tl: e.dma_start(out=O["dbg_IT"][qq], in_=tl[:]), reads=[t_IT])
            ck(6.2)
            for n0 in range(0, n, 4):
                bi = (n0 // 4) % 2
                S.dve(lambda e, bi=bi, n0=n0: e.tensor_tensor(out=B4[bi][:], in0=bc(iotaR[:].unsqueeze(1), [128, 4, 128]),
                                                             in1=bc(I2T[:, n0:n0 + 4].unsqueeze(2), [128, 4, 128]), op=ALU.is_equal),
                      reads=[t_IT, t_pc], writes=[t_B4[bi]])
                S.dve(lambda e, bi=bi, n0=n0: e.tensor_tensor(out=A4[bi][:], in0=bc(iotaR[:].unsqueeze(1), [128, 4, 128]),
                                                             in1=bc(I1T[:, n0:n0 + 4].unsqueeze(2), [128, 4, 128]), op=ALU.is_equal),
                      reads=[t_IT, t_pc], writes=[t_A4[bi]])
                S.dve(lambda e, bi=bi, n0=n0: e.tensor_tensor(out=A4[bi][:], in0=A4[bi][:],
                                                             in1=bc(gT[:, n0:n0 + 4].unsqueeze(2), [128, 4, 128]), op=ALU.mult),
                      reads=[t_IT, t_A4[bi]], writes=[t_A4[bi]])
                bk = 6 + bi
                for q in range(4):
                    S.pe(lambda e, bi=bi, q=q, bk=bk: e.matmul(B[bk][:, q * 128:(q + 1) * 128], lhsT=B4[bi][:, q, :], rhs=A4[bi][:, q, :], start=True, stop=True),
                         reads=[t_A4[bi], t_B4[bi]], writes=[bt[bk]])
                S.act(lambda e, bk=bk, n0=n0: e.activation(out=WT[:, :, n0:n0 + 4].rearrange("p i n -> p n i"),
                                                           in_=B[bk][:].rearrange("p (n i) -> p n i", n=4), func=AF.Copy), reads=[bt[bk]], writes=[t_WT])
            ck(6.3)
            def emit_U(i1):
                blk, k2 = i1 // 2, i1 % 2
                bi = blk % 2
                if k2 == 0:
                    S.dma(lambda e, bi=bi, blk=blk: e.dma_start(out=eub[bi][:], in_=euT_d[blk]), writes=[t_eub[bi]])
                    S.dma(lambda e, bi=bi, blk=blk: e.dma_start(out=evb[bi][:], in_=evv[:, blk * 2:(blk + 1) * 2, :]), writes=[t_evb[bi]])
                ui = i1 % 2
                ubk = 4 + ui
                for c in range(8):
                    S.pe(lambda e, bi=bi, k2=k2, c=c, ubk=ubk, n=n: e.matmul(B[ubk][:, 0:n], lhsT=eub[bi][:, c, k2 * 128:(k2 + 1) * 128], rhs=h2g[:, c, 0:n],
                                                                             start=(c == 0), stop=(c == 7)), reads=[t_eub[bi], t_h2], writes=[bt[ubk]])

            def emit_rest(i1):
                blk, k2 = i1 // 2, i1 % 2
                bi = blk % 2
                ui = i1 % 2
                ubk = 4 + ui
                S.act(lambda e, ui=ui, ubk=ubk, n=n: e.activation(out=gU[ui][:, 0:n], in_=B[ubk][:, 0:n], func=AF.Gelu), reads=[bt[ubk]], writes=[t_gU[ui]])
                S.dve(lambda e, ui=ui, i1=i1, n=n: e.tensor_tensor(out=Wg[ui][:, 0:n], in0=gU[ui][:, 0:n], in1=WT[:, i1, 0:n], op=ALU.mult),
                      reads=[t_gU[ui], t_WT], writes=[t_Wg[ui]])
                for dc in range(8):
                    abk = dc // 2
                    S.pe(lambda e, bi=bi, k2=k2, dc=dc, abk=abk, ui=ui, i1=i1, n=n: e.matmul(
                        B[abk][:, (dc % 2) * 256:(dc % 2) * 256 + n], lhsT=evb[bi][:, k2, dc * 128:(dc + 1) * 128], rhs=Wg[ui][:, 0:n],
                        start=(i1 == 0), stop=(i1 == 127)), reads=[t_evb[bi], t_Wg[ui]], writes=[bt[abk]])

            for i1 in range(129):
                if i1 < 128:
                    emit_U(i1)
                if i1 >= 1:
                    emit_rest(i1 - 1)
            for dc in range(8):
                abk = dc // 2
                src = B[abk][:, (dc % 2) * 256:(dc % 2) * 256 + n]
                if not samp:
                    S.dve(lambda e, dc=dc, src=src, n=n: e.scalar_tensor_tensor(out=x1g[:, dc, 0:n], in0=src, scalar=modT[:, 40 + dc, 0:1], in1=x1g[:, dc, 0:n],
                                                                                 op0=ALU.mult, op1=ALU.add), reads=[bt[abk], t_mod, t_x1], writes=[t_x1])
                else:
                    S.dve(lambda e, dc=dc, src=src: e.tensor_tensor(out=tmpg[:, dc, 0:NS].rearrange("p (b t) -> p b t", t=4),
                                                                    in0=src.rearrange("p (b t) -> p b t", t=4),
                                                                    in1=bc(modT[:, 40 + dc, 1:17].unsqueeze(2), [128, SB, 4]), op=ALU.mult),
                          reads=[bt[abk], t_mod, t_tmp], writes=[t_tmp])
                    S.dve(lambda e, dc=dc: e.tensor_tensor(out=x1g[:, dc, 0:NS], in0=x1g[:, dc, 0:NS], in1=tmpg[:, dc, 0:NS], op=ALU.add),
                          reads=[t_tmp, t_x1], writes=[t_x1])
            for tt0 in range(0, n, 128):
                m = min(128, n - tt0)
                yi = nyt % 2
                nyt += 1
                for dc in range(8):
                    bk = 4 + dc // 4
                    S.pe(lambda e, dc=dc, bk=bk, tt0=tt0, m=m: e.transpose(B[bk][0:m, (dc % 4) * 128:(dc % 4 + 1) * 128], x1g[:, dc, tt0:tt0 + m], ident[:]),
                         reads=[t_x1, t_const], writes=[bt[bk]])
                for hf in range(2):
                    S.act(lambda e, hf=hf, yi=yi, m=m: e.activation(out=ytk[yi][0:m, hf * 512:(hf + 1) * 512], in_=B[4 + hf][0:m, :], func=AF.Copy),
                          reads=[bt[4 + hf]], writes=[t_ytk[yi]])
                S.dma(lambda e, yi=yi, t0=t0, tt0=tt0, m=m: e.dma_start(out=O["y"][t0 + tt0:t0 + tt0 + m, :], in_=ytk[yi][0:m, :]), reads=[t_ytk[yi]])
            ck(6.5 + 0.01 * gi)
    K.st = st
    S.emit()
    st.close()
    return nc


def host_prep(inp, core):
    f = np.float32
    xp = np.asarray(inp["x_prompt"], f)[core]
    xs = np.asarray(inp["x_sample"], f)[core * SB:(core + 1) * SB].reshape(NS, D)
    xT = np.ascontiguousarray(np.concatenate([xp, xs], 0).T)
    cvec = np.concatenate([np.asarray(inp["c_prompt"], f)[core:core + 1],
                           np.asarray(inp["c_sample"], f)[core * SB:(core + 1) * SB]], 0)
    cT = np.ascontiguousarray(cvec.reshape(17, 8, 128).transpose(2, 1, 0))
    m = {}
    m["xT"] = xT
    m["cT"] = cT
    bsl = slice(core * SB, (core + 1) * SB)
    m["ck_s"] = np.ascontiguousarray(np.asarray(inp["cache_k_win"], f)[0, bsl].reshape(SB, 2048, 512))
    m["cv_s"] = np.ascontiguousarray(np.asarray(inp["cache_v_win"], f)[0, bsl].reshape(SB, 2048, 512))
    m["swkv"] = np.ascontiguousarray(np.asarray(inp["state_wkv"], f)[0, bsl].reshape(128, 4096))
    sh = np.asarray(inp["state_shift"], f)[0, bsl]
    m["shT"] = np.ascontiguousarray(sh[:, 0:1536].reshape(SB, 3, 4, 128).transpose(3, 1, 2, 0))
    m["shTl"] = np.ascontiguousarray(sh[:, 1536:1664].T)
    return m


def _t5_bucket(dist):
    import math
    dist = np.asarray(dist, dtype=np.int64)
    max_exact = 16
    safe = np.maximum(dist, 1) / max_exact
    large = max_exact + (np.log(safe) / math.log(2048 / max_exact) * (32 - max_exact)).astype(np.int64)
    large = np.minimum(large, 31)
    return np.where(dist < max_exact, dist, large).astype(np.int32)


def _ohu_table():
    t = np.zeros((32, 3, 384), np.float32)
    for br, dil in enumerate((1, 4, 16)):
        j = np.arange(129)
        b = _t5_bucket(j * dil)
        t[b, br, j + 127] = 1.0
    return t


def host_shared(inp):
    f = np.float32
    m = {}
    m["ada_w"] = np.ascontiguousarray(np.asarray(inp["ada_w"], f)[0])
    m["ada_bT"] = np.ascontiguousarray(np.asarray(inp["ada_b"], f)[0].reshape(48, 128).T)
    m["n1gT"] = np.ascontiguousarray(np.asarray(inp["norm1_g"], f)[0].reshape(8, 128).T)
    m["n2gT"] = np.ascontiguousarray(np.asarray(inp["norm2_g"], f)[0].reshape(8, 128).T)
    m["w_in"] = np.ascontiguousarray(np.asarray(inp["w_in"], f)[0])
    qg = np.asarray(inp["q_norm_g"], f)[0]
    kg = np.asarray(inp["k_norm_g"], f)[0]
    m["qkg"] = np.ascontiguousarray(np.stack([np.tile(qg, 2), np.tile(kg, 2)], 1))
    m["ident"] = np.eye(128, dtype=f)
    m["ones"] = np.ones((128, 128), f)
    b2 = np.zeros((128, 128), f)
    b2[:64, :64] = 1
    b2[64:, 64:] = 1
    m["blk2"] = b2
    m["relb"] = np.ascontiguousarray(np.asarray(inp["rel_bias"], f))
    m["ohu"] = _ohu_table()

    def pf(v):
        return np.asarray(v, f).reshape(4, 128).T
    mu = np.asarray(inp["mu_shift"], f)[0]
    m["rwp"] = np.ascontiguousarray(np.stack([pf(mu[0:512]), pf(mu[512:1024]), pf(mu[1024:1536]), pf(inp["k_k"][0]), pf(inp["k_a"][0]),
                                              pf(np.asarray(inp["r_k"], f)[0].reshape(512)), pf(inp["a0"][0]), pf(inp["w0"][0])], 1))
    m["mul"] = np.ascontiguousarray(mu[1536:1664].reshape(128, 1))
    m["lw3"] = np.ascontiguousarray(np.concatenate([np.asarray(inp["w_w2"], f)[0], np.asarray(inp["w_a2"], f)[0], np.asarray(inp["w_g2"], f)[0]], 0))
    m["w0row"] = np.ascontiguousarray(np.asarray(inp["w0"], f)[0].reshape(1, 512))
    m["lnx"] = np.ascontiguousarray(np.stack([np.asarray(inp["lnx_g"], f)[0], np.asarray(inp["lnx_b"], f)[0]], 0))
    m["w_out"] = np.ascontiguousarray(np.asarray(inp["w_out"], f)[0])
    m["w_pq"] = np.ascontiguousarray(np.asarray(inp["w_peer_q"], f)[0])
    sk = np.asarray(inp["peer_sub_keys"], f)[0]
    m["skT"] = np.ascontiguousarray(sk.reshape(16, 128, 128).transpose(2, 0, 1))
    m["euT"] = np.ascontiguousarray(np.asarray(inp["expert_u"], f)[0].T)
    m["ev"] = np.ascontiguousarray(np.asarray(inp["expert_v"], f)[0])
    m["qkrow"] = np.ascontiguousarray(np.stack([qg, kg], 0))
    t_ = np.zeros((32, 3, 129), f)
    for br_, dil_ in enumerate((1, 4, 16)):
        jp = np.arange(129)
        t_[_t5_bucket((128 - jp) * dil_), br_, jp] = 1.0
    m["ohs"] = t_
    m["iotaR"] = np.ascontiguousarray(np.tile(np.arange(128, dtype=f)[None, :], (128, 1)))
    NEG = -0.6065306597126334
    i = np.arange(128)[:, None]
    t = np.arange(128)[None, :]
    same = (i // 64) == (t // 64)
    m["tri"] = np.ascontiguousarray(np.stack([np.where(same & (i <= t), NEG, 0.0), np.where(same & (i < t), NEG, 0.0)], 1).astype(f))
    ii = (np.arange(128) % 64)[:, None]
    tt = np.arange(64)[None, :]
    m["mk1"] = np.ascontiguousarray(np.stack([(tt > ii), (tt >= ii)], 1).astype(f))
    m["mk3"] = np.ascontiguousarray((tt < ii).astype(f))
    m["id2"] = np.ascontiguousarray((tt == ii).astype(f))
    return m


def kernel(**inp):
    nc = build()
    shared = host_shared(inp)
    in_maps = []
    for c in range(NCORES):
        m = dict(shared)
        m.update(host_prep(inp, c))
        in_maps.append(m)
    res = run_bass_kernel_spmd(nc, in_maps, core_ids=list(range(NCORES)))
    R = res.results
    f = np.float32
    y_p = np.stack([R[c]["y"][0:T] for c in range(NCORES)], 0).astype(f)
    y_s = np.concatenate([R[c]["y"][T:NT].reshape(SB, 4, D) for c in range(NCORES)], 0).astype(f)
    kwp = np.stack([R[c]["kwp"].reshape(2048, 8, 64) for c in range(NCORES)], 0)[None].astype(f)
    vwp = np.stack([R[c]["vwp"].reshape(2048, 8, 64) for c in range(NCORES)], 0)[None].astype(f)
    wkvp = np.stack([R[c]["wkvp"].reshape(2, 64, 4, 64).transpose(2, 0, 3, 1).reshape(8, 64, 64) for c in range(NCORES)], 0)[None].astype(f)
    shp = np.stack([R[c]["shp"].reshape(CR) for c in range(NCORES)], 0)[None].astype(f)
    kws = np.concatenate([R[c]["kws"].reshape(SB, 4, 8, 64) for c in range(NCORES)], 0)[None].astype(f)
    vws = np.concatenate([R[c]["vws"].reshape(SB, 4, 8, 64) for c in range(NCORES)], 0)[None].astype(f)
    wkvs = np.concatenate([R[c]["wkvs"].reshape(SB, 8, 64, 64) for c in range(NCORES)], 0)[None].astype(f)
    shs = np.concatenate([R[c]["shs"].reshape(SB, CR) for c in range(NCORES)], 0)[None].astype(f)
    return (y_p, y_s, kwp, vwp, wkvp, shp, kws, vws, wkvs, shs)
```

```python
import contextlib
import numpy as np
import concourse.bass as bass
import concourse.mybir as mybir
from concourse.bass_utils import run_bass_kernel_spmd

F32 = mybir.dt.float32
BF16 = mybir.dt.bfloat16
I32 = mybir.dt.int32
U32 = mybir.dt.uint32
AF = mybir.ActivationFunctionType
ALU = mybir.AluOpType
AX = mybir.AxisListType

N_DMA_SLOTS = 24
PE_NOSYNC = True
NCORES = 8
D = 1024
T = 4096
NS = 64
NT = T + NS
SB = 16
DIN = 3200
CR = 1664
EPS = 1e-6


class Tok:
    __slots__ = ("lw", "rd", "rd_dma", "name", "excl")

    def __init__(self, name="", excl=False):
        self.excl = excl
        self.lw = None
        self.rd = {}
        self.rd_dma = []
        self.name = name


class Sched:
    ENGS = ("pe", "act", "dve", "pool", "sp")

    def __init__(self, nc):
        self.nc = nc
        self.ins = []
        self.last_by_eng = {}
        self.dmas_since = []
        self.nosync = True

    def barrier(self):
        deps = set(self.last_by_eng.values()) | set(self.dmas_since)
        self.dmas_since = []
        for e in self.ENGS:
            idx = len(self.ins)
            self.ins.append([e, (lambda eh: eh.nop()), set(deps), False, False, 0, 0, False])
            self.last_by_eng[e] = idx

    def op(self, eng, fn, reads=(), writes=(), dma=False, strided=False):
        idx = len(self.ins)
        deps = set()
        for t in reads:
            if t.lw is not None:
                deps.add(t.lw)
            if t.excl:
                deps.update(v for kk_, v in t.rd.items() if kk_ != eng)
        for t in writes:
            if t.lw is not None:
                deps.add(t.lw)
            deps.update(t.rd.values())
            deps.update(t.rd_dma)
        for t in reads:
            if dma:
                t.rd_dma.append(idx)
            else:
                t.rd[eng] = idx
        for t in writes:
            t.lw = idx
            t.rd = {}
            t.rd_dma = []
        deps.discard(idx)
        self.ins.append([eng, fn, deps, dma, False, 0, 0, strided or (not self.nosync)])
        if dma:
            self.dmas_since.append(idx)
        else:
            self.last_by_eng[eng] = idx
        return idx

    def pe(self, fn, reads=(), writes=(), strided=False):
        return self.op("pe", fn, reads, writes, strided=strided)

    def act(self, fn, reads=(), writes=()):
        return self.op("act", fn, reads, writes)

    def dve(self, fn, reads=(), writes=()):
        return self.op("dve", fn, reads, writes)

    def pool(self, fn, reads=(), writes=()):
        return self.op("pool", fn, reads, writes)

    def dma(self, fn, reads=(), writes=(), eng="sp"):
        return self.op(eng, fn, reads, writes, dma=True)

    def emit(self):
        nc = self.nc
        ins = self.ins
        class _Rec:
            def matmul(self, out, lhsT, rhs, **kw):
                self.rg = (lhsT.base_partition(), lhsT.partition_size())
            def transpose(self, out, in_, identity, **kw):
                self.rg = (in_.base_partition(), in_.partition_size())
            def nop(self, *a, **k):
                self.rg = (0, 128)
        prev_pe = None
        prev_strips = None
        for i_, it in enumerate(ins):
            if it[0] == "pe" and not it[3]:
                rec = _Rec()
                it[1](rec)
                b0, sz = rec.rg
                strips = set(range(b0 // 32, (b0 + sz + 31) // 32))
                if PE_NOSYNC and not it[7]:
                    if prev_strips is not None and not (strips & prev_strips):
                        it[2].add(prev_pe)
                    else:
                        it[2] = set(d for d in it[2] if not (ins[d][0] == "pe" and not ins[d][3]))
                prev_pe = i_
                prev_strips = strips
            for d in it[2]:
                ins[d][4] = True
        last = {}
        for i, it in enumerate(ins):
            if not it[3]:
                last[it[0]] = i
        for i in last.values():
            ins[i][4] = True
        cnt = {e: 0 for e in self.ENGS}
        dma_n = {e: 0 for e in self.ENGS}
        for it in ins:
            e = it[0]
            if it[3]:
                it[4] = True
                it[6] = dma_n[e]
                dma_n[e] += 1
            elif it[4]:
                cnt[e] += 1
                it[5] = cnt[e]
        with contextlib.ExitStack() as st:
            sems = {e: st.enter_context(nc.semaphore("s_" + e)) for e in self.ENGS}
            dsems = {e: [st.enter_context(nc.semaphore("d_%s_%d" % (e, i))) for i in range(N_DMA_SLOTS)]
                     for e in self.ENGS if dma_n[e] > 0}
            block = st.enter_context(nc.Block())
            per_eng = {e: [] for e in self.ENGS}
            for i, it in enumerate(ins):
                per_eng[it[0]].append(i)

            def run(eng_name, eh):
                seen = {e: 0 for e in self.ENGS}
                seen_dma = {}
                for i in per_eng[eng_name]:
                    e, fn, deps, is_dma, sig, c, slot = ins[i][:7]
                    need = {}
                    for d in deps:
                        de, _, _, ddma, _, dc, dslot = ins[d][:7]
                        if ddma:
                            key = (de, dslot % N_DMA_SLOTS)
                            val = 16 * (dslot // N_DMA_SLOTS + 1)
                            if seen_dma.get(key, 0) < val:
                                seen_dma[key] = val
                                need[("d",) + key] = val
                        else:
                            if seen[de] < dc:
                                seen[de] = dc
                                need[("c", de)] = dc
                    if is_dma and slot >= N_DMA_SLOTS:
                        key = (e, slot % N_DMA_SLOTS)
                        val = 16 * (slot // N_DMA_SLOTS)
                        if seen_dma.get(key, 0) < val:
                            seen_dma[key] = val
                            need[("d",) + key] = val
                    for k, v in need.items():
                        if k[0] == "c":
                            eh.wait_ge(sems[k[1]], v)
                        else:
                            eh.wait_ge(dsems[k[1]][k[2]], v)
                    inst = fn(eh)
                    if is_dma:
                        inst.then_inc(dsems[e][slot % N_DMA_SLOTS], 16)
                    elif sig:
                        inst.then_inc(sems[e], 1)
                if eng_name == "sp":
                    for e2 in self.ENGS:
                        if cnt[e2] > 0:
                            eh.wait_ge(sems[e2], cnt[e2])
                        n = dma_n.get(e2, 0)
                        for s in range(min(N_DMA_SLOTS, n)):
                            lastslot = ((n - 1 - s) // N_DMA_SLOTS) * N_DMA_SLOTS + s
                            eh.wait_ge(dsems[e2][s], 16 * (lastslot // N_DMA_SLOTS + 1))

            @block.tensor
            def _(eh):
                run("pe", eh)

            @block.scalar
            def _(eh):
                run("act", eh)

            @block.vector
            def _(eh):
                run("dve", eh)

            @block.gpsimd
            def _(eh):
                run("pool", eh)

            @block.sync
            def _(eh):
                run("sp", eh)


class Ctx:
    pass


def bc(ap, shape):
    return ap.to_broadcast(list(shape))


def build(phase_limit=99, debug=False):
    nc = bass.Bass("TRN2", target_bir_lowering=False)
    S = Sched(nc)
    K = Ctx()
    K.nc, K.S = nc, S

    def din(name, shape, dt=F32):
        return nc.dram_tensor(name, list(shape), dt, kind="ExternalInput").ap()

    def dout(name, shape, dt=F32):
        return nc.dram_tensor(name, list(shape), dt, kind="ExternalOutput").ap()

    I = {}
    I["xT"] = din("xT", [D, NT])
    I["cT"] = din("cT", [128, 8, 17])
    I["ada_w"] = din("ada_w", [D, 6 * D])
    I["ada_bT"] = din("ada_bT", [128, 48])
    I["n1gT"] = din("n1gT", [128, 8])
    I["n2gT"] = din("n2gT", [128, 8])
    I["w_in"] = din("w_in", [D, DIN])
    I["qkg"] = din("qkg", [128, 2])
    I["ident"] = din("ident", [128, 128])
    I["ones"] = din("ones", [128, 128])
    I["blk2"] = din("blk2", [128, 128])
    I["relb"] = din("relb", [32, 8])
    I["ohu"] = din("ohu", [32, 3, 384])
    I["w_out"] = din("w_out", [D, D])
    I["w_pq"] = din("w_pq", [D, 2048])
    I["skT"] = din("skT", [128, 16, 128])
    I["euT"] = din("euT", [D, 16384])
    I["ev"] = din("ev", [16384, D])
    I["iotaR"] = din("iotaR", [128, 128])
    euT_d = nc.dram_tensor("euT_d", [64, 128, 8, 256], BF16, kind="Internal").ap()
    ev_d = nc.dram_tensor("ev_d", [16384, D], BF16, kind="Internal").ap()
    wpq_d = nc.dram_tensor("wpq_d", [D, 2048], BF16, kind="Internal").ap()
    I["ck_s"] = din("ck_s", [SB, 2048, 512])
    I["cv_s"] = din("cv_s", [SB, 2048, 512])
    I["swkv"] = din("swkv", [128, 4096])
    I["shT"] = din("shT", [128, 3, 4, 16])
    I["shTl"] = din("shTl", [128, 16])
    I["qkrow"] = din("qkrow", [2, 64])
    I["ohs"] = din("ohs", [32, 3, 129])
    rws_d = nc.dram_tensor("rws_d", [SB, 8, 4, 6, 64], F32, kind="Internal").ap()
    ys_d = nc.dram_tensor("ys_d", [SB, 8, 4, 64], F32, kind="Internal").ap()
    reck_d = nc.dram_tensor("reck_d", [SB, 8, 512], F32, kind="Internal").ap()
    recv_d = nc.dram_tensor("recv_d", [SB, 8, 512], F32, kind="Internal").ap()
    I["rwp"] = din("rwp", [128, 8, 4])
    I["mul"] = din("mul", [128, 1])
    I["lw3"] = din("lw3", [128, 512])
    I["w0row"] = din("w0row", [1, 512])
    I["lnx"] = din("lnx", [2, 512])
    I["tri"] = din("tri", [128, 2, 128])
    I["mk1"] = din("mk1", [128, 2, 64])
    I["mk3"] = din("mk3", [128, 64])
    I["id2"] = din("id2", [128, 64])
    zscr = nc.dram_tensor("zscr", [24, 256, 384], F32, kind="Internal").ap()
    mixT_d = nc.dram_tensor("mixT_d", [8, 128, NT], BF16, kind="Internal").ap()
    O = {}
    O["kwp"] = dout("kwp", [2048, 512])
    O["vwp"] = dout("vwp", [2048, 512])
    O["kws"] = dout("kws", [NS, 512])
    O["vws"] = dout("vws", [NS, 512])
    O["shp"] = dout("shp", [1, CR])
    O["shs"] = dout("shs", [SB, CR])
    O["wkvp"] = dout("wkvp", [128, 4, 64])
    O["y"] = dout("y", [NT, D])
    O["wkvs"] = dout("wkvs", [128, 4096])
    if debug:
        O["dbg_hT"] = dout("dbg_hT", [128, 8, NT], BF16)
        O["dbg_mod"] = dout("dbg_mod", [128, 48, 17])
        O["dbg_ebt"] = dout("dbg_ebt", [128, 24, 2, 128], BF16)
        O["dbg_mixT"] = dout("dbg_mixT", [4, 128, T], BF16)
        O["dbg_yr"] = dout("dbg_yr", [T, 512])
        O["dbg_IT"] = dout("dbg_IT", [3, 128, 256])

    st = contextlib.ExitStack()

    uid = [0]

    def sbt(stack, name, shape, dt=F32):
        uid[0] += 1
        return stack.enter_context(nc.sbuf_tensor("s%d_%s" % (uid[0], name), list(shape), dt))

    def sb(name, shape, dt=F32):
        return sbt(st, name, shape, dt)

    banks = [st.enter_context(nc.psum_tensor("bank%d" % i, [128, 512], F32)) for i in range(8)]
    bt = [Tok("bank%d" % i, excl=True) for i in range(8)]

    ident = sb("ident", [128, 128])
    ones = sb("ones", [128, 128])
    blk2 = sb("blk2", [128, 128])
    identb = sb("identb", [128, 128], BF16)
    epsc = sb("epsc", [128, 1])
    t_const = Tok("const")
    S.dma(lambda e: e.dma_start(out=ident[:], in_=I["ident"]), writes=[t_const])
    S.dma(lambda e: e.dma_start(out=ones[:], in_=I["ones"]), writes=[t_const])
    S.dma(lambda e: e.dma_start(out=blk2[:], in_=I["blk2"]), writes=[t_const])
    S.pool(lambda e: e.memset(epsc[:], EPS), writes=[t_const])
    S.dve(lambda e: e.tensor_copy(out=identb[:], in_=ident[:]), reads=[t_const], writes=[t_const])

    S.nosync = True
    cT = sb("cT", [128, 8, 17])
    scT = sb("scT", [128, 8, 17])
    adab = sb("adab", [128, 48])
    n1g = sb("n1g", [128, 8])
    n2g = sb("n2g", [128, 8])
    qkg = sb("qkg", [128, 2])
    modT = sb("modT", [128, 48, 17])
    A1 = sb("A1", [128, 8, 17])
    A2 = sb("A2", [128, 8, 17])
    t_small = Tok("small")
    t_mod = Tok("mod")
    for dst, src in ((cT, "cT"), (adab, "ada_bT"), (n1g, "n1gT"), (n2g, "n2gT"), (qkg, "qkg")):
        S.dma(lambda e, dst=dst, src=src: e.dma_start(out=dst[:], in_=I[src]), writes=[t_small])
    S.act(lambda e: e.activation(out=scT[:], in_=cT[:], func=AF.Silu), reads=[t_small], writes=[t_small])
    adaw_v = I["ada_w"].rearrange("(c p) n -> p c n", p=128)
    with contextlib.ExitStack() as st2:
        wb = [sbt(st2, "adaw", [128, 8, 512]) for i in range(2)]
        wt = [Tok("adaw%d" % i) for i in range(2)]
        for nb in range(12):
            w = wb[nb % 2]
            wtk = wt[nb % 2]
            S.dma(lambda e, w=w, nb=nb: e.dma_start(out=w[:], in_=adaw_v[:, :, nb * 512:(nb + 1) * 512]), writes=[wtk])
            bk = nb % 2
            for oc in range(4):
                for c in range(8):
                    S.pe(lambda e, w=w, oc=oc, c=c, bk=bk: e.matmul(
                        banks[bk][:, oc * 32:oc * 32 + 17], lhsT=w[:, c, oc * 128:(oc + 1) * 128], rhs=scT[:, c, :],
                        start=(c == 0), stop=(c == 7)), reads=[wtk, t_small], writes=[bt[bk]])
            for oc in range(4):
                j = nb * 4 + oc
                S.act(lambda e, oc=oc, j=j, bk=bk: e.activation(
                    out=modT[:, j, :], in_=banks[bk][:, oc * 32:oc * 32 + 17], func=AF.Identity, bias=adab[:, j:j + 1]),
                    reads=[bt[bk], t_small], writes=[t_mod])
    S.barrier()
    S.dve(lambda e: e.tensor_scalar(out=A1[:], in0=modT[:, 8:16, :], scalar1=1.0, scalar2=None, op0=ALU.add),
          reads=[t_mod], writes=[t_mod])
    S.dve(lambda e: e.tensor_tensor(out=A1[:], in0=A1[:], in1=bc(n1g[:].unsqueeze(2), [128, 8, 17]), op=ALU.mult),
          reads=[t_mod, t_small], writes=[t_mod])
    S.dve(lambda e: e.tensor_scalar(out=A2[:], in0=modT[:, 32:40, :], scalar1=1.0, scalar2=None, op0=ALU.add),
          reads=[t_mod], writes=[t_mod])
    S.dve(lambda e: e.tensor_tensor(out=A2[:], in0=A2[:], in1=bc(n2g[:].unsqueeze(2), [128, 8, 17]), op=ALU.mult),
          reads=[t_mod, t_small], writes=[t_mod])
    if debug:
        S.dma(lambda e: e.dma_start(out=O["dbg_mod"], in_=modT[:]), reads=[t_mod])

    stH = contextlib.ExitStack()
    stC_holder = []
    hT = sbt(stH, "hT", [128, 8, NT], BF16)
    groups = [(g * 512, 512) for g in range(8)] + [(T, NS)]
    t_h = [Tok("h%d" % g) for g in range(9)]
    xT_v = I["xT"].rearrange("(c p) n -> p c n", p=128)

    def norm_groups(src_v, dst, Aap, Bidx, t_dst, extra_reads=()):
        with contextlib.ExitStack() as st2:
            xg = [sbt(st2, "xg", [128, 8, 512]) for i in range(2)]
            xgt = [Tok() for i in range(2)]
            sq = sbt(st2, "sq", [128, 8, 512])
            sqt = Tok()
            rs = sbt(st2, "rs", [128, 512])
            rst = Tok()
            for g, (t0, n) in enumerate(groups):
                x_ = xg[g % 2]
                xt_ = xgt[g % 2]
                bk = 2 + g % 2
                S.dma(lambda e, x_=x_, t0=t0, n=n: e.dma_start(out=x_[:, :, 0:n], in_=src_v[:, :, t0:t0 + n]),
                      writes=[xt_], reads=list(extra_reads))
                S.act(lambda e, x_=x_, n=n: e.activation(out=sq[:, :, 0:n], in_=x_[:, :, 0:n], func=AF.Square),
                      reads=[xt_], writes=[sqt])
                for c in range(8):
                    S.pe(lambda e, c=c, n=n, bk=bk: e.matmul(banks[bk][:, 0:n], lhsT=ones[:], rhs=sq[:, c, 0:n],
                                                              start=(c == 0), stop=(c == 7)),
                         reads=[sqt, t_const], writes=[bt[bk]])
                S.act(lambda e, n=n, bk=bk: e.activation(out=rs[:, 0:n], in_=banks[bk][:, 0:n], func=AF.Sqrt,
                                                          scale=1.0 / D, bias=epsc[:]),
                      reads=[bt[bk], t_const], writes=[rst])
                S.dve(lambda e, n=n: e.reciprocal(out=rs[:, 0:n], in_=rs[:, 0:n]), reads=[rst], writes=[rst])
                S.dve(lambda e, x_=x_, n=n: e.tensor_tensor(out=x_[:, :, 0:n], in0=x_[:, :, 0:n],
                                                             in1=bc(rs[:, 0:n].unsqueeze(1), [128, 8, n]), op=ALU.mult),
                      reads=[xt_, rst], writes=[xt_])
                if g < 8:
                    for c in range(8):
                        eng = S.dve if c % 2 == 0 else S.pool
                        eng(lambda e, x_=x_, c=c, t0=t0, n=n: e.tensor_scalar(
                            out=dst[:, c, t0:t0 + n], in0=x_[:, c, 0:n], scalar1=Aap[:, c, 0:1],
                            scalar2=modT[:, Bidx + c, 0:1], op0=ALU.mult, op1=ALU.add),
                            reads=[xt_, t_mod], writes=[t_dst[g]])
                else:
                    xv = x_[:, :, 0:NS].rearrange("p c (b t) -> p c b t", t=4)
                    S.dve(lambda e, xv=xv: e.tensor_tensor(
                        out=xv, in0=xv, in1=bc(Aap[:, :, 1:17].unsqueeze(3), [128, 8, SB, 4]), op=ALU.mult),
                        reads=[xt_, t_mod], writes=[xt_])
                    S.dve(lambda e, xv=xv, t0=t0: e.tensor_tensor(
                        out=dst[:, :, t0:t0 + NS].rearrange("p c (b t) -> p c b t", t=4), in0=xv,
                        in1=bc(modT[:, Bidx:Bidx + 8, 1:17].unsqueeze(3), [128, 8, SB, 4]), op=ALU.add),
                        reads=[xt_, t_mod], writes=[t_dst[g]])

    norm_groups(xT_v, hT, A1, 0, t_h)
    S.barrier()
    if debug:
        S.dma(lambda e: e.dma_start(out=O["dbg_hT"], in_=hT[:]), reads=t_h)


    if phase_limit < 3:
        S.emit()
        for sk_ in (stC_holder + [stH, st]):
            sk_.close()
        return nc
    S.nosync = True
    stC = contextlib.ExitStack()
    stC_holder.append(stC)
    EBT = sbt(stC, "EBT", [128, 24, 2, 128], BF16)
    onesb = sbt(stC, "onesb", [128, 64], BF16)
    t_ebt = Tok("ebt")
    g0_cst = [sbt(stC, "cst", [128, 1024]) for i in range(3)]
    g0_cbf = [sbt(stC, "cbf", [128, 1024], BF16) for i in range(3)]
    g0_tcs = [Tok() for i in range(3)]
    g0_tcb = [Tok() for i in range(3)]
    g0_blocks = []
    euv_src = I["euT"].rearrange("(c p) e -> p c e", p=128)
    for b_ in range(128):
        g0_blocks.append((lambda i, b_=b_: (g0_cst[i][:].rearrange("p (c e) -> p c e", c=8), euv_src[:, :, b_ * 128:(b_ + 1) * 128]),
                          lambda i, b_=b_: (euT_d[b_ // 2][:, :, (b_ % 2) * 128:(b_ % 2 + 1) * 128], g0_cbf[i][:].rearrange("p (c e) -> p c e", c=8))))
    for src_, dstd_, nblk_ in ((I["ev"], ev_d, 128), (I["w_pq"], wpq_d, 16)):
        sv_ = src_.rearrange("a b -> (a b)").rearrange("(n p f) -> n p f", p=128, f=1024)
        dv_ = dstd_.rearrange("a b -> (a b)").rearrange("(n p f) -> n p f", p=128, f=1024)
        for b_ in range(nblk_):
            g0_blocks.append((lambda i, b_=b_, sv_=sv_: (g0_cst[i][:], sv_[b_]), lambda i, b_=b_, dv_=dv_: (dv_[b_], g0_cbf[i][:])))
    g0_tick = [0]

    def g0_step():
        t = g0_tick[0]
        g0_tick[0] += 1
        nb_ = len(g0_blocks)
        if t - 2 >= 0 and t - 2 < nb_:
            k = t - 2
            i = k % 3
            o_, i_ = g0_blocks[k][1](i)
            S.dma(lambda e, o_=o_, i_=i_: e.dma_start(out=o_, in_=i_), reads=[g0_tcb[i]])
        if t < nb_:
            i = t % 3
            o_, i_ = g0_blocks[t][0](i)
            S.dma(lambda e, o_=o_, i_=i_: e.dma_start(out=o_, in_=i_), writes=[g0_tcs[i]])
        if t - 1 >= 0 and t - 1 < nb_:
            i = (t - 1) % 3
            S.pool(lambda e, i=i: e.tensor_copy(out=g0_cbf[i][:], in_=g0_cst[i][:]), reads=[g0_tcs[i]], writes=[g0_tcb[i]])
    S.dve(lambda e: e.tensor_copy(out=onesb[:], in_=ones[:, 0:64]), reads=[t_const], writes=[t_const])
    with contextlib.ExitStack() as st2:
        relb = sbt(st2, "relb", [32, 8])
        ohu = sbt(st2, "ohu", [32, 3, 384])
        RH = sbt(st2, "RH", [32, 8, 384])
        grep = [sbt(st2, "grep", [128, 384]) for i in range(2)]
        gt = [Tok(), Tok()]
        ebf = [sbt(st2, "ebf", [128, 2, 128]) for i in range(2)]
        et = [Tok(), Tok()]
        t_r = Tok()
        t_rh = Tok()
        S.dma(lambda e: e.dma_start(out=relb[:], in_=I["relb"]), writes=[t_r])
        S.dma(lambda e: e.dma_start(out=ohu[:], in_=I["ohu"]), writes=[t_r])
        S.act(lambda e: e.activation(out=relb[:], in_=relb[:], func=AF.Exp), reads=[t_r], writes=[t_r])
        zt = Tok()
        for br in range(3):
            S.dve(lambda e, br=br: e.tensor_tensor(out=RH[:], in0=bc(relb[:].unsqueeze(2), [32, 8, 384]),
                                                    in1=bc(ohu[:, br, :].unsqueeze(1), [32, 8, 384]), op=ALU.mult),
                  reads=[t_r], writes=[t_rh])
            for h in range(8):
                i = br * 8 + h
                bk = 6 + i % 2
                S.pe(lambda e, h=h, bk=bk: e.matmul(banks[bk][:, 0:384], lhsT=ones[0:32, :], rhs=RH[:, h, :],
                                                     start=True, stop=True), reads=[t_rh, t_const], writes=[bt[bk]])
                g_ = grep[i % 2]
                S.act(lambda e, g_=g_, bk=bk: e.activation(out=g_[:], in_=banks[bk][:, 0:384], func=AF.Copy),
                      reads=[bt[bk]], writes=[gt[i % 2]])
                zi = Tok()
                S.dma(lambda e, g_=g_, i=i: e.dma_start(out=zscr[i, 0:128, :], in_=g_[:]), reads=[gt[i % 2]], writes=[zi])
                S.dma(lambda e, g_=g_, i=i: e.dma_start(out=zscr[i, 128:256, :], in_=g_[:]), reads=[gt[i % 2]], writes=[zi])
                eb_ = ebf[i % 2]
                for part in range(2):
                    src = bass.AP(zscr.tensor, i * 256 * 384 + 255 + part * 128 * 383, [[383, 128], [1, 128]])
                    S.dma(lambda e, eb_=eb_, part=part, src=src: e.dma_start(out=eb_[:, part, :], in_=src),
                          reads=[zi], writes=[et[i % 2]])
                S.dve(lambda e, eb_=eb_, i=i: e.tensor_copy(out=EBT[:, i, :, :], in_=eb_[:]), reads=[et[i % 2]], writes=[t_ebt])
    S.barrier()
    if debug:
        S.dma(lambda e: e.dma_start(out=O["dbg_ebt"], in_=EBT[:]), reads=[t_ebt])

    win_v = I["w_in"].rearrange("(c p) n -> p c n", p=128)

    class _Stop(Exception):
        pass

    def ck(x):
        if phase_limit < x:
            raise _Stop()

    with contextlib.ExitStack() as st2, contextlib.suppress(_Stop):
        ck(3.5)
        wst = [sbt(st2, "wst", [128, 8, 128]) for i in range(2)]
        wstt = [Tok(), Tok()]
        wqkv = sbt(st2, "wqkv", [128, 8, 3, 128], BF16)
        t_w = Tok()
        qT = sbt(st2, "qT", [128, T], BF16)
        kT = sbt(st2, "kT", [128, T], BF16)
        t_q = [Tok() for g in range(8)]
        t_k = [Tok() for g in range(8)]
        V = sbt(st2, "V", [128, 3, 32, 128], BF16)
        t_v = [[Tok() for j in range(32)] for br in range(3)]
        accO = sbt(st2, "accO", [128, 2048])
        accS = sbt(st2, "accS", [128, 2048])
        t_acc = Tok()
        sq = sbt(st2, "sq", [128, 512])
        t_sq = Tok()
        rs = sbt(st2, "rs", [128, 512])
        t_rs = Tok()
        kTf = sbt(st2, "kTf", [128, 512])
        t_kf = Tok()
        ktok = [sbt(st2, "ktok", [128, 4, 128]) for i in range(2)]
        t_kt = [Tok(), Tok()]
        vtok = [sbt(st2, "vtok", [128, 4, 128]) for i in range(2)]
        t_vt = [Tok(), Tok()]
        e0 = [sbt(st2, "e0", [128, 512], BF16) for i in range(3)]
        t_e0 = [Tok(), Tok(), Tok()]
        ee = [sbt(st2, "ee", [128, 512], BF16) for i in range(3)]
        t_ee = [Tok(), Tok(), Tok()]
        mixo = sbt(st2, "mixo", [128, 2048], BF16)
        t_mixo = Tok()
        nld = 0
        nblk = 0
        for hp in range(4):
            for wi in range(3):
                w_ = wst[nld % 2]
                wt_ = wstt[nld % 2]
                nld += 1
                col = wi * 512 + hp * 128
                S.dma(lambda e, w_=w_, col=col: e.dma_start(out=w_[:], in_=win_v[:, :, col:col + 128]), writes=[wt_])
                S.pool(lambda e, w_=w_, wi=wi: e.tensor_copy(out=wqkv[:, :, wi, :], in_=w_[:]), reads=[wt_], writes=[t_w])
            for g in range(8):
                for wi, dstT, tks in ((0, qT, t_q), (1, kT, t_k)):
                    bk = 4 + wi
                    for c in range(8):
                        S.pe(lambda e, c=c, wi=wi, g=g, bk=bk: e.matmul(
                            banks[bk][:], lhsT=wqkv[:, c, wi, :], rhs=hT[:, c, g * 512:(g + 1) * 512],
                            start=(c == 0), stop=(c == 7)), reads=[t_w, t_h[g]], writes=[bt[bk]])
                    S.act(lambda e, bk=bk: e.activation(out=sq[:], in_=banks[bk][:], func=AF.Square),
                          reads=[bt[bk]], writes=[t_sq])
                    S.pe(lambda e: e.matmul(banks[6][:], lhsT=blk2[:], rhs=sq[:], start=True, stop=True),
                         reads=[t_sq, t_const], writes=[bt[6]])
                    S.act(lambda e: e.activation(out=rs[:], in_=banks[6][:], func=AF.Sqrt, scale=1.0 / 64, bias=epsc[:]),
                          reads=[bt[6], t_const], writes=[t_rs])
                    S.dve(lambda e: e.reciprocal(out=rs[:], in_=rs[:]), reads=[t_rs], writes=[t_rs])
                    S.dve(lambda e, bk=bk, wi=wi, g=g, dstT=dstT: e.scalar_tensor_tensor(
                        out=dstT[:, g * 512:(g + 1) * 512], in0=banks[bk][:], scalar=qkg[:, wi:wi + 1], in1=rs[:],
                        op0=ALU.mult, op1=ALU.mult), reads=[bt[bk], t_rs, t_small], writes=[tks[g]])
                    if wi == 1 and g >= 4:
                        S.dve(lambda e, bk=bk: e.scalar_tensor_tensor(
                            out=kTf[:], in0=banks[bk][:], scalar=qkg[:, 1:2], in1=rs[:], op0=ALU.mult, op1=ALU.mult),
                            reads=[bt[bk], t_rs, t_small], writes=[t_kf])
                        for j in range(4):
                            S.pe(lambda e, j=j: e.transpose(banks[7][:, j * 128:(j + 1) * 128], kTf[:, j * 128:(j + 1) * 128], ident[:]),
                                 reads=[t_kf, t_const], writes=[bt[7]])
                        kt_ = ktok[g % 2]
                        S.act(lambda e, kt_=kt_: e.activation(out=kt_[:].rearrange("p a b -> p (a b)"), in_=banks[7][:], func=AF.Copy),
                              reads=[bt[7]], writes=[t_kt[g % 2]])
                        r0 = g * 512 - 2048
                        S.dma(lambda e, kt_=kt_, r0=r0, hp=hp: e.dma_start(
                            out=O["kwp"][r0:r0 + 512, hp * 128:(hp + 1) * 128].rearrange("(j p) c -> p j c", p=128), in_=kt_[:]),
                            reads=[t_kt[g % 2]])
            ck(3.6)
            for br, dil in enumerate((1, 4, 16)):
                if br == 1:
                    ck(3.62)
                G = 32 // dil
                for j0 in range(0, 32, 4):
                    bk = 6 + (j0 // 4) % 2
                    for jj in range(4):
                        j = j0 + jj
                        r, g = j // G, j % G
                        start = r + dil * 128 * g
                        tg = sorted(set([(start) // 512, (start + dil * 127) // 512]))
                        for c in range(8):
                            S.pe(lambda e, c=c, jj=jj, start=start, dil=dil, bk=bk: e.matmul(
                                banks[bk][:, jj * 128:(jj + 1) * 128],
                                lhsT=hT[:, c, start:start + dil * 127 + 1:dil], rhs=wqkv[:, c, 2, :],
                                start=(c == 0), stop=(c == 7)), reads=[t_w] + [t_h[x] for x in tg], writes=[bt[bk]], strided=(dil > 1))
                    S.act(lambda e, br=br, j0=j0, bk=bk: e.activation(
                        out=V[:, br, j0:j0 + 4, :].rearrange("p a b -> p (a b)"), in_=banks[bk][:], func=AF.Copy),
                        reads=[bt[bk]], writes=[t_v[br][j0 + x] for x in range(4)])
                    if br == 0 and j0 >= 16 :
                        vt_ = vtok[(j0 // 4) % 2]
                        tv_ = t_vt[(j0 // 4) % 2]
                        S.act(lambda e, vt_=vt_, bk=bk: e.activation(out=vt_[:].rearrange("p a b -> p (a b)"), in_=banks[bk][:], func=AF.Copy),
                              reads=[bt[bk]], writes=[tv_])
                        r0 = j0 * 128 - 2048
                        S.dma(lambda e, vt_=vt_, r0=r0, hp=hp: e.dma_start(
                            out=O["vwp"][r0:r0 + 512, hp * 128:(hp + 1) * 128].rearrange("(j p) c -> p j c", p=128), in_=vt_[:]),
                            reads=[tv_])
            ck(3.7)
            for half in range(2):
                for br, dil in enumerate((1, 4, 16)):
                    G = 32 // dil
                    Gh = G // 2
                    for r in range(dil):
                        for g in range(half * Gh, (half + 1) * Gh):
                            j = r * G + g
                            start = r + dil * 128 * g
                            qsl = slice(start, start + dil * 127 + 1, dil)
                            tgq = sorted(set([start // 512, (start + dil * 127) // 512]))
                            parts = [1] if g == 0 else [0, 1]
                            g0_step()
                            sbk = nblk % 3
                            obk = 3 + nblk % 3
                            ei = nblk % 3
                            nblk += 1
                            for hh in range(2):
                                ps_ = slice(hh * 64, hh * 64 + 64)
                                for part in parts:
                                    if part == 1:
                                        ksl = qsl
                                        tgk = tgq
                                    else:
                                        ps0 = start - dil * 128
                                        ksl = slice(ps0, ps0 + dil * 127 + 1, dil)
                                        tgk = sorted(set([ps0 // 512, (ps0 + dil * 127) // 512]))
                                    S.pe(lambda e, ps_=ps_, ksl=ksl, qsl=qsl, hh=hh, part=part, sbk=sbk: e.matmul(
                                        banks[sbk][:, (hh * 2 + part) * 128:(hh * 2 + part + 1) * 128],
                                        lhsT=kT[ps_, ksl], rhs=qT[ps_, qsl], start=True, stop=True),
                                        reads=[t_k[x] for x in tgk] + [t_q[x] for x in tgq], writes=[bt[sbk]], strided=(dil > 1))
                            S.act(lambda e, ei=ei, sbk=sbk: e.activation(out=e0[ei][:], in_=banks[sbk][:], func=AF.Exp, scale=0.125),
                                  reads=[bt[sbk]], writes=[t_e0[ei]])
                            S.dve(lambda e, ei=ei, br=br, hp=hp: e.tensor_tensor(
                                out=ee[ei][:], in0=e0[ei][:],
                                in1=EBT[:, br * 8 + 2 * hp:br * 8 + 2 * hp + 2, :, :].rearrange("p a b c -> p (a b c)"), op=ALU.mult),
                                reads=[t_e0[ei], t_ebt], writes=[t_ee[ei]])
                            for hh in range(2):
                                po = slice(hh * 64, hh * 64 + 64)
                                for pi, part in enumerate(parts):
                                    jj = j if part == 1 else j - 1
                                    esl = slice((hh * 2 + part) * 128, (hh * 2 + part + 1) * 128)
                                    S.pe(lambda e, po=po, br=br, jj=jj, hh=hh, esl=esl, ei=ei, obk=obk, pi=pi, parts=parts: e.matmul(
                                        banks[obk][po, 0:128], lhsT=V[:, br, jj, hh * 64:(hh + 1) * 64], rhs=ee[ei][:, esl],
                                        start=(pi == 0), stop=(pi == len(parts) - 1)),
                                        reads=[t_v[br][jj], t_ee[ei]], writes=[bt[obk]])
                                for pi, part in enumerate(parts):
                                    esl = slice((hh * 2 + part) * 128, (hh * 2 + part + 1) * 128)
                                    S.pe(lambda e, po=po, esl=esl, ei=ei, obk=obk, pi=pi, parts=parts: e.matmul(
                                        banks[obk][po, 128:256], lhsT=onesb[:], rhs=ee[ei][:, esl],
                                        start=(pi == 0), stop=(pi == len(parts) - 1)),
                                        reads=[t_const, t_ee[ei]], writes=[bt[obk]])
                            lo = start - half * 2048
                            asl = slice(lo, lo + dil * 127 + 1, dil)
                            if br == 0:
                                S.dve(lambda e, asl=asl, obk=obk: e.tensor_copy(out=accO[:, asl], in_=banks[obk][:, 0:128]),
                                      reads=[bt[obk]], writes=[t_acc])
                                S.dve(lambda e, asl=asl, obk=obk: e.tensor_copy(out=accS[:, asl], in_=banks[obk][:, 128:256]),
                                      reads=[bt[obk]], writes=[t_acc])
                            else:
                                S.dve(lambda e, asl=asl, obk=obk: e.tensor_tensor(out=accO[:, asl], in0=accO[:, asl], in1=banks[obk][:, 0:128], op=ALU.add),
                                      reads=[bt[obk], t_acc], writes=[t_acc])
                                S.dve(lambda e, asl=asl, obk=obk: e.tensor_tensor(out=accS[:, asl], in0=accS[:, asl], in1=banks[obk][:, 128:256], op=ALU.add),
                                      reads=[bt[obk], t_acc], writes=[t_acc])
                S.dve(lambda e: e.reciprocal(out=accS[:], in_=accS[:]), reads=[t_acc], writes=[t_acc])
                S.dve(lambda e: e.tensor_tensor(out=mixo[:], in0=accO[:], in1=accS[:], op=ALU.mult), reads=[t_acc], writes=[t_mixo, t_acc])
                S.dma(lambda e, hp=hp, half=half: e.dma_start(out=mixT_d[hp, :, half * 2048:(half + 1) * 2048], in_=mixo[:]), reads=[t_mixo])
                if debug:
                    S.dma(lambda e, hp=hp, half=half: e.dma_start(out=O["dbg_mixT"][hp, :, half * 2048:(half + 1) * 2048], in_=mixo[:]), reads=[t_mixo])
                ck(3.8)

    while g0_tick[0] < len(g0_blocks) + 2:
        g0_step()
    S.barrier()
    stC.close()
    if phase_limit < 4:
        S.emit()
        for sk_ in (stC_holder + [stH, st]):
            sk_.close()
        return nc
    S.nosync = True
    NEG = -0.6065306597126334
    with contextlib.ExitStack() as st2, contextlib.suppress(_Stop):
        t_rp = Tok("rwparams")
        rwp = sbt(st2, "rwp", [128, 8, 4])
        mul_ = sbt(st2, "mul", [128, 1])
        lw3 = sbt(st2, "lw3", [128, 512])
        w0row = sbt(st2, "w0row", [1, 512])
        lnxg = sbt(st2, "lnxg", [128, 512])
        lnxb = sbt(st2, "lnxb", [128, 512])
        tri = sbt(st2, "tri", [128, 2, 128])
        mk1 = sbt(st2, "mk1", [128, 2, 64])
        mk3 = sbt(st2, "mk3", [128, 64])
        id2 = sbt(st2, "id2", [128, 64])
        omka = sbt(st2, "omka", [128, 4])
        gneps = sbt(st2, "gneps", [128, 1])
        for dst, src in ((rwp, "rwp"), (mul_, "mul"), (lw3, "lw3"), (w0row, "w0row"), (tri, "tri"), (mk1, "mk1"), (mk3, "mk3"), (id2, "id2")):
            S.dma(lambda e, dst=dst, src=src: e.dma_start(out=dst[:], in_=I[src]), writes=[t_rp])
        S.dma(lambda e: e.dma_start(out=lnxg[:], in_=bass.AP(I["lnx"].tensor, 0, [[0, 128], [1, 512]])), writes=[t_rp])
        S.dma(lambda e: e.dma_start(out=lnxb[:], in_=bass.AP(I["lnx"].tensor, 512, [[0, 128], [1, 512]])), writes=[t_rp])
        S.dve(lambda e: e.tensor_scalar(out=omka[:], in0=rwp[:, 4, :], scalar1=-1.0, scalar2=1.0, op0=ALU.mult, op1=ALU.add),
              reads=[t_rp], writes=[t_rp])
        S.pool(lambda e: e.memset(gneps[:], 64e-5), writes=[t_rp])
        wr = sbt(st2, "wr", [128, 8, 1664], BF16)
        t_wr = Tok()
        with contextlib.ExitStack() as st3:
            wst2 = [sbt(st3, "wst2", [128, 8, 416]) for i in range(2)]
            wst2t = [Tok(), Tok()]
            for q4 in range(4):
                w_ = wst2[q4 % 2]
                S.dma(lambda e, w_=w_, q4=q4: e.dma_start(out=w_[:], in_=win_v[:, :, 1536 + q4 * 416:1536 + (q4 + 1) * 416]), writes=[wst2t[q4 % 2]])
                S.pool(lambda e, w_=w_, q4=q4: e.tensor_copy(out=wr[:, :, q4 * 416:(q4 + 1) * 416], in_=w_[:]), reads=[wst2t[q4 % 2]], writes=[t_wr])
        S.barrier()

        def T_(n=""):
            return Tok(n)

        pb = [sbt(st2, "pb", [128, 4, 129]) for x in range(3)]
        pbl = sbt(st2, "pbl", [128, 129])
        t_pb = T_()
        for x in range(3):
            S.pool(lambda e, x=x: e.memset(pb[x][:, :, 0:1], 0.0), writes=[t_pb])
        S.pool(lambda e: e.memset(pbl[:, 0:1], 0.0), writes=[t_pb])
        xm = [sbt(st2, "xm", [128, 4, 128]) for x in range(3)]
        xml = sbt(st2, "xml", [128, 128])
        t_xm = T_()
        twl = sbt(st2, "twl", [128, 128])
        sg_tok = sbt(st2, "sg_tok", [128, 512])
        aT = sbt(st2, "aT", [128, 4, 128])
        g_tok = sbt(st2, "g_tok", [128, 512])
        kk = sbt(st2, "kk", [128, 4, 128])
        sq4 = sbt(st2, "sq4", [128, 4, 128])
        kmod = sbt(st2, "kmod", [128, 4, 128])
        bb = sbt(st2, "bb", [128, 4, 128])
        rk = sbt(st2, "rk", [128, 4, 128])
        dtmp = rk
        bsum = sbt(st2, "bsum", [128, 8])
        ycen = sbt(st2, "ycen", [128, 8, 64])
        ysq = sbt(st2, "ysq", [128, 8, 64])
        gst = sbt(st2, "gst", [128, 8])
        gst2 = sbt(st2, "gst2", [128, 8])
        mixr = sbt(st2, "mixr", [128, 4, 128], BF16)
        st4 = contextlib.ExitStack()
        st2.enter_context(st4)
        Pin = sbt(st4, "Pin", [128, 4, 128])
        Pinv = sbt(st4, "Pinv", [128, 4, 128])
        Pex = sbt(st4, "Pex", [128, 4, 128])
        Phat = sbt(st4, "Phat", [128, 4, 128])
        PCl = sbt(st4, "PCl", [128, 4, 2])
        PC = sbt(st4, "PC", [128, 4, 2])
        AR = sbt(st4, "AR", [128, 4, 2, 2, 64])
        BtT = sbt(st4, "BtT", [128, 4, 128])
        AtT = sbt(st4, "AtT", [128, 4, 128])
        KtT = sbt(st4, "KtT", [128, 4, 128])
        BhT = sbt(st4, "BhT", [128, 4, 128])
        KhT = sbt(st4, "KhT", [128, 4, 128])
        Atok = sbt(st4, "Atok", [128, 512])
        Bhtok = sbt(st4, "Bhtok", [128, 512])
        Khtok = sbt(st4, "Khtok", [128, 512])
        Vtok = sbt(st4, "Vtok", [128, 512])
        NM = sbt(st4, "NM", [128, 8, 2, 64])
        AK = sbt(st4, "AK", [128, 8, 2, 64])
        Aj = [sbt(st4, "Aj", [128, 8, 64])] * 2
        Nj = [sbt(st4, "Nj", [128, 8, 64])] * 2
        Tj = [sbt(st4, "Tj", [128, 8, 64])] * 2
        Z = sbt(st4, "Z", [128, 8, 128])
        AV = sbt(st4, "AV", [128, 8, 128])
        McT = sbt(st4, "McT", [128, 4, 2, 64])
        dPC = sbt(st4, "dPC", [128, 4, 2, 64])
        RpT = sbt(st4, "RpT", [128, 4, 2, 64])
        Hs = sbt(st4, "Hs", [128, 4, 64])
        t_H = T_()
        S.pool(lambda e: e.memset(Hs[:], 0.0), writes=[t_H])
        (t_lora, t_sg, t_a, t_g, t_kk, t_km, t_b, t_rk, t_bs, t_P, t_AR, t_BK, t_BKh, t_tok, t_NM, t_AK, t_A0, t_Z, t_AV,
         t_Mc, t_Rp, t_y, t_mixr) = [T_() for i in range(23)]
        t_Aj = [T_()] * 2
        t_Nj = [T_()] * 2
        t_Tj = [T_()] * 2
        B = banks

        def v4(ap):
            return ap.rearrange("p q (c t) -> p q c t", c=2)

        for sbi in range(32):
            t0 = sbi * 128
            hg = t_h[t0 // 512]
            for x in range(3):
                bk = x
                for p in range(4):
                    for c in range(8):
                        S.pe(lambda e, x=x, p=p, c=c, bk=bk, t0=t0: e.matmul(
                            B[bk][:, p * 128:(p + 1) * 128], lhsT=wr[:, c, x * 512 + p * 128:x * 512 + (p + 1) * 128],
                            rhs=hT[:, c, t0:t0 + 128], start=(c == 0), stop=(c == 7)), reads=[t_wr, hg], writes=[bt[bk]])
                S.act(lambda e, x=x, bk=bk: e.activation(out=pb[x][:, :, 1:129], in_=B[bk][:].rearrange("p (q t) -> p q t", q=4), func=AF.Copy),
                      reads=[bt[bk], t_xm], writes=[t_pb])
            for c in range(8):
                S.pe(lambda e, c=c, t0=t0: e.matmul(B[3][:, 0:128], lhsT=wr[:, c, 1536:1664], rhs=hT[:, c, t0:t0 + 128],
                                                     start=(c == 0), stop=(c == 7)), reads=[t_wr, hg], writes=[bt[3]])
            S.act(lambda e: e.activation(out=pbl[:, 1:129], in_=B[3][:, 0:128], func=AF.Copy), reads=[bt[3], t_xm], writes=[t_pb])
            if sbi == 31:
                for x in range(3):
                    S.dma(lambda e, x=x: e.dma_start(
                        out=bass.AP(O["shp"].tensor, x * 512, [[1, 128], [128, 4], [1, 1]]), in_=pb[x][:, :, 128:129], allow_slow_non_contiguous=True), reads=[t_pb])
                S.dma(lambda e: e.dma_start(out=bass.AP(O["shp"].tensor, 1536, [[1, 128], [1, 1]]), in_=pbl[:, 128:129], allow_slow_non_contiguous=True), reads=[t_pb])
            for x in range(3):
                S.dve(lambda e, x=x: e.tensor_tensor(out=dtmp[:], in0=pb[x][:, :, 0:128], in1=pb[x][:, :, 1:129], op=ALU.subtract),
                      reads=[t_pb], writes=[t_xm, t_rk])
                S.dve(lambda e, x=x: e.tensor_tensor(out=dtmp[:], in0=dtmp[:], in1=bc(rwp[:, x, :].unsqueeze(2), [128, 4, 128]), op=ALU.mult),
                      reads=[t_xm, t_rp, t_rk], writes=[t_xm, t_rk])
                S.dve(lambda e, x=x: e.tensor_tensor(out=xm[x][:], in0=dtmp[:], in1=pb[x][:, :, 1:129], op=ALU.add),
                      reads=[t_xm, t_pb, t_rk], writes=[t_xm])
            S.dve(lambda e: e.tensor_tensor(out=xml[:], in0=pbl[:, 0:128], in1=pbl[:, 1:129], op=ALU.subtract), reads=[t_pb], writes=[t_lora])
            S.dve(lambda e: e.scalar_tensor_tensor(out=xml[:], in0=xml[:], scalar=mul_[:, 0:1], in1=pbl[:, 1:129], op0=ALU.mult, op1=ALU.add),
                  reads=[t_lora, t_pb, t_rp], writes=[t_lora])
            for x in range(3):
                S.pool(lambda e, x=x: e.tensor_copy(out=pb[x][:, :, 0:1], in_=pb[x][:, :, 128:129]), reads=[t_pb, t_xm], writes=[t_pb])
            S.pool(lambda e: e.tensor_copy(out=pbl[:, 0:1], in_=pbl[:, 128:129]), reads=[t_pb, t_lora], writes=[t_pb])
            S.act(lambda e: e.activation(out=twl[0:32, :], in_=xml[0:32, :], func=AF.Tanh), reads=[t_lora], writes=[t_sg])
            S.act(lambda e: e.activation(out=twl[64:128, :], in_=xml[64:128, :], func=AF.Sigmoid), reads=[t_lora], writes=[t_sg])
            S.pe(lambda e: e.matmul(B[4][:], lhsT=twl[0:32, :], rhs=lw3[0:32, :], start=True, stop=False), reads=[t_sg, t_rp], writes=[bt[4]])
            S.pe(lambda e: e.matmul(B[4][:], lhsT=ones[0:1, :], rhs=w0row[0:1, :], start=False, stop=True), reads=[t_const, t_rp], writes=[bt[4]])
            S.act(lambda e: e.activation(out=sg_tok[:], in_=B[4][:], func=AF.Sigmoid), reads=[bt[4]], writes=[t_sg])
            for p in range(4):
                S.pe(lambda e, p=p: e.matmul(B[5][:, p * 128:(p + 1) * 128], lhsT=lw3[32:64, p * 128:(p + 1) * 128], rhs=xml[32:64, :],
                                              start=True, stop=True), reads=[t_lora, t_rp], writes=[bt[5]])
            S.dve(lambda e: e.tensor_tensor(out=aT[:], in0=B[5][:].rearrange("p (q t) -> p q t", q=4),
                                            in1=bc(rwp[:, 6, :].unsqueeze(2), [128, 4, 128]), op=ALU.add), reads=[bt[5], t_rp], writes=[t_a])
            S.act(lambda e: e.activation(out=aT[:], in_=aT[:], func=AF.Sigmoid), reads=[t_a], writes=[t_a])
            S.pe(lambda e: e.matmul(B[6][:], lhsT=twl[64:128, :], rhs=lw3[64:128, :], start=True, stop=True), reads=[t_sg, t_rp], writes=[bt[6]])
            S.act(lambda e: e.activation(out=g_tok[:], in_=B[6][:], func=AF.Copy), reads=[bt[6], t_y], writes=[t_g])
            S.dve(lambda e: e.tensor_tensor(out=kk[:], in0=xm[1][:], in1=bc(rwp[:, 3, :].unsqueeze(2), [128, 4, 128]), op=ALU.mult),
                  reads=[t_xm, t_rp], writes=[t_kk])
            S.act(lambda e: e.activation(out=sq4[:], in_=kk[:], func=AF.Square), reads=[t_kk], writes=[t_kk])
            S.pe(lambda e: e.matmul(B[7][:], lhsT=blk2[:], rhs=sq4[:].rearrange("p q t -> p (q t)"), start=True, stop=True),
                 reads=[t_kk, t_const], writes=[bt[7]])
            S.act(lambda e: e.activation(out=sq4[:].rearrange("p q t -> p (q t)"), in_=B[7][:], func=AF.Sqrt), reads=[bt[7]], writes=[t_kk])
            S.dve(lambda e: e.tensor_scalar(out=sq4[:], in0=sq4[:], scalar1=1e-12, scalar2=None, op0=ALU.max), reads=[t_kk], writes=[t_kk])
            S.dve(lambda e: e.reciprocal(out=sq4[:], in_=sq4[:]), reads=[t_kk], writes=[t_kk])
            S.dve(lambda e: e.tensor_tensor(out=kk[:], in0=kk[:], in1=sq4[:], op=ALU.mult), reads=[t_kk], writes=[t_kk])
            S.dve(lambda e: e.tensor_tensor(out=kmod[:], in0=aT[:], in1=bc(rwp[:, 4, :].unsqueeze(2), [128, 4, 128]), op=ALU.mult),
                  reads=[t_a, t_rp], writes=[t_km])
            S.dve(lambda e: e.tensor_tensor(out=kmod[:], in0=kmod[:], in1=bc(omka[:].unsqueeze(2), [128, 4, 128]), op=ALU.add),
                  reads=[t_km, t_rp], writes=[t_km])
            S.dve(lambda e: e.tensor_tensor(out=kmod[:], in0=kmod[:], in1=xm[1][:], op=ALU.mult), reads=[t_km, t_xm], writes=[t_km])
            S.dve(lambda e: e.tensor_tensor(out=bb[:], in0=kk[:], in1=aT[:], op=ALU.mult), reads=[t_kk, t_a], writes=[t_b])
            S.pool(lambda e: e.tensor_tensor(out=rk[:], in0=xm[0][:], in1=kmod[:], op=ALU.mult), reads=[t_xm, t_km], writes=[t_rk])
            S.pool(lambda e: e.tensor_tensor(out=rk[:], in0=rk[:], in1=bc(rwp[:, 5, :].unsqueeze(2), [128, 4, 128]), op=ALU.mult),
                   reads=[t_rk, t_rp], writes=[t_rk])
            for h in (0, 2, 4, 6, 1, 3, 5, 7):
                p, hh = h // 2, h % 2
                fp = slice(hh * 64, hh * 64 + 64)
                S.pe(lambda e, p=p, fp=fp, h=h: e.matmul(B[6][:, 256 + h:256 + h + 1], lhsT=rk[fp, p, :], rhs=ones[fp, 0:1], start=True, stop=True),
                     reads=[t_rk, t_const], writes=[bt[6]])
            S.act(lambda e: e.activation(out=bsum[:], in_=B[6][:, 256:264], func=AF.Copy), reads=[bt[6], t_y], writes=[t_bs])
            for p in range(4):
                S.pe(lambda e, p=p: e.matmul(B[0][:, p * 128:(p + 1) * 128], lhsT=sg_tok[:, p * 128:(p + 1) * 128], rhs=tri[:, 0, :],
                                              start=True, stop=True), reads=[t_sg, t_rp], writes=[bt[0]])
            for p in range(4):
                S.pe(lambda e, p=p: e.matmul(B[1][:, p * 128:(p + 1) * 128], lhsT=sg_tok[:, p * 128:(p + 1) * 128], rhs=tri[:, 1, :],
                                              start=True, stop=True), reads=[t_sg, t_rp], writes=[bt[1]])
            lp = B[0][:].rearrange("p (q t) -> p q t", q=4)
            S.act(lambda e: e.activation(out=Pin[:], in_=lp, func=AF.Exp), reads=[bt[0]], writes=[t_P])
            S.act(lambda e: e.activation(out=Pinv[:], in_=lp, func=AF.Exp, scale=-1.0), reads=[bt[0]], writes=[t_P])
            S.act(lambda e: e.activation(out=Pex[:], in_=B[1][:].rearrange("p (q t) -> p q t", q=4), func=AF.Exp), reads=[bt[1]], writes=[t_P])
            lp4 = B[0][:].rearrange("p (q c t) -> p q c t", q=4, c=2)
            S.act(lambda e: e.activation(out=PCl[:], in_=lp4[:, :, :, 63], func=AF.Copy), reads=[bt[0]], writes=[t_P])
            S.act(lambda e: e.activation(out=v4(Phat[:]), in_=lp4, func=AF.Copy), reads=[bt[0]], writes=[t_P])
            S.dve(lambda e: e.tensor_tensor(out=v4(Phat[:]), in0=bc(PCl[:].unsqueeze(3), [128, 4, 2, 64]), in1=v4(Phat[:]), op=ALU.subtract),
                  reads=[t_P], writes=[t_P])
            S.act(lambda e: e.activation(out=Phat[:], in_=Phat[:], func=AF.Exp), reads=[t_P], writes=[t_P])
            S.act(lambda e: e.activation(out=PC[:], in_=PCl[:], func=AF.Exp), reads=[t_P], writes=[t_P])
            S.dve(lambda e: e.scalar_tensor_tensor(out=AR[:, :, :, 0, :], in0=v4(kk[:]), scalar=-1.0, in1=v4(Pex[:]), op0=ALU.mult, op1=ALU.mult),
                  reads=[t_kk, t_P], writes=[t_AR])
            S.dve(lambda e: e.tensor_tensor(out=AR[:, :, :, 1, :], in0=v4(xm[0][:]), in1=v4(Pin[:]), op=ALU.mult), reads=[t_xm, t_P], writes=[t_AR])
            S.pool(lambda e: e.tensor_tensor(out=BtT[:], in0=bb[:], in1=Pinv[:], op=ALU.mult), reads=[t_b, t_P], writes=[t_BK])
            S.pool(lambda e: e.tensor_tensor(out=KtT[:], in0=kmod[:], in1=Pinv[:], op=ALU.mult), reads=[t_km, t_P], writes=[t_BK])
            S.pool(lambda e: e.tensor_tensor(out=BhT[:], in0=bb[:], in1=Phat[:], op=ALU.mult), reads=[t_b, t_P], writes=[t_BKh])
            S.pool(lambda e: e.tensor_tensor(out=KhT[:], in0=kmod[:], in1=Phat[:], op=ALU.mult), reads=[t_km, t_P], writes=[t_BKh])
            S.pool(lambda e: e.tensor_copy(out=v4(AtT[:]), in_=AR[:, :, :, 0, :]), reads=[t_AR], writes=[t_BKh])
            for src_fn, dst, rd, bk in ((lambda p: AtT[:, p, :], Atok, [t_BKh], 2), (lambda p: BhT[:, p, :], Bhtok, [t_BKh], 3),
                                        (lambda p: KhT[:, p, :], Khtok, [t_BKh], 4), (lambda p: xm[2][:, p, :], Vtok, [t_xm], 5)):
                for p in range(4):
                    S.pe(lambda e, p=p, src_fn=src_fn, bk=bk: e.transpose(B[bk][:, p * 128:(p + 1) * 128], src_fn(p), ident[:]),
                         reads=rd + [t_const], writes=[bt[bk]])
                S.act(lambda e, dst=dst, bk=bk: e.activation(out=dst[:], in_=B[bk][:], func=AF.Copy), reads=[bt[bk], t_y, t_AV, t_Z], writes=[t_tok])
            for ch in range(2):
                tp = slice(ch * 64, ch * 64 + 64)
                for h in (0, 2, 4, 6, 1, 3, 5, 7):
                    p, hh = h // 2, h % 2
                    fp = slice(hh * 64, hh * 64 + 64)
                    csl = slice(ch * 64, ch * 64 + 64)
                    arr = AR[fp, p, ch, :, :].rearrange("p a t -> p (a t)")
                    S.pe(lambda e, tp=tp, fp=fp, p=p, h=h, csl=csl, arr=arr: e.matmul(
                        B[0][tp, (h % 4) * 128:(h % 4 + 1) * 128] if h < 4 else B[1][tp, (h % 4) * 128:(h % 4 + 1) * 128],
                        lhsT=BtT[fp, p, csl], rhs=arr, start=True, stop=True), reads=[t_BK, t_AR], writes=[bt[0 if h < 4 else 1]])
                    S.pe(lambda e, tp=tp, fp=fp, p=p, h=h, csl=csl, arr=arr: e.matmul(
                        B[2][tp, (h % 4) * 128:(h % 4 + 1) * 128] if h < 4 else B[3][tp, (h % 4) * 128:(h % 4 + 1) * 128],
                        lhsT=KtT[fp, p, csl], rhs=arr, start=True, stop=True), reads=[t_BK, t_AR], writes=[bt[2 if h < 4 else 3]])
                    S.pe(lambda e, tp=tp, fp=fp, p=p, h=h, csl=csl, ch=ch: e.matmul(
                        B[4][tp, h * 64:(h + 1) * 64], lhsT=AR[fp, p, ch, 0, :], rhs=BtT[fp, p, csl], start=True, stop=True),
                        reads=[t_BK, t_AR], writes=[bt[4]])
            mk1b = bc(mk1[:].unsqueeze(1), [128, 4, 2, 64])
            for hf in range(2):
                S.dve(lambda e, hf=hf: e.tensor_tensor(out=NM[:, hf * 4:(hf + 1) * 4, :, :], in0=B[hf][:].rearrange("p (h a t) -> p h a t", h=4, a=2),
                                                        in1=mk1b, op=ALU.mult), reads=[bt[hf], t_rp], writes=[t_NM])
                S.dve(lambda e, hf=hf: e.tensor_tensor(out=AK[:, hf * 4:(hf + 1) * 4, :, :], in0=B[2 + hf][:].rearrange("p (h a t) -> p h a t", h=4, a=2),
                                                        in1=mk1b, op=ALU.mult), reads=[bt[2 + hf], t_rp], writes=[t_AK])
            S.dve(lambda e: e.tensor_tensor(out=Aj[0][:], in0=B[4][:].rearrange("p (h t) -> p h t", h=8), in1=bc(mk3[:].unsqueeze(1), [128, 8, 64]), op=ALU.mult),
                  reads=[bt[4], t_rp], writes=[t_Aj[0]])
            S.pool(lambda e: e.tensor_copy(out=Nj[0][:], in_=NM[:, :, 0, :]), reads=[t_NM], writes=[t_Nj[0]])
            S.pool(lambda e: e.tensor_tensor(out=Tj[0][:], in0=NM[:, :, 0, :], in1=bc(id2[:].unsqueeze(1), [128, 8, 64]), op=ALU.add),
                   reads=[t_NM, t_rp], writes=[t_Tj[0]])
            for lv in range(1, 6):
                a_o = Aj[0]
                n_o = Nj[0]
                t_o = Tj[0]
                ta, tn, tt = t_Aj[0], t_Nj[0], t_Tj[0]
                for ch in range(2):
                    tp = slice(ch * 64, ch * 64 + 64)
                    for h in range(8):
                        S.pe(lambda e, tp=tp, h=h: e.matmul(B[5][tp, h * 64:(h + 1) * 64], lhsT=n_o[tp, h, :], rhs=a_o[tp, h, :],
                                                             start=True, stop=True), reads=[ta, tn], writes=[bt[5]])
                if lv < 5:
                    for ch in range(2):
                        tp = slice(ch * 64, ch * 64 + 64)
                        for h in range(8):
                            S.pe(lambda e, tp=tp, h=h: e.matmul(B[6][tp, h * 64:(h + 1) * 64], lhsT=a_o[tp, h, :], rhs=n_o[tp, h, :],
                                                                 start=True, stop=True), reads=[ta, tn], writes=[bt[6]])
                S.act(lambda e: e.activation(out=a_o[:].rearrange("p h t -> p (h t)"), in_=B[5][:], func=AF.Copy), reads=[bt[5]], writes=[ta])
                if lv < 5:
                    S.act(lambda e: e.activation(out=n_o[:].rearrange("p h t -> p (h t)"), in_=B[6][:], func=AF.Copy), reads=[bt[6]], writes=[tn])
                for ch in range(2):
                    tp = slice(ch * 64, ch * 64 + 64)
                    for h in range(8):
                        S.pe(lambda e, tp=tp, h=h: e.matmul(B[7][tp, h * 64:(h + 1) * 64], lhsT=a_o[tp, h, :], rhs=t_o[tp, h, :],
                                                             start=True, stop=True), reads=[ta, tt], writes=[bt[7]])
                S.dve(lambda e: e.tensor_tensor(out=t_o[:], in0=B[7][:].rearrange("p (h t) -> p h t", h=8), in1=t_o[:], op=ALU.add),
                      reads=[bt[7], tt], writes=[tt])
            TT = Tj[5 % 2]
            t_TT = t_Tj[5 % 2]
            for ch in range(2):
                tp = slice(ch * 64, ch * 64 + 64)
                for h in range(8):
                    S.pe(lambda e, tp=tp, h=h: e.matmul(B[4][tp, h * 64:(h + 1) * 64], lhsT=AK[tp, h, 0, :], rhs=Vtok[tp, h * 64:(h + 1) * 64],
                                                         start=True, stop=True), reads=[t_AK, t_tok], writes=[bt[4]])
            S.act(lambda e: e.activation(out=Z[:, :, 64:128], in_=B[4][:].rearrange("p (h t) -> p h t", h=8), func=AF.Copy), reads=[bt[4]], writes=[t_Z])
            S.pool(lambda e: e.tensor_copy(out=Z[:, :, 0:64], in_=Atok[:].rearrange("p (h t) -> p h t", h=8)), reads=[t_tok], writes=[t_Z])
            for ch in range(2):
                tp = slice(ch * 64, ch * 64 + 64)
                for h in range(8):
                    S.pe(lambda e, tp=tp, h=h: e.matmul(B[h // 4][tp, (h % 4) * 128:(h % 4 + 1) * 128], lhsT=TT[tp, h, :], rhs=Z[tp, h, :],
                                                         start=True, stop=True), reads=[t_TT, t_Z], writes=[bt[h // 4]])
            for hf in range(2):
                S.act(lambda e, hf=hf: e.activation(out=AV[:, hf * 4:(hf + 1) * 4, :].rearrange("p h t -> p (h t)"), in_=B[hf][:], func=AF.Copy),
                      reads=[bt[hf]], writes=[t_AV])
            for ch in range(2):
                tp = slice(ch * 64, ch * 64 + 64)
                for h in range(8):
                    p, hh = h // 2, h % 2
                    fp = slice(hh * 64, hh * 64 + 64)
                    col = (p * 2 + ch) * 64
                    S.pe(lambda e, tp=tp, fp=fp, h=h, col=col: e.matmul(B[2][fp, col:col + 64], lhsT=AV[tp, h, 0:64], rhs=Bhtok[tp, h * 64:(h + 1) * 64],
                                                                         start=True, stop=True), reads=[t_AV, t_tok], writes=[bt[2]])
                    S.pe(lambda e, tp=tp, fp=fp, h=h, col=col: e.matmul(B[3][fp, col:col + 64], lhsT=AV[tp, h, 0:64], rhs=NM[tp, h, 1, :],
                                                                         start=True, stop=True), reads=[t_AV, t_NM], writes=[bt[3]])
            S.dve(lambda e: e.tensor_tensor(out=dPC[:], in0=bc(PC[:].unsqueeze(3), [128, 4, 2, 64]),
                                            in1=bc(id2[:].unsqueeze(1).unsqueeze(1), [128, 4, 2, 64]), op=ALU.mult), reads=[t_P, t_rp], writes=[t_Mc])
            S.dve(lambda e: e.tensor_tensor(out=McT[:], in0=B[2][:].rearrange("p (q c t) -> p q c t", q=4, c=2), in1=dPC[:], op=ALU.add),
                  reads=[bt[2], t_Mc], writes=[t_Mc])
            S.dve(lambda e: e.tensor_tensor(out=RpT[:], in0=B[3][:].rearrange("p (q c t) -> p q c t", q=4, c=2), in1=AR[:, :, :, 1, :], op=ALU.add),
                  reads=[bt[3], t_AR], writes=[t_Rp])
            for ch in range(2):
                tp = slice(ch * 64, ch * 64 + 64)
                for h in range(8):
                    p, hh = h // 2, h % 2
                    fp = slice(hh * 64, hh * 64 + 64)
                    S.pe(lambda e, tp=tp, h=h: e.matmul(B[6][tp, h * 64:(h + 1) * 64], lhsT=NM[tp, h, 1, :], rhs=AV[tp, h, 64:128], start=True, stop=False),
                         reads=[t_NM, t_AV], writes=[bt[6]])
                    S.pe(lambda e, tp=tp, h=h: e.matmul(B[6][tp, h * 64:(h + 1) * 64], lhsT=AK[tp, h, 1, :], rhs=Vtok[tp, h * 64:(h + 1) * 64], start=False, stop=False),
                         reads=[t_AK, t_tok], writes=[bt[6]])
                    S.pe(lambda e, tp=tp, fp=fp, p=p, h=h, ch=ch: e.matmul(B[6][tp, h * 64:(h + 1) * 64], lhsT=RpT[fp, p, ch, :], rhs=Hs[fp, p, :], start=False, stop=True),
                         reads=[t_Rp, t_H], writes=[bt[6]])
                for h in range(8):
                    p, hh = h // 2, h % 2
                    fp = slice(hh * 64, hh * 64 + 64)
                    S.pe(lambda e, tp=tp, fp=fp, p=p, h=h: e.matmul(B[5][fp, p * 64:(p + 1) * 64], lhsT=Bhtok[tp, h * 64:(h + 1) * 64], rhs=AV[tp, h, 64:128], start=True, stop=False),
                         reads=[t_tok, t_AV], writes=[bt[5]])
                    S.pe(lambda e, tp=tp, fp=fp, p=p, h=h: e.matmul(B[5][fp, p * 64:(p + 1) * 64], lhsT=Khtok[tp, h * 64:(h + 1) * 64], rhs=Vtok[tp, h * 64:(h + 1) * 64], start=False, stop=False),
                         reads=[t_tok], writes=[bt[5]])
                    S.pe(lambda e, fp=fp, p=p, ch=ch: e.matmul(B[5][fp, p * 64:(p + 1) * 64], lhsT=McT[fp, p, ch, :], rhs=Hs[fp, p, :], start=False, stop=True),
                         reads=[t_Mc, t_H], writes=[bt[5]])
                S.act(lambda e: e.activation(out=Hs[:].rearrange("p q v -> p (q v)"), in_=B[5][:, 0:256], func=AF.Copy), reads=[bt[5]], writes=[t_H])
            yps = B[6][:].rearrange("p (h v) -> p h v", h=8)
            S.dve(lambda e: e.tensor_reduce(out=gst[:], in_=yps, axis=AX.X, op=ALU.add), reads=[bt[6]], writes=[t_y])
            S.dve(lambda e: e.tensor_scalar(out=gst[:], in0=gst[:], scalar1=1.0 / 64, scalar2=None, op0=ALU.mult), reads=[t_y], writes=[t_y])
            S.dve(lambda e: e.tensor_tensor(out=ycen[:], in0=yps, in1=bc(gst[:].unsqueeze(2), [128, 8, 64]), op=ALU.subtract), reads=[t_y, bt[6]], writes=[t_y])
            S.act(lambda e: e.activation(out=ysq[:], in_=ycen[:], func=AF.Square), reads=[t_y], writes=[t_y])
            S.dve(lambda e: e.tensor_reduce(out=gst2[:], in_=ysq[:], axis=AX.X, op=ALU.add), reads=[t_y], writes=[t_y])
            S.act(lambda e: e.activation(out=gst2[:], in_=gst2[:], func=AF.Sqrt, scale=1.0 / 64, bias=gneps[:]), reads=[t_y, t_rp], writes=[t_y])
            S.dve(lambda e: e.reciprocal(out=gst2[:], in_=gst2[:]), reads=[t_y], writes=[t_y])
            S.dve(lambda e: e.tensor_tensor(out=ycen[:], in0=ycen[:], in1=bc(gst2[:].unsqueeze(2), [128, 8, 64]), op=ALU.mult), reads=[t_y], writes=[t_y])
            yc2 = ycen[:].rearrange("p h v -> p (h v)")
            S.dve(lambda e: e.tensor_tensor(out=yc2, in0=yc2, in1=lnxg[:], op=ALU.mult), reads=[t_y, t_rp], writes=[t_y])
            S.dve(lambda e: e.tensor_tensor(out=yc2, in0=yc2, in1=lnxb[:], op=ALU.add), reads=[t_y, t_rp], writes=[t_y])
            S.pool(lambda e: e.tensor_tensor(out=ysq[:], in0=Vtok[:].rearrange("p (h v) -> p h v", h=8), in1=bc(bsum[:].unsqueeze(2), [128, 8, 64]), op=ALU.mult),
                   reads=[t_tok, t_bs, t_y], writes=[t_y])
            S.dve(lambda e: e.tensor_tensor(out=ycen[:], in0=ycen[:], in1=ysq[:], op=ALU.add), reads=[t_y], writes=[t_y])
            S.dve(lambda e: e.tensor_tensor(out=yc2, in0=yc2, in1=g_tok[:], op=ALU.mult), reads=[t_y, t_g], writes=[t_y])
            if debug:
                S.dma(lambda e, t0=t0: e.dma_start(out=O["dbg_yr"][t0:t0 + 128, :], in_=yc2), reads=[t_y])
            for p in range(4):
                S.pe(lambda e, p=p: e.transpose(B[7][:, p * 128:(p + 1) * 128], ycen[:, 2 * p:2 * p + 2, :].rearrange("p h v -> p (h v)"), ident[:]),
                     reads=[t_y, t_const], writes=[bt[7]])
            S.act(lambda e: e.activation(out=mixr[:].rearrange("p q t -> p (q t)"), in_=B[7][:], func=AF.Copy), reads=[bt[7]], writes=[t_mixr])
            S.dma(lambda e, t0=t0: e.dma_start(out=mixT_d[4:8, :, t0:t0 + 128].rearrange("q p t -> p q t"), in_=mixr[:]), reads=[t_mixr])
            ck(4.0 + 0.01 * (sbi + 1))
        S.dma(lambda e: e.dma_start(out=O["wkvp"], in_=Hs[:]), reads=[t_H])

        S.barrier()
        st4.close()
        ck(4.95)
        NSS = 64
        hs_perm = lambda c: hT[:, c, T:T + NS].rearrange("p (b t) -> p t b", t=4)
        for x in range(3):
            S.dma(lambda e, x=x: e.dma_start(out=pb[x][:, :, 0:16], in_=I["shT"][:, x, :, :]), writes=[t_pb])
        S.dma(lambda e: e.dma_start(out=pbl[:, 0:16], in_=I["shTl"]), writes=[t_pb])
        for x in range(3):
            bk = x
            for p in range(4):
                for c in range(8):
                    S.pe(lambda e, x=x, p=p, c=c, bk=bk: e.matmul(
                        B[bk][:, p * 128:p * 128 + NSS], lhsT=wr[:, c, x * 512 + p * 128:x * 512 + (p + 1) * 128],
                        rhs=hs_perm(c), start=(c == 0), stop=(c == 7)), reads=[t_wr, t_h[8]], writes=[bt[bk]])
            S.act(lambda e, x=x, bk=bk: e.activation(out=pb[x][:, :, 16:80], in_=B[bk][:].rearrange("p (q t) -> p q t", q=4)[:, :, 0:NSS], func=AF.Copy),
                  reads=[bt[bk], t_xm], writes=[t_pb])
        for c in range(8):
            S.pe(lambda e, c=c: e.matmul(B[3][:, 0:NSS], lhsT=wr[:, c, 1536:1664], rhs=hs_perm(c), start=(c == 0), stop=(c == 7)),
                 reads=[t_wr, t_h[8]], writes=[bt[3]])
        S.act(lambda e: e.activation(out=pbl[:, 16:80], in_=B[3][:, 0:NSS], func=AF.Copy), reads=[bt[3], t_xm], writes=[t_pb])
        shrow = sbt(st2, "shrow", [16, 1664])
        t_shrow = T_()
        hl = sbt(st2, "hl", [128, 8, 16], BF16)
        t_hl = T_()
        S.pool(lambda e: e.tensor_copy(out=hl[:], in_=hT[:, :, T + 3:T + NS:4]), reads=[t_h[8]], writes=[t_hl])
        for q4 in range(4):
            for c in range(8):
                S.pe(lambda e, q4=q4, c=c: e.matmul(B[4][0:16, 0:416], lhsT=hl[:, c, :], rhs=wr[:, c, q4 * 416:(q4 + 1) * 416],
                                                    start=(c == 0), stop=(c == 7)), reads=[t_wr, t_hl], writes=[bt[4]])
            S.act(lambda e, q4=q4: e.activation(out=shrow[:, q4 * 416:(q4 + 1) * 416], in_=B[4][0:16, 0:416], func=AF.Copy), reads=[bt[4]], writes=[t_shrow])
        S.dma(lambda e: e.dma_start(out=O["shs"], in_=shrow[:]), reads=[t_shrow])
        n_ = NSS
        for x in range(3):
            S.dve(lambda e, x=x: e.tensor_tensor(out=dtmp[:, :, 0:n_], in0=pb[x][:, :, 0:n_], in1=pb[x][:, :, 16:16 + n_], op=ALU.subtract),
                  reads=[t_pb], writes=[t_xm, t_rk])
            S.dve(lambda e, x=x: e.tensor_tensor(out=dtmp[:, :, 0:n_], in0=dtmp[:, :, 0:n_], in1=bc(rwp[:, x, :].unsqueeze(2), [128, 4, n_]), op=ALU.mult),
                  reads=[t_xm, t_rp, t_rk], writes=[t_xm, t_rk])
            S.dve(lambda e, x=x: e.tensor_tensor(out=xm[x][:, :, 0:n_], in0=dtmp[:, :, 0:n_], in1=pb[x][:, :, 16:16 + n_], op=ALU.add),
                  reads=[t_xm, t_pb, t_rk], writes=[t_xm])
        S.dve(lambda e: e.tensor_tensor(out=xml[:, 0:n_], in0=pbl[:, 0:n_], in1=pbl[:, 16:16 + n_], op=ALU.subtract), reads=[t_pb], writes=[t_lora])
        S.dve(lambda e: e.scalar_tensor_tensor(out=xml[:, 0:n_], in0=xml[:, 0:n_], scalar=mul_[:, 0:1], in1=pbl[:, 16:16 + n_], op0=ALU.mult, op1=ALU.add),
              reads=[t_lora, t_pb, t_rp], writes=[t_lora])
        S.act(lambda e: e.activation(out=twl[0:32, 0:n_], in_=xml[0:32, 0:n_], func=AF.Tanh), reads=[t_lora], writes=[t_sg])
        S.act(lambda e: e.activation(out=twl[64:128, 0:n_], in_=xml[64:128, 0:n_], func=AF.Sigmoid), reads=[t_lora], writes=[t_sg])
        S.pe(lambda e: e.matmul(B[4][0:n_, :], lhsT=twl[0:32, 0:n_], rhs=lw3[0:32, :], start=True, stop=False), reads=[t_sg, t_rp], writes=[bt[4]])
        S.pe(lambda e: e.matmul(B[4][0:n_, :], lhsT=ones[0:1, 0:n_], rhs=w0row[0:1, :], start=False, stop=True), reads=[t_const, t_rp], writes=[bt[4]])
        S.act(lambda e: e.activation(out=sg_tok[0:n_, :], in_=B[4][0:n_, :], func=AF.Sigmoid), reads=[bt[4]], writes=[t_sg])
        for p in range(4):
            S.pe(lambda e, p=p: e.matmul(B[5][:, p * 128:p * 128 + n_], lhsT=lw3[32:64, p * 128:(p + 1) * 128], rhs=xml[32:64, 0:n_],
                                          start=True, stop=True), reads=[t_lora, t_rp], writes=[bt[5]])
        S.dve(lambda e: e.tensor_tensor(out=aT[:, :, 0:n_], in0=B[5][:].rearrange("p (q t) -> p q t", q=4)[:, :, 0:n_],
                                        in1=bc(rwp[:, 6, :].unsqueeze(2), [128, 4, n_]), op=ALU.add), reads=[bt[5], t_rp], writes=[t_a])
        S.act(lambda e: e.activation(out=aT[:, :, 0:n_], in_=aT[:, :, 0:n_], func=AF.Sigmoid), reads=[t_a], writes=[t_a])
        S.pe(lambda e: e.matmul(B[6][0:n_, :], lhsT=twl[64:128, 0:n_], rhs=lw3[64:128, :], start=True, stop=True), reads=[t_sg, t_rp], writes=[bt[6]])
        S.act(lambda e: e.activation(out=g_tok[0:n_, :], in_=B[6][0:n_, :], func=AF.Copy), reads=[bt[6], t_y], writes=[t_g])
        S.dve(lambda e: e.tensor_tensor(out=kk[:, :, 0:n_], in0=xm[1][:, :, 0:n_], in1=bc(rwp[:, 3, :].unsqueeze(2), [128, 4, n_]), op=ALU.mult),
              reads=[t_xm, t_rp], writes=[t_kk])
        S.act(lambda e: e.activation(out=sq4[:, :, 0:n_], in_=kk[:, :, 0:n_], func=AF.Square), reads=[t_kk], writes=[t_kk])
        for p in range(4):
            S.pe(lambda e, p=p: e.matmul(B[7][:, p * 128:p * 128 + n_], lhsT=blk2[:], rhs=sq4[:, p, 0:n_], start=True, stop=True),
                 reads=[t_kk, t_const], writes=[bt[7]])
        S.act(lambda e: e.activation(out=sq4[:, :, 0:n_], in_=B[7][:].rearrange("p (q t) -> p q t", q=4)[:, :, 0:n_], func=AF.Sqrt), reads=[bt[7]], writes=[t_kk])
        S.dve(lambda e: e.tensor_scalar(out=sq4[:, :, 0:n_], in0=sq4[:, :, 0:n_], scalar1=1e-12, scalar2=None, op0=ALU.max), reads=[t_kk], writes=[t_kk])
        S.dve(lambda e: e.reciprocal(out=sq4[:, :, 0:n_], in_=sq4[:, :, 0:n_]), reads=[t_kk], writes=[t_kk])
        S.dve(lambda e: e.tensor_tensor(out=kk[:, :, 0:n_], in0=kk[:, :, 0:n_], in1=sq4[:, :, 0:n_], op=ALU.mult), reads=[t_kk], writes=[t_kk])
        S.dve(lambda e: e.tensor_tensor(out=kmod[:, :, 0:n_], in0=aT[:, :, 0:n_], in1=bc(rwp[:, 4, :].unsqueeze(2), [128, 4, n_]), op=ALU.mult),
              reads=[t_a, t_rp], writes=[t_km])
        S.dve(lambda e: e.tensor_tensor(out=kmod[:, :, 0:n_], in0=kmod[:, :, 0:n_], in1=bc(omka[:].unsqueeze(2), [128, 4, n_]), op=ALU.add),
              reads=[t_km, t_rp], writes=[t_km])
        S.dve(lambda e: e.tensor_tensor(out=kmod[:, :, 0:n_], in0=kmod[:, :, 0:n_], in1=xm[1][:, :, 0:n_], op=ALU.mult), reads=[t_km, t_xm], writes=[t_km])
        S.dve(lambda e: e.tensor_tensor(out=bb[:, :, 0:n_], in0=kk[:, :, 0:n_], in1=aT[:, :, 0:n_], op=ALU.mult), reads=[t_kk, t_a], writes=[t_b])
        S.pool(lambda e: e.tensor_tensor(out=rk[:, :, 0:n_], in0=xm[0][:, :, 0:n_], in1=kmod[:, :, 0:n_], op=ALU.mult), reads=[t_xm, t_km], writes=[t_rk])
        S.pool(lambda e: e.tensor_tensor(out=rk[:, :, 0:n_], in0=rk[:, :, 0:n_], in1=bc(rwp[:, 5, :].unsqueeze(2), [128, 4, n_]), op=ALU.mult),
               reads=[t_rk, t_rp], writes=[t_rk])
        for h in range(8):
            p, hh = h // 2, h % 2
            fp = slice(hh * 64, hh * 64 + 64)
            S.pe(lambda e, p=p, fp=fp, h=h: e.matmul(B[6][0:n_, 256 + h:256 + h + 1], lhsT=rk[fp, p, 0:n_], rhs=ones[fp, 0:1], start=True, stop=True),
                 reads=[t_rk, t_const], writes=[bt[6]])
        S.act(lambda e: e.activation(out=bsum[0:n_, :], in_=B[6][0:n_, 256:264], func=AF.Copy), reads=[bt[6], t_y], writes=[t_bs])
        tok6 = sbt(st2, "tok6", [64, 6, 512])
        t_tok6 = T_()
        S.act(lambda e: e.activation(out=tok6[:, 1, :], in_=sg_tok[0:n_, :], func=AF.Exp, scale=NEG), reads=[t_sg], writes=[t_tok6])
        for xi, (srcT, rd) in enumerate(((xm[0], t_xm), (None, None), (kmod, t_km), (xm[2], t_xm), (kk, t_kk), (bb, t_b))):
            if srcT is None:
                continue
            bk = 2 + xi % 2
            for p in range(4):
                S.pe(lambda e, p=p, srcT=srcT, bk=bk: e.transpose(B[bk][0:n_, p * 128:(p + 1) * 128], srcT[:, p, 0:n_], ident[:]),
                     reads=[rd, t_const], writes=[bt[bk]])
            S.act(lambda e, xi=xi, bk=bk: e.activation(out=tok6[:, xi, :], in_=B[bk][0:n_, :], func=AF.Copy), reads=[bt[bk]], writes=[t_tok6])
        t_rwsd = T_()
        t_rwsd_l = [T_() for i in range(24)]
        for t in range(4):
            for xi in range(6):
                S.dma(lambda e, t=t, xi=xi: e.dma_start(out=rws_d[:, :, t, xi, :], in_=tok6[16 * t:16 * t + 16, xi, :].rearrange("p (h c) -> p h c", h=8)),
                      reads=[t_tok6], writes=[t_rwsd_l[t * 6 + xi]])
        X6 = sbt(st2, "X6", [128, 4, 6, 64])
        St = sbt(st2, "St", [128, 64, 64])
        tmpS = sbt(st2, "tmpS", [128, 64, 64])
        sp_ = sbt(st2, "sp", [128, 64])
        ys = sbt(st2, "ys", [128, 4, 64])
        t_X6, t_St, t_tmpS, t_sp, t_ys = [T_() for i in range(5)]
        S.dma(lambda e: e.dma_start(out=X6[:].rearrange("p t x c -> p (t x c)"), in_=rws_d.rearrange("b h t x c -> (b h) (t x c)")), reads=t_rwsd_l, writes=[t_X6])
        S.dma(lambda e: e.dma_start(out=St[:].rearrange("p v k -> p (v k)"), in_=I["swkv"]), writes=[t_St])
        for t in range(4):
            def bk_(xi, t=t):
                return bc(X6[:, t, xi, :].unsqueeze(1), [128, 64, 64])
            def bv_(ap):
                return bc(ap.unsqueeze(2), [128, 64, 64])
            S.dve(lambda e, t=t: e.tensor_tensor(out=tmpS[:], in0=St[:], in1=bk_(4, t), op=ALU.mult), reads=[t_St, t_X6], writes=[t_tmpS])
            S.dve(lambda e: e.tensor_reduce(out=sp_[:], in_=tmpS[:], axis=AX.X, op=ALU.add), reads=[t_tmpS], writes=[t_sp])
            S.dve(lambda e, t=t: e.tensor_tensor(out=St[:], in0=St[:], in1=bk_(1, t), op=ALU.mult), reads=[t_St, t_X6, t_tmpS], writes=[t_St])
            S.pool(lambda e, t=t: e.tensor_tensor(out=tmpS[:], in0=bv_(sp_[:]), in1=bk_(5, t), op=ALU.mult), reads=[t_sp, t_X6], writes=[t_tmpS])
            S.dve(lambda e: e.tensor_tensor(out=St[:], in0=St[:], in1=tmpS[:], op=ALU.subtract), reads=[t_St, t_tmpS], writes=[t_St])
            S.pool(lambda e, t=t: e.tensor_tensor(out=tmpS[:], in0=bv_(X6[:, t, 3, :]), in1=bk_(2, t), op=ALU.mult), reads=[t_X6, t_St], writes=[t_tmpS])
            S.dve(lambda e: e.tensor_tensor(out=St[:], in0=St[:], in1=tmpS[:], op=ALU.add), reads=[t_St, t_tmpS], writes=[t_St])
            S.dve(lambda e, t=t: e.tensor_tensor(out=tmpS[:], in0=St[:], in1=bk_(0, t), op=ALU.mult), reads=[t_St, t_X6], writes=[t_tmpS])
            S.dve(lambda e, t=t: e.tensor_reduce(out=ys[:, t, :], in_=tmpS[:], axis=AX.X, op=ALU.add), reads=[t_tmpS], writes=[t_ys])
        S.dma(lambda e: e.dma_start(out=O["wkvs"], in_=St[:].rearrange("p v k -> p (v k)")), reads=[t_St])
        t_ysd = T_()
        S.dma(lambda e: e.dma_start(out=ys_d.rearrange("b h t v -> (b h) (t v)"), in_=ys[:].rearrange("p t v -> p (t v)")), reads=[t_ys], writes=[t_ysd])
        ysr = sbt(st2, "ysr", [64, 8, 64])
        t_ysr = T_()
        for t in range(4):
            S.dma(lambda e, t=t: e.dma_start(out=ysr[16 * t:16 * t + 16, :, :], in_=ys_d[:, :, t, :]), reads=[t_ysd], writes=[t_ysr])
        yps_s = ysr[:]
        Y0 = slice(0, 64)
        S.dve(lambda e: e.tensor_reduce(out=gst[Y0], in_=yps_s, axis=AX.X, op=ALU.add), reads=[t_ysr], writes=[t_y])
        S.dve(lambda e: e.tensor_scalar(out=gst[Y0], in0=gst[Y0], scalar1=1.0 / 64, scalar2=None, op0=ALU.mult), reads=[t_y], writes=[t_y])
        S.dve(lambda e: e.tensor_tensor(out=ycen[Y0], in0=yps_s, in1=bc(gst[Y0].unsqueeze(2), [64, 8, 64]), op=ALU.subtract), reads=[t_y, t_ysr], writes=[t_y])
        S.act(lambda e: e.activation(out=ysq[Y0], in_=ycen[Y0], func=AF.Square), reads=[t_y], writes=[t_y])
        S.dve(lambda e: e.tensor_reduce(out=gst2[Y0], in_=ysq[Y0], axis=AX.X, op=ALU.add), reads=[t_y], writes=[t_y])
        S.act(lambda e: e.activation(out=gst2[Y0], in_=gst2[Y0], func=AF.Sqrt, scale=1.0 / 64, bias=gneps[Y0]), reads=[t_y, t_rp], writes=[t_y])
        S.dve(lambda e: e.reciprocal(out=gst2[Y0], in_=gst2[Y0]), reads=[t_y], writes=[t_y])
        S.dve(lambda e: e.tensor_tensor(out=ycen[Y0], in0=ycen[Y0], in1=bc(gst2[Y0].unsqueeze(2), [64, 8, 64]), op=ALU.mult), reads=[t_y], writes=[t_y])
        yc2_s = ycen[Y0].rearrange("p h v -> p (h v)")
        S.dve(lambda e: e.tensor_tensor(out=yc2_s, in0=yc2_s, in1=lnxg[Y0], op=ALU.mult), reads=[t_y, t_rp], writes=[t_y])
        S.dve(lambda e: e.tensor_tensor(out=yc2_s, in0=yc2_s, in1=lnxb[Y0], op=ALU.add), reads=[t_y, t_rp], writes=[t_y])
        S.pool(lambda e: e.tensor_tensor(out=ysq[Y0], in0=tok6[:, 3, :].rearrange("p (h v) -> p h v", h=8), in1=bc(bsum[Y0].unsqueeze(2), [64, 8, 64]), op=ALU.mult),
               reads=[t_tok6, t_bs, t_y], writes=[t_y])
        S.dve(lambda e: e.tensor_tensor(out=ycen[Y0], in0=ycen[Y0], in1=ysq[Y0], op=ALU.add), reads=[t_y], writes=[t_y])
        S.dve(lambda e: e.tensor_tensor(out=yc2_s, in0=yc2_s, in1=g_tok[Y0], op=ALU.mult), reads=[t_y, t_g], writes=[t_y])
        for p in range(4):
            S.pe(lambda e, p=p: e.transpose(B[7][:, p * 128:p * 128 + 64], ycen[Y0, 2 * p:2 * p + 2, :].rearrange("p h v -> p (h v)"), ident[0:64, 0:64]),
                 reads=[t_y, t_const], writes=[bt[7]])
        S.act(lambda e: e.activation(out=mixr[:, :, 0:64].rearrange("p q (b t) -> p q t b", t=4),
                                     in_=B[7][:].rearrange("p (q x) -> p q x", q=4)[:, :, 0:64].rearrange("p q (t b) -> p q t b", t=4), func=AF.Copy),
              reads=[bt[7]], writes=[t_mixr])
        S.dma(lambda e: e.dma_start(out=mixT_d[4:8, :, T:T + NS].rearrange("q p t -> p q t"), in_=mixr[:, :, 0:64]), reads=[t_mixr])


    S.barrier()
    with contextlib.ExitStack() as st2, contextlib.suppress(_Stop):
        ck(5.0)
        wq3 = sbt(st2, "wq3", [128, 8, 1536], BF16)
        t_wq3 = Tok()
        with contextlib.ExitStack() as st3:
            wst3 = [sbt(st3, "wst3", [128, 8, 512]) for i in range(2)]
            wst3t = [Tok(), Tok()]
            for q3 in range(3):
                w_ = wst3[q3 % 2]
                S.dma(lambda e, w_=w_, q3=q3: e.dma_start(out=w_[:], in_=win_v[:, :, q3 * 512:(q3 + 1) * 512]), writes=[wst3t[q3 % 2]])
                S.pool(lambda e, w_=w_, q3=q3: e.tensor_copy(out=wq3[:, :, q3 * 512:(q3 + 1) * 512], in_=w_[:]), reads=[wst3t[q3 % 2]], writes=[t_wq3])
        S.barrier()
        B = banks
        NQ = 64
        hsp = sbt(st2, "hsp", [128, 8, 64], BF16)
        t_hsp = Tok()
        S.pool(lambda e: e.tensor_copy(out=hsp[:].rearrange("p c (s b) -> p c s b", s=4), in_=hT[:, :, T:T + NS].rearrange("p c (b s) -> p c s b", s=4)),
               reads=[t_h[8]], writes=[t_hsp])
        hs_sb = lambda c: hsp[:, c, :]
        qkvs = sbt(st2, "qkvs", [64, 3, 8, 64])
        sqs = sbt(st2, "sqs", [64, 8, 64])
        ssn = sbt(st2, "ssn", [64, 8])
        gqk = sbt(st2, "gqk", [64, 2, 64])
        t_qkv, t_sqs, t_gqk = Tok(), Tok(), Tok()
        for wi in range(2):
            S.dma(lambda e, wi=wi: e.dma_start(out=gqk[:, wi, :], in_=bass.AP(I["qkrow"].tensor, wi * 64, [[0, 64], [1, 64]])), writes=[t_gqk])
        for wi in range(3):
            for c in range(8):
                S.pe(lambda e, wi=wi, c=c: e.matmul(B[wi][0:NQ, :], lhsT=hs_sb(c), rhs=wq3[:, c, wi * 512:(wi + 1) * 512], start=(c == 0), stop=(c == 7)),
                     reads=[t_wq3, t_hsp], writes=[bt[wi]])
            pv = B[wi][0:NQ, :].rearrange("p (h c) -> p h c", h=8)
            if wi == 2:
                S.act(lambda e, pv=pv: e.activation(out=qkvs[:, 2, :, :], in_=pv, func=AF.Copy), reads=[bt[wi]], writes=[t_qkv])
            else:
                S.act(lambda e, pv=pv: e.activation(out=sqs[:], in_=pv, func=AF.Square), reads=[bt[wi]], writes=[t_sqs])
                S.dve(lambda e: e.tensor_reduce(out=ssn[:], in_=sqs[:], axis=AX.X, op=ALU.add), reads=[t_sqs], writes=[t_sqs])
                S.act(lambda e: e.activation(out=ssn[:], in_=ssn[:], func=AF.Sqrt, scale=1.0 / 64, bias=epsc[0:64]), reads=[t_sqs, t_const], writes=[t_sqs])
                S.dve(lambda e: e.reciprocal(out=ssn[:], in_=ssn[:]), reads=[t_sqs], writes=[t_sqs])
                S.dve(lambda e, pv=pv, wi=wi: e.tensor_tensor(out=qkvs[:, wi, :, :], in0=pv, in1=bc(ssn[:].unsqueeze(2), [64, 8, 64]), op=ALU.mult),
                      reads=[bt[wi], t_sqs], writes=[t_qkv])
                S.dve(lambda e, wi=wi: e.tensor_tensor(out=qkvs[:, wi, :, :], in0=qkvs[:, wi, :, :], in1=bc(gqk[:, wi, :].unsqueeze(1), [64, 8, 64]), op=ALU.mult),
                      reads=[t_qkv, t_gqk], writes=[t_qkv])
        ck(5.1)
        t_rec = Tok()
        for wi, oname, recd, cname in ((1, "kws", reck_d, "ck_s"), (2, "vws", recv_d, "cv_s")):
            for s_ in range(4):
                S.dma(lambda e, wi=wi, oname=oname, s_=s_: e.dma_start(out=O[oname].rearrange("(b s) c -> s b c", s=4)[s_],
                                                                        in_=qkvs[16 * s_:16 * s_ + 16, wi, :, :].rearrange("p h c -> p (h c)")), reads=[t_qkv])
                S.dma(lambda e, wi=wi, recd=recd, s_=s_: e.dma_start(out=recd[:, 4 + s_, :], in_=qkvs[16 * s_:16 * s_ + 16, wi, :, :].rearrange("p h c -> p (h c)")),
                      reads=[t_qkv], writes=[t_rec])
            S.dma(lambda e, recd=recd, cname=cname: e.dma_start(out=recd[:, 0:4, :], in_=I[cname][:, 2044:2048, :]), writes=[t_rec])
        ck(5.2)
        btab = sbt(st2, "btab", [64, 3, 8, 129])
        t_btab = Tok()
        with contextlib.ExitStack() as st3:
            relr = sbt(st3, "relr", [32, 8])
            ohs = sbt(st3, "ohs", [32, 3, 129])
            RHs = sbt(st3, "RHs", [32, 8, 129])
            t_r2, t_rh2 = Tok(), Tok()
            S.dma(lambda e: e.dma_start(out=relr[:], in_=I["relb"]), writes=[t_r2])
            S.dma(lambda e: e.dma_start(out=ohs[:], in_=I["ohs"]), writes=[t_r2])
            for br in range(3):
                S.dve(lambda e, br=br: e.tensor_tensor(out=RHs[:], in0=bc(relr[:].unsqueeze(2), [32, 8, 129]), in1=bc(ohs[:, br, :].unsqueeze(1), [32, 8, 129]), op=ALU.mult),
                      reads=[t_r2], writes=[t_rh2])
                for h in range(8):
                    bk = 3 + h % 2
                    S.pe(lambda e, h=h, bk=bk: e.matmul(B[bk][0:64, 0:129], lhsT=ones[0:32, 0:64], rhs=RHs[:, h, :], start=True, stop=True),
                         reads=[t_rh2, t_const], writes=[bt[bk]])
                    S.act(lambda e, h=h, bk=bk, br=br: e.activation(out=btab[:, br, h, :], in_=B[bk][0:64, 0:129], func=AF.Copy), reads=[bt[bk]], writes=[t_btab])
        S.barrier()
        ck(5.3)
        Kt = [sbt(st2, "Kt", [64, 129, 64])] * 2
        Vt = [sbt(st2, "Vt", [64, 129, 64])] * 2
        t_Kt = [[Tok() for i in range(24)]] * 2
        t_Vt = [[Tok() for i in range(24)]] * 2
        lg = sbt(st2, "lg", [64, 129])
        zz = sbt(st2, "zz", [64, 1])
        ov = sbt(st2, "ov", [64, 64])
        Oacc = sbt(st2, "Oacc", [64, 8, 64])
        Zacc = sbt(st2, "Zacc", [64, 8])
        t_lg, t_zz, t_ov, t_acc2 = Tok(), Tok(), Tok(), Tok()
        S.pool(lambda e: e.memset(Oacc[:], 0.0), writes=[t_acc2])
        S.pool(lambda e: e.memset(Zacc[:], 0.0), writes=[t_acc2])
        it = 0
        for br, dil in enumerate((1, 4, 16)):
            ncache = 125 if dil == 1 else 128
            for h in range(8):
                bi = it % 2
                it += 1
                for tl, tk, cname, recd in ((Kt[bi], t_Kt[bi], "ck_s", reck_d), (Vt[bi], t_Vt[bi], "cv_s", recv_d)):
                    for s_ in range(4):
                        ps_ = slice(16 * s_, 16 * s_ + 16)
                        for j0 in range(0, ncache, 32):
                            j1 = min(ncache, j0 + 32)
                            src = bass.AP(I[cname].tensor, (2048 + s_ - 128 * dil + j0 * dil) * 512 + h * 64, [[2048 * 512, 16], [dil * 512, j1 - j0], [1, 64]])
                            S.dma(lambda e, tl=tl, ps_=ps_, src=src, j0=j0, j1=j1: e.dma_start(out=tl[ps_, j0:j1, :], in_=src), writes=[tk[8 + 4 * s_ + j0 // 32]])
                        r0 = (1 + s_) if dil == 1 else (4 + s_)
                        src2 = bass.AP(recd.tensor, r0 * 512 + h * 64, [[8 * 512, 16], [512, 129 - ncache], [1, 64]])
                        S.dma(lambda e, tl=tl, ps_=ps_, src2=src2, ncache=ncache: e.dma_start(out=tl[ps_, ncache:129, :], in_=src2), reads=[t_rec], writes=[tk[2 * s_ + 1]])
                K_, V_ = Kt[bi], Vt[bi]
                S.dve(lambda e, K_=K_, h=h: e.tensor_tensor(out=K_[:], in0=K_[:], in1=bc(qkvs[:, 0, h, :].unsqueeze(1), [64, 129, 64]), op=ALU.mult),
                      reads=t_Kt[bi] + [t_qkv], writes=t_Kt[bi])
                S.dve(lambda e, K_=K_: e.tensor_reduce(out=lg[:], in_=K_[:], axis=AX.X, op=ALU.add), reads=t_Kt[bi], writes=[t_lg])
                S.dve(lambda e, br=br, h=h: e.scalar_tensor_tensor(out=lg[:], in0=lg[:], scalar=0.125, in1=btab[:, br, h, :], op0=ALU.mult, op1=ALU.add),
                      reads=[t_lg, t_btab], writes=[t_lg])
                S.act(lambda e: e.activation(out=lg[:], in_=lg[:], func=AF.Exp), reads=[t_lg], writes=[t_lg])
                S.dve(lambda e: e.tensor_reduce(out=zz[:], in_=lg[:], axis=AX.X, op=ALU.add), reads=[t_lg], writes=[t_zz])
                S.dve(lambda e, h=h: e.tensor_tensor(out=Zacc[:, h:h + 1], in0=Zacc[:, h:h + 1], in1=zz[:], op=ALU.add), reads=[t_zz, t_acc2], writes=[t_acc2])
                S.pool(lambda e, V_=V_: e.tensor_tensor(out=V_[:], in0=V_[:], in1=bc(lg[:].unsqueeze(2), [64, 129, 64]), op=ALU.mult),
                       reads=t_Vt[bi] + [t_lg], writes=t_Vt[bi])
                S.dve(lambda e, V_=V_: e.tensor_reduce(out=ov[:], in_=V_[:].rearrange("p j c -> p c j"), axis=AX.X, op=ALU.add), reads=t_Vt[bi], writes=[t_ov])
                S.dve(lambda e, h=h: e.tensor_tensor(out=Oacc[:, h, :], in0=Oacc[:, h, :], in1=ov[:], op=ALU.add), reads=[t_ov, t_acc2], writes=[t_acc2])
                ck(5.4 + 0.001 * it)
        S.dve(lambda e: e.reciprocal(out=Zacc[:], in_=Zacc[:]), reads=[t_acc2], writes=[t_acc2])
        S.dve(lambda e: e.tensor_tensor(out=Oacc[:], in0=Oacc[:], in1=bc(Zacc[:].unsqueeze(2), [64, 8, 64]), op=ALU.mult), reads=[t_acc2], writes=[t_acc2])
        mixs = sbt(st2, "mixs", [128, 4, 64], BF16)
        t_mixs = Tok()
        for p in range(4):
            S.pe(lambda e, p=p: e.transpose(B[5][:, p * 128:p * 128 + 64], Oacc[:, 2 * p:2 * p + 2, :].rearrange("p h c -> p (h c)"), ident[0:64, 0:64]),
                 reads=[t_acc2, t_const], writes=[bt[5]])
        S.act(lambda e: e.activation(out=mixs[:].rearrange("p q (b s) -> p q s b", s=4),
                                     in_=B[5][:].rearrange("p (q x) -> p q x", q=4)[:, :, 0:64].rearrange("p q (s b) -> p q s b", s=4), func=AF.Copy),
              reads=[bt[5]], writes=[t_mixs])
        S.dma(lambda e: e.dma_start(out=mixT_d[0:4, :, T:T + NS].rearrange("q p t -> p q t"), in_=mixs[:]), reads=[t_mixs])
    S.barrier()
    stH.close()
    if phase_limit < 6:
        S.emit()
        for sk_ in (stC_holder + [stH, st]):
            sk_.close()
        return nc
    S.nosync = True
    with contextlib.ExitStack() as st2, contextlib.suppress(_Stop):
        t_pc = Tok("peerconst")
        wout = sbt(st2, "wout", [128, 8, 1024], BF16)
        skT = sbt(st2, "skT", [128, 16, 128], BF16)
        iotaR = sbt(st2, "iotaR", [128, 128])
        with contextlib.ExitStack() as st3:
            wo_st = sbt(st3, "wo_st", [128, 8, 1024])
            sk_st = sbt(st3, "sk_st", [128, 16, 128])
            S.dma(lambda e: e.dma_start(out=wo_st[:], in_=I["w_out"].rearrange("(c p) n -> p c n", p=128)), writes=[t_pc])
            S.dma(lambda e: e.dma_start(out=sk_st[:], in_=I["skT"]), writes=[t_pc])
            S.dma(lambda e: e.dma_start(out=iotaR[:], in_=I["iotaR"]), writes=[t_pc])
            S.dve(lambda e: e.tensor_copy(out=wout[:], in_=wo_st[:]), reads=[t_pc], writes=[t_pc])
            S.dve(lambda e: e.tensor_copy(out=skT[:], in_=sk_st[:]), reads=[t_pc], writes=[t_pc])
        S.barrier()
        GS = 256
        x1g = sbt(st2, "x1g", [128, 8, GS])
        tmpg = sbt(st2, "tmpg", [128, 8, GS])
        h2g = sbt(st2, "h2g", [128, 8, GS], BF16)
        mixg = sbt(st2, "mixg", [128, 8, GS], BF16)
        rsg = sbt(st2, "rsg", [128, GS])
        qpT = sbt(st2, "qpT", [128, 16, GS], BF16)
        wpqb = sbt(st2, "wpqb", [128, 8, 512], BF16)
        s_sb = sbt(st2, "s_sb", [128, 16, 128])
        s2 = sbt(st2, "s2", [128, 256])
        vals = sbt(st2, "vals", [128, 16, 16])
        idxu = sbt(st2, "idxu", [128, 16, 16], U32)
        idxf = sbt(st2, "idxf", [128, 16, 16])
        cand = sbt(st2, "cand", [128, 8, 256])
        ts_ = sbt(st2, "ts", [128, 8, 16])
        posu = sbt(st2, "posu", [128, 8, 16], U32)
        au = sbt(st2, "au", [128, 8, 16], U32)
        bu = sbt(st2, "bu", [128, 8, 16], U32)
        a_f = sbt(st2, "a_f", [128, 8, 16])
        b_f = sbt(st2, "b_f", [128, 8, 16])
        eq = sbt(st2, "eq", [128, 8, 16, 16])
        I1 = sbt(st2, "I1", [128, 8, 16])
        I2 = sbt(st2, "I2", [128, 8, 16])
        gt_ = sbt(st2, "gt", [128, 8, 16])
        zs = sbt(st2, "zs", [128, 8])
        I1T = sbt(st2, "I1T", [128, GS], BF16)
        I2T = sbt(st2, "I2T", [128, GS], BF16)
        gT = sbt(st2, "gT", [128, GS], BF16)
        iotaRb = sbt(st2, "iotaRb", [128, 128], BF16)
        S.dve(lambda e: e.tensor_copy(out=iotaRb[:], in_=iotaR[:]), reads=[t_pc], writes=[t_pc])
        A4 = [sbt(st2, "A4", [128, 4, 128], BF16) for i in range(2)]
        B4 = [sbt(st2, "B4", [128, 4, 128], BF16) for i in range(2)]
        WT = sbt(st2, "WT", [128, GS, 128], BF16)
        eub = [sbt(st2, "eub", [128, 8, 256], BF16) for i in range(2)]
        evb = [sbt(st2, "evb", [128, 2, 1024], BF16) for i in range(2)]
        gU = [sbt(st2, "gU", [128, GS], BF16) for i in range(2)]
        Wg = [sbt(st2, "Wg", [128, GS], BF16) for i in range(2)]
        ytk = [sbt(st2, "ytk", [128, 1024]) for i in range(2)]
        (t_x1, t_tmp, t_h2, t_mixg, t_rsg, t_qp, t_wpq, t_s, t_s2, t_vals, t_cand, t_ts, t_ab, t_eq, t_I, t_g, t_IT, t_WT) = [Tok() for i in range(18)]
        t_A4 = [Tok(), Tok()]
        t_B4 = [Tok(), Tok()]
        t_eub = [Tok(), Tok()]
        t_evb = [Tok(), Tok()]
        t_gU = [Tok(), Tok()]
        t_Wg = [Tok(), Tok()]
        t_ytk = [Tok(), Tok()]
        B = banks
        evv = ev_d.rearrange("(k p) d -> p k d", p=128)
        wpqv = wpq_d.rearrange("(c p) n -> p c n", p=128)
        iota16 = iotaR[:, 0:16]
        pgroups = [(g * GS, GS) for g in range(T // GS)] + [(T, NS)]
        nyt = 0
        for gi, (t0, n) in enumerate(pgroups):
            samp = (gi == len(pgroups) - 1)
            S.dma(lambda e, t0=t0, n=n: e.dma_start(out=mixg[:, :, 0:n], in_=mixT_d[:, :, t0:t0 + n].rearrange("j p t -> p j t")), writes=[t_mixg])
            S.dma(lambda e, t0=t0, n=n: e.dma_start(out=x1g[:, :, 0:n], in_=xT_v[:, :, t0:t0 + n]), writes=[t_x1])
            for dc in range(8):
                bk = dc % 2
                for j in range(8):
                    S.pe(lambda e, dc=dc, j=j, bk=bk, n=n: e.matmul(B[bk][:, 0:n], lhsT=wout[:, j, dc * 128:(dc + 1) * 128], rhs=mixg[:, j, 0:n],
                                                                     start=(j == 0), stop=(j == 7)), reads=[t_pc, t_mixg], writes=[bt[bk]])
                if not samp:
                    S.dve(lambda e, dc=dc, bk=bk, n=n: e.scalar_tensor_tensor(out=x1g[:, dc, 0:n], in0=B[bk][:, 0:n], scalar=modT[:, 16 + dc, 0:1],
                                                                               in1=x1g[:, dc, 0:n], op0=ALU.mult, op1=ALU.add),
                          reads=[bt[bk], t_mod, t_x1], writes=[t_x1])
                else:
                    S.dve(lambda e, dc=dc, bk=bk: e.tensor_tensor(out=tmpg[:, dc, 0:NS].rearrange("p (b t) -> p b t", t=4),
                                                                  in0=B[bk][:, 0:NS].rearrange("p (b t) -> p b t", t=4),
                                                                  in1=bc(modT[:, 16 + dc, 1:17].unsqueeze(2), [128, SB, 4]), op=ALU.mult),
                          reads=[bt[bk], t_mod], writes=[t_tmp])
                    S.dve(lambda e, dc=dc: e.tensor_tensor(out=x1g[:, dc, 0:NS], in0=x1g[:, dc, 0:NS], in1=tmpg[:, dc, 0:NS], op=ALU.add),
                          reads=[t_tmp, t_x1], writes=[t_x1])
            S.act(lambda e, n=n: e.activation(out=tmpg[:, :, 0:n], in_=x1g[:, :, 0:n], func=AF.Square), reads=[t_x1], writes=[t_tmp])
            for c in range(8):
                S.pe(lambda e, c=c, n=n: e.matmul(B[2][:, 0:n], lhsT=ones[:], rhs=tmpg[:, c, 0:n], start=(c == 0), stop=(c == 7)),
                     reads=[t_tmp, t_const], writes=[bt[2]])
            S.act(lambda e, n=n: e.activation(out=rsg[:, 0:n], in_=B[2][:, 0:n], func=AF.Sqrt, scale=1.0 / D, bias=epsc[:]),
                  reads=[bt[2], t_const], writes=[t_rsg])
            S.dve(lambda e, n=n: e.reciprocal(out=rsg[:, 0:n], in_=rsg[:, 0:n]), reads=[t_rsg], writes=[t_rsg])
            S.dve(lambda e, n=n: e.tensor_tensor(out=tmpg[:, :, 0:n], in0=x1g[:, :, 0:n], in1=bc(rsg[:, 0:n].unsqueeze(1), [128, 8, n]), op=ALU.mult),
                  reads=[t_x1, t_rsg, t_tmp], writes=[t_tmp])
            if not samp:
                for c in range(8):
                    eng = S.dve if c % 2 == 0 else S.pool
                    eng(lambda e, c=c, n=n: e.tensor_scalar(out=h2g[:, c, 0:n], in0=tmpg[:, c, 0:n], scalar1=A2[:, c, 0:1], scalar2=modT[:, 24 + c, 0:1],
                                                            op0=ALU.mult, op1=ALU.add), reads=[t_tmp, t_mod], writes=[t_h2])
            else:
                tv = tmpg[:, :, 0:NS].rearrange("p c (b t) -> p c b t", t=4)
                S.dve(lambda e, tv=tv: e.tensor_tensor(out=tv, in0=tv, in1=bc(A2[:, :, 1:17].unsqueeze(3), [128, 8, SB, 4]), op=ALU.mult),
                      reads=[t_tmp, t_mod], writes=[t_tmp])
                S.dve(lambda e, tv=tv: e.tensor_tensor(out=h2g[:, :, 0:NS].rearrange("p c (b t) -> p c b t", t=4), in0=tv,
                                                       in1=bc(modT[:, 24:32, 1:17].unsqueeze(3), [128, 8, SB, 4]), op=ALU.add),
                      reads=[t_tmp, t_mod], writes=[t_h2])
            ck(6.1)
            for jb in range(4):
                S.dma(lambda e, jb=jb: e.dma_start(out=wpqb[:], in_=wpqv[:, :, jb * 512:(jb + 1) * 512]), writes=[t_wpq])
                for j in range(4):
                    for c in range(8):
                        S.pe(lambda e, j=j, c=c, n=n: e.matmul(B[j][:, 0:n], lhsT=wpqb[:, c, j * 128:(j + 1) * 128], rhs=h2g[:, c, 0:n],
                                                               start=(c == 0), stop=(c == 7)), reads=[t_wpq, t_h2], writes=[bt[j]])
                for j in range(4):
                    S.act(lambda e, j=j, jb=jb, n=n: e.activation(out=qpT[:, jb * 4 + j, 0:n], in_=B[j][:, 0:n], func=AF.Copy), reads=[bt[j]], writes=[t_qp])
            for tt0 in range(0, n, 128):
                m = min(128, n - tt0)
                for j in range(16):
                    bk = 4 + j // 4
                    S.pe(lambda e, j=j, bk=bk, tt0=tt0, m=m: e.matmul(B[bk][0:m, (j % 4) * 128:(j % 4 + 1) * 128], lhsT=qpT[:, j, tt0:tt0 + m], rhs=skT[:, j, :],
                                                                      start=True, stop=True), reads=[t_qp, t_pc], writes=[bt[bk]])
                for q in range(4):
                    S.act(lambda e, q=q, m=m: e.activation(out=s_sb[0:m, q * 4:(q + 1) * 4, :].rearrange("p a k -> p (a k)"), in_=B[4 + q][0:m, :], func=AF.Copy),
                          reads=[bt[4 + q]], writes=[t_s])
                for j in range(16):
                    S.dve(lambda e, j=j, m=m: e.max(out=vals[0:m, j, 0:8], in_=s_sb[0:m, j, :]), reads=[t_s], writes=[t_vals])
                    S.dve(lambda e, j=j, m=m: e.match_replace(out=s2[0:m, 0:128], in_to_replace=vals[0:m, j, 0:8], in_values=s_sb[0:m, j, :], imm_value=-1e30),
                          reads=[t_s, t_vals], writes=[t_s2])
                    S.dve(lambda e, j=j, m=m: e.max(out=vals[0:m, j, 8:16], in_=s2[0:m, 0:128]), reads=[t_s2], writes=[t_vals])
                    S.dve(lambda e, j=j, m=m: e.max_index(out=idxu[0:m, j, 0:8], in_max=vals[0:m, j, 0:8], in_values=s_sb[0:m, j, :]), reads=[t_s, t_vals], writes=[t_vals])
                    S.dve(lambda e, j=j, m=m: e.max_index(out=idxu[0:m, j, 8:16], in_max=vals[0:m, j, 8:16], in_values=s_sb[0:m, j, :]), reads=[t_s, t_vals], writes=[t_vals])
                S.dve(lambda e, m=m: e.tensor_copy(out=idxf[0:m], in_=idxu[0:m]), reads=[t_vals], writes=[t_vals])
                v2 = vals[0:m].rearrange("p (h two) k -> p h two k", two=2)
                i2v = idxf[0:m].rearrange("p (h two) k -> p h two k", two=2)
                S.dve(lambda e, m=m, v2=v2: e.tensor_tensor(out=cand[0:m].rearrange("p h (a b) -> p h a b", a=16),
                                                            in0=bc(v2[:, :, 0, :].unsqueeze(3), [m, 8, 16, 16]),
                                                            in1=bc(v2[:, :, 1, :].unsqueeze(2), [m, 8, 16, 16]), op=ALU.add), reads=[t_vals], writes=[t_cand])
                for h in range(8):
                    S.dve(lambda e, h=h, m=m: e.max(out=ts_[0:m, h, 0:8], in_=cand[0:m, h, :]), reads=[t_cand], writes=[t_ts])
                    S.dve(lambda e, h=h, m=m: e.match_replace(out=s2[0:m, :], in_to_replace=ts_[0:m, h, 0:8], in_values=cand[0:m, h, :], imm_value=-1e30),
                          reads=[t_cand, t_ts], writes=[t_s2])
                    S.dve(lambda e, h=h, m=m: e.max(out=ts_[0:m, h, 8:16], in_=s2[0:m, :]), reads=[t_s2], writes=[t_ts])
                    S.dve(lambda e, h=h, m=m: e.max_index(out=posu[0:m, h, 0:8], in_max=ts_[0:m, h, 0:8], in_values=cand[0:m, h, :]), reads=[t_cand, t_ts], writes=[t_ts])
                    S.dve(lambda e, h=h, m=m: e.max_index(out=posu[0:m, h, 8:16], in_max=ts_[0:m, h, 8:16], in_values=cand[0:m, h, :]), reads=[t_cand, t_ts], writes=[t_ts])
                S.dve(lambda e, m=m: e.tensor_single_scalar(out=au[0:m], in_=posu[0:m], scalar=4, op=ALU.logical_shift_right), reads=[t_ts], writes=[t_ab])
                S.dve(lambda e, m=m: e.tensor_single_scalar(out=bu[0:m], in_=posu[0:m], scalar=15, op=ALU.bitwise_and), reads=[t_ts], writes=[t_ab])
                S.dve(lambda e, m=m: e.tensor_copy(out=a_f[0:m], in_=au[0:m]), reads=[t_ab], writes=[t_ab])
                S.dve(lambda e, m=m: e.tensor_copy(out=b_f[0:m], in_=bu[0:m]), reads=[t_ab], writes=[t_ab])
                for which, sel, dstI in ((0, a_f, I1), (1, b_f, I2)):
                    S.dve(lambda e, m=m, sel=sel: e.tensor_tensor(out=eq[0:m], in0=bc(iota16[0:m].unsqueeze(1).unsqueeze(1), [m, 8, 16, 16]),
                                                                  in1=bc(sel[0:m].unsqueeze(3), [m, 8, 16, 16]), op=ALU.is_equal),
                          reads=[t_ab, t_pc], writes=[t_eq])
                    S.dve(lambda e, m=m, which=which, i2v=i2v: e.tensor_tensor(out=eq[0:m], in0=eq[0:m], in1=bc(i2v[:, :, which, :].unsqueeze(2), [m, 8, 16, 16]), op=ALU.mult),
                          reads=[t_eq, t_vals], writes=[t_eq])
                    S.dve(lambda e, m=m, dstI=dstI: e.tensor_reduce(out=dstI[0:m], in_=eq[0:m], axis=AX.X, op=ALU.add), reads=[t_eq], writes=[t_I])
                S.dve(lambda e, m=m: e.tensor_tensor(out=gt_[0:m], in0=ts_[0:m], in1=bc(ts_[0:m, :, 0:1], [m, 8, 16]), op=ALU.subtract), reads=[t_ts], writes=[t_g])
                S.act(lambda e, m=m: e.activation(out=gt_[0:m], in_=gt_[0:m], func=AF.Exp), reads=[t_g], writes=[t_g])
                S.dve(lambda e, m=m: e.tensor_reduce(out=zs[0:m], in_=gt_[0:m], axis=AX.X, op=ALU.add), reads=[t_g], writes=[t_g])
                S.dve(lambda e, m=m: e.reciprocal(out=zs[0:m], in_=zs[0:m]), reads=[t_g], writes=[t_g])
                S.dve(lambda e, m=m: e.tensor_tensor(out=gt_[0:m], in0=gt_[0:m], in1=bc(zs[0:m].unsqueeze(2), [m, 8, 16]), op=ALU.mult), reads=[t_g], writes=[t_g])
                for srcI, dstT, rd in ((I1, I1T, t_I), (I2, I2T, t_I), (gt_, gT, t_g)):
                    S.pe(lambda e, srcI=srcI, m=m: e.transpose(B[0][:, 0:m], srcI[0:m].rearrange("p h k -> p (h k)"), ident[0:m, 0:m]),
                         reads=[rd, t_const], writes=[bt[0]])
                    S.act(lambda e, dstT=dstT, tt0=tt0, m=m: e.activation(out=dstT[:, tt0:tt0 + m], in_=B[0][:, 0:m], func=AF.Copy), reads=[bt[0]], writes=[t_IT])
            if debug and gi == 0 and False:
                for qq, tl in enumerate((I1T, I2T, gT)):
                    S.dma(lambda e, qq=qq, tl=tl: e.dma_start(out=O["dbg_IT"][qq], in_=tl[:]), reads=[t_IT])
            ck(6.2)
            for n0 in range(0, n, 4):
                bi = (n0 // 4) % 2
                S.dve(lambda e, bi=bi, n0=n0: e.tensor_tensor(out=B4[bi][:], in0=bc(iotaRb[:].unsqueeze(1), [128, 4, 128]),
                                                             in1=bc(I2T[:, n0:n0 + 4].unsqueeze(2), [128, 4, 128]), op=ALU.is_equal),
                      reads=[t_IT, t_pc], writes=[t_B4[bi]])
                S.dve(lambda e, bi=bi, n0=n0: e.tensor_tensor(out=A4[bi][:], in0=bc(iotaRb[:].unsqueeze(1), [128, 4, 128]),
                                                             in1=bc(I1T[:, n0:n0 + 4].unsqueeze(2), [128, 4, 128]), op=ALU.is_equal),
                      reads=[t_IT, t_pc], writes=[t_A4[bi]])
                S.pool(lambda e, bi=bi, n0=n0: e.tensor_tensor(out=A4[bi][:], in0=A4[bi][:],
                                                              in1=bc(gT[:, n0:n0 + 4].unsqueeze(2), [128, 4, 128]), op=ALU.mult),
                      reads=[t_IT, t_A4[bi]], writes=[t_A4[bi]])
                bk = 6 + bi
                for q in range(4):
                    S.pe(lambda e, bi=bi, q=q, bk=bk: e.matmul(B[bk][:, q * 128:(q + 1) * 128], lhsT=B4[bi][:, q, :], rhs=A4[bi][:, q, :], start=True, stop=True),
                         reads=[t_A4[bi], t_B4[bi]], writes=[bt[bk]])
                S.act(lambda e, bk=bk, n0=n0: e.activation(out=WT[:, n0:n0 + 4, :].rearrange("p n i -> p (n i)"),
                                                           in_=B[bk][:], func=AF.Copy), reads=[bt[bk]], writes=[t_WT])
            ck(6.3)
            def emit_U(i1):
                blk, k2 = i1 // 2, i1 % 2
                bi = blk % 2
                if k2 == 0:
                    S.dma(lambda e, bi=bi, blk=blk: e.dma_start(out=eub[bi][:], in_=euT_d[blk]), writes=[t_eub[bi]])
                    S.dma(lambda e, bi=bi, blk=blk: e.dma_start(out=evb[bi][:], in_=evv[:, blk * 2:(blk + 1) * 2, :]), writes=[t_evb[bi]])
                ui = i1 % 2
                ubk = 4 + ui
                for c in range(8):
                    S.pe(lambda e, bi=bi, k2=k2, c=c, ubk=ubk, n=n: e.matmul(B[ubk][:, 0:n], lhsT=eub[bi][:, c, k2 * 128:(k2 + 1) * 128], rhs=h2g[:, c, 0:n],
                                                                             start=(c == 0), stop=(c == 7)), reads=[t_eub[bi], t_h2], writes=[bt[ubk]])

            def emit_rest(i1):
                blk, k2 = i1 // 2, i1 % 2
                bi = blk % 2
                ui = i1 % 2
                ubk = 4 + ui
                S.act(lambda e, ui=ui, ubk=ubk, n=n: e.activation(out=gU[ui][:, 0:n], in_=B[ubk][:, 0:n], func=AF.Gelu), reads=[bt[ubk]], writes=[t_gU[ui]])
                S.dve(lambda e, ui=ui, i1=i1, n=n: e.tensor_tensor(out=Wg[ui][:, 0:n], in0=gU[ui][:, 0:n], in1=WT[:, 0:n, i1], op=ALU.mult),
                      reads=[t_gU[ui], t_WT], writes=[t_Wg[ui]])
                for dc in range(8):
                    abk = dc // 2
                    S.pe(lambda e, bi=bi, k2=k2, dc=dc, abk=abk, ui=ui, i1=i1, n=n: e.matmul(
                        B[abk][:, (dc % 2) * 256:(dc % 2) * 256 + n], lhsT=evb[bi][:, k2, dc * 128:(dc + 1) * 128], rhs=Wg[ui][:, 0:n],
                        start=(i1 == 0 and dc % 2 == 0), stop=(i1 == 127)), reads=[t_evb[bi], t_Wg[ui]], writes=[bt[abk]])

            for i1 in range(129):
                if i1 < 128:
                    emit_U(i1)
                if i1 >= 1:
                    emit_rest(i1 - 1)
            for dc in range(8):
                abk = dc // 2
                src = B[abk][:, (dc % 2) * 256:(dc % 2) * 256 + n]
                if not samp:
                    S.dve(lambda e, dc=dc, src=src, n=n: e.scalar_tensor_tensor(out=x1g[:, dc, 0:n], in0=src, scalar=modT[:, 40 + dc, 0:1], in1=x1g[:, dc, 0:n],
                                                                                 op0=ALU.mult, op1=ALU.add), reads=[bt[abk], t_mod, t_x1], writes=[t_x1])
                else:
                    S.dve(lambda e, dc=dc, src=src: e.tensor_tensor(out=tmpg[:, dc, 0:NS].rearrange("p (b t) -> p b t", t=4),
                                                                    in0=src.rearrange("p (b t) -> p b t", t=4),
                                                                    in1=bc(modT[:, 40 + dc, 1:17].unsqueeze(2), [128, SB, 4]), op=ALU.mult),
                          reads=[bt[abk], t_mod, t_tmp], writes=[t_tmp])
                    S.dve(lambda e, dc=dc: e.tensor_tensor(out=x1g[:, dc, 0:NS], in0=x1g[:, dc, 0:NS], in1=tmpg[:, dc, 0:NS], op=ALU.add),
                          reads=[t_tmp, t_x1], writes=[t_x1])
            for tt0 in range(0, n, 128):
                m = min(128, n - tt0)
                yi = nyt % 2
                nyt += 1
                for dc in range(8):
                    bk = 4 + dc // 4
                    S.pe(lambda e, dc=dc, bk=bk, tt0=tt0, m=m: e.transpose(B[bk][0:m, (dc % 4) * 128:(dc % 4 + 1) * 128], x1g[:, dc, tt0:tt0 + m], ident[:]),
                         reads=[t_x1, t_const], writes=[bt[bk]])
                for hf in range(2):
                    S.act(lambda e, hf=hf, yi=yi, m=m: e.activation(out=ytk[yi][0:m, hf * 512:(hf + 1) * 512], in_=B[4 + hf][0:m, :], func=AF.Copy),
                          reads=[bt[4 + hf]], writes=[t_ytk[yi]])
                S.dma(lambda e, yi=yi, t0=t0, tt0=tt0, m=m: e.dma_start(out=O["y"][t0 + tt0:t0 + tt0 + m, :], in_=ytk[yi][0:m, :]), reads=[t_ytk[yi]])
            ck(6.5 + 0.01 * gi)
    K.st = st
    S.emit()
    st.close()
    return nc


def host_prep(inp, core):
    f = np.float32
    xp = np.asarray(inp["x_prompt"], f)[core]
    xs = np.asarray(inp["x_sample"], f)[core * SB:(core + 1) * SB].reshape(NS, D)
    xT = np.ascontiguousarray(np.concatenate([xp, xs], 0).T)
    cvec = np.concatenate([np.asarray(inp["c_prompt"], f)[core:core + 1],
                           np.asarray(inp["c_sample"], f)[core * SB:(core + 1) * SB]], 0)
    cT = np.ascontiguousarray(cvec.reshape(17, 8, 128).transpose(2, 1, 0))
    m = {}
    m["xT"] = xT
    m["cT"] = cT
    bsl = slice(core * SB, (core + 1) * SB)
    m["ck_s"] = np.ascontiguousarray(np.asarray(inp["cache_k_win"], f)[0, bsl].reshape(SB, 2048, 512))
    m["cv_s"] = np.ascontiguousarray(np.asarray(inp["cache_v_win"], f)[0, bsl].reshape(SB, 2048, 512))
    m["swkv"] = np.ascontiguousarray(np.asarray(inp["state_wkv"], f)[0, bsl].reshape(128, 4096))
    sh = np.asarray(inp["state_shift"], f)[0, bsl]
    m["shT"] = np.ascontiguousarray(sh[:, 0:1536].reshape(SB, 3, 4, 128).transpose(3, 1, 2, 0))
    m["shTl"] = np.ascontiguousarray(sh[:, 1536:1664].T)
    return m


def _t5_bucket(dist):
    import math
    dist = np.asarray(dist, dtype=np.int64)
    max_exact = 16
    safe = np.maximum(dist, 1) / max_exact
    large = max_exact + (np.log(safe) / math.log(2048 / max_exact) * (32 - max_exact)).astype(np.int64)
    large = np.minimum(large, 31)
    return np.where(dist < max_exact, dist, large).astype(np.int32)


def _ohu_table():
    t = np.zeros((32, 3, 384), np.float32)
    for br, dil in enumerate((1, 4, 16)):
        j = np.arange(129)
        b = _t5_bucket(j * dil)
        t[b, br, j + 127] = 1.0
    return t


def host_shared(inp):
    f = np.float32
    m = {}
    m["ada_w"] = np.ascontiguousarray(np.asarray(inp["ada_w"], f)[0])
    m["ada_bT"] = np.ascontiguousarray(np.asarray(inp["ada_b"], f)[0].reshape(48, 128).T)
    m["n1gT"] = np.ascontiguousarray(np.asarray(inp["norm1_g"], f)[0].reshape(8, 128).T)
    m["n2gT"] = np.ascontiguousarray(np.asarray(inp["norm2_g"], f)[0].reshape(8, 128).T)
    m["w_in"] = np.ascontiguousarray(np.asarray(inp["w_in"], f)[0])
    qg = np.asarray(inp["q_norm_g"], f)[0]
    kg = np.asarray(inp["k_norm_g"], f)[0]
    m["qkg"] = np.ascontiguousarray(np.stack([np.tile(qg, 2), np.tile(kg, 2)], 1))
    m["ident"] = np.eye(128, dtype=f)
    m["ones"] = np.ones((128, 128), f)
    b2 = np.zeros((128, 128), f)
    b2[:64, :64] = 1
    b2[64:, 64:] = 1
    m["blk2"] = b2
    m["relb"] = np.ascontiguousarray(np.asarray(inp["rel_bias"], f))
    m["ohu"] = _ohu_table()

    def pf(v):
        return np.asarray(v, f).reshape(4, 128).T
    mu = np.asarray(inp["mu_shift"], f)[0]
    m["rwp"] = np.ascontiguousarray(np.stack([pf(mu[0:512]), pf(mu[512:1024]), pf(mu[1024:1536]), pf(inp["k_k"][0]), pf(inp["k_a"][0]),
                                              pf(np.asarray(inp["r_k"], f)[0].reshape(512)), pf(inp["a0"][0]), pf(inp["w0"][0])], 1))
    m["mul"] = np.ascontiguousarray(mu[1536:1664].reshape(128, 1))
    m["lw3"] = np.ascontiguousarray(np.concatenate([np.asarray(inp["w_w2"], f)[0], np.asarray(inp["w_a2"], f)[0], np.asarray(inp["w_g2"], f)[0]], 0))
    m["w0row"] = np.ascontiguousarray(np.asarray(inp["w0"], f)[0].reshape(1, 512))
    m["lnx"] = np.ascontiguousarray(np.stack([np.asarray(inp["lnx_g"], f)[0], np.asarray(inp["lnx_b"], f)[0]], 0))
    m["w_out"] = np.ascontiguousarray(np.asarray(inp["w_out"], f)[0])
    m["w_pq"] = np.ascontiguousarray(np.asarray(inp["w_peer_q"], f)[0])
    sk = np.asarray(inp["peer_sub_keys"], f)[0]
    m["skT"] = np.ascontiguousarray(sk.reshape(16, 128, 128).transpose(2, 0, 1))
    m["euT"] = np.ascontiguousarray(np.asarray(inp["expert_u"], f)[0].T)
    m["ev"] = np.ascontiguousarray(np.asarray(inp["expert_v"], f)[0])
    m["qkrow"] = np.ascontiguousarray(np.stack([qg, kg], 0))
    t_ = np.zeros((32, 3, 129), f)
    for br_, dil_ in enumerate((1, 4, 16)):
        jp = np.arange(129)
        t_[_t5_bucket((128 - jp) * dil_), br_, jp] = 1.0
    m["ohs"] = t_
    m["iotaR"] = np.ascontiguousarray(np.tile(np.arange(128, dtype=f)[None, :], (128, 1)))
    NEG = -0.6065306597126334
    i = np.arange(128)[:, None]
    t = np.arange(128)[None, :]
    same = (i // 64) == (t // 64)
    m["tri"] = np.ascontiguousarray(np.stack([np.where(same & (i <= t), NEG, 0.0), np.where(same & (i < t), NEG, 0.0)], 1).astype(f))
    ii = (np.arange(128) % 64)[:, None]
    tt = np.arange(64)[None, :]
    m["mk1"] = np.ascontiguousarray(np.stack([(tt > ii), (tt >= ii)], 1).astype(f))
    m["mk3"] = np.ascontiguousarray((tt < ii).astype(f))
    m["id2"] = np.ascontiguousarray((tt == ii).astype(f))
    return m


def kernel(**inp):
    nc = build()
    shared = host_shared(inp)
    in_maps = []
    for c in range(NCORES):
        m = dict(shared)
        m.update(host_prep(inp, c))
        in_maps.append(m)
    res = run_bass_kernel_spmd(nc, in_maps, core_ids=list(range(NCORES)))
    R = res.results
    f = np.float32
    y_p = np.stack([R[c]["y"][0:T] for c in range(NCORES)], 0).astype(f)
    y_s = np.concatenate([R[c]["y"][T:NT].reshape(SB, 4, D) for c in range(NCORES)], 0).astype(f)
    kwp = np.stack([R[c]["kwp"].reshape(2048, 8, 64) for c in range(NCORES)], 0)[None].astype(f)
    vwp = np.stack([R[c]["vwp"].reshape(2048, 8, 64) for c in range(NCORES)], 0)[None].astype(f)
    wkvp = np.stack([R[c]["wkvp"].reshape(2, 64, 4, 64).transpose(2, 0, 3, 1).reshape(8, 64, 64) for c in range(NCORES)], 0)[None].astype(f)
    shp = np.stack([R[c]["shp"].reshape(CR) for c in range(NCORES)], 0)[None].astype(f)
    kws = np.concatenate([R[c]["kws"].reshape(SB, 4, 8, 64) for c in range(NCORES)], 0)[None].astype(f)
    vws = np.concatenate([R[c]["vws"].reshape(SB, 4, 8, 64) for c in range(NCORES)], 0)[None].astype(f)
    wkvs = np.concatenate([R[c]["wkvs"].reshape(SB, 8, 64, 64) for c in range(NCORES)], 0)[None].astype(f)
    shs = np.concatenate([R[c]["shs"].reshape(SB, CR) for c in range(NCORES)], 0)[None].astype(f)
    return (y_p, y_s, kwp, vwp, wkvp, shp, kws, vws, wkvs, shs)
```

```python
import contextlib
import numpy as np
import concourse.bass as bass
import concourse.mybir as mybir
from concourse.bass_utils import run_bass_kernel_spmd

F32 = mybir.dt.float32
BF16 = mybir.dt.bfloat16
I32 = mybir.dt.int32
U32 = mybir.dt.uint32
AF = mybir.ActivationFunctionType
ALU = mybir.AluOpType
AX = mybir.AxisListType

N_DMA_SLOTS = 24
PE_NOSYNC = True
NCORES = 8
D = 1024
T = 4096
NS = 64
NT = T + NS
SB = 16
DIN = 3200
CR = 1664
EPS = 1e-6


class Tok:
    __slots__ = ("lw", "rd", "rd_dma", "name", "excl")

    def __init__(self, name="", excl=False):
        self.excl = excl
        self.lw = None
        self.rd = {}
        self.rd_dma = []
        self.name = name


class Sched:
    ENGS = ("pe", "act", "dve", "pool", "sp")

    def __init__(self, nc):
        self.nc = nc
        self.ins = []
        self.last_by_eng = {}
        self.dmas_since = []
        self.nosync = True

    def barrier(self):
        deps = set(self.last_by_eng.values()) | set(self.dmas_since)
        self.dmas_since = []
        for e in self.ENGS:
            idx = len(self.ins)
            self.ins.append([e, (lambda eh: eh.nop()), set(deps), False, False, 0, 0, False])
            self.last_by_eng[e] = idx

    def op(self, eng, fn, reads=(), writes=(), dma=False, strided=False):
        idx = len(self.ins)
        deps = set()
        for t in reads:
            if t.lw is not None:
                deps.add(t.lw)
            if t.excl:
                deps.update(v for kk_, v in t.rd.items() if kk_ != eng)
        for t in writes:
            if t.lw is not None:
                deps.add(t.lw)
            deps.update(t.rd.values())
            deps.update(t.rd_dma)
        for t in reads:
            if dma:
                t.rd_dma.append(idx)
            else:
                t.rd[eng] = idx
        for t in writes:
            t.lw = idx
            t.rd = {}
            t.rd_dma = []
        deps.discard(idx)
        self.ins.append([eng, fn, deps, dma, False, 0, 0, strided or (not self.nosync)])
        if dma:
            self.dmas_since.append(idx)
        else:
            self.last_by_eng[eng] = idx
        return idx

    def pe(self, fn, reads=(), writes=(), strided=False):
        return self.op("pe", fn, reads, writes, strided=strided)

    def act(self, fn, reads=(), writes=()):
        return self.op("act", fn, reads, writes)

    def dve(self, fn, reads=(), writes=()):
        return self.op("dve", fn, reads, writes)

    def pool(self, fn, reads=(), writes=()):
        return self.op("pool", fn, reads, writes)

    def dma(self, fn, reads=(), writes=(), eng="sp"):
        return self.op(eng, fn, reads, writes, dma=True)

    def emit(self):
        nc = self.nc
        ins = self.ins
        class _Rec:
            def matmul(self, out, lhsT, rhs, **kw):
                self.rg = (lhsT.base_partition(), lhsT.partition_size())
            def transpose(self, out, in_, identity, **kw):
                self.rg = (in_.base_partition(), in_.partition_size())
            def nop(self, *a, **k):
                self.rg = (0, 128)
        prev_pe = None
        prev_strips = None
        for i_, it in enumerate(ins):
            if it[0] == "pe" and not it[3]:
                rec = _Rec()
                it[1](rec)
                b0, sz = rec.rg
                strips = set(range(b0 // 32, (b0 + sz + 31) // 32))
                if PE_NOSYNC and not it[7]:
                    if prev_strips is not None and not (strips & prev_strips):
                        it[2].add(prev_pe)
                    else:
                        it[2] = set(d for d in it[2] if not (ins[d][0] == "pe" and not ins[d][3]))
                prev_pe = i_
                prev_strips = strips
            for d in it[2]:
                ins[d][4] = True
        last = {}
        for i, it in enumerate(ins):
            if not it[3]:
                last[it[0]] = i
        for i in last.values():
            ins[i][4] = True
        cnt = {e: 0 for e in self.ENGS}
        dma_n = {e: 0 for e in self.ENGS}
        for it in ins:
            e = it[0]
            if it[3]:
                it[4] = True
                it[6] = dma_n[e]
                dma_n[e] += 1
            elif it[4]:
                cnt[e] += 1
                it[5] = cnt[e]
        with contextlib.ExitStack() as st:
            sems = {e: st.enter_context(nc.semaphore("s_" + e)) for e in self.ENGS}
            dsems = {e: [st.enter_context(nc.semaphore("d_%s_%d" % (e, i))) for i in range(N_DMA_SLOTS)]
                     for e in self.ENGS if dma_n[e] > 0}
            block = st.enter_context(nc.Block())
            per_eng = {e: [] for e in self.ENGS}
            for i, it in enumerate(ins):
                per_eng[it[0]].append(i)

            def run(eng_name, eh):
                seen = {e: 0 for e in self.ENGS}
                seen_dma = {}
                for i in per_eng[eng_name]:
                    e, fn, deps, is_dma, sig, c, slot = ins[i][:7]
                    need = {}
                    for d in deps:
                        de, _, _, ddma, _, dc, dslot = ins[d][:7]
                        if ddma:
                            key = (de, dslot % N_DMA_SLOTS)
                            val = 16 * (dslot // N_DMA_SLOTS + 1)
                            if seen_dma.get(key, 0) < val:
                                seen_dma[key] = val
                                need[("d",) + key] = val
                        else:
                            if seen[de] < dc:
                                seen[de] = dc
                                need[("c", de)] = dc
                    if is_dma and slot >= N_DMA_SLOTS:
                        key = (e, slot % N_DMA_SLOTS)
                        val = 16 * (slot // N_DMA_SLOTS)
                        if seen_dma.get(key, 0) < val:
                            seen_dma[key] = val
                            need[("d",) + key] = val
                    for k, v in need.items():
                        if k[0] == "c":
                            eh.wait_ge(sems[k[1]], v)
                        else:
                            eh.wait_ge(dsems[k[1]][k[2]], v)
                    inst = fn(eh)
                    if is_dma:
                        inst.then_inc(dsems[e][slot % N_DMA_SLOTS], 16)
                    elif sig:
                        inst.then_inc(sems[e], 1)
                if eng_name == "sp":
                    for e2 in self.ENGS:
                        if cnt[e2] > 0:
                            eh.wait_ge(sems[e2], cnt[e2])
                        n = dma_n.get(e2, 0)
                        for s in range(min(N_DMA_SLOTS, n)):
                            lastslot = ((n - 1 - s) // N_DMA_SLOTS) * N_DMA_SLOTS + s
                            eh.wait_ge(dsems[e2][s], 16 * (lastslot // N_DMA_SLOTS + 1))

            @block.tensor
            def _(eh):
                run("pe", eh)

            @block.scalar
            def _(eh):
                run("act", eh)

            @block.vector
            def _(eh):
                run("dve", eh)

            @block.gpsimd
            def _(eh):
                run("pool", eh)

            @block.sync
            def _(eh):
                run("sp", eh)


class Ctx:
    pass


def bc(ap, shape):
    return ap.to_broadcast(list(shape))


def build(phase_limit=99, debug=False):
    nc = bass.Bass("TRN2", target_bir_lowering=False)
    S = Sched(nc)
    K = Ctx()
    K.nc, K.S = nc, S

    def din(name, shape, dt=F32):
        return nc.dram_tensor(name, list(shape), dt, kind="ExternalInput").ap()

    def dout(name, shape, dt=F32):
        return nc.dram_tensor(name, list(shape), dt, kind="ExternalOutput").ap()

    I = {}
    I["xT"] = din("xT", [D, NT])
    I["cT"] = din("cT", [128, 8, 17])
    I["ada_w"] = din("ada_w", [D, 6 * D])
    I["ada_bT"] = din("ada_bT", [128, 48])
    I["n1gT"] = din("n1gT", [128, 8])
    I["n2gT"] = din("n2gT", [128, 8])
    I["w_in"] = din("w_in", [D, DIN])
    I["qkg"] = din("qkg", [128, 2])
    I["ident"] = din("ident", [128, 128])
    I["ones"] = din("ones", [128, 128])
    I["blk2"] = din("blk2", [128, 128])
    I["relb"] = din("relb", [32, 8])
    I["ohu"] = din("ohu", [32, 3, 384])
    I["w_out"] = din("w_out", [D, D])
    I["w_pq"] = din("w_pq", [D, 2048])
    I["skT"] = din("skT", [128, 16, 128])
    I["euT"] = din("euT", [D, 16384])
    I["ev"] = din("ev", [16384, D])
    I["iotaR"] = din("iotaR", [128, 128])
    euT_d = nc.dram_tensor("euT_d", [64, 128, 8, 256], BF16, kind="Internal").ap()
    ev_d = nc.dram_tensor("ev_d", [16384, D], BF16, kind="Internal").ap()
    wpq_d = nc.dram_tensor("wpq_d", [D, 2048], BF16, kind="Internal").ap()
    I["ck_s"] = din("ck_s", [SB, 2048, 512])
    I["cv_s"] = din("cv_s", [SB, 2048, 512])
    I["swkv"] = din("swkv", [128, 4096])
    I["shT"] = din("shT", [128, 3, 4, 16])
    I["shTl"] = din("shTl", [128, 16])
    I["qkrow"] = din("qkrow", [2, 64])
    I["ohs"] = din("ohs", [32, 3, 129])
    rws_d = nc.dram_tensor("rws_d", [SB, 8, 4, 6, 64], F32, kind="Internal").ap()
    ys_d = nc.dram_tensor("ys_d", [SB, 8, 4, 64], F32, kind="Internal").ap()
    reck_d = nc.dram_tensor("reck_d", [SB, 8, 512], F32, kind="Internal").ap()
    recv_d = nc.dram_tensor("recv_d", [SB, 8, 512], F32, kind="Internal").ap()
    I["rwp"] = din("rwp", [128, 8, 4])
    I["mul"] = din("mul", [128, 1])
    I["lw3"] = din("lw3", [128, 512])
    I["w0row"] = din("w0row", [1, 512])
    I["lnx"] = din("lnx", [2, 512])
    I["tri"] = din("tri", [128, 2, 128])
    I["mk1"] = din("mk1", [128, 2, 64])
    I["mk3"] = din("mk3", [128, 64])
    I["id2"] = din("id2", [128, 64])
    zscr = nc.dram_tensor("zscr", [24, 256, 384], F32, kind="Internal").ap()
    mixT_d = nc.dram_tensor("mixT_d", [8, 128, NT], BF16, kind="Internal").ap()
    O = {}
    O["kwp"] = dout("kwp", [2048, 512])
    O["vwp"] = dout("vwp", [2048, 512])
    O["kws"] = dout("kws", [NS, 512])
    O["vws"] = dout("vws", [NS, 512])
    O["shp"] = dout("shp", [1, CR])
    O["shs"] = dout("shs", [SB, CR])
    O["wkvp"] = dout("wkvp", [128, 4, 64])
    O["y"] = dout("y", [NT, D])
    O["wkvs"] = dout("wkvs", [128, 4096])
    if debug:
        O["dbg_hT"] = dout("dbg_hT", [128, 8, NT], BF16)
        O["dbg_mod"] = dout("dbg_mod", [128, 48, 17])
        O["dbg_ebt"] = dout("dbg_ebt", [128, 24, 2, 128], BF16)
        O["dbg_mixT"] = dout("dbg_mixT", [4, 128, T], BF16)
        O["dbg_yr"] = dout("dbg_yr", [T, 512])
        O["dbg_IT"] = dout("dbg_IT", [3, 128, 256])

    st = contextlib.ExitStack()

    uid = [0]

    def sbt(stack, name, shape, dt=F32):
        uid[0] += 1
        return stack.enter_context(nc.sbuf_tensor("s%d_%s" % (uid[0], name), list(shape), dt))

    def sb(name, shape, dt=F32):
        return sbt(st, name, shape, dt)

    banks = [st.enter_context(nc.psum_tensor("bank%d" % i, [128, 512], F32)) for i in range(8)]
    bt = [Tok("bank%d" % i, excl=True) for i in range(8)]

    ident = sb("ident", [128, 128])
    ones = sb("ones", [128, 128])
    blk2 = sb("blk2", [128, 128])
    identb = sb("identb", [128, 128], BF16)
    epsc = sb("epsc", [128, 1])
    t_const = Tok("const")
    S.dma(lambda e: e.dma_start(out=ident[:], in_=I["ident"]), writes=[t_const])
    S.dma(lambda e: e.dma_start(out=ones[:], in_=I["ones"]), writes=[t_const])
    S.dma(lambda e: e.dma_start(out=blk2[:], in_=I["blk2"]), writes=[t_const])
    S.pool(lambda e: e.memset(epsc[:], EPS), writes=[t_const])
    S.dve(lambda e: e.tensor_copy(out=identb[:], in_=ident[:]), reads=[t_const], writes=[t_const])

    S.nosync = True
    cT = sb("cT", [128, 8, 17])
    scT = sb("scT", [128, 8, 17])
    adab = sb("adab", [128, 48])
    n1g = sb("n1g", [128, 8])
    n2g = sb("n2g", [128, 8])
    qkg = sb("qkg", [128, 2])
    modT = sb("modT", [128, 48, 17])
    A1 = sb("A1", [128, 8, 17])
    A2 = sb("A2", [128, 8, 17])
    t_small = Tok("small")
    t_mod = Tok("mod")
    for dst, src in ((cT, "cT"), (adab, "ada_bT"), (n1g, "n1gT"), (n2g, "n2gT"), (qkg, "qkg")):
        S.dma(lambda e, dst=dst, src=src: e.dma_start(out=dst[:], in_=I[src]), writes=[t_small])
    S.act(lambda e: e.activation(out=scT[:], in_=cT[:], func=AF.Silu), reads=[t_small], writes=[t_small])
    adaw_v = I["ada_w"].rearrange("(c p) n -> p c n", p=128)
    with contextlib.ExitStack() as st2:
        wb = [sbt(st2, "adaw", [128, 8, 512]) for i in range(2)]
        wt = [Tok("adaw%d" % i) for i in range(2)]
        for nb in range(12):
            w = wb[nb % 2]
            wtk = wt[nb % 2]
            S.dma(lambda e, w=w, nb=nb: e.dma_start(out=w[:], in_=adaw_v[:, :, nb * 512:(nb + 1) * 512]), writes=[wtk])
            bk = nb % 2
            for oc in range(4):
                for c in range(8):
                    S.pe(lambda e, w=w, oc=oc, c=c, bk=bk: e.matmul(
                        banks[bk][:, oc * 32:oc * 32 + 17], lhsT=w[:, c, oc * 128:(oc + 1) * 128], rhs=scT[:, c, :],
                        start=(c == 0), stop=(c == 7)), reads=[wtk, t_small], writes=[bt[bk]])
            for oc in range(4):
                j = nb * 4 + oc
                S.act(lambda e, oc=oc, j=j, bk=bk: e.activation(
                    out=modT[:, j, :], in_=banks[bk][:, oc * 32:oc * 32 + 17], func=AF.Identity, bias=adab[:, j:j + 1]),
                    reads=[bt[bk], t_small], writes=[t_mod])
    S.barrier()
    S.dve(lambda e: e.tensor_scalar(out=A1[:], in0=modT[:, 8:16, :], scalar1=1.0, scalar2=None, op0=ALU.add),
          reads=[t_mod], writes=[t_mod])
    S.dve(lambda e: e.tensor_tensor(out=A1[:], in0=A1[:], in1=bc(n1g[:].unsqueeze(2), [128, 8, 17]), op=ALU.mult),
          reads=[t_mod, t_small], writes=[t_mod])
    S.dve(lambda e: e.tensor_scalar(out=A2[:], in0=modT[:, 32:40, :], scalar1=1.0, scalar2=None, op0=ALU.add),
          reads=[t_mod], writes=[t_mod])
    S.dve(lambda e: e.tensor_tensor(out=A2[:], in0=A2[:], in1=bc(n2g[:].unsqueeze(2), [128, 8, 17]), op=ALU.mult),
          reads=[t_mod, t_small], writes=[t_mod])
    if debug:
        S.dma(lambda e: e.dma_start(out=O["dbg_mod"], in_=modT[:]), reads=[t_mod])

    stH = contextlib.ExitStack()
    stC_holder = []
    hT = sbt(stH, "hT", [128, 8, NT], BF16)
    groups = [(g * 512, 512) for g in range(8)] + [(T, NS)]
    t_h = [Tok("h%d" % g) for g in range(9)]
    xT_v = I["xT"].rearrange("(c p) n -> p c n", p=128)

    def norm_groups(src_v, dst, Aap, Bidx, t_dst, extra_reads=()):
        with contextlib.ExitStack() as st2:
            xg = [sbt(st2, "xg", [128, 8, 512]) for i in range(2)]
            xgt = [Tok() for i in range(2)]
            sq = sbt(st2, "sq", [128, 8, 512])
            sqt = Tok()
            rs = sbt(st2, "rs", [128, 512])
            rst = Tok()
            for g, (t0, n) in enumerate(groups):
                x_ = xg[g % 2]
                xt_ = xgt[g % 2]
                bk = 2 + g % 2
                S.dma(lambda e, x_=x_, t0=t0, n=n: e.dma_start(out=x_[:, :, 0:n], in_=src_v[:, :, t0:t0 + n]),
                      writes=[xt_], reads=list(extra_reads))
                S.act(lambda e, x_=x_, n=n: e.activation(out=sq[:, :, 0:n], in_=x_[:, :, 0:n], func=AF.Square),
                      reads=[xt_], writes=[sqt])
                for c in range(8):
                    S.pe(lambda e, c=c, n=n, bk=bk: e.matmul(banks[bk][:, 0:n], lhsT=ones[:], rhs=sq[:, c, 0:n],
                                                              start=(c == 0), stop=(c == 7)),
                         reads=[sqt, t_const], writes=[bt[bk]])
                S.act(lambda e, n=n, bk=bk: e.activation(out=rs[:, 0:n], in_=banks[bk][:, 0:n], func=AF.Sqrt,
                                                          scale=1.0 / D, bias=epsc[:]),
                      reads=[bt[bk], t_const], writes=[rst])
                S.dve(lambda e, n=n: e.reciprocal(out=rs[:, 0:n], in_=rs[:, 0:n]), reads=[rst], writes=[rst])
                S.dve(lambda e, x_=x_, n=n: e.tensor_tensor(out=x_[:, :, 0:n], in0=x_[:, :, 0:n],
                                                             in1=bc(rs[:, 0:n].unsqueeze(1), [128, 8, n]), op=ALU.mult),
                      reads=[xt_, rst], writes=[xt_])
                if g < 8:
                    for c in range(8):
                        eng = S.dve if c % 2 == 0 else S.pool
                        eng(lambda e, x_=x_, c=c, t0=t0, n=n: e.tensor_scalar(
                            out=dst[:, c, t0:t0 + n], in0=x_[:, c, 0:n], scalar1=Aap[:, c, 0:1],
                            scalar2=modT[:, Bidx + c, 0:1], op0=ALU.mult, op1=ALU.add),
                            reads=[xt_, t_mod], writes=[t_dst[g]])
                else:
                    xv = x_[:, :, 0:NS].rearrange("p c (b t) -> p c b t", t=4)
                    S.dve(lambda e, xv=xv: e.tensor_tensor(
                        out=xv, in0=xv, in1=bc(Aap[:, :, 1:17].unsqueeze(3), [128, 8, SB, 4]), op=ALU.mult),
                        reads=[xt_, t_mod], writes=[xt_])
                    S.dve(lambda e, xv=xv, t0=t0: e.tensor_tensor(
                        out=dst[:, :, t0:t0 + NS].rearrange("p c (b t) -> p c b t", t=4), in0=xv,
                        in1=bc(modT[:, Bidx:Bidx + 8, 1:17].unsqueeze(3), [128, 8, SB, 4]), op=ALU.add),
                        reads=[xt_, t_mod], writes=[t_dst[g]])

    norm_groups(xT_v, hT, A1, 0, t_h)
    S.barrier()
    if debug:
        S.dma(lambda e: e.dma_start(out=O["dbg_hT"], in_=hT[:]), reads=t_h)


    if phase_limit < 3:
        S.emit()
        for sk_ in (stC_holder + [stH, st]):
            sk_.close()
        return nc
    S.nosync = True
    stC = contextlib.ExitStack()
    stC_holder.append(stC)
    EBT = sbt(stC, "EBT", [128, 24, 2, 128], BF16)
    onesb = sbt(stC, "onesb", [128, 64], BF16)
    t_ebt = Tok("ebt")
    g0_cst = [sbt(stC, "cst", [128, 1024]) for i in range(3)]
    g0_cbf = [sbt(stC, "cbf", [128, 1024], BF16) for i in range(3)]
    g0_tcs = [Tok() for i in range(3)]
    g0_tcb = [Tok() for i in range(3)]
    g0_blocks = []
    euv_src = I["euT"].rearrange("(c p) e -> p c e", p=128)
    for b_ in range(128):
        g0_blocks.append((lambda i, b_=b_: (g0_cst[i][:].rearrange("p (c e) -> p c e", c=8), euv_src[:, :, b_ * 128:(b_ + 1) * 128]),
                          lambda i, b_=b_: (euT_d[b_ // 2][:, :, (b_ % 2) * 128:(b_ % 2 + 1) * 128], g0_cbf[i][:].rearrange("p (c e) -> p c e", c=8))))
    for src_, dstd_, nblk_ in ((I["ev"], ev_d, 128), (I["w_pq"], wpq_d, 16)):
        sv_ = src_.rearrange("a b -> (a b)").rearrange("(n p f) -> n p f", p=128, f=1024)
        dv_ = dstd_.rearrange("a b -> (a b)").rearrange("(n p f) -> n p f", p=128, f=1024)
        for b_ in range(nblk_):
            g0_blocks.append((lambda i, b_=b_, sv_=sv_: (g0_cst[i][:], sv_[b_]), lambda i, b_=b_, dv_=dv_: (dv_[b_], g0_cbf[i][:])))
    g0_tick = [0]

    def g0_step():
        t = g0_tick[0]
        g0_tick[0] += 1
        nb_ = len(g0_blocks)
        if t - 2 >= 0 and t - 2 < nb_:
            k = t - 2
            i = k % 3
            o_, i_ = g0_blocks[k][1](i)
            S.dma(lambda e, o_=o_, i_=i_: e.dma_start(out=o_, in_=i_), reads=[g0_tcb[i]])
        if t < nb_:
            i = t % 3
            o_, i_ = g0_blocks[t][0](i)
            S.dma(lambda e, o_=o_, i_=i_: e.dma_start(out=o_, in_=i_), writes=[g0_tcs[i]])
        if t - 1 >= 0 and t - 1 < nb_:
            i = (t - 1) % 3
            S.pool(lambda e, i=i: e.tensor_copy(out=g0_cbf[i][:], in_=g0_cst[i][:]), reads=[g0_tcs[i]], writes=[g0_tcb[i]])
    S.dve(lambda e: e.tensor_copy(out=onesb[:], in_=ones[:, 0:64]), reads=[t_const], writes=[t_const])
    with contextlib.ExitStack() as st2:
        relb = sbt(st2, "relb", [32, 8])
        ohu = sbt(st2, "ohu", [32, 3, 384])
        RH = sbt(st2, "RH", [32, 8, 384])
        grep = [sbt(st2, "grep", [128, 384]) for i in range(2)]
        gt = [Tok(), Tok()]
        ebf = [sbt(st2, "ebf", [128, 2, 128]) for i in range(2)]
        et = [Tok(), Tok()]
        t_r = Tok()
        t_rh = Tok()
        S.dma(lambda e: e.dma_start(out=relb[:], in_=I["relb"]), writes=[t_r])
        S.dma(lambda e: e.dma_start(out=ohu[:], in_=I["ohu"]), writes=[t_r])
        S.act(lambda e: e.activation(out=relb[:], in_=relb[:], func=AF.Exp), reads=[t_r], writes=[t_r])
        zt = Tok()
        for br in range(3):
            S.dve(lambda e, br=br: e.tensor_tensor(out=RH[:], in0=bc(relb[:].unsqueeze(2), [32, 8, 384]),
                                                    in1=bc(ohu[:, br, :].unsqueeze(1), [32, 8, 384]), op=ALU.mult),
                  reads=[t_r], writes=[t_rh])
            for h in range(8):
                i = br * 8 + h
                bk = 6 + i % 2
                S.pe(lambda e, h=h, bk=bk: e.matmul(banks[bk][:, 0:384], lhsT=ones[0:32, :], rhs=RH[:, h, :],
                                                     start=True, stop=True), reads=[t_rh, t_const], writes=[bt[bk]])
                g_ = grep[i % 2]
                S.act(lambda e, g_=g_, bk=bk: e.activation(out=g_[:], in_=banks[bk][:, 0:384], func=AF.Copy),
                      reads=[bt[bk]], writes=[gt[i % 2]])
                zi = Tok()
                S.dma(lambda e, g_=g_, i=i: e.dma_start(out=zscr[i, 0:128, :], in_=g_[:]), reads=[gt[i % 2]], writes=[zi])
                S.dma(lambda e, g_=g_, i=i: e.dma_start(out=zscr[i, 128:256, :], in_=g_[:]), reads=[gt[i % 2]], writes=[zi])
                eb_ = ebf[i % 2]
                for part in range(2):
                    src = bass.AP(zscr.tensor, i * 256 * 384 + 255 + part * 128 * 383, [[383, 128], [1, 128]])
                    S.dma(lambda e, eb_=eb_, part=part, src=src: e.dma_start(out=eb_[:, part, :], in_=src),
                          reads=[zi], writes=[et[i % 2]])
                S.dve(lambda e, eb_=eb_, i=i: e.tensor_copy(out=EBT[:, i, :, :], in_=eb_[:]), reads=[et[i % 2]], writes=[t_ebt])
    S.barrier()
    if debug:
        S.dma(lambda e: e.dma_start(out=O["dbg_ebt"], in_=EBT[:]), reads=[t_ebt])

    win_v = I["w_in"].rearrange("(c p) n -> p c n", p=128)

    class _Stop(Exception):
        pass

    def ck(x):
        if phase_limit < x:
            raise _Stop()

    with contextlib.ExitStack() as st2, contextlib.suppress(_Stop):
        ck(3.5)
        wst = [sbt(st2, "wst", [128, 8, 128]) for i in range(2)]
        wstt = [Tok(), Tok()]
        wqkv = sbt(st2, "wqkv", [128, 8, 3, 128], BF16)
        t_w = Tok()
        qT = sbt(st2, "qT", [128, T], BF16)
        kT = sbt(st2, "kT", [128, T], BF16)
        t_q = [Tok() for g in range(8)]
        t_k = [Tok() for g in range(8)]
        V = sbt(st2, "V", [128, 3, 32, 128], BF16)
        t_v = [[Tok() for j in range(32)] for br in range(3)]
        accO = sbt(st2, "accO", [128, 2048])
        accS = sbt(st2, "accS", [128, 2048])
        t_acc = Tok()
        sq = sbt(st2, "sq", [128, 512])
        t_sq = Tok()
        rs = sbt(st2, "rs", [128, 512])
        t_rs = Tok()
        kTf = sbt(st2, "kTf", [128, 512])
        t_kf = Tok()
        ktok = [sbt(st2, "ktok", [128, 4, 128]) for i in range(2)]
        t_kt = [Tok(), Tok()]
        vtok = [sbt(st2, "vtok", [128, 4, 128]) for i in range(2)]
        t_vt = [Tok(), Tok()]
        e0 = [sbt(st2, "e0", [128, 512], BF16) for i in range(4)]
        t_e0 = [Tok() for i in range(4)]
        ee = [sbt(st2, "ee", [128, 512], BF16) for i in range(4)]
        t_ee = [Tok() for i in range(4)]
        mixo = sbt(st2, "mixo", [128, 2048], BF16)
        t_mixo = Tok()
        nld = 0
        nblk = 0
        for hp in range(4):
            for wi in range(3):
                w_ = wst[nld % 2]
                wt_ = wstt[nld % 2]
                nld += 1
                col = wi * 512 + hp * 128
                S.dma(lambda e, w_=w_, col=col: e.dma_start(out=w_[:], in_=win_v[:, :, col:col + 128]), writes=[wt_])
                S.pool(lambda e, w_=w_, wi=wi: e.tensor_copy(out=wqkv[:, :, wi, :], in_=w_[:]), reads=[wt_], writes=[t_w])
            for g in range(8):
                for wi, dstT, tks in ((0, qT, t_q), (1, kT, t_k)):
                    bk = 4 + wi
                    for c in range(8):
                        S.pe(lambda e, c=c, wi=wi, g=g, bk=bk: e.matmul(
                            banks[bk][:], lhsT=wqkv[:, c, wi, :], rhs=hT[:, c, g * 512:(g + 1) * 512],
                            start=(c == 0), stop=(c == 7)), reads=[t_w, t_h[g]], writes=[bt[bk]])
                    S.act(lambda e, bk=bk: e.activation(out=sq[:], in_=banks[bk][:], func=AF.Square),
                          reads=[bt[bk]], writes=[t_sq])
                    S.pe(lambda e: e.matmul(banks[6][:], lhsT=blk2[:], rhs=sq[:], start=True, stop=True),
                         reads=[t_sq, t_const], writes=[bt[6]])
                    S.act(lambda e: e.activation(out=rs[:], in_=banks[6][:], func=AF.Sqrt, scale=1.0 / 64, bias=epsc[:]),
                          reads=[bt[6], t_const], writes=[t_rs])
                    S.dve(lambda e: e.reciprocal(out=rs[:], in_=rs[:]), reads=[t_rs], writes=[t_rs])
                    S.dve(lambda e, bk=bk, wi=wi, g=g, dstT=dstT: e.scalar_tensor_tensor(
                        out=dstT[:, g * 512:(g + 1) * 512], in0=banks[bk][:], scalar=qkg[:, wi:wi + 1], in1=rs[:],
                        op0=ALU.mult, op1=ALU.mult), reads=[bt[bk], t_rs, t_small], writes=[tks[g]])
                    if wi == 1 and g >= 4:
                        S.dve(lambda e, bk=bk: e.scalar_tensor_tensor(
                            out=kTf[:], in0=banks[bk][:], scalar=qkg[:, 1:2], in1=rs[:], op0=ALU.mult, op1=ALU.mult),
                            reads=[bt[bk], t_rs, t_small], writes=[t_kf])
                        for j in range(4):
                            S.pe(lambda e, j=j: e.transpose(banks[7][:, j * 128:(j + 1) * 128], kTf[:, j * 128:(j + 1) * 128], ident[:]),
                                 reads=[t_kf, t_const], writes=[bt[7]])
                        kt_ = ktok[g % 2]
                        S.act(lambda e, kt_=kt_: e.activation(out=kt_[:].rearrange("p a b -> p (a b)"), in_=banks[7][:], func=AF.Copy),
                              reads=[bt[7]], writes=[t_kt[g % 2]])
                        r0 = g * 512 - 2048
                        S.dma(lambda e, kt_=kt_, r0=r0, hp=hp: e.dma_start(
                            out=O["kwp"][r0:r0 + 512, hp * 128:(hp + 1) * 128].rearrange("(j p) c -> p j c", p=128), in_=kt_[:]),
                            reads=[t_kt[g % 2]])
            ck(3.6)
            for br, dil in enumerate((1, 4, 16)):
                if br == 1:
                    ck(3.62)
                G = 32 // dil
                for j0 in range(0, 32, 4):
                    bk = 6 + (j0 // 4) % 2
                    for jj in range(4):
                        j = j0 + jj
                        r, g = j // G, j % G
                        start = r + dil * 128 * g
                        tg = sorted(set([(start) // 512, (start + dil * 127) // 512]))
                        for c in range(8):
                            S.pe(lambda e, c=c, jj=jj, start=start, dil=dil, bk=bk: e.matmul(
                                banks[bk][:, jj * 128:(jj + 1) * 128],
                                lhsT=hT[:, c, start:start + dil * 127 + 1:dil], rhs=wqkv[:, c, 2, :],
                                start=(c == 0), stop=(c == 7)), reads=[t_w] + [t_h[x] for x in tg], writes=[bt[bk]], strided=(dil > 1))
                    S.act(lambda e, br=br, j0=j0, bk=bk: e.activation(
                        out=V[:, br, j0:j0 + 4, :].rearrange("p a b -> p (a b)"), in_=banks[bk][:], func=AF.Copy),
                        reads=[bt[bk]], writes=[t_v[br][j0 + x] for x in range(4)])
                    if br == 0 and j0 >= 16 :
                        vt_ = vtok[(j0 // 4) % 2]
                        tv_ = t_vt[(j0 // 4) % 2]
                        S.act(lambda e, vt_=vt_, bk=bk: e.activation(out=vt_[:].rearrange("p a b -> p (a b)"), in_=banks[bk][:], func=AF.Copy),
                              reads=[bt[bk]], writes=[tv_])
                        r0 = j0 * 128 - 2048
                        S.dma(lambda e, vt_=vt_, r0=r0, hp=hp: e.dma_start(
                            out=O["vwp"][r0:r0 + 512, hp * 128:(hp + 1) * 128].rearrange("(j p) c -> p j c", p=128), in_=vt_[:]),
                            reads=[tv_])
            ck(3.7)
            for half in range(2):
                for br, dil in enumerate((1, 4, 16)):
                    G = 32 // dil
                    Gh = G // 2
                    for r in range(dil):
                        for g in range(half * Gh, (half + 1) * Gh):
                            j = r * G + g
                            start = r + dil * 128 * g
                            qsl = slice(start, start + dil * 127 + 1, dil)
                            tgq = sorted(set([start // 512, (start + dil * 127) // 512]))
                            parts = [1] if g == 0 else [0, 1]
                            g0_step()
                            sbk = (0, 1, 2, 6)[nblk % 4]
                            obk = (3, 4, 5, 7)[nblk % 4]
                            ei = nblk % 4
                            nblk += 1
                            for hh in range(2):
                                ps_ = slice(hh * 64, hh * 64 + 64)
                                for part in parts:
                                    if part == 1:
                                        ksl = qsl
                                        tgk = tgq
                                    else:
                                        ps0 = start - dil * 128
                                        ksl = slice(ps0, ps0 + dil * 127 + 1, dil)
                                        tgk = sorted(set([ps0 // 512, (ps0 + dil * 127) // 512]))
                                    S.pe(lambda e, ps_=ps_, ksl=ksl, qsl=qsl, hh=hh, part=part, sbk=sbk: e.matmul(
                                        banks[sbk][:, (hh * 2 + part) * 128:(hh * 2 + part + 1) * 128],
                                        lhsT=kT[ps_, ksl], rhs=qT[ps_, qsl], start=True, stop=True),
                                        reads=[t_k[x] for x in tgk] + [t_q[x] for x in tgq], writes=[bt[sbk]], strided=(dil > 1))
                            S.act(lambda e, ei=ei, sbk=sbk: e.activation(out=e0[ei][:], in_=banks[sbk][:], func=AF.Exp, scale=0.125),
                                  reads=[bt[sbk]], writes=[t_e0[ei]])
                            S.dve(lambda e, ei=ei, br=br, hp=hp: e.tensor_tensor(
                                out=ee[ei][:], in0=e0[ei][:],
                                in1=EBT[:, br * 8 + 2 * hp:br * 8 + 2 * hp + 2, :, :].rearrange("p a b c -> p (a b c)"), op=ALU.mult),
                                reads=[t_e0[ei], t_ebt], writes=[t_ee[ei]])
                            for hh in range(2):
                                po = slice(hh * 64, hh * 64 + 64)
                                for pi, part in enumerate(parts):
                                    jj = j if part == 1 else j - 1
                                    esl = slice((hh * 2 + part) * 128, (hh * 2 + part + 1) * 128)
                                    S.pe(lambda e, po=po, br=br, jj=jj, hh=hh, esl=esl, ei=ei, obk=obk, pi=pi, parts=parts: e.matmul(
                                        banks[obk][po, 0:128], lhsT=V[:, br, jj, hh * 64:(hh + 1) * 64], rhs=ee[ei][:, esl],
                                        start=(pi == 0), stop=(pi == len(parts) - 1)),
                                        reads=[t_v[br][jj], t_ee[ei]], writes=[bt[obk]])
                                for pi, part in enumerate(parts):
                                    esl = slice((hh * 2 + part) * 128, (hh * 2 + part + 1) * 128)
                                    S.pe(lambda e, po=po, esl=esl, ei=ei, obk=obk, pi=pi, parts=parts: e.matmul(
                                        banks[obk][po, 128:256], lhsT=onesb[:], rhs=ee[ei][:, esl],
                                        start=(pi == 0), stop=(pi == len(parts) - 1)),
                                        reads=[t_const, t_ee[ei]], writes=[bt[obk]])
                            lo = start - half * 2048
                            asl = slice(lo, lo + dil * 127 + 1, dil)
                            if br == 0:
                                S.dve(lambda e, asl=asl, obk=obk: e.tensor_copy(out=accO[:, asl], in_=banks[obk][:, 0:128]),
                                      reads=[bt[obk]], writes=[t_acc])
                                S.dve(lambda e, asl=asl, obk=obk: e.tensor_copy(out=accS[:, asl], in_=banks[obk][:, 128:256]),
                                      reads=[bt[obk]], writes=[t_acc])
                            else:
                                S.dve(lambda e, asl=asl, obk=obk: e.tensor_tensor(out=accO[:, asl], in0=accO[:, asl], in1=banks[obk][:, 0:128], op=ALU.add),
                                      reads=[bt[obk], t_acc], writes=[t_acc])
                                S.dve(lambda e, asl=asl, obk=obk: e.tensor_tensor(out=accS[:, asl], in0=accS[:, asl], in1=banks[obk][:, 128:256], op=ALU.add),
                                      reads=[bt[obk], t_acc], writes=[t_acc])
                S.dve(lambda e: e.reciprocal(out=accS[:], in_=accS[:]), reads=[t_acc], writes=[t_acc])
                S.dve(lambda e: e.tensor_tensor(out=mixo[:], in0=accO[:], in1=accS[:], op=ALU.mult), reads=[t_acc], writes=[t_mixo, t_acc])
                S.dma(lambda e, hp=hp, half=half: e.dma_start(out=mixT_d[hp, :, half * 2048:(half + 1) * 2048], in_=mixo[:]), reads=[t_mixo])
                if debug:
                    S.dma(lambda e, hp=hp, half=half: e.dma_start(out=O["dbg_mixT"][hp, :, half * 2048:(half + 1) * 2048], in_=mixo[:]), reads=[t_mixo])
                ck(3.8)

    while g0_tick[0] < len(g0_blocks) + 2:
        g0_step()
    S.barrier()
    stC.close()
    if phase_limit < 4:
        S.emit()
        for sk_ in (stC_holder + [stH, st]):
            sk_.close()
        return nc
    S.nosync = True
    NEG = -0.6065306597126334
    with contextlib.ExitStack() as st2, contextlib.suppress(_Stop):
        t_rp = Tok("rwparams")
        rwp = sbt(st2, "rwp", [128, 8, 4])
        mul_ = sbt(st2, "mul", [128, 1])
        lw3 = sbt(st2, "lw3", [128, 512])
        w0row = sbt(st2, "w0row", [1, 512])
        lnxg = sbt(st2, "lnxg", [128, 512])
        lnxb = sbt(st2, "lnxb", [128, 512])
        tri = sbt(st2, "tri", [128, 2, 128])
        mk1 = sbt(st2, "mk1", [128, 2, 64])
        mk3 = sbt(st2, "mk3", [128, 64])
        id2 = sbt(st2, "id2", [128, 64])
        omka = sbt(st2, "omka", [128, 4])
        gneps = sbt(st2, "gneps", [128, 1])
        for dst, src in ((rwp, "rwp"), (mul_, "mul"), (lw3, "lw3"), (w0row, "w0row"), (tri, "tri"), (mk1, "mk1"), (mk3, "mk3"), (id2, "id2")):
            S.dma(lambda e, dst=dst, src=src: e.dma_start(out=dst[:], in_=I[src]), writes=[t_rp])
        S.dma(lambda e: e.dma_start(out=lnxg[:], in_=bass.AP(I["lnx"].tensor, 0, [[0, 128], [1, 512]])), writes=[t_rp])
        S.dma(lambda e: e.dma_start(out=lnxb[:], in_=bass.AP(I["lnx"].tensor, 512, [[0, 128], [1, 512]])), writes=[t_rp])
        S.dve(lambda e: e.tensor_scalar(out=omka[:], in0=rwp[:, 4, :], scalar1=-1.0, scalar2=1.0, op0=ALU.mult, op1=ALU.add),
              reads=[t_rp], writes=[t_rp])
        S.pool(lambda e: e.memset(gneps[:], 64e-5), writes=[t_rp])
        wr = sbt(st2, "wr", [128, 8, 1664], BF16)
        t_wr = Tok()
        with contextlib.ExitStack() as st3:
            wst2 = [sbt(st3, "wst2", [128, 8, 416]) for i in range(2)]
            wst2t = [Tok(), Tok()]
            for q4 in range(4):
                w_ = wst2[q4 % 2]
                S.dma(lambda e, w_=w_, q4=q4: e.dma_start(out=w_[:], in_=win_v[:, :, 1536 + q4 * 416:1536 + (q4 + 1) * 416]), writes=[wst2t[q4 % 2]])
                S.pool(lambda e, w_=w_, q4=q4: e.tensor_copy(out=wr[:, :, q4 * 416:(q4 + 1) * 416], in_=w_[:]), reads=[wst2t[q4 % 2]], writes=[t_wr])
        S.barrier()

        def T_(n=""):
            return Tok(n)

        pb = [sbt(st2, "pb", [128, 4, 129]) for x in range(3)]
        pbl = sbt(st2, "pbl", [128, 129])
        t_pb = T_()
        for x in range(3):
            S.pool(lambda e, x=x: e.memset(pb[x][:, :, 0:1], 0.0), writes=[t_pb])
        S.pool(lambda e: e.memset(pbl[:, 0:1], 0.0), writes=[t_pb])
        xm = [sbt(st2, "xm", [128, 4, 128]) for x in range(3)]
        xml = sbt(st2, "xml", [128, 128])
        t_xm = T_()
        twl = sbt(st2, "twl", [128, 128])
        sg_tok = sbt(st2, "sg_tok", [128, 512])
        aT = sbt(st2, "aT", [128, 4, 128])
        g_tok = sbt(st2, "g_tok", [128, 512])
        kk = sbt(st2, "kk", [128, 4, 128])
        sq4 = sbt(st2, "sq4", [128, 4, 128])
        kmod = sbt(st2, "kmod", [128, 4, 128])
        bb = sbt(st2, "bb", [128, 4, 128])
        rk = sbt(st2, "rk", [128, 4, 128])
        dtmp = rk
        bsum = sbt(st2, "bsum", [128, 8])
        ycen = sbt(st2, "ycen", [128, 8, 64])
        ysq = sbt(st2, "ysq", [128, 8, 64])
        gst = sbt(st2, "gst", [128, 8])
        gst2 = sbt(st2, "gst2", [128, 8])
        mixr = sbt(st2, "mixr", [128, 4, 128], BF16)
        st4 = contextlib.ExitStack()
        st2.enter_context(st4)
        Pin = sbt(st4, "Pin", [128, 4, 128])
        Pinv = sbt(st4, "Pinv", [128, 4, 128])
        Pex = sbt(st4, "Pex", [128, 4, 128])
        Phat = sbt(st4, "Phat", [128, 4, 128])
        PCl = sbt(st4, "PCl", [128, 4, 2])
        PC = sbt(st4, "PC", [128, 4, 2])
        AR = sbt(st4, "AR", [128, 4, 2, 2, 64])
        BtT = sbt(st4, "BtT", [128, 4, 128])
        AtT = sbt(st4, "AtT", [128, 4, 128])
        KtT = sbt(st4, "KtT", [128, 4, 128])
        BhT = sbt(st4, "BhT", [128, 4, 128])
        KhT = sbt(st4, "KhT", [128, 4, 128])
        Atok = sbt(st4, "Atok", [128, 512])
        Bhtok = sbt(st4, "Bhtok", [128, 512])
        Khtok = sbt(st4, "Khtok", [128, 512])
        Vtok = sbt(st4, "Vtok", [128, 512])
        NM = sbt(st4, "NM", [128, 8, 2, 64])
        AK = sbt(st4, "AK", [128, 8, 2, 64])
        Aj = [sbt(st4, "Aj", [128, 8, 64])] * 2
        Nj = [sbt(st4, "Nj", [128, 8, 64])] * 2
        Tj = [sbt(st4, "Tj", [128, 8, 64])] * 2
        Z = sbt(st4, "Z", [128, 8, 128])
        AV = sbt(st4, "AV", [128, 8, 128])
        McT = sbt(st4, "McT", [128, 4, 2, 64])
        dPC = sbt(st4, "dPC", [128, 4, 2, 64])
        RpT = sbt(st4, "RpT", [128, 4, 2, 64])
        Hs = sbt(st4, "Hs", [128, 4, 64])
        t_H = T_()
        S.pool(lambda e: e.memset(Hs[:], 0.0), writes=[t_H])
        (t_lora, t_sg, t_a, t_g, t_kk, t_km, t_b, t_rk, t_bs, t_P, t_AR, t_BK, t_BKh, t_tok, t_NM, t_AK, t_A0, t_Z, t_AV,
         t_Mc, t_Rp, t_y, t_mixr) = [T_() for i in range(23)]
        t_Aj = [T_()] * 2
        t_Nj = [T_()] * 2
        t_Tj = [T_()] * 2
        B = banks

        def v4(ap):
            return ap.rearrange("p q (c t) -> p q c t", c=2)

        for sbi in range(32):
            t0 = sbi * 128
            hg = t_h[t0 // 512]
            for x in range(3):
                bk = x
                for p in range(4):
                    for c in range(8):
                        S.pe(lambda e, x=x, p=p, c=c, bk=bk, t0=t0: e.matmul(
                            B[bk][:, p * 128:(p + 1) * 128], lhsT=wr[:, c, x * 512 + p * 128:x * 512 + (p + 1) * 128],
                            rhs=hT[:, c, t0:t0 + 128], start=(c == 0), stop=(c == 7)), reads=[t_wr, hg], writes=[bt[bk]])
                S.act(lambda e, x=x, bk=bk: e.activation(out=pb[x][:, :, 1:129], in_=B[bk][:].rearrange("p (q t) -> p q t", q=4), func=AF.Copy),
                      reads=[bt[bk], t_xm], writes=[t_pb])
            for c in range(8):
                S.pe(lambda e, c=c, t0=t0: e.matmul(B[3][:, 0:128], lhsT=wr[:, c, 1536:1664], rhs=hT[:, c, t0:t0 + 128],
                                                     start=(c == 0), stop=(c == 7)), reads=[t_wr, hg], writes=[bt[3]])
            S.act(lambda e: e.activation(out=pbl[:, 1:129], in_=B[3][:, 0:128], func=AF.Copy), reads=[bt[3], t_xm], writes=[t_pb])
            if sbi == 31:
                for x in range(3):
                    S.dma(lambda e, x=x: e.dma_start(
                        out=bass.AP(O["shp"].tensor, x * 512, [[1, 128], [128, 4], [1, 1]]), in_=pb[x][:, :, 128:129], allow_slow_non_contiguous=True), reads=[t_pb])
                S.dma(lambda e: e.dma_start(out=bass.AP(O["shp"].tensor, 1536, [[1, 128], [1, 1]]), in_=pbl[:, 128:129], allow_slow_non_contiguous=True), reads=[t_pb])
            for x in range(3):
                S.dve(lambda e, x=x: e.tensor_tensor(out=dtmp[:], in0=pb[x][:, :, 0:128], in1=pb[x][:, :, 1:129], op=ALU.subtract),
                      reads=[t_pb], writes=[t_xm, t_rk])
                S.dve(lambda e, x=x: e.tensor_tensor(out=dtmp[:], in0=dtmp[:], in1=bc(rwp[:, x, :].unsqueeze(2), [128, 4, 128]), op=ALU.mult),
                      reads=[t_xm, t_rp, t_rk], writes=[t_xm, t_rk])
                S.dve(lambda e, x=x: e.tensor_tensor(out=xm[x][:], in0=dtmp[:], in1=pb[x][:, :, 1:129], op=ALU.add),
                      reads=[t_xm, t_pb, t_rk], writes=[t_xm])
            S.dve(lambda e: e.tensor_tensor(out=xml[:], in0=pbl[:, 0:128], in1=pbl[:, 1:129], op=ALU.subtract), reads=[t_pb], writes=[t_lora])
            S.dve(lambda e: e.scalar_tensor_tensor(out=xml[:], in0=xml[:], scalar=mul_[:, 0:1], in1=pbl[:, 1:129], op0=ALU.mult, op1=ALU.add),
                  reads=[t_lora, t_pb, t_rp], writes=[t_lora])
            for x in range(3):
                S.pool(lambda e, x=x: e.tensor_copy(out=pb[x][:, :, 0:1], in_=pb[x][:, :, 128:129]), reads=[t_pb, t_xm], writes=[t_pb])
            S.pool(lambda e: e.tensor_copy(out=pbl[:, 0:1], in_=pbl[:, 128:129]), reads=[t_pb, t_lora], writes=[t_pb])
            S.act(lambda e: e.activation(out=twl[0:32, :], in_=xml[0:32, :], func=AF.Tanh), reads=[t_lora], writes=[t_sg])
            S.act(lambda e: e.activation(out=twl[64:128, :], in_=xml[64:128, :], func=AF.Sigmoid), reads=[t_lora], writes=[t_sg])
            S.pe(lambda e: e.matmul(B[4][:], lhsT=twl[0:32, :], rhs=lw3[0:32, :], start=True, stop=False), reads=[t_sg, t_rp], writes=[bt[4]])
            S.pe(lambda e: e.matmul(B[4][:], lhsT=ones[0:1, :], rhs=w0row[0:1, :], start=False, stop=True), reads=[t_const, t_rp], writes=[bt[4]])
            S.act(lambda e: e.activation(out=sg_tok[:], in_=B[4][:], func=AF.Sigmoid), reads=[bt[4]], writes=[t_sg])
            for p in range(4):
                S.pe(lambda e, p=p: e.matmul(B[5][:, p * 128:(p + 1) * 128], lhsT=lw3[32:64, p * 128:(p + 1) * 128], rhs=xml[32:64, :],
                                              start=True, stop=True), reads=[t_lora, t_rp], writes=[bt[5]])
            S.dve(lambda e: e.tensor_tensor(out=aT[:], in0=B[5][:].rearrange("p (q t) -> p q t", q=4),
                                            in1=bc(rwp[:, 6, :].unsqueeze(2), [128, 4, 128]), op=ALU.add), reads=[bt[5], t_rp], writes=[t_a])
            S.act(lambda e: e.activation(out=aT[:], in_=aT[:], func=AF.Sigmoid), reads=[t_a], writes=[t_a])
            S.pe(lambda e: e.matmul(B[6][:], lhsT=twl[64:128, :], rhs=lw3[64:128, :], start=True, stop=True), reads=[t_sg, t_rp], writes=[bt[6]])
            S.act(lambda e: e.activation(out=g_tok[:], in_=B[6][:], func=AF.Copy), reads=[bt[6], t_y], writes=[t_g])
            S.dve(lambda e: e.tensor_tensor(out=kk[:], in0=xm[1][:], in1=bc(rwp[:, 3, :].unsqueeze(2), [128, 4, 128]), op=ALU.mult),
                  reads=[t_xm, t_rp], writes=[t_kk])
            S.act(lambda e: e.activation(out=sq4[:], in_=kk[:], func=AF.Square), reads=[t_kk], writes=[t_kk])
            S.pe(lambda e: e.matmul(B[7][:], lhsT=blk2[:], rhs=sq4[:].rearrange("p q t -> p (q t)"), start=True, stop=True),
                 reads=[t_kk, t_const], writes=[bt[7]])
            S.act(lambda e: e.activation(out=sq4[:].rearrange("p q t -> p (q t)"), in_=B[7][:], func=AF.Sqrt), reads=[bt[7]], writes=[t_kk])
            S.dve(lambda e: e.tensor_scalar(out=sq4[:], in0=sq4[:], scalar1=1e-12, scalar2=None, op0=ALU.max), reads=[t_kk], writes=[t_kk])
            S.dve(lambda e: e.reciprocal(out=sq4[:], in_=sq4[:]), reads=[t_kk], writes=[t_kk])
            S.dve(lambda e: e.tensor_tensor(out=kk[:], in0=kk[:], in1=sq4[:], op=ALU.mult), reads=[t_kk], writes=[t_kk])
            S.dve(lambda e: e.tensor_tensor(out=kmod[:], in0=aT[:], in1=bc(rwp[:, 4, :].unsqueeze(2), [128, 4, 128]), op=ALU.mult),
                  reads=[t_a, t_rp], writes=[t_km])
            S.dve(lambda e: e.tensor_tensor(out=kmod[:], in0=kmod[:], in1=bc(omka[:].unsqueeze(2), [128, 4, 128]), op=ALU.add),
                  reads=[t_km, t_rp], writes=[t_km])
            S.dve(lambda e: e.tensor_tensor(out=kmod[:], in0=kmod[:], in1=xm[1][:], op=ALU.mult), reads=[t_km, t_xm], writes=[t_km])
            S.dve(lambda e: e.tensor_tensor(out=bb[:], in0=kk[:], in1=aT[:], op=ALU.mult), reads=[t_kk, t_a], writes=[t_b])
            S.pool(lambda e: e.tensor_tensor(out=rk[:], in0=xm[0][:], in1=kmod[:], op=ALU.mult), reads=[t_xm, t_km], writes=[t_rk])
            S.pool(lambda e: e.tensor_tensor(out=rk[:], in0=rk[:], in1=bc(rwp[:, 5, :].unsqueeze(2), [128, 4, 128]), op=ALU.mult),
                   reads=[t_rk, t_rp], writes=[t_rk])
            for h in (0, 2, 4, 6, 1, 3, 5, 7):
                p, hh = h // 2, h % 2
                fp = slice(hh * 64, hh * 64 + 64)
                S.pe(lambda e, p=p, fp=fp, h=h: e.matmul(B[6][:, 256 + h:256 + h + 1], lhsT=rk[fp, p, :], rhs=ones[fp, 0:1], start=True, stop=True),
                     reads=[t_rk, t_const], writes=[bt[6]])
            S.act(lambda e: e.activation(out=bsum[:], in_=B[6][:, 256:264], func=AF.Copy), reads=[bt[6], t_y], writes=[t_bs])
            for p in range(4):
                S.pe(lambda e, p=p: e.matmul(B[0][:, p * 128:(p + 1) * 128], lhsT=sg_tok[:, p * 128:(p + 1) * 128], rhs=tri[:, 0, :],
                                              start=True, stop=True), reads=[t_sg, t_rp], writes=[bt[0]])
            for p in range(4):
                S.pe(lambda e, p=p: e.matmul(B[1][:, p * 128:(p + 1) * 128], lhsT=sg_tok[:, p * 128:(p + 1) * 128], rhs=tri[:, 1, :],
                                              start=True, stop=True), reads=[t_sg, t_rp], writes=[bt[1]])
            lp = B[0][:].rearrange("p (q t) -> p q t", q=4)
            S.act(lambda e: e.activation(out=Pin[:], in_=lp, func=AF.Exp), reads=[bt[0]], writes=[t_P])
            S.act(lambda e: e.activation(out=Pinv[:], in_=lp, func=AF.Exp, scale=-1.0), reads=[bt[0]], writes=[t_P])
            S.act(lambda e: e.activation(out=Pex[:], in_=B[1][:].rearrange("p (q t) -> p q t", q=4), func=AF.Exp), reads=[bt[1]], writes=[t_P])
            lp4 = B[0][:].rearrange("p (q c t) -> p q c t", q=4, c=2)
            S.act(lambda e: e.activation(out=PCl[:], in_=lp4[:, :, :, 63], func=AF.Copy), reads=[bt[0]], writes=[t_P])
            S.act(lambda e: e.activation(out=v4(Phat[:]), in_=lp4, func=AF.Copy), reads=[bt[0]], writes=[t_P])
            S.dve(lambda e: e.tensor_tensor(out=v4(Phat[:]), in0=bc(PCl[:].unsqueeze(3), [128, 4, 2, 64]), in1=v4(Phat[:]), op=ALU.subtract),
                  reads=[t_P], writes=[t_P])
            S.act(lambda e: e.activation(out=Phat[:], in_=Phat[:], func=AF.Exp), reads=[t_P], writes=[t_P])
            S.act(lambda e: e.activation(out=PC[:], in_=PCl[:], func=AF.Exp), reads=[t_P], writes=[t_P])
            S.dve(lambda e: e.scalar_tensor_tensor(out=AR[:, :, :, 0, :], in0=v4(kk[:]), scalar=-1.0, in1=v4(Pex[:]), op0=ALU.mult, op1=ALU.mult),
                  reads=[t_kk, t_P], writes=[t_AR])
            S.dve(lambda e: e.tensor_tensor(out=AR[:, :, :, 1, :], in0=v4(xm[0][:]), in1=v4(Pin[:]), op=ALU.mult), reads=[t_xm, t_P], writes=[t_AR])
            S.pool(lambda e: e.tensor_tensor(out=BtT[:], in0=bb[:], in1=Pinv[:], op=ALU.mult), reads=[t_b, t_P], writes=[t_BK])
            S.pool(lambda e: e.tensor_tensor(out=KtT[:], in0=kmod[:], in1=Pinv[:], op=ALU.mult), reads=[t_km, t_P], writes=[t_BK])
            S.pool(lambda e: e.tensor_tensor(out=BhT[:], in0=bb[:], in1=Phat[:], op=ALU.mult), reads=[t_b, t_P], writes=[t_BKh])
            S.pool(lambda e: e.tensor_tensor(out=KhT[:], in0=kmod[:], in1=Phat[:], op=ALU.mult), reads=[t_km, t_P], writes=[t_BKh])
            S.pool(lambda e: e.tensor_copy(out=v4(AtT[:]), in_=AR[:, :, :, 0, :]), reads=[t_AR], writes=[t_BKh])
            for src_fn, dst, rd, bk in ((lambda p: AtT[:, p, :], Atok, [t_BKh], 2), (lambda p: BhT[:, p, :], Bhtok, [t_BKh], 3),
                                        (lambda p: KhT[:, p, :], Khtok, [t_BKh], 4), (lambda p: xm[2][:, p, :], Vtok, [t_xm], 5)):
                for p in range(4):
                    S.pe(lambda e, p=p, src_fn=src_fn, bk=bk: e.transpose(B[bk][:, p * 128:(p + 1) * 128], src_fn(p), ident[:]),
                         reads=rd + [t_const], writes=[bt[bk]])
                S.act(lambda e, dst=dst, bk=bk: e.activation(out=dst[:], in_=B[bk][:], func=AF.Copy), reads=[bt[bk], t_y, t_AV, t_Z], writes=[t_tok])
            for ch in range(2):
                tp = slice(ch * 64, ch * 64 + 64)
                for h in (0, 2, 4, 6, 1, 3, 5, 7):
                    p, hh = h // 2, h % 2
                    fp = slice(hh * 64, hh * 64 + 64)
                    csl = slice(ch * 64, ch * 64 + 64)
                    arr = AR[fp, p, ch, :, :].rearrange("p a t -> p (a t)")
                    S.pe(lambda e, tp=tp, fp=fp, p=p, h=h, csl=csl, arr=arr: e.matmul(
                        B[0][tp, (h % 4) * 128:(h % 4 + 1) * 128] if h < 4 else B[1][tp, (h % 4) * 128:(h % 4 + 1) * 128],
                        lhsT=BtT[fp, p, csl], rhs=arr, start=True, stop=True), reads=[t_BK, t_AR], writes=[bt[0 if h < 4 else 1]])
                    S.pe(lambda e, tp=tp, fp=fp, p=p, h=h, csl=csl, arr=arr: e.matmul(
                        B[2][tp, (h % 4) * 128:(h % 4 + 1) * 128] if h < 4 else B[3][tp, (h % 4) * 128:(h % 4 + 1) * 128],
                        lhsT=KtT[fp, p, csl], rhs=arr, start=True, stop=True), reads=[t_BK, t_AR], writes=[bt[2 if h < 4 else 3]])
                    S.pe(lambda e, tp=tp, fp=fp, p=p, h=h, csl=csl, ch=ch: e.matmul(
                        B[4][tp, h * 64:(h + 1) * 64], lhsT=AR[fp, p, ch, 0, :], rhs=BtT[fp, p, csl], start=True, stop=True),
                        reads=[t_BK, t_AR], writes=[bt[4]])
            mk1b = bc(mk1[:].unsqueeze(1), [128, 4, 2, 64])
            for hf in range(2):
                S.dve(lambda e, hf=hf: e.tensor_tensor(out=NM[:, hf * 4:(hf + 1) * 4, :, :], in0=B[hf][:].rearrange("p (h a t) -> p h a t", h=4, a=2),
                                                        in1=mk1b, op=ALU.mult), reads=[bt[hf], t_rp], writes=[t_NM])
                S.dve(lambda e, hf=hf: e.tensor_tensor(out=AK[:, hf * 4:(hf + 1) * 4, :, :], in0=B[2 + hf][:].rearrange("p (h a t) -> p h a t", h=4, a=2),
                                                        in1=mk1b, op=ALU.mult), reads=[bt[2 + hf], t_rp], writes=[t_AK])
            S.dve(lambda e: e.tensor_tensor(out=Aj[0][:], in0=B[4][:].rearrange("p (h t) -> p h t", h=8), in1=bc(mk3[:].unsqueeze(1), [128, 8, 64]), op=ALU.mult),
                  reads=[bt[4], t_rp], writes=[t_Aj[0]])
            S.pool(lambda e: e.tensor_copy(out=Nj[0][:], in_=NM[:, :, 0, :]), reads=[t_NM], writes=[t_Nj[0]])
            S.pool(lambda e: e.tensor_tensor(out=Tj[0][:], in0=NM[:, :, 0, :], in1=bc(id2[:].unsqueeze(1), [128, 8, 64]), op=ALU.add),
                   reads=[t_NM, t_rp], writes=[t_Tj[0]])
            for lv in range(1, 6):
                a_o = Aj[0]
                n_o = Nj[0]
                t_o = Tj[0]
                ta, tn, tt = t_Aj[0], t_Nj[0], t_Tj[0]
                for ch in range(2):
                    tp = slice(ch * 64, ch * 64 + 64)
                    for h in range(8):
                        S.pe(lambda e, tp=tp, h=h: e.matmul(B[5][tp, h * 64:(h + 1) * 64], lhsT=n_o[tp, h, :], rhs=a_o[tp, h, :],
                                                             start=True, stop=True), reads=[ta, tn], writes=[bt[5]])
                if lv < 5:
                    for ch in range(2):
                        tp = slice(ch * 64, ch * 64 + 64)
                        for h in range(8):
                            S.pe(lambda e, tp=tp, h=h: e.matmul(B[6][tp, h * 64:(h + 1) * 64], lhsT=a_o[tp, h, :], rhs=n_o[tp, h, :],
                                                                 start=True, stop=True), reads=[ta, tn], writes=[bt[6]])
                S.act(lambda e: e.activation(out=a_o[:].rearrange("p h t -> p (h t)"), in_=B[5][:], func=AF.Copy), reads=[bt[5]], writes=[ta])
                if lv < 5:
                    S.act(lambda e: e.activation(out=n_o[:].rearrange("p h t -> p (h t)"), in_=B[6][:], func=AF.Copy), reads=[bt[6]], writes=[tn])
                for ch in range(2):
                    tp = slice(ch * 64, ch * 64 + 64)
                    for h in range(8):
                        S.pe(lambda e, tp=tp, h=h: e.matmul(B[7][tp, h * 64:(h + 1) * 64], lhsT=a_o[tp, h, :], rhs=t_o[tp, h, :],
                                                             start=True, stop=True), reads=[ta, tt], writes=[bt[7]])
                S.dve(lambda e: e.tensor_tensor(out=t_o[:], in0=B[7][:].rearrange("p (h t) -> p h t", h=8), in1=t_o[:], op=ALU.add),
                      reads=[bt[7], tt], writes=[tt])
            TT = Tj[5 % 2]
            t_TT = t_Tj[5 % 2]
            for ch in range(2):
                tp = slice(ch * 64, ch * 64 + 64)
                for h in range(8):
                    S.pe(lambda e, tp=tp, h=h: e.matmul(B[4][tp, h * 64:(h + 1) * 64], lhsT=AK[tp, h, 0, :], rhs=Vtok[tp, h * 64:(h + 1) * 64],
                                                         start=True, stop=True), reads=[t_AK, t_tok], writes=[bt[4]])
            S.act(lambda e: e.activation(out=Z[:, :, 64:128], in_=B[4][:].rearrange("p (h t) -> p h t", h=8), func=AF.Copy), reads=[bt[4]], writes=[t_Z])
            S.pool(lambda e: e.tensor_copy(out=Z[:, :, 0:64], in_=Atok[:].rearrange("p (h t) -> p h t", h=8)), reads=[t_tok], writes=[t_Z])
            for ch in range(2):
                tp = slice(ch * 64, ch * 64 + 64)
                for h in range(8):
                    S.pe(lambda e, tp=tp, h=h: e.matmul(B[h // 4][tp, (h % 4) * 128:(h % 4 + 1) * 128], lhsT=TT[tp, h, :], rhs=Z[tp, h, :],
                                                         start=True, stop=True), reads=[t_TT, t_Z], writes=[bt[h // 4]])
            for hf in range(2):
                S.act(lambda e, hf=hf: e.activation(out=AV[:, hf * 4:(hf + 1) * 4, :].rearrange("p h t -> p (h t)"), in_=B[hf][:], func=AF.Copy),
                      reads=[bt[hf]], writes=[t_AV])
            for ch in range(2):
                tp = slice(ch * 64, ch * 64 + 64)
                for h in range(8):
                    p, hh = h // 2, h % 2
                    fp = slice(hh * 64, hh * 64 + 64)
                    col = (p * 2 + ch) * 64
                    S.pe(lambda e, tp=tp, fp=fp, h=h, col=col: e.matmul(B[2][fp, col:col + 64], lhsT=AV[tp, h, 0:64], rhs=Bhtok[tp, h * 64:(h + 1) * 64],
                                                                         start=True, stop=True), reads=[t_AV, t_tok], writes=[bt[2]])
                    S.pe(lambda e, tp=tp, fp=fp, h=h, col=col: e.matmul(B[3][fp, col:col + 64], lhsT=AV[tp, h, 0:64], rhs=NM[tp, h, 1, :],
                                                                         start=True, stop=True), reads=[t_AV, t_NM], writes=[bt[3]])
            S.dve(lambda e: e.tensor_tensor(out=dPC[:], in0=bc(PC[:].unsqueeze(3), [128, 4, 2, 64]),
                                            in1=bc(id2[:].unsqueeze(1).unsqueeze(1), [128, 4, 2, 64]), op=ALU.mult), reads=[t_P, t_rp], writes=[t_Mc])
            S.dve(lambda e: e.tensor_tensor(out=McT[:], in0=B[2][:].rearrange("p (q c t) -> p q c t", q=4, c=2), in1=dPC[:], op=ALU.add),
                  reads=[bt[2], t_Mc], writes=[t_Mc])
            S.dve(lambda e: e.tensor_tensor(out=RpT[:], in0=B[3][:].rearrange("p (q c t) -> p q c t", q=4, c=2), in1=AR[:, :, :, 1, :], op=ALU.add),
                  reads=[bt[3], t_AR], writes=[t_Rp])
            for ch in range(2):
                tp = slice(ch * 64, ch * 64 + 64)
                for h in range(8):
                    p, hh = h // 2, h % 2
                    fp = slice(hh * 64, hh * 64 + 64)
                    S.pe(lambda e, tp=tp, h=h: e.matmul(B[6][tp, h * 64:(h + 1) * 64], lhsT=NM[tp, h, 1, :], rhs=AV[tp, h, 64:128], start=True, stop=False),
                         reads=[t_NM, t_AV], writes=[bt[6]])
                    S.pe(lambda e, tp=tp, h=h: e.matmul(B[6][tp, h * 64:(h + 1) * 64], lhsT=AK[tp, h, 1, :], rhs=Vtok[tp, h * 64:(h + 1) * 64], start=False, stop=False),
                         reads=[t_AK, t_tok], writes=[bt[6]])
                    S.pe(lambda e, tp=tp, fp=fp, p=p, h=h, ch=ch: e.matmul(B[6][tp, h * 64:(h + 1) * 64], lhsT=RpT[fp, p, ch, :], rhs=Hs[fp, p, :], start=False, stop=True),
                         reads=[t_Rp, t_H], writes=[bt[6]])
                for h in range(8):
                    p, hh = h // 2, h % 2
                    fp = slice(hh * 64, hh * 64 + 64)
                    S.pe(lambda e, tp=tp, fp=fp, p=p, h=h: e.matmul(B[5][fp, p * 64:(p + 1) * 64], lhsT=Bhtok[tp, h * 64:(h + 1) * 64], rhs=AV[tp, h, 64:128], start=True, stop=False),
                         reads=[t_tok, t_AV], writes=[bt[5]])
                    S.pe(lambda e, tp=tp, fp=fp, p=p, h=h: e.matmul(B[5][fp, p * 64:(p + 1) * 64], lhsT=Khtok[tp, h * 64:(h + 1) * 64], rhs=Vtok[tp, h * 64:(h + 1) * 64], start=False, stop=False),
                         reads=[t_tok], writes=[bt[5]])
                    S.pe(lambda e, fp=fp, p=p, ch=ch: e.matmul(B[5][fp, p * 64:(p + 1) * 64], lhsT=McT[fp, p, ch, :], rhs=Hs[fp, p, :], start=False, stop=True),
                         reads=[t_Mc, t_H], writes=[bt[5]])
                S.act(lambda e: e.activation(out=Hs[:].rearrange("p q v -> p (q v)"), in_=B[5][:, 0:256], func=AF.Copy), reads=[bt[5]], writes=[t_H])
            yps = B[6][:].rearrange("p (h v) -> p h v", h=8)
            S.dve(lambda e: e.tensor_reduce(out=gst[:], in_=yps, axis=AX.X, op=ALU.add), reads=[bt[6]], writes=[t_y])
            S.dve(lambda e: e.tensor_scalar(out=gst[:], in0=gst[:], scalar1=1.0 / 64, scalar2=None, op0=ALU.mult), reads=[t_y], writes=[t_y])
            S.dve(lambda e: e.tensor_tensor(out=ycen[:], in0=yps, in1=bc(gst[:].unsqueeze(2), [128, 8, 64]), op=ALU.subtract), reads=[t_y, bt[6]], writes=[t_y])
            S.act(lambda e: e.activation(out=ysq[:], in_=ycen[:], func=AF.Square), reads=[t_y], writes=[t_y])
            S.dve(lambda e: e.tensor_reduce(out=gst2[:], in_=ysq[:], axis=AX.X, op=ALU.add), reads=[t_y], writes=[t_y])
            S.act(lambda e: e.activation(out=gst2[:], in_=gst2[:], func=AF.Sqrt, scale=1.0 / 64, bias=gneps[:]), reads=[t_y, t_rp], writes=[t_y])
            S.dve(lambda e: e.reciprocal(out=gst2[:], in_=gst2[:]), reads=[t_y], writes=[t_y])
            S.dve(lambda e: e.tensor_tensor(out=ycen[:], in0=ycen[:], in1=bc(gst2[:].unsqueeze(2), [128, 8, 64]), op=ALU.mult), reads=[t_y], writes=[t_y])
            yc2 = ycen[:].rearrange("p h v -> p (h v)")
            S.dve(lambda e: e.tensor_tensor(out=yc2, in0=yc2, in1=lnxg[:], op=ALU.mult), reads=[t_y, t_rp], writes=[t_y])
            S.dve(lambda e: e.tensor_tensor(out=yc2, in0=yc2, in1=lnxb[:], op=ALU.add), reads=[t_y, t_rp], writes=[t_y])
            S.pool(lambda e: e.tensor_tensor(out=ysq[:], in0=Vtok[:].rearrange("p (h v) -> p h v", h=8), in1=bc(bsum[:].unsqueeze(2), [128, 8, 64]), op=ALU.mult),
                   reads=[t_tok, t_bs, t_y], writes=[t_y])
            S.dve(lambda e: e.tensor_tensor(out=ycen[:], in0=ycen[:], in1=ysq[:], op=ALU.add), reads=[t_y], writes=[t_y])
            S.dve(lambda e: e.tensor_tensor(out=yc2, in0=yc2, in1=g_tok[:], op=ALU.mult), reads=[t_y, t_g], writes=[t_y])
            if debug:
                S.dma(lambda e, t0=t0: e.dma_start(out=O["dbg_yr"][t0:t0 + 128, :], in_=yc2), reads=[t_y])
            for p in range(4):
                S.pe(lambda e, p=p: e.transpose(B[7][:, p * 128:(p + 1) * 128], ycen[:, 2 * p:2 * p + 2, :].rearrange("p h v -> p (h v)"), ident[:]),
                     reads=[t_y, t_const], writes=[bt[7]])
            S.act(lambda e: e.activation(out=mixr[:].rearrange("p q t -> p (q t)"), in_=B[7][:], func=AF.Copy), reads=[bt[7]], writes=[t_mixr])
            S.dma(lambda e, t0=t0: e.dma_start(out=mixT_d[4:8, :, t0:t0 + 128].rearrange("q p t -> p q t"), in_=mixr[:]), reads=[t_mixr])
            ck(4.0 + 0.01 * (sbi + 1))
        S.dma(lambda e: e.dma_start(out=O["wkvp"], in_=Hs[:]), reads=[t_H])

        S.barrier()
        st4.close()
        ck(4.95)
        NSS = 64
        hs_perm = lambda c: hT[:, c, T:T + NS].rearrange("p (b t) -> p t b", t=4)
        for x in range(3):
            S.dma(lambda e, x=x: e.dma_start(out=pb[x][:, :, 0:16], in_=I["shT"][:, x, :, :]), writes=[t_pb])
        S.dma(lambda e: e.dma_start(out=pbl[:, 0:16], in_=I["shTl"]), writes=[t_pb])
        for x in range(3):
            bk = x
            for p in range(4):
                for c in range(8):
                    S.pe(lambda e, x=x, p=p, c=c, bk=bk: e.matmul(
                        B[bk][:, p * 128:p * 128 + NSS], lhsT=wr[:, c, x * 512 + p * 128:x * 512 + (p + 1) * 128],
                        rhs=hs_perm(c), start=(c == 0), stop=(c == 7)), reads=[t_wr, t_h[8]], writes=[bt[bk]])
            S.act(lambda e, x=x, bk=bk: e.activation(out=pb[x][:, :, 16:80], in_=B[bk][:].rearrange("p (q t) -> p q t", q=4)[:, :, 0:NSS], func=AF.Copy),
                  reads=[bt[bk], t_xm], writes=[t_pb])
        for c in range(8):
            S.pe(lambda e, c=c: e.matmul(B[3][:, 0:NSS], lhsT=wr[:, c, 1536:1664], rhs=hs_perm(c), start=(c == 0), stop=(c == 7)),
                 reads=[t_wr, t_h[8]], writes=[bt[3]])
        S.act(lambda e: e.activation(out=pbl[:, 16:80], in_=B[3][:, 0:NSS], func=AF.Copy), reads=[bt[3], t_xm], writes=[t_pb])
        shrow = sbt(st2, "shrow", [16, 1664])
        t_shrow = T_()
        hl = sbt(st2, "hl", [128, 8, 16], BF16)
        t_hl = T_()
        S.pool(lambda e: e.tensor_copy(out=hl[:], in_=hT[:, :, T + 3:T + NS:4]), reads=[t_h[8]], writes=[t_hl])
        for q4 in range(4):
            for c in range(8):
                S.pe(lambda e, q4=q4, c=c: e.matmul(B[4][0:16, 0:416], lhsT=hl[:, c, :], rhs=wr[:, c, q4 * 416:(q4 + 1) * 416],
                                                    start=(c == 0), stop=(c == 7)), reads=[t_wr, t_hl], writes=[bt[4]])
            S.act(lambda e, q4=q4: e.activation(out=shrow[:, q4 * 416:(q4 + 1) * 416], in_=B[4][0:16, 0:416], func=AF.Copy), reads=[bt[4]], writes=[t_shrow])
        S.dma(lambda e: e.dma_start(out=O["shs"], in_=shrow[:]), reads=[t_shrow])
        n_ = NSS
        for x in range(3):
            S.dve(lambda e, x=x: e.tensor_tensor(out=dtmp[:, :, 0:n_], in0=pb[x][:, :, 0:n_], in1=pb[x][:, :, 16:16 + n_], op=ALU.subtract),
                  reads=[t_pb], writes=[t_xm, t_rk])
            S.dve(lambda e, x=x: e.tensor_tensor(out=dtmp[:, :, 0:n_], in0=dtmp[:, :, 0:n_], in1=bc(rwp[:, x, :].unsqueeze(2), [128, 4, n_]), op=ALU.mult),
                  reads=[t_xm, t_rp, t_rk], writes=[t_xm, t_rk])
            S.dve(lambda e, x=x: e.tensor_tensor(out=xm[x][:, :, 0:n_], in0=dtmp[:, :, 0:n_], in1=pb[x][:, :, 16:16 + n_], op=ALU.add),
                  reads=[t_xm, t_pb, t_rk], writes=[t_xm])
        S.dve(lambda e: e.tensor_tensor(out=xml[:, 0:n_], in0=pbl[:, 0:n_], in1=pbl[:, 16:16 + n_], op=ALU.subtract), reads=[t_pb], writes=[t_lora])
        S.dve(lambda e: e.scalar_tensor_tensor(out=xml[:, 0:n_], in0=xml[:, 0:n_], scalar=mul_[:, 0:1], in1=pbl[:, 16:16 + n_], op0=ALU.mult, op1=ALU.add),
              reads=[t_lora, t_pb, t_rp], writes=[t_lora])
        S.act(lambda e: e.activation(out=twl[0:32, 0:n_], in_=xml[0:32, 0:n_], func=AF.Tanh), reads=[t_lora], writes=[t_sg])
        S.act(lambda e: e.activation(out=twl[64:128, 0:n_], in_=xml[64:128, 0:n_], func=AF.Sigmoid), reads=[t_lora], writes=[t_sg])
        S.pe(lambda e: e.matmul(B[4][0:n_, :], lhsT=twl[0:32, 0:n_], rhs=lw3[0:32, :], start=True, stop=False), reads=[t_sg, t_rp], writes=[bt[4]])
        S.pe(lambda e: e.matmul(B[4][0:n_, :], lhsT=ones[0:1, 0:n_], rhs=w0row[0:1, :], start=False, stop=True), reads=[t_const, t_rp], writes=[bt[4]])
        S.act(lambda e: e.activation(out=sg_tok[0:n_, :], in_=B[4][0:n_, :], func=AF.Sigmoid), reads=[bt[4]], writes=[t_sg])
        for p in range(4):
            S.pe(lambda e, p=p: e.matmul(B[5][:, p * 128:p * 128 + n_], lhsT=lw3[32:64, p * 128:(p + 1) * 128], rhs=xml[32:64, 0:n_],
                                          start=True, stop=True), reads=[t_lora, t_rp], writes=[bt[5]])
        S.dve(lambda e: e.tensor_tensor(out=aT[:, :, 0:n_], in0=B[5][:].rearrange("p (q t) -> p q t", q=4)[:, :, 0:n_],
                                        in1=bc(rwp[:, 6, :].unsqueeze(2), [128, 4, n_]), op=ALU.add), reads=[bt[5], t_rp], writes=[t_a])
        S.act(lambda e: e.activation(out=aT[:, :, 0:n_], in_=aT[:, :, 0:n_], func=AF.Sigmoid), reads=[t_a], writes=[t_a])
        S.pe(lambda e: e.matmul(B[6][0:n_, :], lhsT=twl[64:128, 0:n_], rhs=lw3[64:128, :], start=True, stop=True), reads=[t_sg, t_rp], writes=[bt[6]])
        S.act(lambda e: e.activation(out=g_tok[0:n_, :], in_=B[6][0:n_, :], func=AF.Copy), reads=[bt[6], t_y], writes=[t_g])
        S.dve(lambda e: e.tensor_tensor(out=kk[:, :, 0:n_], in0=xm[1][:, :, 0:n_], in1=bc(rwp[:, 3, :].unsqueeze(2), [128, 4, n_]), op=ALU.mult),
              reads=[t_xm, t_rp], writes=[t_kk])
        S.act(lambda e: e.activation(out=sq4[:, :, 0:n_], in_=kk[:, :, 0:n_], func=AF.Square), reads=[t_kk], writes=[t_kk])
        for p in range(4):
            S.pe(lambda e, p=p: e.matmul(B[7][:, p * 128:p * 128 + n_], lhsT=blk2[:], rhs=sq4[:, p, 0:n_], start=True, stop=True),
                 reads=[t_kk, t_const], writes=[bt[7]])
        S.act(lambda e: e.activation(out=sq4[:, :, 0:n_], in_=B[7][:].rearrange("p (q t) -> p q t", q=4)[:, :, 0:n_], func=AF.Sqrt), reads=[bt[7]], writes=[t_kk])
        S.dve(lambda e: e.tensor_scalar(out=sq4[:, :, 0:n_], in0=sq4[:, :, 0:n_], scalar1=1e-12, scalar2=None, op0=ALU.max), reads=[t_kk], writes=[t_kk])
        S.dve(lambda e: e.reciprocal(out=sq4[:, :, 0:n_], in_=sq4[:, :, 0:n_]), reads=[t_kk], writes=[t_kk])
        S.dve(lambda e: e.tensor_tensor(out=kk[:, :, 0:n_], in0=kk[:, :, 0:n_], in1=sq4[:, :, 0:n_], op=ALU.mult), reads=[t_kk], writes=[t_kk])
        S.dve(lambda e: e.tensor_tensor(out=kmod[:, :, 0:n_], in0=aT[:, :, 0:n_], in1=bc(rwp[:, 4, :].unsqueeze(2), [128, 4, n_]), op=ALU.mult),
              reads=[t_a, t_rp], writes=[t_km])
        S.dve(lambda e: e.tensor_tensor(out=kmod[:, :, 0:n_], in0=kmod[:, :, 0:n_], in1=bc(omka[:].unsqueeze(2), [128, 4, n_]), op=ALU.add),
              reads=[t_km, t_rp], writes=[t_km])
        S.dve(lambda e: e.tensor_tensor(out=kmod[:, :, 0:n_], in0=kmod[:, :, 0:n_], in1=xm[1][:, :, 0:n_], op=ALU.mult), reads=[t_km, t_xm], writes=[t_km])
        S.dve(lambda e: e.tensor_tensor(out=bb[:, :, 0:n_], in0=kk[:, :, 0:n_], in1=aT[:, :, 0:n_], op=ALU.mult), reads=[t_kk, t_a], writes=[t_b])
        S.pool(lambda e: e.tensor_tensor(out=rk[:, :, 0:n_], in0=xm[0][:, :, 0:n_], in1=kmod[:, :, 0:n_], op=ALU.mult), reads=[t_xm, t_km], writes=[t_rk])
        S.pool(lambda e: e.tensor_tensor(out=rk[:, :, 0:n_], in0=rk[:, :, 0:n_], in1=bc(rwp[:, 5, :].unsqueeze(2), [128, 4, n_]), op=ALU.mult),
               reads=[t_rk, t_rp], writes=[t_rk])
        for h in range(8):
            p, hh = h // 2, h % 2
            fp = slice(hh * 64, hh * 64 + 64)
            S.pe(lambda e, p=p, fp=fp, h=h: e.matmul(B[6][0:n_, 256 + h:256 + h + 1], lhsT=rk[fp, p, 0:n_], rhs=ones[fp, 0:1], start=True, stop=True),
                 reads=[t_rk, t_const], writes=[bt[6]])
        S.act(lambda e: e.activation(out=bsum[0:n_, :], in_=B[6][0:n_, 256:264], func=AF.Copy), reads=[bt[6], t_y], writes=[t_bs])
        tok6 = sbt(st2, "tok6", [64, 6, 512])
        t_tok6 = T_()
        S.act(lambda e: e.activation(out=tok6[:, 1, :], in_=sg_tok[0:n_, :], func=AF.Exp, scale=NEG), reads=[t_sg], writes=[t_tok6])
        for xi, (srcT, rd) in enumerate(((xm[0], t_xm), (None, None), (kmod, t_km), (xm[2], t_xm), (kk, t_kk), (bb, t_b))):
            if srcT is None:
                continue
            bk = 2 + xi % 2
            for p in range(4):
                S.pe(lambda e, p=p, srcT=srcT, bk=bk: e.transpose(B[bk][0:n_, p * 128:(p + 1) * 128], srcT[:, p, 0:n_], ident[:]),
                     reads=[rd, t_const], writes=[bt[bk]])
            S.act(lambda e, xi=xi, bk=bk: e.activation(out=tok6[:, xi, :], in_=B[bk][0:n_, :], func=AF.Copy), reads=[bt[bk]], writes=[t_tok6])
        t_rwsd = T_()
        t_rwsd_l = [T_() for i in range(24)]
        for t in range(4):
            for xi in range(6):
                S.dma(lambda e, t=t, xi=xi: e.dma_start(out=rws_d[:, :, t, xi, :], in_=tok6[16 * t:16 * t + 16, xi, :].rearrange("p (h c) -> p h c", h=8)),
                      reads=[t_tok6], writes=[t_rwsd_l[t * 6 + xi]])
        X6 = sbt(st2, "X6", [128, 4, 6, 64])
        St = sbt(st2, "St", [128, 64, 64])
        tmpS = sbt(st2, "tmpS", [128, 64, 64])
        sp_ = sbt(st2, "sp", [128, 64])
        ys = sbt(st2, "ys", [128, 4, 64])
        t_X6, t_St, t_tmpS, t_sp, t_ys = [T_() for i in range(5)]
        S.dma(lambda e: e.dma_start(out=X6[:].rearrange("p t x c -> p (t x c)"), in_=rws_d.rearrange("b h t x c -> (b h) (t x c)")), reads=t_rwsd_l, writes=[t_X6])
        S.dma(lambda e: e.dma_start(out=St[:].rearrange("p v k -> p (v k)"), in_=I["swkv"]), writes=[t_St])
        for t in range(4):
            def bk_(xi, t=t):
                return bc(X6[:, t, xi, :].unsqueeze(1), [128, 64, 64])
            def bv_(ap):
                return bc(ap.unsqueeze(2), [128, 64, 64])
            S.dve(lambda e, t=t: e.tensor_tensor(out=tmpS[:], in0=St[:], in1=bk_(4, t), op=ALU.mult), reads=[t_St, t_X6], writes=[t_tmpS])
            S.dve(lambda e: e.tensor_reduce(out=sp_[:], in_=tmpS[:], axis=AX.X, op=ALU.add), reads=[t_tmpS], writes=[t_sp])
            S.dve(lambda e, t=t: e.tensor_tensor(out=St[:], in0=St[:], in1=bk_(1, t), op=ALU.mult), reads=[t_St, t_X6, t_tmpS], writes=[t_St])
            S.pool(lambda e, t=t: e.tensor_tensor(out=tmpS[:], in0=bv_(sp_[:]), in1=bk_(5, t), op=ALU.mult), reads=[t_sp, t_X6], writes=[t_tmpS])
            S.dve(lambda e: e.tensor_tensor(out=St[:], in0=St[:], in1=tmpS[:], op=ALU.subtract), reads=[t_St, t_tmpS], writes=[t_St])
            S.pool(lambda e, t=t: e.tensor_tensor(out=tmpS[:], in0=bv_(X6[:, t, 3, :]), in1=bk_(2, t), op=ALU.mult), reads=[t_X6, t_St], writes=[t_tmpS])
            S.dve(lambda e: e.tensor_tensor(out=St[:], in0=St[:], in1=tmpS[:], op=ALU.add), reads=[t_St, t_tmpS], writes=[t_St])
            S.dve(lambda e, t=t: e.tensor_tensor(out=tmpS[:], in0=St[:], in1=bk_(0, t), op=ALU.mult), reads=[t_St, t_X6], writes=[t_tmpS])
            S.dve(lambda e, t=t: e.tensor_reduce(out=ys[:, t, :], in_=tmpS[:], axis=AX.X, op=ALU.add), reads=[t_tmpS], writes=[t_ys])
        S.dma(lambda e: e.dma_start(out=O["wkvs"], in_=St[:].rearrange("p v k -> p (v k)")), reads=[t_St])
        t_ysd = T_()
        S.dma(lambda e: e.dma_start(out=ys_d.rearrange("b h t v -> (b h) (t v)"), in_=ys[:].rearrange("p t v -> p (t v)")), reads=[t_ys], writes=[t_ysd])
        ysr = sbt(st2, "ysr", [64, 8, 64])
        t_ysr = T_()
        for t in range(4):
            S.dma(lambda e, t=t: e.dma_start(out=ysr[16 * t:16 * t + 16, :, :], in_=ys_d[:, :, t, :]), reads=[t_ysd], writes=[t_ysr])
        yps_s = ysr[:]
        Y0 = slice(0, 64)
        S.dve(lambda e: e.tensor_reduce(out=gst[Y0], in_=yps_s, axis=AX.X, op=ALU.add), reads=[t_ysr], writes=[t_y])
        S.dve(lambda e: e.tensor_scalar(out=gst[Y0], in0=gst[Y0], scalar1=1.0 / 64, scalar2=None, op0=ALU.mult), reads=[t_y], writes=[t_y])
        S.dve(lambda e: e.tensor_tensor(out=ycen[Y0], in0=yps_s, in1=bc(gst[Y0].unsqueeze(2), [64, 8, 64]), op=ALU.subtract), reads=[t_y, t_ysr], writes=[t_y])
        S.act(lambda e: e.activation(out=ysq[Y0], in_=ycen[Y0], func=AF.Square), reads=[t_y], writes=[t_y])
        S.dve(lambda e: e.tensor_reduce(out=gst2[Y0], in_=ysq[Y0], axis=AX.X, op=ALU.add), reads=[t_y], writes=[t_y])
        S.act(lambda e: e.activation(out=gst2[Y0], in_=gst2[Y0], func=AF.Sqrt, scale=1.0 / 64, bias=gneps[Y0]), reads=[t_y, t_rp], writes=[t_y])
        S.dve(lambda e: e.reciprocal(out=gst2[Y0], in_=gst2[Y0]), reads=[t_y], writes=[t_y])
        S.dve(lambda e: e.tensor_tensor(out=ycen[Y0], in0=ycen[Y0], in1=bc(gst2[Y0].unsqueeze(2), [64, 8, 64]), op=ALU.mult), reads=[t_y], writes=[t_y])
        yc2_s = ycen[Y0].rearrange("p h v -> p (h v)")
        S.dve(lambda e: e.tensor_tensor(out=yc2_s, in0=yc2_s, in1=lnxg[Y0], op=ALU.mult), reads=[t_y, t_rp], writes=[t_y])
        S.dve(lambda e: e.tensor_tensor(out=yc2_s, in0=yc2_s, in1=lnxb[Y0], op=ALU.add), reads=[t_y, t_rp], writes=[t_y])
        S.pool(lambda e: e.tensor_tensor(out=ysq[Y0], in0=tok6[:, 3, :].rearrange("p (h v) -> p h v", h=8), in1=bc(bsum[Y0].unsqueeze(2), [64, 8, 64]), op=ALU.mult),
               reads=[t_tok6, t_bs, t_y], writes=[t_y])
        S.dve(lambda e: e.tensor_tensor(out=ycen[Y0], in0=ycen[Y0], in1=ysq[Y0], op=ALU.add), reads=[t_y], writes=[t_y])
        S.dve(lambda e: e.tensor_tensor(out=yc2_s, in0=yc2_s, in1=g_tok[Y0], op=ALU.mult), reads=[t_y, t_g], writes=[t_y])
        for p in range(4):
            S.pe(lambda e, p=p: e.transpose(B[7][:, p * 128:p * 128 + 64], ycen[Y0, 2 * p:2 * p + 2, :].rearrange("p h v -> p (h v)"), ident[0:64, 0:64]),
                 reads=[t_y, t_const], writes=[bt[7]])
        S.act(lambda e: e.activation(out=mixr[:, :, 0:64].rearrange("p q (b t) -> p q t b", t=4),
                                     in_=B[7][:].rearrange("p (q x) -> p q x", q=4)[:, :, 0:64].rearrange("p q (t b) -> p q t b", t=4), func=AF.Copy),
              reads=[bt[7]], writes=[t_mixr])
        S.dma(lambda e: e.dma_start(out=mixT_d[4:8, :, T:T + NS].rearrange("q p t -> p q t"), in_=mixr[:, :, 0:64]), reads=[t_mixr])


    S.barrier()
    with contextlib.ExitStack() as st2, contextlib.suppress(_Stop):
        ck(5.0)
        wq3 = sbt(st2, "wq3", [128, 8, 1536], BF16)
        t_wq3 = Tok()
        with contextlib.ExitStack() as st3:
            wst3 = [sbt(st3, "wst3", [128, 8, 512]) for i in range(2)]
            wst3t = [Tok(), Tok()]
            for q3 in range(3):
                w_ = wst3[q3 % 2]
                S.dma(lambda e, w_=w_, q3=q3: e.dma_start(out=w_[:], in_=win_v[:, :, q3 * 512:(q3 + 1) * 512]), writes=[wst3t[q3 % 2]])
                S.pool(lambda e, w_=w_, q3=q3: e.tensor_copy(out=wq3[:, :, q3 * 512:(q3 + 1) * 512], in_=w_[:]), reads=[wst3t[q3 % 2]], writes=[t_wq3])
        S.barrier()
        B = banks
        NQ = 64
        hsp = sbt(st2, "hsp", [128, 8, 64], BF16)
        t_hsp = Tok()
        S.pool(lambda e: e.tensor_copy(out=hsp[:].rearrange("p c (s b) -> p c s b", s=4), in_=hT[:, :, T:T + NS].rearrange("p c (b s) -> p c s b", s=4)),
               reads=[t_h[8]], writes=[t_hsp])
        hs_sb = lambda c: hsp[:, c, :]
        qkvs = sbt(st2, "qkvs", [64, 3, 8, 64])
        sqs = sbt(st2, "sqs", [64, 8, 64])
        ssn = sbt(st2, "ssn", [64, 8])
        gqk = sbt(st2, "gqk", [64, 2, 64])
        t_qkv, t_sqs, t_gqk = Tok(), Tok(), Tok()
        for wi in range(2):
            S.dma(lambda e, wi=wi: e.dma_start(out=gqk[:, wi, :], in_=bass.AP(I["qkrow"].tensor, wi * 64, [[0, 64], [1, 64]])), writes=[t_gqk])
        for wi in range(3):
            for c in range(8):
                S.pe(lambda e, wi=wi, c=c: e.matmul(B[wi][0:NQ, :], lhsT=hs_sb(c), rhs=wq3[:, c, wi * 512:(wi + 1) * 512], start=(c == 0), stop=(c == 7)),
                     reads=[t_wq3, t_hsp], writes=[bt[wi]])
            pv = B[wi][0:NQ, :].rearrange("p (h c) -> p h c", h=8)
            if wi == 2:
                S.act(lambda e, pv=pv: e.activation(out=qkvs[:, 2, :, :], in_=pv, func=AF.Copy), reads=[bt[wi]], writes=[t_qkv])
            else:
                S.act(lambda e, pv=pv: e.activation(out=sqs[:], in_=pv, func=AF.Square), reads=[bt[wi]], writes=[t_sqs])
                S.dve(lambda e: e.tensor_reduce(out=ssn[:], in_=sqs[:], axis=AX.X, op=ALU.add), reads=[t_sqs], writes=[t_sqs])
                S.act(lambda e: e.activation(out=ssn[:], in_=ssn[:], func=AF.Sqrt, scale=1.0 / 64, bias=epsc[0:64]), reads=[t_sqs, t_const], writes=[t_sqs])
                S.dve(lambda e: e.reciprocal(out=ssn[:], in_=ssn[:]), reads=[t_sqs], writes=[t_sqs])
                S.dve(lambda e, pv=pv, wi=wi: e.tensor_tensor(out=qkvs[:, wi, :, :], in0=pv, in1=bc(ssn[:].unsqueeze(2), [64, 8, 64]), op=ALU.mult),
                      reads=[bt[wi], t_sqs], writes=[t_qkv])
                S.dve(lambda e, wi=wi: e.tensor_tensor(out=qkvs[:, wi, :, :], in0=qkvs[:, wi, :, :], in1=bc(gqk[:, wi, :].unsqueeze(1), [64, 8, 64]), op=ALU.mult),
                      reads=[t_qkv, t_gqk], writes=[t_qkv])
        ck(5.1)
        t_rec = Tok()
        for wi, oname, recd, cname in ((1, "kws", reck_d, "ck_s"), (2, "vws", recv_d, "cv_s")):
            for s_ in range(4):
                S.dma(lambda e, wi=wi, oname=oname, s_=s_: e.dma_start(out=O[oname].rearrange("(b s) c -> s b c", s=4)[s_],
                                                                        in_=qkvs[16 * s_:16 * s_ + 16, wi, :, :].rearrange("p h c -> p (h c)")), reads=[t_qkv])
                S.dma(lambda e, wi=wi, recd=recd, s_=s_: e.dma_start(out=recd[:, 4 + s_, :], in_=qkvs[16 * s_:16 * s_ + 16, wi, :, :].rearrange("p h c -> p (h c)")),
                      reads=[t_qkv], writes=[t_rec])
            S.dma(lambda e, recd=recd, cname=cname: e.dma_start(out=recd[:, 0:4, :], in_=I[cname][:, 2044:2048, :]), writes=[t_rec])
        ck(5.2)
        btab = sbt(st2, "btab", [64, 3, 8, 129])
        t_btab = Tok()
        with contextlib.ExitStack() as st3:
            relr = sbt(st3, "relr", [32, 8])
            ohs = sbt(st3, "ohs", [32, 3, 129])
            RHs = sbt(st3, "RHs", [32, 8, 129])
            t_r2, t_rh2 = Tok(), Tok()
            S.dma(lambda e: e.dma_start(out=relr[:], in_=I["relb"]), writes=[t_r2])
            S.dma(lambda e: e.dma_start(out=ohs[:], in_=I["ohs"]), writes=[t_r2])
            for br in range(3):
                S.dve(lambda e, br=br: e.tensor_tensor(out=RHs[:], in0=bc(relr[:].unsqueeze(2), [32, 8, 129]), in1=bc(ohs[:, br, :].unsqueeze(1), [32, 8, 129]), op=ALU.mult),
                      reads=[t_r2], writes=[t_rh2])
                for h in range(8):
                    bk = 3 + h % 2
                    S.pe(lambda e, h=h, bk=bk: e.matmul(B[bk][0:64, 0:129], lhsT=ones[0:32, 0:64], rhs=RHs[:, h, :], start=True, stop=True),
                         reads=[t_rh2, t_const], writes=[bt[bk]])
                    S.act(lambda e, h=h, bk=bk, br=br: e.activation(out=btab[:, br, h, :], in_=B[bk][0:64, 0:129], func=AF.Copy), reads=[bt[bk]], writes=[t_btab])
        S.barrier()
        ck(5.3)
        Kt = [sbt(st2, "Kt", [64, 129, 64])] * 2
        Vt = [sbt(st2, "Vt", [64, 129, 64])] * 2
        t_Kt = [[Tok() for i in range(24)]] * 2
        t_Vt = [[Tok() for i in range(24)]] * 2
        lg = sbt(st2, "lg", [64, 129])
        zz = sbt(st2, "zz", [64, 1])
        ov = sbt(st2, "ov", [64, 64])
        Oacc = sbt(st2, "Oacc", [64, 8, 64])
        Zacc = sbt(st2, "Zacc", [64, 8])
        t_lg, t_zz, t_ov, t_acc2 = Tok(), Tok(), Tok(), Tok()
        S.pool(lambda e: e.memset(Oacc[:], 0.0), writes=[t_acc2])
        S.pool(lambda e: e.memset(Zacc[:], 0.0), writes=[t_acc2])
        it = 0
        for br, dil in enumerate((1, 4, 16)):
            ncache = 125 if dil == 1 else 128
            for h in range(8):
                bi = it % 2
                it += 1
                for tl, tk, cname, recd in ((Kt[bi], t_Kt[bi], "ck_s", reck_d), (Vt[bi], t_Vt[bi], "cv_s", recv_d)):
                    for s_ in range(4):
                        ps_ = slice(16 * s_, 16 * s_ + 16)
                        for j0 in range(0, ncache, 32):
                            j1 = min(ncache, j0 + 32)
                            src = bass.AP(I[cname].tensor, (2048 + s_ - 128 * dil + j0 * dil) * 512 + h * 64, [[2048 * 512, 16], [dil * 512, j1 - j0], [1, 64]])
                            S.dma(lambda e, tl=tl, ps_=ps_, src=src, j0=j0, j1=j1: e.dma_start(out=tl[ps_, j0:j1, :], in_=src), writes=[tk[8 + 4 * s_ + j0 // 32]])
                        r0 = (1 + s_) if dil == 1 else (4 + s_)
                        src2 = bass.AP(recd.tensor, r0 * 512 + h * 64, [[8 * 512, 16], [512, 129 - ncache], [1, 64]])
                        S.dma(lambda e, tl=tl, ps_=ps_, src2=src2, ncache=ncache: e.dma_start(out=tl[ps_, ncache:129, :], in_=src2), reads=[t_rec], writes=[tk[2 * s_ + 1]])
                K_, V_ = Kt[bi], Vt[bi]
                S.dve(lambda e, K_=K_, h=h: e.tensor_tensor(out=K_[:], in0=K_[:], in1=bc(qkvs[:, 0, h, :].unsqueeze(1), [64, 129, 64]), op=ALU.mult),
                      reads=t_Kt[bi] + [t_qkv], writes=t_Kt[bi])
                S.dve(lambda e, K_=K_: e.tensor_reduce(out=lg[:], in_=K_[:], axis=AX.X, op=ALU.add), reads=t_Kt[bi], writes=[t_lg])
                S.dve(lambda e, br=br, h=h: e.scalar_tensor_tensor(out=lg[:], in0=lg[:], scalar=0.125, in1=btab[:, br, h, :], op0=ALU.mult, op1=ALU.add),
                      reads=[t_lg, t_btab], writes=[t_lg])
                S.act(lambda e: e.activation(out=lg[:], in_=lg[:], func=AF.Exp), reads=[t_lg], writes=[t_lg])
                S.dve(lambda e: e.tensor_reduce(out=zz[:], in_=lg[:], axis=AX.X, op=ALU.add), reads=[t_lg], writes=[t_zz])
                S.dve(lambda e, h=h: e.tensor_tensor(out=Zacc[:, h:h + 1], in0=Zacc[:, h:h + 1], in1=zz[:], op=ALU.add), reads=[t_zz, t_acc2], writes=[t_acc2])
                S.pool(lambda e, V_=V_: e.tensor_tensor(out=V_[:], in0=V_[:], in1=bc(lg[:].unsqueeze(2), [64, 129, 64]), op=ALU.mult),
                       reads=t_Vt[bi] + [t_lg], writes=t_Vt[bi])
                S.dve(lambda e, V_=V_: e.tensor_reduce(out=ov[:], in_=V_[:].rearrange("p j c -> p c j"), axis=AX.X, op=ALU.add), reads=t_Vt[bi], writes=[t_ov])
                S.dve(lambda e, h=h: e.tensor_tensor(out=Oacc[:, h, :], in0=Oacc[:, h, :], in1=ov[:], op=ALU.add), reads=[t_ov, t_acc2], writes=[t_acc2])
                ck(5.4 + 0.001 * it)
        S.dve(lambda e: e.reciprocal(out=Zacc[:], in_=Zacc[:]), reads=[t_acc2], writes=[t_acc2])
        S.dve(lambda e: e.tensor_tensor(out=Oacc[:], in0=Oacc[:], in1=bc(Zacc[:].unsqueeze(2), [64, 8, 64]), op=ALU.mult), reads=[t_acc2], writes=[t_acc2])
        mixs = sbt(st2, "mixs", [128, 4, 64], BF16)
        t_mixs = Tok()
        for p in range(4):
            S.pe(lambda e, p=p: e.transpose(B[5][:, p * 128:p * 128 + 64], Oacc[:, 2 * p:2 * p + 2, :].rearrange("p h c -> p (h c)"), ident[0:64, 0:64]),
                 reads=[t_acc2, t_const], writes=[bt[5]])
        S.act(lambda e: e.activation(out=mixs[:].rearrange("p q (b s) -> p q s b", s=4),
                                     in_=B[5][:].rearrange("p (q x) -> p q x", q=4)[:, :, 0:64].rearrange("p q (s b) -> p q s b", s=4), func=AF.Copy),
              reads=[bt[5]], writes=[t_mixs])
        S.dma(lambda e: e.dma_start(out=mixT_d[0:4, :, T:T + NS].rearrange("q p t -> p q t"), in_=mixs[:]), reads=[t_mixs])
    S.barrier()
    stH.close()
    if phase_limit < 6:
        S.emit()
        for sk_ in (stC_holder + [stH, st]):
            sk_.close()
        return nc
    S.nosync = True
    with contextlib.ExitStack() as st2, contextlib.suppress(_Stop):
        t_pc = Tok("peerconst")
        wout = sbt(st2, "wout", [128, 8, 1024], BF16)
        skT = sbt(st2, "skT", [128, 16, 128], BF16)
        iotaR = sbt(st2, "iotaR", [128, 128])
        with contextlib.ExitStack() as st3:
            wo_st = sbt(st3, "wo_st", [128, 8, 1024])
            sk_st = sbt(st3, "sk_st", [128, 16, 128])
            S.dma(lambda e: e.dma_start(out=wo_st[:], in_=I["w_out"].rearrange("(c p) n -> p c n", p=128)), writes=[t_pc])
            S.dma(lambda e: e.dma_start(out=sk_st[:], in_=I["skT"]), writes=[t_pc])
            S.dma(lambda e: e.dma_start(out=iotaR[:], in_=I["iotaR"]), writes=[t_pc])
            S.dve(lambda e: e.tensor_copy(out=wout[:], in_=wo_st[:]), reads=[t_pc], writes=[t_pc])
            S.dve(lambda e: e.tensor_copy(out=skT[:], in_=sk_st[:]), reads=[t_pc], writes=[t_pc])
        S.barrier()
        GS = 256
        x1g = sbt(st2, "x1g", [128, 8, GS])
        tmpg = sbt(st2, "tmpg", [128, 8, GS])
        h2g = sbt(st2, "h2g", [128, 8, GS], BF16)
        mixg = sbt(st2, "mixg", [128, 8, GS], BF16)
        rsg = sbt(st2, "rsg", [128, GS])
        qpT = sbt(st2, "qpT", [128, 16, GS], BF16)
        wpqb = sbt(st2, "wpqb", [128, 8, 512], BF16)
        s_sb = sbt(st2, "s_sb", [128, 16, 128])
        s2 = sbt(st2, "s2", [128, 256])
        vals = sbt(st2, "vals", [128, 16, 16])
        idxu = sbt(st2, "idxu", [128, 16, 16], U32)
        idxf = sbt(st2, "idxf", [128, 16, 16])
        cand = sbt(st2, "cand", [128, 8, 256])
        ts_ = sbt(st2, "ts", [128, 8, 16])
        posu = sbt(st2, "posu", [128, 8, 16], U32)
        au = sbt(st2, "au", [128, 8, 16], U32)
        bu = sbt(st2, "bu", [128, 8, 16], U32)
        a_f = sbt(st2, "a_f", [128, 8, 16])
        b_f = sbt(st2, "b_f", [128, 8, 16])
        eq = sbt(st2, "eq", [128, 8, 16, 16])
        I1 = sbt(st2, "I1", [128, 8, 16])
        I2 = sbt(st2, "I2", [128, 8, 16])
        gt_ = sbt(st2, "gt", [128, 8, 16])
        zs = sbt(st2, "zs", [128, 8])
        I1T = sbt(st2, "I1T", [128, GS], BF16)
        I2T = sbt(st2, "I2T", [128, GS], BF16)
        gT = sbt(st2, "gT", [128, GS], BF16)
        iotaRb = sbt(st2, "iotaRb", [128, 128], BF16)
        S.dve(lambda e: e.tensor_copy(out=iotaRb[:], in_=iotaR[:]), reads=[t_pc], writes=[t_pc])
        A4 = [sbt(st2, "A4", [128, 4, 128], BF16) for i in range(2)]
        B4 = [sbt(st2, "B4", [128, 4, 128], BF16) for i in range(2)]
        WT = sbt(st2, "WT", [128, GS, 128], BF16)
        eub = [sbt(st2, "eub", [128, 8, 256], BF16) for i in range(2)]
        evb = [sbt(st2, "evb", [128, 2, 1024], BF16) for i in range(2)]
        gU = [sbt(st2, "gU", [128, GS], BF16) for i in range(2)]
        Wg = [sbt(st2, "Wg", [128, GS], BF16) for i in range(2)]
        ytk = [sbt(st2, "ytk", [128, 1024]) for i in range(2)]
        (t_x1, t_tmp, t_h2, t_mixg, t_rsg, t_qp, t_wpq, t_s, t_s2, t_vals, t_cand, t_ts, t_ab, t_eq, t_I, t_g, t_IT, t_WT) = [Tok() for i in range(18)]
        t_A4 = [Tok(), Tok()]
        t_B4 = [Tok(), Tok()]
        t_eub = [Tok(), Tok()]
        t_evb = [Tok(), Tok()]
        t_gU = [Tok(), Tok()]
        t_Wg = [Tok(), Tok()]
        t_ytk = [Tok(), Tok()]
        B = banks
        evv = ev_d.rearrange("(k p) d -> p k d", p=128)
        wpqv = wpq_d.rearrange("(c p) n -> p c n", p=128)
        iota16 = iotaR[:, 0:16]
        pgroups = [(g * GS, GS) for g in range(T // GS)] + [(T, NS)]
        nyt = 0
        for gi, (t0, n) in enumerate(pgroups):
            samp = (gi == len(pgroups) - 1)
            S.dma(lambda e, t0=t0, n=n: e.dma_start(out=mixg[:, :, 0:n], in_=mixT_d[:, :, t0:t0 + n].rearrange("j p t -> p j t")), writes=[t_mixg])
            S.dma(lambda e, t0=t0, n=n: e.dma_start(out=x1g[:, :, 0:n], in_=xT_v[:, :, t0:t0 + n]), writes=[t_x1])
            for dc in range(8):
                bk = dc % 2
                for j in range(8):
                    S.pe(lambda e, dc=dc, j=j, bk=bk, n=n: e.matmul(B[bk][:, 0:n], lhsT=wout[:, j, dc * 128:(dc + 1) * 128], rhs=mixg[:, j, 0:n],
                                                                     start=(j == 0), stop=(j == 7)), reads=[t_pc, t_mixg], writes=[bt[bk]])
                if not samp:
                    S.dve(lambda e, dc=dc, bk=bk, n=n: e.scalar_tensor_tensor(out=x1g[:, dc, 0:n], in0=B[bk][:, 0:n], scalar=modT[:, 16 + dc, 0:1],
                                                                               in1=x1g[:, dc, 0:n], op0=ALU.mult, op1=ALU.add),
                          reads=[bt[bk], t_mod, t_x1], writes=[t_x1])
                else:
                    S.dve(lambda e, dc=dc, bk=bk: e.tensor_tensor(out=tmpg[:, dc, 0:NS].rearrange("p (b t) -> p b t", t=4),
                                                                  in0=B[bk][:, 0:NS].rearrange("p (b t) -> p b t", t=4),
                                                                  in1=bc(modT[:, 16 + dc, 1:17].unsqueeze(2), [128, SB, 4]), op=ALU.mult),
                          reads=[bt[bk], t_mod], writes=[t_tmp])
                    S.dve(lambda e, dc=dc: e.tensor_tensor(out=x1g[:, dc, 0:NS], in0=x1g[:, dc, 0:NS], in1=tmpg[:, dc, 0:NS], op=ALU.add),
                          reads=[t_tmp, t_x1], writes=[t_x1])
            S.act(lambda e, n=n: e.activation(out=tmpg[:, :, 0:n], in_=x1g[:, :, 0:n], func=AF.Square), reads=[t_x1], writes=[t_tmp])
            for c in range(8):
                S.pe(lambda e, c=c, n=n: e.matmul(B[2][:, 0:n], lhsT=ones[:], rhs=tmpg[:, c, 0:n], start=(c == 0), stop=(c == 7)),
                     reads=[t_tmp, t_const], writes=[bt[2]])
            S.act(lambda e, n=n: e.activation(out=rsg[:, 0:n], in_=B[2][:, 0:n], func=AF.Sqrt, scale=1.0 / D, bias=epsc[:]),
                  reads=[bt[2], t_const], writes=[t_rsg])
            S.dve(lambda e, n=n: e.reciprocal(out=rsg[:, 0:n], in_=rsg[:, 0:n]), reads=[t_rsg], writes=[t_rsg])
            S.dve(lambda e, n=n: e.tensor_tensor(out=tmpg[:, :, 0:n], in0=x1g[:, :, 0:n], in1=bc(rsg[:, 0:n].unsqueeze(1), [128, 8, n]), op=ALU.mult),
                  reads=[t_x1, t_rsg, t_tmp], writes=[t_tmp])
            if not samp:
                for c in range(8):
                    eng = S.dve if c % 2 == 0 else S.pool
                    eng(lambda e, c=c, n=n: e.tensor_scalar(out=h2g[:, c, 0:n], in0=tmpg[:, c, 0:n], scalar1=A2[:, c, 0:1], scalar2=modT[:, 24 + c, 0:1],
                                                            op0=ALU.mult, op1=ALU.add), reads=[t_tmp, t_mod], writes=[t_h2])
            else:
                tv = tmpg[:, :, 0:NS].rearrange("p c (b t) -> p c b t", t=4)
                S.dve(lambda e, tv=tv: e.tensor_tensor(out=tv, in0=tv, in1=bc(A2[:, :, 1:17].unsqueeze(3), [128, 8, SB, 4]), op=ALU.mult),
                      reads=[t_tmp, t_mod], writes=[t_tmp])
                S.dve(lambda e, tv=tv: e.tensor_tensor(out=h2g[:, :, 0:NS].rearrange("p c (b t) -> p c b t", t=4), in0=tv,
                                                       in1=bc(modT[:, 24:32, 1:17].unsqueeze(3), [128, 8, SB, 4]), op=ALU.add),
                      reads=[t_tmp, t_mod], writes=[t_h2])
            ck(6.1)
            for jb in range(4):
                S.dma(lambda e, jb=jb: e.dma_start(out=wpqb[:], in_=wpqv[:, :, jb * 512:(jb + 1) * 512]), writes=[t_wpq])
                for j in range(4):
                    for c in range(8):
                        S.pe(lambda e, j=j, c=c, n=n: e.matmul(B[j][:, 0:n], lhsT=wpqb[:, c, j * 128:(j + 1) * 128], rhs=h2g[:, c, 0:n],
                                                               start=(c == 0), stop=(c == 7)), reads=[t_wpq, t_h2], writes=[bt[j]])
                for j in range(4):
                    S.act(lambda e, j=j, jb=jb, n=n: e.activation(out=qpT[:, jb * 4 + j, 0:n], in_=B[j][:, 0:n], func=AF.Copy), reads=[bt[j]], writes=[t_qp])
            for tt0 in range(0, n, 128):
                m = min(128, n - tt0)
                for j in range(16):
                    bk = 4 + j // 4
                    S.pe(lambda e, j=j, bk=bk, tt0=tt0, m=m: e.matmul(B[bk][0:m, (j % 4) * 128:(j % 4 + 1) * 128], lhsT=qpT[:, j, tt0:tt0 + m], rhs=skT[:, j, :],
                                                                      start=True, stop=True), reads=[t_qp, t_pc], writes=[bt[bk]])
                for q in range(4):
                    S.act(lambda e, q=q, m=m: e.activation(out=s_sb[0:m, q * 4:(q + 1) * 4, :].rearrange("p a k -> p (a k)"), in_=B[4 + q][0:m, :], func=AF.Copy),
                          reads=[bt[4 + q]], writes=[t_s])
                for j in range(16):
                    S.dve(lambda e, j=j, m=m: e.max(out=vals[0:m, j, 0:8], in_=s_sb[0:m, j, :]), reads=[t_s], writes=[t_vals])
                    S.dve(lambda e, j=j, m=m: e.match_replace(out=s2[0:m, 0:128], in_to_replace=vals[0:m, j, 0:8], in_values=s_sb[0:m, j, :], imm_value=-1e30),
                          reads=[t_s, t_vals], writes=[t_s2])
                    S.dve(lambda e, j=j, m=m: e.max(out=vals[0:m, j, 8:16], in_=s2[0:m, 0:128]), reads=[t_s2], writes=[t_vals])
                    S.dve(lambda e, j=j, m=m: e.max_index(out=idxu[0:m, j, 0:8], in_max=vals[0:m, j, 0:8], in_values=s_sb[0:m, j, :]), reads=[t_s, t_vals], writes=[t_vals])
                    S.dve(lambda e, j=j, m=m: e.max_index(out=idxu[0:m, j, 8:16], in_max=vals[0:m, j, 8:16], in_values=s_sb[0:m, j, :]), reads=[t_s, t_vals], writes=[t_vals])
                S.dve(lambda e, m=m: e.tensor_copy(out=idxf[0:m], in_=idxu[0:m]), reads=[t_vals], writes=[t_vals])
                v2 = vals[0:m].rearrange("p (h two) k -> p h two k", two=2)
                i2v = idxf[0:m].rearrange("p (h two) k -> p h two k", two=2)
                S.dve(lambda e, m=m, v2=v2: e.tensor_tensor(out=cand[0:m].rearrange("p h (a b) -> p h a b", a=16),
                                                            in0=bc(v2[:, :, 0, :].unsqueeze(3), [m, 8, 16, 16]),
                                                            in1=bc(v2[:, :, 1, :].unsqueeze(2), [m, 8, 16, 16]), op=ALU.add), reads=[t_vals], writes=[t_cand])
                for h in range(8):
                    S.dve(lambda e, h=h, m=m: e.max(out=ts_[0:m, h, 0:8], in_=cand[0:m, h, :]), reads=[t_cand], writes=[t_ts])
                    S.dve(lambda e, h=h, m=m: e.match_replace(out=s2[0:m, :], in_to_replace=ts_[0:m, h, 0:8], in_values=cand[0:m, h, :], imm_value=-1e30),
                          reads=[t_cand, t_ts], writes=[t_s2])
                    S.dve(lambda e, h=h, m=m: e.max(out=ts_[0:m, h, 8:16], in_=s2[0:m, :]), reads=[t_s2], writes=[t_ts])
                    S.dve(lambda e, h=h, m=m: e.max_index(out=posu[0:m, h, 0:8], in_max=ts_[0:m, h, 0:8], in_values=cand[0:m, h, :]), reads=[t_cand, t_ts], writes=[t_ts])
                    S.dve(lambda e, h=h, m=m: e.max_index(out=posu[0:m, h, 8:16], in_max=ts_[0:m, h, 8:16], in_values=cand[0:m, h, :]), reads=[t_cand, t_ts], writes=[t_ts])
                S.dve(lambda e, m=m: e.tensor_single_scalar(out=au[0:m], in_=posu[0:m], scalar=4, op=ALU.logical_shift_right), reads=[t_ts], writes=[t_ab])
                S.dve(lambda e, m=m: e.tensor_single_scalar(out=bu[0:m], in_=posu[0:m], scalar=15, op=ALU.bitwise_and), reads=[t_ts], writes=[t_ab])
                S.dve(lambda e, m=m: e.tensor_copy(out=a_f[0:m], in_=au[0:m]), reads=[t_ab], writes=[t_ab])
                S.dve(lambda e, m=m: e.tensor_copy(out=b_f[0:m], in_=bu[0:m]), reads=[t_ab], writes=[t_ab])
                for which, sel, dstI in ((0, a_f, I1), (1, b_f, I2)):
                    S.dve(lambda e, m=m, sel=sel: e.tensor_tensor(out=eq[0:m], in0=bc(iota16[0:m].unsqueeze(1).unsqueeze(1), [m, 8, 16, 16]),
                                                                  in1=bc(sel[0:m].unsqueeze(3), [m, 8, 16, 16]), op=ALU.is_equal),
                          reads=[t_ab, t_pc], writes=[t_eq])
                    S.dve(lambda e, m=m, which=which, i2v=i2v: e.tensor_tensor(out=eq[0:m], in0=eq[0:m], in1=bc(i2v[:, :, which, :].unsqueeze(2), [m, 8, 16, 16]), op=ALU.mult),
                          reads=[t_eq, t_vals], writes=[t_eq])
                    S.dve(lambda e, m=m, dstI=dstI: e.tensor_reduce(out=dstI[0:m], in_=eq[0:m], axis=AX.X, op=ALU.add), reads=[t_eq], writes=[t_I])
                S.dve(lambda e, m=m: e.tensor_tensor(out=gt_[0:m], in0=ts_[0:m], in1=bc(ts_[0:m, :, 0:1], [m, 8, 16]), op=ALU.subtract), reads=[t_ts], writes=[t_g])
                S.act(lambda e, m=m: e.activation(out=gt_[0:m], in_=gt_[0:m], func=AF.Exp), reads=[t_g], writes=[t_g])
                S.dve(lambda e, m=m: e.tensor_reduce(out=zs[0:m], in_=gt_[0:m], axis=AX.X, op=ALU.add), reads=[t_g], writes=[t_g])
                S.dve(lambda e, m=m: e.reciprocal(out=zs[0:m], in_=zs[0:m]), reads=[t_g], writes=[t_g])
                S.dve(lambda e, m=m: e.tensor_tensor(out=gt_[0:m], in0=gt_[0:m], in1=bc(zs[0:m].unsqueeze(2), [m, 8, 16]), op=ALU.mult), reads=[t_g], writes=[t_g])
                for srcI, dstT, rd in ((I1, I1T, t_I), (I2, I2T, t_I), (gt_, gT, t_g)):
                    S.pe(lambda e, srcI=srcI, m=m: e.transpose(B[0][:, 0:m], srcI[0:m].rearrange("p h k -> p (h k)"), ident[0:m, 0:m]),
                         reads=[rd, t_const], writes=[bt[0]])
                    S.act(lambda e, dstT=dstT, tt0=tt0, m=m: e.activation(out=dstT[:, tt0:tt0 + m], in_=B[0][:, 0:m], func=AF.Copy), reads=[bt[0]], writes=[t_IT])
            if debug and gi == 0 and False:
                for qq, tl in enumerate((I1T, I2T, gT)):
                    S.dma(lambda e, qq=qq, tl=tl: e.dma_start(out=O["dbg_IT"][qq], in_=tl[:]), reads=[t_IT])
            ck(6.2)
            for n0 in range(0, n, 4):
                bi = (n0 // 4) % 2
                S.dve(lambda e, bi=bi, n0=n0: e.tensor_tensor(out=B4[bi][:], in0=bc(iotaRb[:].unsqueeze(1), [128, 4, 128]),
                                                             in1=bc(I2T[:, n0:n0 + 4].unsqueeze(2), [128, 4, 128]), op=ALU.is_equal),
                      reads=[t_IT, t_pc], writes=[t_B4[bi]])
                S.dve(lambda e, bi=bi, n0=n0: e.tensor_tensor(out=A4[bi][:], in0=bc(iotaRb[:].unsqueeze(1), [128, 4, 128]),
                                                             in1=bc(I1T[:, n0:n0 + 4].unsqueeze(2), [128, 4, 128]), op=ALU.is_equal),
                      reads=[t_IT, t_pc], writes=[t_A4[bi]])
                S.pool(lambda e, bi=bi, n0=n0: e.tensor_tensor(out=A4[bi][:], in0=A4[bi][:],
                                                              in1=bc(gT[:, n0:n0 + 4].unsqueeze(2), [128, 4, 128]), op=ALU.mult),
                      reads=[t_IT, t_A4[bi]], writes=[t_A4[bi]])
                bk = 6 + bi
                for q in range(4):
                    S.pe(lambda e, bi=bi, q=q, bk=bk: e.matmul(B[bk][:, q * 128:(q + 1) * 128], lhsT=B4[bi][:, q, :], rhs=A4[bi][:, q, :], start=True, stop=True),
                         reads=[t_A4[bi], t_B4[bi]], writes=[bt[bk]])
                S.act(lambda e, bk=bk, n0=n0: e.activation(out=WT[:, n0:n0 + 4, :].rearrange("p n i -> p (n i)"),
                                                           in_=B[bk][:], func=AF.Copy), reads=[bt[bk]], writes=[t_WT])
            ck(6.3)
            def emit_U(i1):
                blk, k2 = i1 // 2, i1 % 2
                bi = blk % 2
                if k2 == 0:
                    S.dma(lambda e, bi=bi, blk=blk: e.dma_start(out=eub[bi][:], in_=euT_d[blk]), writes=[t_eub[bi]])
                    S.dma(lambda e, bi=bi, blk=blk: e.dma_start(out=evb[bi][:], in_=evv[:, blk * 2:(blk + 1) * 2, :]), writes=[t_evb[bi]])
                ui = i1 % 2
                ubk = 4 + ui
                for c in range(8):
                    S.pe(lambda e, bi=bi, k2=k2, c=c, ubk=ubk, n=n: e.matmul(B[ubk][:, 0:n], lhsT=eub[bi][:, c, k2 * 128:(k2 + 1) * 128], rhs=h2g[:, c, 0:n],
                                                                             start=(c == 0), stop=(c == 7)), reads=[t_eub[bi], t_h2], writes=[bt[ubk]])

            def emit_rest(i1):
                blk, k2 = i1 // 2, i1 % 2
                bi = blk % 2
                ui = i1 % 2
                ubk = 4 + ui
                S.act(lambda e, ui=ui, ubk=ubk, n=n: e.activation(out=gU[ui][:, 0:n], in_=B[ubk][:, 0:n], func=AF.Gelu), reads=[bt[ubk]], writes=[t_gU[ui]])
                S.dve(lambda e, ui=ui, i1=i1, n=n: e.tensor_tensor(out=Wg[ui][:, 0:n], in0=gU[ui][:, 0:n], in1=WT[:, 0:n, i1], op=ALU.mult),
                      reads=[t_gU[ui], t_WT], writes=[t_Wg[ui]])
                for dc in range(8):
                    abk = dc // 2
                    S.pe(lambda e, bi=bi, k2=k2, dc=dc, abk=abk, ui=ui, i1=i1, n=n: e.matmul(
                        B[abk][:, (dc % 2) * 256:(dc % 2) * 256 + n], lhsT=evb[bi][:, k2, dc * 128:(dc + 1) * 128], rhs=Wg[ui][:, 0:n],
                        start=(i1 == 0 and dc % 2 == 0), stop=(i1 == 127)), reads=[t_evb[bi], t_Wg[ui]], writes=[bt[abk]])

            for i1 in range(129):
                if i1 < 128:
                    emit_U(i1)
                if i1 >= 1:
                    emit_rest(i1 - 1)
            for dc in range(8):
                abk = dc // 2
                src = B[abk][:, (dc % 2) * 256:(dc % 2) * 256 + n]
                if not samp:
                    S.dve(lambda e, dc=dc, src=src, n=n: e.scalar_tensor_tensor(out=x1g[:, dc, 0:n], in0=src, scalar=modT[:, 40 + dc, 0:1], in1=x1g[:, dc, 0:n],
                                                                                 op0=ALU.mult, op1=ALU.add), reads=[bt[abk], t_mod, t_x1], writes=[t_x1])
                else:
                    S.dve(lambda e, dc=dc, src=src: e.tensor_tensor(out=tmpg[:, dc, 0:NS].rearrange("p (b t) -> p b t", t=4),
                                                                    in0=src.rearrange("p (b t) -> p b t", t=4),
                                                                    in1=bc(modT[:, 40 + dc, 1:17].unsqueeze(2), [128, SB, 4]), op=ALU.mult),
                          reads=[bt[abk], t_mod, t_tmp], writes=[t_tmp])
                    S.dve(lambda e, dc=dc: e.tensor_tensor(out=x1g[:, dc, 0:NS], in0=x1g[:, dc, 0:NS], in1=tmpg[:, dc, 0:NS], op=ALU.add),
                          reads=[t_tmp, t_x1], writes=[t_x1])
            for tt0 in range(0, n, 128):
                m = min(128, n - tt0)
                yi = nyt % 2
                nyt += 1
                for dc in range(8):
                    bk = 4 + dc // 4
                    S.pe(lambda e, dc=dc, bk=bk, tt0=tt0, m=m: e.transpose(B[bk][0:m, (dc % 4) * 128:(dc % 4 + 1) * 128], x1g[:, dc, tt0:tt0 + m], ident[:]),
                         reads=[t_x1, t_const], writes=[bt[bk]])
                for hf in range(2):
                    S.act(lambda e, hf=hf, yi=yi, m=m: e.activation(out=ytk[yi][0:m, hf * 512:(hf + 1) * 512], in_=B[4 + hf][0:m, :], func=AF.Copy),
                          reads=[bt[4 + hf]], writes=[t_ytk[yi]])
                S.dma(lambda e, yi=yi, t0=t0, tt0=tt0, m=m: e.dma_start(out=O["y"][t0 + tt0:t0 + tt0 + m, :], in_=ytk[yi][0:m, :]), reads=[t_ytk[yi]])
            ck(6.5 + 0.01 * gi)
    K.st = st
    S.emit()
    st.close()
    return nc


def host_prep(inp, core):
    f = np.float32
    xp = np.asarray(inp["x_prompt"], f)[core]
    xs = np.asarray(inp["x_sample"], f)[core * SB:(core + 1) * SB].reshape(NS, D)
    xT = np.ascontiguousarray(np.concatenate([xp, xs], 0).T)
    cvec = np.concatenate([np.asarray(inp["c_prompt"], f)[core:core + 1],
                           np.asarray(inp["c_sample"], f)[core * SB:(core + 1) * SB]], 0)
    cT = np.ascontiguousarray(cvec.reshape(17, 8, 128).transpose(2, 1, 0))
    m = {}
    m["xT"] = xT
    m["cT"] = cT
    bsl = slice(core * SB, (core + 1) * SB)
    m["ck_s"] = np.ascontiguousarray(np.asarray(inp["cache_k_win"], f)[0, bsl].reshape(SB, 2048, 512))
    m["cv_s"] = np.ascontiguousarray(np.asarray(inp["cache_v_win"], f)[0, bsl].reshape(SB, 2048, 512))
    m["swkv"] = np.ascontiguousarray(np.asarray(inp["state_wkv"], f)[0, bsl].reshape(128, 4096))
    sh = np.asarray(inp["state_shift"], f)[0, bsl]
    m["shT"] = np.ascontiguousarray(sh[:, 0:1536].reshape(SB, 3, 4, 128).transpose(3, 1, 2, 0))
    m["shTl"] = np.ascontiguousarray(sh[:, 1536:1664].T)
    return m


def _t5_bucket(dist):
    import math
    dist = np.asarray(dist, dtype=np.int64)
    max_exact = 16
    safe = np.maximum(dist, 1) / max_exact
    large = max_exact + (np.log(safe) / math.log(2048 / max_exact) * (32 - max_exact)).astype(np.int64)
    large = np.minimum(large, 31)
    return np.where(dist < max_exact, dist, large).astype(np.int32)


def _ohu_table():
    t = np.zeros((32, 3, 384), np.float32)
    for br, dil in enumerate((1, 4, 16)):
        j = np.arange(129)
        b = _t5_bucket(j * dil)
        t[b, br, j + 127] = 1.0
    return t


def host_shared(inp):
    f = np.float32
    m = {}
    m["ada_w"] = np.ascontiguousarray(np.asarray(inp["ada_w"], f)[0])
    m["ada_bT"] = np.ascontiguousarray(np.asarray(inp["ada_b"], f)[0].reshape(48, 128).T)
    m["n1gT"] = np.ascontiguousarray(np.asarray(inp["norm1_g"], f)[0].reshape(8, 128).T)
    m["n2gT"] = np.ascontiguousarray(np.asarray(inp["norm2_g"], f)[0].reshape(8, 128).T)
    m["w_in"] = np.ascontiguousarray(np.asarray(inp["w_in"], f)[0])
    qg = np.asarray(inp["q_norm_g"], f)[0]
    kg = np.asarray(inp["k_norm_g"], f)[0]
    m["qkg"] = np.ascontiguousarray(np.stack([np.tile(qg, 2), np.tile(kg, 2)], 1))
    m["ident"] = np.eye(128, dtype=f)
    m["ones"] = np.ones((128, 128), f)
    b2 = np.zeros((128, 128), f)
    b2[:64, :64] = 1
    b2[64:, 64:] = 1
    m["blk2"] = b2
    m["relb"] = np.ascontiguousarray(np.asarray(inp["rel_bias"], f))
    m["ohu"] = _ohu_table()

    def pf(v):
        return np.asarray(v, f).reshape(4, 128).T
    mu = np.asarray(inp["mu_shift"], f)[0]
    m["rwp"] = np.ascontiguousarray(np.stack([pf(mu[0:512]), pf(mu[512:1024]), pf(mu[1024:1536]), pf(inp["k_k"][0]), pf(inp["k_a"][0]),
                                              pf(np.asarray(inp["r_k"], f)[0].reshape(512)), pf(inp["a0"][0]), pf(inp["w0"][0])], 1))
    m["mul"] = np.ascontiguousarray(mu[1536:1664].reshape(128, 1))
    m["lw3"] = np.ascontiguousarray(np.concatenate([np.asarray(inp["w_w2"], f)[0], np.asarray(inp["w_a2"], f)[0], np.asarray(inp["w_g2"], f)[0]], 0))
    m["w0row"] = np.ascontiguousarray(np.asarray(inp["w0"], f)[0].reshape(1, 512))
    m["lnx"] = np.ascontiguousarray(np.stack([np.asarray(inp["lnx_g"], f)[0], np.asarray(inp["lnx_b"], f)[0]], 0))
    m["w_out"] = np.ascontiguousarray(np.asarray(inp["w_out"], f)[0])
    m["w_pq"] = np.ascontiguousarray(np.asarray(inp["w_peer_q"], f)[0])
    sk = np.asarray(inp["peer_sub_keys"], f)[0]
    m["skT"] = np.ascontiguousarray(sk.reshape(16, 128, 128).transpose(2, 0, 1))
    m["euT"] = np.ascontiguousarray(np.asarray(inp["expert_u"], f)[0].T)
    m["ev"] = np.ascontiguousarray(np.asarray(inp["expert_v"], f)[0])
    m["qkrow"] = np.ascontiguousarray(np.stack([qg, kg], 0))
    t_ = np.zeros((32, 3, 129), f)
    for br_, dil_ in enumerate((1, 4, 16)):
        jp = np.arange(129)
        t_[_t5_bucket((128 - jp) * dil_), br_, jp] = 1.0
    m["ohs"] = t_
    m["iotaR"] = np.ascontiguousarray(np.tile(np.arange(128, dtype=f)[None, :], (128, 1)))
    NEG = -0.6065306597126334
    i = np.arange(128)[:, None]
    t = np.arange(128)[None, :]
    same = (i // 64) == (t // 64)
    m["tri"] = np.ascontiguousarray(np.stack([np.where(same & (i <= t), NEG, 0.0), np.where(same & (i < t), NEG, 0.0)], 1).astype(f))
    ii = (np.arange(128) % 64)[:, None]
    tt = np.arange(64)[None, :]
    m["mk1"] = np.ascontiguousarray(np.stack([(tt > ii), (tt >= ii)], 1).astype(f))
    m["mk3"] = np.ascontiguousarray((tt < ii).astype(f))
    m["id2"] = np.ascontiguousarray((tt == ii).astype(f))
    return m


def kernel(**inp):
    nc = build()
    shared = host_shared(inp)
    in_maps = []
    for c in range(NCORES):
        m = dict(shared)
        m.update(host_prep(inp, c))
        in_maps.append(m)
    res = run_bass_kernel_spmd(nc, in_maps, core_ids=list(range(NCORES)))
    R = res.results
    f = np.float32
    y_p = np.stack([R[c]["y"][0:T] for c in range(NCORES)], 0).astype(f)
    y_s = np.concatenate([R[c]["y"][T:NT].reshape(SB, 4, D) for c in range(NCORES)], 0).astype(f)
    kwp = np.stack([R[c]["kwp"].reshape(2048, 8, 64) for c in range(NCORES)], 0)[None].astype(f)
    vwp = np.stack([R[c]["vwp"].reshape(2048, 8, 64) for c in range(NCORES)], 0)[None].astype(f)
    wkvp = np.stack([R[c]["wkvp"].reshape(2, 64, 4, 64).transpose(2, 0, 3, 1).reshape(8, 64, 64) for c in range(NCORES)], 0)[None].astype(f)
    shp = np.stack([R[c]["shp"].reshape(CR) for c in range(NCORES)], 0)[None].astype(f)
    kws = np.concatenate([R[c]["kws"].reshape(SB, 4, 8, 64) for c in range(NCORES)], 0)[None].astype(f)
    vws = np.concatenate([R[c]["vws"].reshape(SB, 4, 8, 64) for c in range(NCORES)], 0)[None].astype(f)
    wkvs = np.concatenate([R[c]["wkvs"].reshape(SB, 8, 64, 64) for c in range(NCORES)], 0)[None].astype(f)
    shs = np.concatenate([R[c]["shs"].reshape(SB, CR) for c in range(NCORES)], 0)[None].astype(f)
    return (y_p, y_s, kwp, vwp, wkvp, shp, kws, vws, wkvs, shs)
```

```python
import contextlib
import numpy as np
import concourse.bass as bass
import concourse.mybir as mybir
from concourse.bass_utils import run_bass_kernel_spmd

F32 = mybir.dt.float32
BF16 = mybir.dt.bfloat16
I32 = mybir.dt.int32
U32 = mybir.dt.uint32
AF = mybir.ActivationFunctionType
ALU = mybir.AluOpType
AX = mybir.AxisListType

N_DMA_SLOTS = 24
PE_NOSYNC = True
NCORES = 8
D = 1024
T = 4096
NS = 64
NT = T + NS
SB = 16
DIN = 3200
CR = 1664
EPS = 1e-6


class Tok:
    __slots__ = ("lw", "rd", "rd_dma", "name", "excl")

    def __init__(self, name="", excl=False):
        self.excl = excl
        self.lw = None
        self.rd = {}
        self.rd_dma = []
        self.name = name


class Sched:
    ENGS = ("pe", "act", "dve", "pool", "sp")

    def __init__(self, nc):
        self.nc = nc
        self.ins = []
        self.last_by_eng = {}
        self.dmas_since = []
        self.nosync = True

    def barrier(self):
        deps = set(self.last_by_eng.values()) | set(self.dmas_since)
        self.dmas_since = []
        for e in self.ENGS:
            idx = len(self.ins)
            self.ins.append([e, (lambda eh: eh.nop()), set(deps), False, False, 0, 0, False])
            self.last_by_eng[e] = idx

    def op(self, eng, fn, reads=(), writes=(), dma=False, strided=False):
        idx = len(self.ins)
        deps = set()
        for t in reads:
            if t.lw is not None:
                deps.add(t.lw)
            if t.excl:
                deps.update(v for kk_, v in t.rd.items() if kk_ != eng)
        for t in writes:
            if t.lw is not None:
                deps.add(t.lw)
            deps.update(t.rd.values())
            deps.update(t.rd_dma)
        for t in reads:
            if dma:
                t.rd_dma.append(idx)
            else:
                t.rd[eng] = idx
        for t in writes:
            t.lw = idx
            t.rd = {}
            t.rd_dma = []
        deps.discard(idx)
        self.ins.append([eng, fn, deps, dma, False, 0, 0, strided or (not self.nosync)])
        if dma:
            self.dmas_since.append(idx)
        else:
            self.last_by_eng[eng] = idx
        return idx

    def pe(self, fn, reads=(), writes=(), strided=False):
        return self.op("pe", fn, reads, writes, strided=strided)

    def act(self, fn, reads=(), writes=()):
        return self.op("act", fn, reads, writes)

    def dve(self, fn, reads=(), writes=()):
        return self.op("dve", fn, reads, writes)

    def pool(self, fn, reads=(), writes=()):
        return self.op("pool", fn, reads, writes)

    def dma(self, fn, reads=(), writes=(), eng="sp"):
        return self.op(eng, fn, reads, writes, dma=True)

    def emit(self):
        nc = self.nc
        ins = self.ins
        class _Rec:
            def matmul(self, out, lhsT, rhs, **kw):
                self.rg = (lhsT.base_partition(), lhsT.partition_size())
            def transpose(self, out, in_, identity, **kw):
                self.rg = (in_.base_partition(), in_.partition_size())
            def nop(self, *a, **k):
                self.rg = (0, 128)
        prev_pe = None
        prev_strips = None
        for i_, it in enumerate(ins):
            if it[0] == "pe" and not it[3]:
                rec = _Rec()
                it[1](rec)
                b0, sz = rec.rg
                strips = set(range(b0 // 32, (b0 + sz + 31) // 32))
                if PE_NOSYNC and not it[7]:
                    if prev_strips is not None and not (strips & prev_strips):
                        it[2].add(prev_pe)
                    else:
                        it[2] = set(d for d in it[2] if not (ins[d][0] == "pe" and not ins[d][3]))
                prev_pe = i_
                prev_strips = strips
            for d in it[2]:
                ins[d][4] = True
        last = {}
        for i, it in enumerate(ins):
            if not it[3]:
                last[it[0]] = i
        for i in last.values():
            ins[i][4] = True
        cnt = {e: 0 for e in self.ENGS}
        dma_n = {e: 0 for e in self.ENGS}
        for it in ins:
            e = it[0]
            if it[3]:
                it[4] = True
                it[6] = dma_n[e]
                dma_n[e] += 1
            elif it[4]:
                cnt[e] += 1
                it[5] = cnt[e]
        with contextlib.ExitStack() as st:
            sems = {e: st.enter_context(nc.semaphore("s_" + e)) for e in self.ENGS}
            dsems = {e: [st.enter_context(nc.semaphore("d_%s_%d" % (e, i))) for i in range(N_DMA_SLOTS)]
                     for e in self.ENGS if dma_n[e] > 0}
            block = st.enter_context(nc.Block())
            per_eng = {e: [] for e in self.ENGS}
            for i, it in enumerate(ins):
                per_eng[it[0]].append(i)

            def run(eng_name, eh):
                seen = {e: 0 for e in self.ENGS}
                seen_dma = {}
                for i in per_eng[eng_name]:
                    e, fn, deps, is_dma, sig, c, slot = ins[i][:7]
                    need = {}
                    for d in deps:
                        de, _, _, ddma, _, dc, dslot = ins[d][:7]
                        if ddma:
                            key = (de, dslot % N_DMA_SLOTS)
                            val = 16 * (dslot // N_DMA_SLOTS + 1)
                            if seen_dma.get(key, 0) < val:
                                seen_dma[key] = val
                                need[("d",) + key] = val
                        else:
                            if seen[de] < dc:
                                seen[de] = dc
                                need[("c", de)] = dc
                    if is_dma and slot >= N_DMA_SLOTS:
                        key = (e, slot % N_DMA_SLOTS)
                        val = 16 * (slot // N_DMA_SLOTS)
                        if seen_dma.get(key, 0) < val:
                            seen_dma[key] = val
                            need[("d",) + key] = val
                    for k, v in need.items():
                        if k[0] == "c":
                            eh.wait_ge(sems[k[1]], v)
                        else:
                            eh.wait_ge(dsems[k[1]][k[2]], v)
                    inst = fn(eh)
                    if is_dma:
                        inst.then_inc(dsems[e][slot % N_DMA_SLOTS], 16)
                    elif sig:
                        inst.then_inc(sems[e], 1)
                if eng_name == "sp":
                    for e2 in self.ENGS:
                        if cnt[e2] > 0:
                            eh.wait_ge(sems[e2], cnt[e2])
                        n = dma_n.get(e2, 0)
                        for s in range(min(N_DMA_SLOTS, n)):
                            lastslot = ((n - 1 - s) // N_DMA_SLOTS) * N_DMA_SLOTS + s
                            eh.wait_ge(dsems[e2][s], 16 * (lastslot // N_DMA_SLOTS + 1))

            @block.tensor
            def _(eh):
                run("pe", eh)

            @block.scalar
            def _(eh):
                run("act", eh)

            @block.vector
            def _(eh):
                run("dve", eh)

            @block.gpsimd
            def _(eh):
                run("pool", eh)

            @block.sync
            def _(eh):
                run("sp", eh)


class Ctx:
    pass


def bc(ap, shape):
    return ap.to_broadcast(list(shape))


def build(phase_limit=99, debug=False):
    nc = bass.Bass("TRN2", target_bir_lowering=False)
    S = Sched(nc)
    K = Ctx()
    K.nc, K.S = nc, S

    def din(name, shape, dt=F32):
        return nc.dram_tensor(name, list(shape), dt, kind="ExternalInput").ap()

    def dout(name, shape, dt=F32):
        return nc.dram_tensor(name, list(shape), dt, kind="ExternalOutput").ap()

    I = {}
    I["xT"] = din("xT", [D, NT])
    I["cT"] = din("cT", [128, 8, 17])
    I["ada_w"] = din("ada_w", [D, 6 * D])
    I["ada_bT"] = din("ada_bT", [128, 48])
    I["n1gT"] = din("n1gT", [128, 8])
    I["n2gT"] = din("n2gT", [128, 8])
    I["w_in"] = din("w_in", [D, DIN])
    I["qkg"] = din("qkg", [128, 2])
    I["ident"] = din("ident", [128, 128])
    I["ones"] = din("ones", [128, 128])
    I["blk2"] = din("blk2", [128, 128])
    I["relb"] = din("relb", [32, 8])
    I["ohu"] = din("ohu", [32, 3, 384])
    I["w_out"] = din("w_out", [D, D])
    I["w_pq"] = din("w_pq", [D, 2048])
    I["skT"] = din("skT", [128, 16, 128])
    I["euT"] = din("euT", [D, 16384])
    I["ev"] = din("ev", [16384, D])
    I["iotaR"] = din("iotaR", [128, 128])
    euT_d = nc.dram_tensor("euT_d", [64, 128, 8, 256], BF16, kind="Internal").ap()
    ev_d = nc.dram_tensor("ev_d", [16384, D], BF16, kind="Internal").ap()
    wpq_d = nc.dram_tensor("wpq_d", [D, 2048], BF16, kind="Internal").ap()
    I["ck_s"] = din("ck_s", [SB, 2048, 512])
    I["cv_s"] = din("cv_s", [SB, 2048, 512])
    I["swkv"] = din("swkv", [128, 4096])
    I["shT"] = din("shT", [128, 3, 4, 16])
    I["shTl"] = din("shTl", [128, 16])
    I["qkrow"] = din("qkrow", [2, 64])
    I["ohs"] = din("ohs", [32, 3, 129])
    rws_d = nc.dram_tensor("rws_d", [SB, 8, 4, 6, 64], F32, kind="Internal").ap()
    ys_d = nc.dram_tensor("ys_d", [SB, 8, 4, 64], F32, kind="Internal").ap()
    reck_d = nc.dram_tensor("reck_d", [SB, 8, 512], F32, kind="Internal").ap()
    recv_d = nc.dram_tensor("recv_d", [SB, 8, 512], F32, kind="Internal").ap()
    I["rwp"] = din("rwp", [128, 8, 4])
    I["mul"] = din("mul", [128, 1])
    I["lw3"] = din("lw3", [128, 512])
    I["w0row"] = din("w0row", [1, 512])
    I["lnx"] = din("lnx", [2, 512])
    I["tri"] = din("tri", [128, 2, 128])
    I["mk1"] = din("mk1", [128, 2, 64])
    I["mk3"] = din("mk3", [128, 64])
    I["id2"] = din("id2", [128, 64])
    zscr = nc.dram_tensor("zscr", [24, 256, 384], F32, kind="Internal").ap()
    mixT_d = nc.dram_tensor("mixT_d", [8, 128, NT], BF16, kind="Internal").ap()
    O = {}
    O["kwp"] = dout("kwp", [2048, 512])
    O["vwp"] = dout("vwp", [2048, 512])
    O["kws"] = dout("kws", [NS, 512])
    O["vws"] = dout("vws", [NS, 512])
    O["shp"] = dout("shp", [1, CR])
    O["shs"] = dout("shs", [SB, CR])
    O["wkvp"] = dout("wkvp", [128, 4, 64])
    O["y"] = dout("y", [NT, D])
    O["wkvs"] = dout("wkvs", [128, 4096])
    if debug:
        O["dbg_hT"] = dout("dbg_hT", [128, 8, NT], BF16)
        O["dbg_mod"] = dout("dbg_mod", [128, 48, 17])
        O["dbg_ebt"] = dout("dbg_ebt", [128, 24, 2, 128], BF16)
        O["dbg_mixT"] = dout("dbg_mixT", [4, 128, T], BF16)
        O["dbg_yr"] = dout("dbg_yr", [T, 512])
        O["dbg_IT"] = dout("dbg_IT", [3, 128, 256])

    st = contextlib.ExitStack()

    uid = [0]

    def sbt(stack, name, shape, dt=F32):
        uid[0] += 1
        return stack.enter_context(nc.sbuf_tensor("s%d_%s" % (uid[0], name), list(shape), dt))

    def sb(name, shape, dt=F32):
        return sbt(st, name, shape, dt)

    banks = [st.enter_context(nc.psum_tensor("bank%d" % i, [128, 512], F32)) for i in range(8)]
    bt = [Tok("bank%d" % i, excl=True) for i in range(8)]

    ident = sb("ident", [128, 128])
    ones = sb("ones", [128, 128])
    blk2 = sb("blk2", [128, 128])
    identb = sb("identb", [128, 128], BF16)
    epsc = sb("epsc", [128, 1])
    t_const = Tok("const")
    S.dma(lambda e: e.dma_start(out=ident[:], in_=I["ident"]), writes=[t_const])
    S.dma(lambda e: e.dma_start(out=ones[:], in_=I["ones"]), writes=[t_const])
    S.dma(lambda e: e.dma_start(out=blk2[:], in_=I["blk2"]), writes=[t_const])
    S.pool(lambda e: e.memset(epsc[:], EPS), writes=[t_const])
    S.dve(lambda e: e.tensor_copy(out=identb[:], in_=ident[:]), reads=[t_const], writes=[t_const])

    S.nosync = True
    cT = sb("cT", [128, 8, 17])
    scT = sb("scT", [128, 8, 17])
    adab = sb("adab", [128, 48])
    n1g = sb("n1g", [128, 8])
    n2g = sb("n2g", [128, 8])
    qkg = sb("qkg", [128, 2])
    modT = sb("modT", [128, 48, 17])
    A1 = sb("A1", [128, 8, 17])
    A2 = sb("A2", [128, 8, 17])
    t_small = Tok("small")
    t_mod = Tok("mod")
    for dst, src in ((cT, "cT"), (adab, "ada_bT"), (n1g, "n1gT"), (n2g, "n2gT"), (qkg, "qkg")):
        S.dma(lambda e, dst=dst, src=src: e.dma_start(out=dst[:], in_=I[src]), writes=[t_small])
    S.act(lambda e: e.activation(out=scT[:], in_=cT[:], func=AF.Silu), reads=[t_small], writes=[t_small])
    adaw_v = I["ada_w"].rearrange("(c p) n -> p c n", p=128)
    with contextlib.ExitStack() as st2:
        wb = [sbt(st2, "adaw", [128, 8, 512]) for i in range(2)]
        wt = [Tok("adaw%d" % i) for i in range(2)]
        for nb in range(12):
            w = wb[nb % 2]
            wtk = wt[nb % 2]
            S.dma(lambda e, w=w, nb=nb: e.dma_start(out=w[:], in_=adaw_v[:, :, nb * 512:(nb + 1) * 512]), writes=[wtk])
            bk = nb % 2
            for oc in range(4):
                for c in range(8):
                    S.pe(lambda e, w=w, oc=oc, c=c, bk=bk: e.matmul(
                        banks[bk][:, oc * 32:oc * 32 + 17], lhsT=w[:, c, oc * 128:(oc + 1) * 128], rhs=scT[:, c, :],
                        start=(c == 0), stop=(c == 7)), reads=[wtk, t_small], writes=[bt[bk]])
            for oc in range(4):
                j = nb * 4 + oc
                S.act(lambda e, oc=oc, j=j, bk=bk: e.activation(
                    out=modT[:, j, :], in_=banks[bk][:, oc * 32:oc * 32 + 17], func=AF.Identity, bias=adab[:, j:j + 1]),
                    reads=[bt[bk], t_small], writes=[t_mod])
    S.barrier()
    S.dve(lambda e: e.tensor_scalar(out=A1[:], in0=modT[:, 8:16, :], scalar1=1.0, scalar2=None, op0=ALU.add),
          reads=[t_mod], writes=[t_mod])
    S.dve(lambda e: e.tensor_tensor(out=A1[:], in0=A1[:], in1=bc(n1g[:].unsqueeze(2), [128, 8, 17]), op=ALU.mult),
          reads=[t_mod, t_small], writes=[t_mod])
    S.dve(lambda e: e.tensor_scalar(out=A2[:], in0=modT[:, 32:40, :], scalar1=1.0, scalar2=None, op0=ALU.add),
          reads=[t_mod], writes=[t_mod])
    S.dve(lambda e: e.tensor_tensor(out=A2[:], in0=A2[:], in1=bc(n2g[:].unsqueeze(2), [128, 8, 17]), op=ALU.mult),
          reads=[t_mod, t_small], writes=[t_mod])
    if debug:
        S.dma(lambda e: e.dma_start(out=O["dbg_mod"], in_=modT[:]), reads=[t_mod])

    stH = contextlib.ExitStack()
    stC_holder = []
    hT = sbt(stH, "hT", [128, 8, NT], BF16)
    groups = [(g * 512, 512) for g in range(8)] + [(T, NS)]
    t_h = [Tok("h%d" % g) for g in range(9)]
    xT_v = I["xT"].rearrange("(c p) n -> p c n", p=128)

    def norm_groups(src_v, dst, Aap, Bidx, t_dst, extra_reads=()):
        with contextlib.ExitStack() as st2:
            xg = [sbt(st2, "xg", [128, 8, 512]) for i in range(2)]
            xgt = [Tok() for i in range(2)]
            sq = sbt(st2, "sq", [128, 8, 512])
            sqt = Tok()
            rs = sbt(st2, "rs", [128, 512])
            rst = Tok()
            for g, (t0, n) in enumerate(groups):
                x_ = xg[g % 2]
                xt_ = xgt[g % 2]
                bk = 2 + g % 2
                S.dma(lambda e, x_=x_, t0=t0, n=n: e.dma_start(out=x_[:, :, 0:n], in_=src_v[:, :, t0:t0 + n]),
                      writes=[xt_], reads=list(extra_reads))
                S.act(lambda e, x_=x_, n=n: e.activation(out=sq[:, :, 0:n], in_=x_[:, :, 0:n], func=AF.Square),
                      reads=[xt_], writes=[sqt])
                for c in range(8):
                    S.pe(lambda e, c=c, n=n, bk=bk: e.matmul(banks[bk][:, 0:n], lhsT=ones[:], rhs=sq[:, c, 0:n],
                                                              start=(c == 0), stop=(c == 7)),
                         reads=[sqt, t_const], writes=[bt[bk]])
                S.act(lambda e, n=n, bk=bk: e.activation(out=rs[:, 0:n], in_=banks[bk][:, 0:n], func=AF.Sqrt,
                                                          scale=1.0 / D, bias=epsc[:]),
                      reads=[bt[bk], t_const], writes=[rst])
                S.dve(lambda e, n=n: e.reciprocal(out=rs[:, 0:n], in_=rs[:, 0:n]), reads=[rst], writes=[rst])
                S.dve(lambda e, x_=x_, n=n: e.tensor_tensor(out=x_[:, :, 0:n], in0=x_[:, :, 0:n],
                                                             in1=bc(rs[:, 0:n].unsqueeze(1), [128, 8, n]), op=ALU.mult),
                      reads=[xt_, rst], writes=[xt_])
                if g < 8:
                    for c in range(8):
                        eng = S.dve if c % 2 == 0 else S.pool
                        eng(lambda e, x_=x_, c=c, t0=t0, n=n: e.tensor_scalar(
                            out=dst[:, c, t0:t0 + n], in0=x_[:, c, 0:n], scalar1=Aap[:, c, 0:1],
                            scalar2=modT[:, Bidx + c, 0:1], op0=ALU.mult, op1=ALU.add),
                            reads=[xt_, t_mod], writes=[t_dst[g]])
                else:
                    xv = x_[:, :, 0:NS].rearrange("p c (b t) -> p c b t", t=4)
                    S.dve(lambda e, xv=xv: e.tensor_tensor(
                        out=xv, in0=xv, in1=bc(Aap[:, :, 1:17].unsqueeze(3), [128, 8, SB, 4]), op=ALU.mult),
                        reads=[xt_, t_mod], writes=[xt_])
                    S.dve(lambda e, xv=xv, t0=t0: e.tensor_tensor(
                        out=dst[:, :, t0:t0 + NS].rearrange("p c (b t) -> p c b t", t=4), in0=xv,
                        in1=bc(modT[:, Bidx:Bidx + 8, 1:17].unsqueeze(3), [128, 8, SB, 4]), op=ALU.add),
                        reads=[xt_, t_mod], writes=[t_dst[g]])

    norm_groups(xT_v, hT, A1, 0, t_h)
    S.barrier()
    if debug:
        S.dma(lambda e: e.dma_start(out=O["dbg_hT"], in_=hT[:]), reads=t_h)


    if phase_limit < 3:
        S.emit()
        for sk_ in (stC_holder + [stH, st]):
            sk_.close()
        return nc
    S.nosync = True
    stC = contextlib.ExitStack()
    stC_holder.append(stC)
    EBT = sbt(stC, "EBT", [128, 24, 2, 128], BF16)
    onesb = sbt(stC, "onesb", [128, 64], BF16)
    t_ebt = Tok("ebt")
    g0_cst = [sbt(stC, "cst", [128, 1024]) for i in range(3)]
    g0_cbf = [sbt(stC, "cbf", [128, 1024], BF16) for i in range(3)]
    g0_tcs = [Tok() for i in range(3)]
    g0_tcb = [Tok() for i in range(3)]
    g0_blocks = []
    euv_src = I["euT"].rearrange("(c p) e -> p c e", p=128)
    for b_ in range(128):
        g0_blocks.append((lambda i, b_=b_: (g0_cst[i][:].rearrange("p (c e) -> p c e", c=8), euv_src[:, :, b_ * 128:(b_ + 1) * 128]),
                          lambda i, b_=b_: (euT_d[b_ // 2][:, :, (b_ % 2) * 128:(b_ % 2 + 1) * 128], g0_cbf[i][:].rearrange("p (c e) -> p c e", c=8))))
    for src_, dstd_, nblk_ in ((I["ev"], ev_d, 128), (I["w_pq"], wpq_d, 16)):
        sv_ = src_.rearrange("a b -> (a b)").rearrange("(n p f) -> n p f", p=128, f=1024)
        dv_ = dstd_.rearrange("a b -> (a b)").rearrange("(n p f) -> n p f", p=128, f=1024)
        for b_ in range(nblk_):
            g0_blocks.append((lambda i, b_=b_, sv_=sv_: (g0_cst[i][:], sv_[b_]), lambda i, b_=b_, dv_=dv_: (dv_[b_], g0_cbf[i][:])))
    g0_tick = [0]

    def g0_step():
        t = g0_tick[0]
        g0_tick[0] += 1
        nb_ = len(g0_blocks)
        if t - 2 >= 0 and t - 2 < nb_:
            k = t - 2
            i = k % 3
            o_, i_ = g0_blocks[k][1](i)
            S.dma(lambda e, o_=o_, i_=i_: e.dma_start(out=o_, in_=i_), reads=[g0_tcb[i]])
        if t < nb_:
            i = t % 3
            o_, i_ = g0_blocks[t][0](i)
            S.dma(lambda e, o_=o_, i_=i_: e.dma_start(out=o_, in_=i_), writes=[g0_tcs[i]])
        if t - 1 >= 0 and t - 1 < nb_:
            i = (t - 1) % 3
            S.pool(lambda e, i=i: e.tensor_copy(out=g0_cbf[i][:], in_=g0_cst[i][:]), reads=[g0_tcs[i]], writes=[g0_tcb[i]])
    S.dve(lambda e: e.tensor_copy(out=onesb[:], in_=ones[:, 0:64]), reads=[t_const], writes=[t_const])
    with contextlib.ExitStack() as st2:
        relb = sbt(st2, "relb", [32, 8])
        ohu = sbt(st2, "ohu", [32, 3, 384])
        RH = sbt(st2, "RH", [32, 8, 384])
        grep = [sbt(st2, "grep", [128, 384]) for i in range(2)]
        gt = [Tok(), Tok()]
        ebf = [sbt(st2, "ebf", [128, 2, 128]) for i in range(2)]
        et = [Tok(), Tok()]
        t_r = Tok()
        t_rh = Tok()
        S.dma(lambda e: e.dma_start(out=relb[:], in_=I["relb"]), writes=[t_r])
        S.dma(lambda e: e.dma_start(out=ohu[:], in_=I["ohu"]), writes=[t_r])
        S.act(lambda e: e.activation(out=relb[:], in_=relb[:], func=AF.Exp), reads=[t_r], writes=[t_r])
        zt = Tok()
        for br in range(3):
            S.dve(lambda e, br=br: e.tensor_tensor(out=RH[:], in0=bc(relb[:].unsqueeze(2), [32, 8, 384]),
                                                    in1=bc(ohu[:, br, :].unsqueeze(1), [32, 8, 384]), op=ALU.mult),
                  reads=[t_r], writes=[t_rh])
            for h in range(8):
                i = br * 8 + h
                bk = 6 + i % 2
                S.pe(lambda e, h=h, bk=bk: e.matmul(banks[bk][:, 0:384], lhsT=ones[0:32, :], rhs=RH[:, h, :],
                                                     start=True, stop=True), reads=[t_rh, t_const], writes=[bt[bk]])
                g_ = grep[i % 2]
                S.act(lambda e, g_=g_, bk=bk: e.activation(out=g_[:], in_=banks[bk][:, 0:384], func=AF.Copy),
                      reads=[bt[bk]], writes=[gt[i % 2]])
                zi = Tok()
                S.dma(lambda e, g_=g_, i=i: e.dma_start(out=zscr[i, 0:128, :], in_=g_[:]), reads=[gt[i % 2]], writes=[zi])
                S.dma(lambda e, g_=g_, i=i: e.dma_start(out=zscr[i, 128:256, :], in_=g_[:]), reads=[gt[i % 2]], writes=[zi])
                eb_ = ebf[i % 2]
                for part in range(2):
                    src = bass.AP(zscr.tensor, i * 256 * 384 + 255 + part * 128 * 383, [[383, 128], [1, 128]])
                    S.dma(lambda e, eb_=eb_, part=part, src=src: e.dma_start(out=eb_[:, part, :], in_=src),
                          reads=[zi], writes=[et[i % 2]])
                S.dve(lambda e, eb_=eb_, i=i: e.tensor_copy(out=EBT[:, i, :, :], in_=eb_[:]), reads=[et[i % 2]], writes=[t_ebt])
    S.barrier()
    if debug:
        S.dma(lambda e: e.dma_start(out=O["dbg_ebt"], in_=EBT[:]), reads=[t_ebt])

    win_v = I["w_in"].rearrange("(c p) n -> p c n", p=128)

    class _Stop(Exception):
        pass

    def ck(x):
        if phase_limit < x:
            raise _Stop()

    with contextlib.ExitStack() as st2, contextlib.suppress(_Stop):
        ck(3.5)
        wst = [sbt(st2, "wst", [128, 8, 128]) for i in range(2)]
        wstt = [Tok(), Tok()]
        wqkv = sbt(st2, "wqkv", [128, 8, 3, 128], BF16)
        t_w = Tok()
        qT = sbt(st2, "qT", [128, T], BF16)
        kT = sbt(st2, "kT", [128, T], BF16)
        t_q = [Tok() for g in range(8)]
        t_k = [Tok() for g in range(8)]
        V = sbt(st2, "V", [128, 3, 32, 128], BF16)
        t_v = [[Tok() for j in range(32)] for br in range(3)]
        accO = sbt(st2, "accO", [128, 2048])
        accS = sbt(st2, "accS", [128, 2048])
        t_acc = Tok()
        sq = sbt(st2, "sq", [128, 512])
        t_sq = Tok()
        rs = sbt(st2, "rs", [128, 512])
        t_rs = Tok()
        kTf = sbt(st2, "kTf", [128, 512])
        t_kf = Tok()
        ktok = [sbt(st2, "ktok", [128, 4, 128]) for i in range(2)]
        t_kt = [Tok(), Tok()]
        vtok = [sbt(st2, "vtok", [128, 4, 128]) for i in range(2)]
        t_vt = [Tok(), Tok()]
        e0 = [sbt(st2, "e0", [128, 512], BF16) for i in range(4)]
        t_e0 = [Tok() for i in range(4)]
        ee = [sbt(st2, "ee", [128, 512], BF16) for i in range(4)]
        t_ee = [Tok() for i in range(4)]
        mixo = sbt(st2, "mixo", [128, 2048], BF16)
        t_mixo = Tok()
        nld = 0
        nblk = 0
        for hp in range(4):
            for wi in range(3):
                w_ = wst[nld % 2]
                wt_ = wstt[nld % 2]
                nld += 1
                col = wi * 512 + hp * 128
                S.dma(lambda e, w_=w_, col=col: e.dma_start(out=w_[:], in_=win_v[:, :, col:col + 128]), writes=[wt_])
                S.pool(lambda e, w_=w_, wi=wi: e.tensor_copy(out=wqkv[:, :, wi, :], in_=w_[:]), reads=[wt_], writes=[t_w])
            for g in range(8):
                for wi, dstT, tks in ((0, qT, t_q), (1, kT, t_k)):
                    bk = 4 + wi
                    for c in range(8):
                        S.pe(lambda e, c=c, wi=wi, g=g, bk=bk: e.matmul(
                            banks[bk][:], lhsT=wqkv[:, c, wi, :], rhs=hT[:, c, g * 512:(g + 1) * 512],
                            start=(c == 0), stop=(c == 7)), reads=[t_w, t_h[g]], writes=[bt[bk]])
                    S.act(lambda e, bk=bk: e.activation(out=sq[:], in_=banks[bk][:], func=AF.Square),
                          reads=[bt[bk]], writes=[t_sq])
                    S.pe(lambda e: e.matmul(banks[6][:], lhsT=blk2[:], rhs=sq[:], start=True, stop=True),
                         reads=[t_sq, t_const], writes=[bt[6]])
                    S.act(lambda e: e.activation(out=rs[:], in_=banks[6][:], func=AF.Sqrt, scale=1.0 / 64, bias=epsc[:]),
                          reads=[bt[6], t_const], writes=[t_rs])
                    S.dve(lambda e: e.reciprocal(out=rs[:], in_=rs[:]), reads=[t_rs], writes=[t_rs])
                    S.dve(lambda e, bk=bk, wi=wi, g=g, dstT=dstT: e.scalar_tensor_tensor(
                        out=dstT[:, g * 512:(g + 1) * 512], in0=banks[bk][:], scalar=qkg[:, wi:wi + 1], in1=rs[:],
                        op0=ALU.mult, op1=ALU.mult), reads=[bt[bk], t_rs, t_small], writes=[tks[g]])
                    if wi == 1 and g >= 4:
                        S.dve(lambda e, bk=bk: e.scalar_tensor_tensor(
                            out=kTf[:], in0=banks[bk][:], scalar=qkg[:, 1:2], in1=rs[:], op0=ALU.mult, op1=ALU.mult),
                            reads=[bt[bk], t_rs, t_small], writes=[t_kf])
                        for j in range(4):
                            S.pe(lambda e, j=j: e.transpose(banks[7][:, j * 128:(j + 1) * 128], kTf[:, j * 128:(j + 1) * 128], ident[:]),
                                 reads=[t_kf, t_const], writes=[bt[7]])
                        kt_ = ktok[g % 2]
                        S.act(lambda e, kt_=kt_: e.activation(out=kt_[:].rearrange("p a b -> p (a b)"), in_=banks[7][:], func=AF.Copy),
                              reads=[bt[7]], writes=[t_kt[g % 2]])
                        r0 = g * 512 - 2048
                        S.dma(lambda e, kt_=kt_, r0=r0, hp=hp: e.dma_start(
                            out=O["kwp"][r0:r0 + 512, hp * 128:(hp + 1) * 128].rearrange("(j p) c -> p j c", p=128), in_=kt_[:]),
                            reads=[t_kt[g % 2]])
            ck(3.6)
            for br, dil in enumerate((1, 4, 16)):
                if br == 1:
                    ck(3.62)
                G = 32 // dil
                for j0 in range(0, 32, 4):
                    bk = 6 + (j0 // 4) % 2
                    for jj in range(4):
                        j = j0 + jj
                        r, g = j // G, j % G
                        start = r + dil * 128 * g
                        tg = sorted(set([(start) // 512, (start + dil * 127) // 512]))
                        for c in range(8):
                            S.pe(lambda e, c=c, jj=jj, start=start, dil=dil, bk=bk: e.matmul(
                                banks[bk][:, jj * 128:(jj + 1) * 128],
                                lhsT=hT[:, c, start:start + dil * 127 + 1:dil], rhs=wqkv[:, c, 2, :],
                                start=(c == 0), stop=(c == 7)), reads=[t_w] + [t_h[x] for x in tg], writes=[bt[bk]], strided=(dil > 1))
                    S.act(lambda e, br=br, j0=j0, bk=bk: e.activation(
                        out=V[:, br, j0:j0 + 4, :].rearrange("p a b -> p (a b)"), in_=banks[bk][:], func=AF.Copy),
                        reads=[bt[bk]], writes=[t_v[br][j0 + x] for x in range(4)])
                    if br == 0 and j0 >= 16 :
                        vt_ = vtok[(j0 // 4) % 2]
                        tv_ = t_vt[(j0 // 4) % 2]
                        S.act(lambda e, vt_=vt_, bk=bk: e.activation(out=vt_[:].rearrange("p a b -> p (a b)"), in_=banks[bk][:], func=AF.Copy),
                              reads=[bt[bk]], writes=[tv_])
                        r0 = j0 * 128 - 2048
                        S.dma(lambda e, vt_=vt_, r0=r0, hp=hp: e.dma_start(
                            out=O["vwp"][r0:r0 + 512, hp * 128:(hp + 1) * 128].rearrange("(j p) c -> p j c", p=128), in_=vt_[:]),
                            reads=[tv_])
            ck(3.7)
            for half in range(2):
                for br, dil in enumerate((1, 4, 16)):
                    G = 32 // dil
                    Gh = G // 2
                    for r in range(dil):
                        for g in range(half * Gh, (half + 1) * Gh):
                            j = r * G + g
                            start = r + dil * 128 * g
                            qsl = slice(start, start + dil * 127 + 1, dil)
                            tgq = sorted(set([start // 512, (start + dil * 127) // 512]))
                            parts = [1] if g == 0 else [0, 1]
                            g0_step()
                            sbk = (0, 1, 2, 6)[nblk % 4]
                            obk = (3, 4, 5, 7)[nblk % 4]
                            ei = nblk % 4
                            nblk += 1
                            for hh in range(2):
                                ps_ = slice(hh * 64, hh * 64 + 64)
                                for part in parts:
                                    if part == 1:
                                        ksl = qsl
                                        tgk = tgq
                                    else:
                                        ps0 = start - dil * 128
                                        ksl = slice(ps0, ps0 + dil * 127 + 1, dil)
                                        tgk = sorted(set([ps0 // 512, (ps0 + dil * 127) // 512]))
                                    S.pe(lambda e, ps_=ps_, ksl=ksl, qsl=qsl, hh=hh, part=part, sbk=sbk: e.matmul(
                                        banks[sbk][:, (hh * 2 + part) * 128:(hh * 2 + part + 1) * 128],
                                        lhsT=kT[ps_, ksl], rhs=qT[ps_, qsl], start=True, stop=True),
                                        reads=[t_k[x] for x in tgk] + [t_q[x] for x in tgq], writes=[bt[sbk]], strided=(dil > 1))
                            S.act(lambda e, ei=ei, sbk=sbk: e.activation(out=e0[ei][:], in_=banks[sbk][:], func=AF.Exp, scale=0.125),
                                  reads=[bt[sbk]], writes=[t_e0[ei]])
                            S.dve(lambda e, ei=ei, br=br, hp=hp: e.tensor_tensor(
                                out=ee[ei][:], in0=e0[ei][:],
                                in1=EBT[:, br * 8 + 2 * hp:br * 8 + 2 * hp + 2, :, :].rearrange("p a b c -> p (a b c)"), op=ALU.mult),
                                reads=[t_e0[ei], t_ebt], writes=[t_ee[ei]])
                            for hh in range(2):
                                po = slice(hh * 64, hh * 64 + 64)
                                for pi, part in enumerate(parts):
                                    jj = j if part == 1 else j - 1
                                    esl = slice((hh * 2 + part) * 128, (hh * 2 + part + 1) * 128)
                                    S.pe(lambda e, po=po, br=br, jj=jj, hh=hh, esl=esl, ei=ei, obk=obk, pi=pi, parts=parts: e.matmul(
                                        banks[obk][po, 0:128], lhsT=V[:, br, jj, hh * 64:(hh + 1) * 64], rhs=ee[ei][:, esl],
                                        start=(pi == 0), stop=(pi == len(parts) - 1)),
                                        reads=[t_v[br][jj], t_ee[ei]], writes=[bt[obk]])
                                for pi, part in enumerate(parts):
                                    esl = slice((hh * 2 + part) * 128, (hh * 2 + part + 1) * 128)
                                    S.pe(lambda e, po=po, esl=esl, ei=ei, obk=obk, pi=pi, parts=parts: e.matmul(
                                        banks[obk][po, 128:256], lhsT=onesb[:], rhs=ee[ei][:, esl],
                                        start=(pi == 0), stop=(pi == len(parts) - 1)),
                                        reads=[t_const, t_ee[ei]], writes=[bt[obk]])
                            lo = start - half * 2048
                            asl = slice(lo, lo + dil * 127 + 1, dil)
                            if br == 0:
                                S.dve(lambda e, asl=asl, obk=obk: e.tensor_copy(out=accO[:, asl], in_=banks[obk][:, 0:128]),
                                      reads=[bt[obk]], writes=[t_acc])
                                S.dve(lambda e, asl=asl, obk=obk: e.tensor_copy(out=accS[:, asl], in_=banks[obk][:, 128:256]),
                                      reads=[bt[obk]], writes=[t_acc])
                            else:
                                S.dve(lambda e, asl=asl, obk=obk: e.tensor_tensor(out=accO[:, asl], in0=accO[:, asl], in1=banks[obk][:, 0:128], op=ALU.add),
                                      reads=[bt[obk], t_acc], writes=[t_acc])
                                S.dve(lambda e, asl=asl, obk=obk: e.tensor_tensor(out=accS[:, asl], in0=accS[:, asl], in1=banks[obk][:, 128:256], op=ALU.add),
                                      reads=[bt[obk], t_acc], writes=[t_acc])
                S.dve(lambda e: e.reciprocal(out=accS[:], in_=accS[:]), reads=[t_acc], writes=[t_acc])
                S.dve(lambda e: e.tensor_tensor(out=mixo[:], in0=accO[:], in1=accS[:], op=ALU.mult), reads=[t_acc], writes=[t_mixo, t_acc])
                S.dma(lambda e, hp=hp, half=half: e.dma_start(out=mixT_d[hp, :, half * 2048:(half + 1) * 2048], in_=mixo[:]), reads=[t_mixo])
                if debug:
                    S.dma(lambda e, hp=hp, half=half: e.dma_start(out=O["dbg_mixT"][hp, :, half * 2048:(half + 1) * 2048], in_=mixo[:]), reads=[t_mixo])
                ck(3.8)

    while g0_tick[0] < len(g0_blocks) + 2:
        g0_step()
    S.barrier()
    stC.close()
    if phase_limit < 4:
        S.emit()
        for sk_ in (stC_holder + [stH, st]):
            sk_.close()
        return nc
    S.nosync = True
    NEG = -0.6065306597126334
    with contextlib.ExitStack() as st2, contextlib.suppress(_Stop):
        t_rp = Tok("rwparams")
        rwp = sbt(st2, "rwp", [128, 8, 4])
        mul_ = sbt(st2, "mul", [128, 1])
        lw3 = sbt(st2, "lw3", [128, 512])
        w0row = sbt(st2, "w0row", [1, 512])
        lnxg = sbt(st2, "lnxg", [128, 512])
        lnxb = sbt(st2, "lnxb", [128, 512])
        tri = sbt(st2, "tri", [128, 2, 128])
        mk1 = sbt(st2, "mk1", [128, 2, 64])
        mk3 = sbt(st2, "mk3", [128, 64])
        id2 = sbt(st2, "id2", [128, 64])
        omka = sbt(st2, "omka", [128, 4])
        gneps = sbt(st2, "gneps", [128, 1])
        for dst, src in ((rwp, "rwp"), (mul_, "mul"), (lw3, "lw3"), (w0row, "w0row"), (tri, "tri"), (mk1, "mk1"), (mk3, "mk3"), (id2, "id2")):
            S.dma(lambda e, dst=dst, src=src: e.dma_start(out=dst[:], in_=I[src]), writes=[t_rp])
        S.dma(lambda e: e.dma_start(out=lnxg[:], in_=bass.AP(I["lnx"].tensor, 0, [[0, 128], [1, 512]])), writes=[t_rp])
        S.dma(lambda e: e.dma_start(out=lnxb[:], in_=bass.AP(I["lnx"].tensor, 512, [[0, 128], [1, 512]])), writes=[t_rp])
        S.dve(lambda e: e.tensor_scalar(out=omka[:], in0=rwp[:, 4, :], scalar1=-1.0, scalar2=1.0, op0=ALU.mult, op1=ALU.add),
              reads=[t_rp], writes=[t_rp])
        S.pool(lambda e: e.memset(gneps[:], 64e-5), writes=[t_rp])
        wr = sbt(st2, "wr", [128, 8, 1664], BF16)
        t_wr = Tok()
        with contextlib.ExitStack() as st3:
            wst2 = [sbt(st3, "wst2", [128, 8, 416]) for i in range(2)]
            wst2t = [Tok(), Tok()]
            for q4 in range(4):
                w_ = wst2[q4 % 2]
                S.dma(lambda e, w_=w_, q4=q4: e.dma_start(out=w_[:], in_=win_v[:, :, 1536 + q4 * 416:1536 + (q4 + 1) * 416]), writes=[wst2t[q4 % 2]])
                S.pool(lambda e, w_=w_, q4=q4: e.tensor_copy(out=wr[:, :, q4 * 416:(q4 + 1) * 416], in_=w_[:]), reads=[wst2t[q4 % 2]], writes=[t_wr])
        S.barrier()

        def T_(n=""):
            return Tok(n)

        pb = [sbt(st2, "pb", [128, 4, 129]) for x in range(3)]
        pbl = sbt(st2, "pbl", [128, 129])
        t_pb = T_()
        for x in range(3):
            S.pool(lambda e, x=x: e.memset(pb[x][:, :, 0:1], 0.0), writes=[t_pb])
        S.pool(lambda e: e.memset(pbl[:, 0:1], 0.0), writes=[t_pb])
        xm = [sbt(st2, "xm", [128, 4, 128]) for x in range(3)]
        xml = sbt(st2, "xml", [128, 128])
        t_xm = T_()
        twl = sbt(st2, "twl", [128, 128])
        sg_tok = sbt(st2, "sg_tok", [128, 512])
        aT = sbt(st2, "aT", [128, 4, 128])
        g_tok = sbt(st2, "g_tok", [128, 512])
        kk = sbt(st2, "kk", [128, 4, 128])
        sq4 = sbt(st2, "sq4", [128, 4, 128])
        kmod = sbt(st2, "kmod", [128, 4, 128])
        bb = sbt(st2, "bb", [128, 4, 128])
        rk = sbt(st2, "rk", [128, 4, 128])
        dtmp = rk
        bsum = sbt(st2, "bsum", [128, 8])
        ycen = sbt(st2, "ycen", [128, 8, 64])
        ysq = sbt(st2, "ysq", [128, 8, 64])
        gst = sbt(st2, "gst", [128, 8])
        gst2 = sbt(st2, "gst2", [128, 8])
        mixr = sbt(st2, "mixr", [128, 4, 128], BF16)
        st4 = contextlib.ExitStack()
        st2.enter_context(st4)
        Pin = sbt(st4, "Pin", [128, 4, 128])
        Pinv = sbt(st4, "Pinv", [128, 4, 128])
        Pex = sbt(st4, "Pex", [128, 4, 128])
        Phat = sbt(st4, "Phat", [128, 4, 128])
        PCl = sbt(st4, "PCl", [128, 4, 2])
        PC = sbt(st4, "PC", [128, 4, 2])
        AR = sbt(st4, "AR", [128, 4, 2, 2, 64])
        BtT = sbt(st4, "BtT", [128, 4, 128])
        AtT = sbt(st4, "AtT", [128, 4, 128])
        KtT = sbt(st4, "KtT", [128, 4, 128])
        BhT = sbt(st4, "BhT", [128, 4, 128])
        KhT = sbt(st4, "KhT", [128, 4, 128])
        Atok = sbt(st4, "Atok", [128, 512])
        Bhtok = sbt(st4, "Bhtok", [128, 512])
        Khtok = sbt(st4, "Khtok", [128, 512])
        Vtok = sbt(st4, "Vtok", [128, 512])
        NM = sbt(st4, "NM", [128, 8, 2, 64])
        AK = sbt(st4, "AK", [128, 8, 2, 64])
        Aj = [sbt(st4, "Aj", [128, 8, 64])] * 2
        Nj = [sbt(st4, "Nj", [128, 8, 64])] * 2
        Tj = [sbt(st4, "Tj", [128, 8, 64])] * 2
        Z = sbt(st4, "Z", [128, 8, 128])
        AV = sbt(st4, "AV", [128, 8, 128])
        McT = sbt(st4, "McT", [128, 4, 2, 64])
        dPC = sbt(st4, "dPC", [128, 4, 2, 64])
        RpT = sbt(st4, "RpT", [128, 4, 2, 64])
        Hs = sbt(st4, "Hs", [128, 4, 64])
        t_H = T_()
        S.pool(lambda e: e.memset(Hs[:], 0.0), writes=[t_H])
        (t_lora, t_sg, t_a, t_g, t_kk, t_km, t_b, t_rk, t_bs, t_P, t_AR, t_BK, t_BKh, t_tok, t_NM, t_AK, t_A0, t_Z, t_AV,
         t_Mc, t_Rp, t_y, t_mixr) = [T_() for i in range(23)]
        t_Aj = [T_()] * 2
        t_Nj = [T_()] * 2
        t_Tj = [T_()] * 2
        B = banks

        def v4(ap):
            return ap.rearrange("p q (c t) -> p q c t", c=2)

        for sbi in range(32):
            t0 = sbi * 128
            hg = t_h[t0 // 512]
            for x in range(3):
                bk = x
                for p in range(4):
                    for c in range(8):
                        S.pe(lambda e, x=x, p=p, c=c, bk=bk, t0=t0: e.matmul(
                            B[bk][:, p * 128:(p + 1) * 128], lhsT=wr[:, c, x * 512 + p * 128:x * 512 + (p + 1) * 128],
                            rhs=hT[:, c, t0:t0 + 128], start=(c == 0), stop=(c == 7)), reads=[t_wr, hg], writes=[bt[bk]])
                S.act(lambda e, x=x, bk=bk: e.activation(out=pb[x][:, :, 1:129], in_=B[bk][:].rearrange("p (q t) -> p q t", q=4), func=AF.Copy),
                      reads=[bt[bk], t_xm], writes=[t_pb])
            for c in range(8):
                S.pe(lambda e, c=c, t0=t0: e.matmul(B[3][:, 0:128], lhsT=wr[:, c, 1536:1664], rhs=hT[:, c, t0:t0 + 128],
                                                     start=(c == 0), stop=(c == 7)), reads=[t_wr, hg], writes=[bt[3]])
            S.act(lambda e: e.activation(out=pbl[:, 1:129], in_=B[3][:, 0:128], func=AF.Copy), reads=[bt[3], t_xm], writes=[t_pb])
            if sbi == 31:
                for x in range(3):
                    S.dma(lambda e, x=x: e.dma_start(
                        out=bass.AP(O["shp"].tensor, x * 512, [[1, 128], [128, 4], [1, 1]]), in_=pb[x][:, :, 128:129], allow_slow_non_contiguous=True), reads=[t_pb])
                S.dma(lambda e: e.dma_start(out=bass.AP(O["shp"].tensor, 1536, [[1, 128], [1, 1]]), in_=pbl[:, 128:129], allow_slow_non_contiguous=True), reads=[t_pb])
            for x in range(3):
                S.dve(lambda e, x=x: e.tensor_tensor(out=dtmp[:], in0=pb[x][:, :, 0:128], in1=pb[x][:, :, 1:129], op=ALU.subtract),
                      reads=[t_pb], writes=[t_xm, t_rk])
                S.dve(lambda e, x=x: e.tensor_tensor(out=dtmp[:], in0=dtmp[:], in1=bc(rwp[:, x, :].unsqueeze(2), [128, 4, 128]), op=ALU.mult),
                      reads=[t_xm, t_rp, t_rk], writes=[t_xm, t_rk])
                S.dve(lambda e, x=x: e.tensor_tensor(out=xm[x][:], in0=dtmp[:], in1=pb[x][:, :, 1:129], op=ALU.add),
                      reads=[t_xm, t_pb, t_rk], writes=[t_xm])
            S.dve(lambda e: e.tensor_tensor(out=xml[:], in0=pbl[:, 0:128], in1=pbl[:, 1:129], op=ALU.subtract), reads=[t_pb], writes=[t_lora])
            S.dve(lambda e: e.scalar_tensor_tensor(out=xml[:], in0=xml[:], scalar=mul_[:, 0:1], in1=pbl[:, 1:129], op0=ALU.mult, op1=ALU.add),
                  reads=[t_lora, t_pb, t_rp], writes=[t_lora])
            for x in range(3):
                S.pool(lambda e, x=x: e.tensor_copy(out=pb[x][:, :, 0:1], in_=pb[x][:, :, 128:129]), reads=[t_pb, t_xm], writes=[t_pb])
            S.pool(lambda e: e.tensor_copy(out=pbl[:, 0:1], in_=pbl[:, 128:129]), reads=[t_pb, t_lora], writes=[t_pb])
            S.act(lambda e: e.activation(out=twl[0:32, :], in_=xml[0:32, :], func=AF.Tanh), reads=[t_lora], writes=[t_sg])
            S.act(lambda e: e.activation(out=twl[64:128, :], in_=xml[64:128, :], func=AF.Sigmoid), reads=[t_lora], writes=[t_sg])
            S.pe(lambda e: e.matmul(B[4][:], lhsT=twl[0:32, :], rhs=lw3[0:32, :], start=True, stop=False), reads=[t_sg, t_rp], writes=[bt[4]])
            S.pe(lambda e: e.matmul(B[4][:], lhsT=ones[0:1, :], rhs=w0row[0:1, :], start=False, stop=True), reads=[t_const, t_rp], writes=[bt[4]])
            S.act(lambda e: e.activation(out=sg_tok[:], in_=B[4][:], func=AF.Sigmoid), reads=[bt[4]], writes=[t_sg])
            for p in range(4):
                S.pe(lambda e, p=p: e.matmul(B[5][:, p * 128:(p + 1) * 128], lhsT=lw3[32:64, p * 128:(p + 1) * 128], rhs=xml[32:64, :],
                                              start=True, stop=True), reads=[t_lora, t_rp], writes=[bt[5]])
            S.dve(lambda e: e.tensor_tensor(out=aT[:], in0=B[5][:].rearrange("p (q t) -> p q t", q=4),
                                            in1=bc(rwp[:, 6, :].unsqueeze(2), [128, 4, 128]), op=ALU.add), reads=[bt[5], t_rp], writes=[t_a])
            S.act(lambda e: e.activation(out=aT[:], in_=aT[:], func=AF.Sigmoid), reads=[t_a], writes=[t_a])
            S.pe(lambda e: e.matmul(B[6][:], lhsT=twl[64:128, :], rhs=lw3[64:128, :], start=True, stop=True), reads=[t_sg, t_rp], writes=[bt[6]])
            S.act(lambda e: e.activation(out=g_tok[:], in_=B[6][:], func=AF.Copy), reads=[bt[6], t_y], writes=[t_g])
            S.dve(lambda e: e.tensor_tensor(out=kk[:], in0=xm[1][:], in1=bc(rwp[:, 3, :].unsqueeze(2), [128, 4, 128]), op=ALU.mult),
                  reads=[t_xm, t_rp], writes=[t_kk])
            S.act(lambda e: e.activation(out=sq4[:], in_=kk[:], func=AF.Square), reads=[t_kk], writes=[t_kk])
            S.pe(lambda e: e.matmul(B[7][:], lhsT=blk2[:], rhs=sq4[:].rearrange("p q t -> p (q t)"), start=True, stop=True),
                 reads=[t_kk, t_const], writes=[bt[7]])
            S.act(lambda e: e.activation(out=sq4[:].rearrange("p q t -> p (q t)"), in_=B[7][:], func=AF.Sqrt), reads=[bt[7]], writes=[t_kk])
            S.dve(lambda e: e.tensor_scalar(out=sq4[:], in0=sq4[:], scalar1=1e-12, scalar2=None, op0=ALU.max), reads=[t_kk], writes=[t_kk])
            S.dve(lambda e: e.reciprocal(out=sq4[:], in_=sq4[:]), reads=[t_kk], writes=[t_kk])
            S.dve(lambda e: e.tensor_tensor(out=kk[:], in0=kk[:], in1=sq4[:], op=ALU.mult), reads=[t_kk], writes=[t_kk])
            S.dve(lambda e: e.tensor_tensor(out=kmod[:], in0=aT[:], in1=bc(rwp[:, 4, :].unsqueeze(2), [128, 4, 128]), op=ALU.mult),
                  reads=[t_a, t_rp], writes=[t_km])
            S.dve(lambda e: e.tensor_tensor(out=kmod[:], in0=kmod[:], in1=bc(omka[:].unsqueeze(2), [128, 4, 128]), op=ALU.add),
                  reads=[t_km, t_rp], writes=[t_km])
            S.dve(lambda e: e.tensor_tensor(out=kmod[:], in0=kmod[:], in1=xm[1][:], op=ALU.mult), reads=[t_km, t_xm], writes=[t_km])
            S.dve(lambda e: e.tensor_tensor(out=bb[:], in0=kk[:], in1=aT[:], op=ALU.mult), reads=[t_kk, t_a], writes=[t_b])
            S.pool(lambda e: e.tensor_tensor(out=rk[:], in0=xm[0][:], in1=kmod[:], op=ALU.mult), reads=[t_xm, t_km], writes=[t_rk])
            S.pool(lambda e: e.tensor_tensor(out=rk[:], in0=rk[:], in1=bc(rwp[:, 5, :].unsqueeze(2), [128, 4, 128]), op=ALU.mult),
                   reads=[t_rk, t_rp], writes=[t_rk])
            for h in (0, 2, 4, 6, 1, 3, 5, 7):
                p, hh = h // 2, h % 2
                fp = slice(hh * 64, hh * 64 + 64)
                S.pe(lambda e, p=p, fp=fp, h=h: e.matmul(B[6][:, 256 + h:256 + h + 1], lhsT=rk[fp, p, :], rhs=ones[fp, 0:1], start=True, stop=True),
                     reads=[t_rk, t_const], writes=[bt[6]])
            S.act(lambda e: e.activation(out=bsum[:], in_=B[6][:, 256:264], func=AF.Copy), reads=[bt[6], t_y], writes=[t_bs])
            for p in range(4):
                S.pe(lambda e, p=p: e.matmul(B[0][:, p * 128:(p + 1) * 128], lhsT=sg_tok[:, p * 128:(p + 1) * 128], rhs=tri[:, 0, :],
                                              start=True, stop=True), reads=[t_sg, t_rp], writes=[bt[0]])
            for p in range(4):
                S.pe(lambda e, p=p: e.matmul(B[1][:, p * 128:(p + 1) * 128], lhsT=sg_tok[:, p * 128:(p + 1) * 128], rhs=tri[:, 1, :],
                                              start=True, stop=True), reads=[t_sg, t_rp], writes=[bt[1]])
            lp = B[0][:].rearrange("p (q t) -> p q t", q=4)
            S.act(lambda e: e.activation(out=Pin[:], in_=lp, func=AF.Exp), reads=[bt[0]], writes=[t_P])
            S.act(lambda e: e.activation(out=Pinv[:], in_=lp, func=AF.Exp, scale=-1.0), reads=[bt[0]], writes=[t_P])
            S.act(lambda e: e.activation(out=Pex[:], in_=B[1][:].rearrange("p (q t) -> p q t", q=4), func=AF.Exp), reads=[bt[1]], writes=[t_P])
            lp4 = B[0][:].rearrange("p (q c t) -> p q c t", q=4, c=2)
            S.act(lambda e: e.activation(out=PCl[:], in_=lp4[:, :, :, 63], func=AF.Copy), reads=[bt[0]], writes=[t_P])
            S.act(lambda e: e.activation(out=v4(Phat[:]), in_=lp4, func=AF.Copy), reads=[bt[0]], writes=[t_P])
            S.dve(lambda e: e.tensor_tensor(out=v4(Phat[:]), in0=bc(PCl[:].unsqueeze(3), [128, 4, 2, 64]), in1=v4(Phat[:]), op=ALU.subtract),
                  reads=[t_P], writes=[t_P])
            S.act(lambda e: e.activation(out=Phat[:], in_=Phat[:], func=AF.Exp), reads=[t_P], writes=[t_P])
            S.act(lambda e: e.activation(out=PC[:], in_=PCl[:], func=AF.Exp), reads=[t_P], writes=[t_P])
            S.dve(lambda e: e.scalar_tensor_tensor(out=AR[:, :, :, 0, :], in0=v4(kk[:]), scalar=-1.0, in1=v4(Pex[:]), op0=ALU.mult, op1=ALU.mult),
                  reads=[t_kk, t_P], writes=[t_AR])
            S.dve(lambda e: e.tensor_tensor(out=AR[:, :, :, 1, :], in0=v4(xm[0][:]), in1=v4(Pin[:]), op=ALU.mult), reads=[t_xm, t_P], writes=[t_AR])
            S.pool(lambda e: e.tensor_tensor(out=BtT[:], in0=bb[:], in1=Pinv[:], op=ALU.mult), reads=[t_b, t_P], writes=[t_BK])
            S.pool(lambda e: e.tensor_tensor(out=KtT[:], in0=kmod[:], in1=Pinv[:], op=ALU.mult), reads=[t_km, t_P], writes=[t_BK])
            S.pool(lambda e: e.tensor_tensor(out=BhT[:], in0=bb[:], in1=Phat[:], op=ALU.mult), reads=[t_b, t_P], writes=[t_BKh])
            S.pool(lambda e: e.tensor_tensor(out=KhT[:], in0=kmod[:], in1=Phat[:], op=ALU.mult), reads=[t_km, t_P], writes=[t_BKh])
            S.pool(lambda e: e.tensor_copy(out=v4(AtT[:]), in_=AR[:, :, :, 0, :]), reads=[t_AR], writes=[t_BKh])
            for src_fn, dst, rd, bk in ((lambda p: AtT[:, p, :], Atok, [t_BKh], 2), (lambda p: BhT[:, p, :], Bhtok, [t_BKh], 3),
                                        (lambda p: KhT[:, p, :], Khtok, [t_BKh], 4), (lambda p: xm[2][:, p, :], Vtok, [t_xm], 5)):
                for p in range(4):
                    S.pe(lambda e, p=p, src_fn=src_fn, bk=bk: e.transpose(B[bk][:, p * 128:(p + 1) * 128], src_fn(p), ident[:]),
                         reads=rd + [t_const], writes=[bt[bk]])
                S.act(lambda e, dst=dst, bk=bk: e.activation(out=dst[:], in_=B[bk][:], func=AF.Copy), reads=[bt[bk], t_y, t_AV, t_Z], writes=[t_tok])
            for ch in range(2):
                tp = slice(ch * 64, ch * 64 + 64)
                for h in (0, 2, 4, 6, 1, 3, 5, 7):
                    p, hh = h // 2, h % 2
                    fp = slice(hh * 64, hh * 64 + 64)
                    csl = slice(ch * 64, ch * 64 + 64)
                    arr = AR[fp, p, ch, :, :].rearrange("p a t -> p (a t)")
                    S.pe(lambda e, tp=tp, fp=fp, p=p, h=h, csl=csl, arr=arr: e.matmul(
                        B[0][tp, (h % 4) * 128:(h % 4 + 1) * 128] if h < 4 else B[1][tp, (h % 4) * 128:(h % 4 + 1) * 128],
                        lhsT=BtT[fp, p, csl], rhs=arr, start=True, stop=True), reads=[t_BK, t_AR], writes=[bt[0 if h < 4 else 1]])
                    S.pe(lambda e, tp=tp, fp=fp, p=p, h=h, csl=csl, arr=arr: e.matmul(
                        B[2][tp, (h % 4) * 128:(h % 4 + 1) * 128] if h < 4 else B[3][tp, (h % 4) * 128:(h % 4 + 1) * 128],
                        lhsT=KtT[fp, p, csl], rhs=arr, start=True, stop=True), reads=[t_BK, t_AR], writes=[bt[2 if h < 4 else 3]])
                    S.pe(lambda e, tp=tp, fp=fp, p=p, h=h, csl=csl, ch=ch: e.matmul(
                        B[4][tp, h * 64:(h + 1) * 64], lhsT=AR[fp, p, ch, 0, :], rhs=BtT[fp, p, csl], start=True, stop=True),
                        reads=[t_BK, t_AR], writes=[bt[4]])
            mk1b = bc(mk1[:].unsqueeze(1), [128, 4, 2, 64])
            for hf in range(2):
                S.dve(lambda e, hf=hf: e.tensor_tensor(out=NM[:, hf * 4:(hf + 1) * 4, :, :], in0=B[hf][:].rearrange("p (h a t) -> p h a t", h=4, a=2),
                                                        in1=mk1b, op=ALU.mult), reads=[bt[hf], t_rp], writes=[t_NM])
                S.dve(lambda e, hf=hf: e.tensor_tensor(out=AK[:, hf * 4:(hf + 1) * 4, :, :], in0=B[2 + hf][:].rearrange("p (h a t) -> p h a t", h=4, a=2),
                                                        in1=mk1b, op=ALU.mult), reads=[bt[2 + hf], t_rp], writes=[t_AK])
            S.dve(lambda e: e.tensor_tensor(out=Aj[0][:], in0=B[4][:].rearrange("p (h t) -> p h t", h=8), in1=bc(mk3[:].unsqueeze(1), [128, 8, 64]), op=ALU.mult),
                  reads=[bt[4], t_rp], writes=[t_Aj[0]])
            S.pool(lambda e: e.tensor_copy(out=Nj[0][:], in_=NM[:, :, 0, :]), reads=[t_NM], writes=[t_Nj[0]])
            S.pool(lambda e: e.tensor_tensor(out=Tj[0][:], in0=NM[:, :, 0, :], in1=bc(id2[:].unsqueeze(1), [128, 8, 64]), op=ALU.add),
                   reads=[t_NM, t_rp], writes=[t_Tj[0]])
            for lv in range(1, 6):
                a_o = Aj[0]
                n_o = Nj[0]
                t_o = Tj[0]
                ta, tn, tt = t_Aj[0], t_Nj[0], t_Tj[0]
                for ch in range(2):
                    tp = slice(ch * 64, ch * 64 + 64)
                    for h in range(8):
                        S.pe(lambda e, tp=tp, h=h: e.matmul(B[5][tp, h * 64:(h + 1) * 64], lhsT=n_o[tp, h, :], rhs=a_o[tp, h, :],
                                                             start=True, stop=True), reads=[ta, tn], writes=[bt[5]])
                if lv < 5:
                    for ch in range(2):
                        tp = slice(ch * 64, ch * 64 + 64)
                        for h in range(8):
                            S.pe(lambda e, tp=tp, h=h: e.matmul(B[6][tp, h * 64:(h + 1) * 64], lhsT=a_o[tp, h, :], rhs=n_o[tp, h, :],
                                                                 start=True, stop=True), reads=[ta, tn], writes=[bt[6]])
                S.act(lambda e: e.activation(out=a_o[:].rearrange("p h t -> p (h t)"), in_=B[5][:], func=AF.Copy), reads=[bt[5]], writes=[ta])
                if lv < 5:
                    S.act(lambda e: e.activation(out=n_o[:].rearrange("p h t -> p (h t)"), in_=B[6][:], func=AF.Copy), reads=[bt[6]], writes=[tn])
                for ch in range(2):
                    tp = slice(ch * 64, ch * 64 + 64)
                    for h in range(8):
                        S.pe(lambda e, tp=tp, h=h: e.matmul(B[7][tp, h * 64:(h + 1) * 64], lhsT=a_o[tp, h, :], rhs=t_o[tp, h, :],
                                                             start=True, stop=True), reads=[ta, tt], writes=[bt[7]])
                S.dve(lambda e: e.tensor_tensor(out=t_o[:], in0=B[7][:].rearrange("p (h t) -> p h t", h=8), in1=t_o[:], op=ALU.add),
                      reads=[bt[7], tt], writes=[tt])
            TT = Tj[5 % 2]
            t_TT = t_Tj[5 % 2]
            for ch in range(2):
                tp = slice(ch * 64, ch * 64 + 64)
                for h in range(8):
                    S.pe(lambda e, tp=tp, h=h: e.matmul(B[4][tp, h * 64:(h + 1) * 64], lhsT=AK[tp, h, 0, :], rhs=Vtok[tp, h * 64:(h + 1) * 64],
                                                         start=True, stop=True), reads=[t_AK, t_tok], writes=[bt[4]])
            S.act(lambda e: e.activation(out=Z[:, :, 64:128], in_=B[4][:].rearrange("p (h t) -> p h t", h=8), func=AF.Copy), reads=[bt[4]], writes=[t_Z])
            S.pool(lambda e: e.tensor_copy(out=Z[:, :, 0:64], in_=Atok[:].rearrange("p (h t) -> p h t", h=8)), reads=[t_tok], writes=[t_Z])
            for ch in range(2):
                tp = slice(ch * 64, ch * 64 + 64)
                for h in range(8):
                    S.pe(lambda e, tp=tp, h=h: e.matmul(B[h // 4][tp, (h % 4) * 128:(h % 4 + 1) * 128], lhsT=TT[tp, h, :], rhs=Z[tp, h, :],
                                                         start=True, stop=True), reads=[t_TT, t_Z], writes=[bt[h // 4]])
            for hf in range(2):
                S.act(lambda e, hf=hf: e.activation(out=AV[:, hf * 4:(hf + 1) * 4, :].rearrange("p h t -> p (h t)"), in_=B[hf][:], func=AF.Copy),
                      reads=[bt[hf]], writes=[t_AV])
            for ch in range(2):
                tp = slice(ch * 64, ch * 64 + 64)
                for h in range(8):
                    p, hh = h // 2, h % 2
                    fp = slice(hh * 64, hh * 64 + 64)
                    col = (p * 2 + ch) * 64
                    S.pe(lambda e, tp=tp, fp=fp, h=h, col=col: e.matmul(B[2][fp, col:col + 64], lhsT=AV[tp, h, 0:64], rhs=Bhtok[tp, h * 64:(h + 1) * 64],
                                                                         start=True, stop=True), reads=[t_AV, t_tok], writes=[bt[2]])
                    S.pe(lambda e, tp=tp, fp=fp, h=h, col=col: e.matmul(B[3][fp, col:col + 64], lhsT=AV[tp, h, 0:64], rhs=NM[tp, h, 1, :],
                                                                         start=True, stop=True), reads=[t_AV, t_NM], writes=[bt[3]])
            S.dve(lambda e: e.tensor_tensor(out=dPC[:], in0=bc(PC[:].unsqueeze(3), [128, 4, 2, 64]),
                                            in1=bc(id2[:].unsqueeze(1).unsqueeze(1), [128, 4, 2, 64]), op=ALU.mult), reads=[t_P, t_rp], writes=[t_Mc])
            S.dve(lambda e: e.tensor_tensor(out=McT[:], in0=B[2][:].rearrange("p (q c t) -> p q c t", q=4, c=2), in1=dPC[:], op=ALU.add),
                  reads=[bt[2], t_Mc], writes=[t_Mc])
            S.dve(lambda e: e.tensor_tensor(out=RpT[:], in0=B[3][:].rearrange("p (q c t) -> p q c t", q=4, c=2), in1=AR[:, :, :, 1, :], op=ALU.add),
                  reads=[bt[3], t_AR], writes=[t_Rp])
            for ch in range(2):
                tp = slice(ch * 64, ch * 64 + 64)
                for h in range(8):
                    p, hh = h // 2, h % 2
                    fp = slice(hh * 64, hh * 64 + 64)
                    S.pe(lambda e, tp=tp, h=h: e.matmul(B[6][tp, h * 64:(h + 1) * 64], lhsT=NM[tp, h, 1, :], rhs=AV[tp, h, 64:128], start=True, stop=False),
                         reads=[t_NM, t_AV], writes=[bt[6]])
                    S.pe(lambda e, tp=tp, h=h: e.matmul(B[6][tp, h * 64:(h + 1) * 64], lhsT=AK[tp, h, 1, :], rhs=Vtok[tp, h * 64:(h + 1) * 64], start=False, stop=False),
                         reads=[t_AK, t_tok], writes=[bt[6]])
                    S.pe(lambda e, tp=tp, fp=fp, p=p, h=h, ch=ch: e.matmul(B[6][tp, h * 64:(h + 1) * 64], lhsT=RpT[fp, p, ch, :], rhs=Hs[fp, p, :], start=False, stop=True),
                         reads=[t_Rp, t_H], writes=[bt[6]])
                for h in range(8):
                    p, hh = h // 2, h % 2
                    fp = slice(hh * 64, hh * 64 + 64)
                    S.pe(lambda e, tp=tp, fp=fp, p=p, h=h: e.matmul(B[5][fp, p * 64:(p + 1) * 64], lhsT=Bhtok[tp, h * 64:(h + 1) * 64], rhs=AV[tp, h, 64:128], start=True, stop=False),
                         reads=[t_tok, t_AV], writes=[bt[5]])
                    S.pe(lambda e, tp=tp, fp=fp, p=p, h=h: e.matmul(B[5][fp, p * 64:(p + 1) * 64], lhsT=Khtok[tp, h * 64:(h + 1) * 64], rhs=Vtok[tp, h * 64:(h + 1) * 64], start=False, stop=False),
                         reads=[t_tok], writes=[bt[5]])
                    S.pe(lambda e, fp=fp, p=p, ch=ch: e.matmul(B[5][fp, p * 64:(p + 1) * 64], lhsT=McT[fp, p, ch, :], rhs=Hs[fp, p, :], start=False, stop=True),
                         reads=[t_Mc, t_H], writes=[bt[5]])
                S.act(lambda e: e.activation(out=Hs[:].rearrange("p q v -> p (q v)"), in_=B[5][:, 0:256], func=AF.Copy), reads=[bt[5]], writes=[t_H])
            yps = B[6][:].rearrange("p (h v) -> p h v", h=8)
            S.dve(lambda e: e.tensor_reduce(out=gst[:], in_=yps, axis=AX.X, op=ALU.add), reads=[bt[6]], writes=[t_y])
            S.dve(lambda e: e.tensor_scalar(out=gst[:], in0=gst[:], scalar1=1.0 / 64, scalar2=None, op0=ALU.mult), reads=[t_y], writes=[t_y])
            S.dve(lambda e: e.tensor_tensor(out=ycen[:], in0=yps, in1=bc(gst[:].unsqueeze(2), [128, 8, 64]), op=ALU.subtract), reads=[t_y, bt[6]], writes=[t_y])
            S.act(lambda e: e.activation(out=ysq[:], in_=ycen[:], func=AF.Square), reads=[t_y], writes=[t_y])
            S.dve(lambda e: e.tensor_reduce(out=gst2[:], in_=ysq[:], axis=AX.X, op=ALU.add), reads=[t_y], writes=[t_y])
            S.act(lambda e: e.activation(out=gst2[:], in_=gst2[:], func=AF.Sqrt, scale=1.0 / 64, bias=gneps[:]), reads=[t_y, t_rp], writes=[t_y])
            S.dve(lambda e: e.reciprocal(out=gst2[:], in_=gst2[:]), reads=[t_y], writes=[t_y])
            S.dve(lambda e: e.tensor_tensor(out=ycen[:], in0=ycen[:], in1=bc(gst2[:].unsqueeze(2), [128, 8, 64]), op=ALU.mult), reads=[t_y], writes=[t_y])
            yc2 = ycen[:].rearrange("p h v -> p (h v)")
            S.dve(lambda e: e.tensor_tensor(out=yc2, in0=yc2, in1=lnxg[:], op=ALU.mult), reads=[t_y, t_rp], writes=[t_y])
            S.dve(lambda e: e.tensor_tensor(out=yc2, in0=yc2, in1=lnxb[:], op=ALU.add), reads=[t_y, t_rp], writes=[t_y])
            S.pool(lambda e: e.tensor_tensor(out=ysq[:], in0=Vtok[:].rearrange("p (h v) -> p h v", h=8), in1=bc(bsum[:].unsqueeze(2), [128, 8, 64]), op=ALU.mult),
                   reads=[t_tok, t_bs, t_y], writes=[t_y])
            S.dve(lambda e: e.tensor_tensor(out=ycen[:], in0=ycen[:], in1=ysq[:], op=ALU.add), reads=[t_y], writes=[t_y])
            S.dve(lambda e: e.tensor_tensor(out=yc2, in0=yc2, in1=g_tok[:], op=ALU.mult), reads=[t_y, t_g], writes=[t_y])
            if debug:
                S.dma(lambda e, t0=t0: e.dma_start(out=O["dbg_yr"][t0:t0 + 128, :], in_=yc2), reads=[t_y])
            for p in range(4):
                S.pe(lambda e, p=p: e.transpose(B[7][:, p * 128:(p + 1) * 128], ycen[:, 2 * p:2 * p + 2, :].rearrange("p h v -> p (h v)"), ident[:]),
                     reads=[t_y, t_const], writes=[bt[7]])
            S.act(lambda e: e.activation(out=mixr[:].rearrange("p q t -> p (q t)"), in_=B[7][:], func=AF.Copy), reads=[bt[7]], writes=[t_mixr])
            S.dma(lambda e, t0=t0: e.dma_start(out=mixT_d[4:8, :, t0:t0 + 128].rearrange("q p t -> p q t"), in_=mixr[:]), reads=[t_mixr])
            ck(4.0 + 0.01 * (sbi + 1))
        S.dma(lambda e: e.dma_start(out=O["wkvp"], in_=Hs[:]), reads=[t_H])

        S.barrier()
        st4.close()
        ck(4.95)
        NSS = 64
        hs_perm = lambda c: hT[:, c, T:T + NS].rearrange("p (b t) -> p t b", t=4)
        for x in range(3):
            S.dma(lambda e, x=x: e.dma_start(out=pb[x][:, :, 0:16], in_=I["shT"][:, x, :, :]), writes=[t_pb])
        S.dma(lambda e: e.dma_start(out=pbl[:, 0:16], in_=I["shTl"]), writes=[t_pb])
        for x in range(3):
            bk = x
            for p in range(4):
                for c in range(8):
                    S.pe(lambda e, x=x, p=p, c=c, bk=bk: e.matmul(
                        B[bk][:, p * 128:p * 128 + NSS], lhsT=wr[:, c, x * 512 + p * 128:x * 512 + (p + 1) * 128],
                        rhs=hs_perm(c), start=(c == 0), stop=(c == 7)), reads=[t_wr, t_h[8]], writes=[bt[bk]])
            S.act(lambda e, x=x, bk=bk: e.activation(out=pb[x][:, :, 16:80], in_=B[bk][:].rearrange("p (q t) -> p q t", q=4)[:, :, 0:NSS], func=AF.Copy),
                  reads=[bt[bk], t_xm], writes=[t_pb])
        for c in range(8):
            S.pe(lambda e, c=c: e.matmul(B[3][:, 0:NSS], lhsT=wr[:, c, 1536:1664], rhs=hs_perm(c), start=(c == 0), stop=(c == 7)),
                 reads=[t_wr, t_h[8]], writes=[bt[3]])
        S.act(lambda e: e.activation(out=pbl[:, 16:80], in_=B[3][:, 0:NSS], func=AF.Copy), reads=[bt[3], t_xm], writes=[t_pb])
        shrow = sbt(st2, "shrow", [16, 1664])
        t_shrow = T_()
        hl = sbt(st2, "hl", [128, 8, 16], BF16)
        t_hl = T_()
        S.pool(lambda e: e.tensor_copy(out=hl[:], in_=hT[:, :, T + 3:T + NS:4]), reads=[t_h[8]], writes=[t_hl])
        for q4 in range(4):
            for c in range(8):
                S.pe(lambda e, q4=q4, c=c: e.matmul(B[4][0:16, 0:416], lhsT=hl[:, c, :], rhs=wr[:, c, q4 * 416:(q4 + 1) * 416],
                                                    start=(c == 0), stop=(c == 7)), reads=[t_wr, t_hl], writes=[bt[4]])
            S.act(lambda e, q4=q4: e.activation(out=shrow[:, q4 * 416:(q4 + 1) * 416], in_=B[4][0:16, 0:416], func=AF.Copy), reads=[bt[4]], writes=[t_shrow])
        S.dma(lambda e: e.dma_start(out=O["shs"], in_=shrow[:]), reads=[t_shrow])
        n_ = NSS
        for x in range(3):
            S.dve(lambda e, x=x: e.tensor_tensor(out=dtmp[:, :, 0:n_], in0=pb[x][:, :, 0:n_], in1=pb[x][:, :, 16:16 + n_], op=ALU.subtract),
                  reads=[t_pb], writes=[t_xm, t_rk])
            S.dve(lambda e, x=x: e.tensor_tensor(out=dtmp[:, :, 0:n_], in0=dtmp[:, :, 0:n_], in1=bc(rwp[:, x, :].unsqueeze(2), [128, 4, n_]), op=ALU.mult),
                  reads=[t_xm, t_rp, t_rk], writes=[t_xm, t_rk])
            S.dve(lambda e, x=x: e.tensor_tensor(out=xm[x][:, :, 0:n_], in0=dtmp[:, :, 0:n_], in1=pb[x][:, :, 16:16 + n_], op=ALU.add),
                  reads=[t_xm, t_pb, t_rk], writes=[t_xm])
        S.dve(lambda e: e.tensor_tensor(out=xml[:, 0:n_], in0=pbl[:, 0:n_], in1=pbl[:, 16:16 + n_], op=ALU.subtract), reads=[t_pb], writes=[t_lora])
        S.dve(lambda e: e.scalar_tensor_tensor(out=xml[:, 0:n_], in0=xml[:, 0:n_], scalar=mul_[:, 0:1], in1=pbl[:, 16:16 + n_], op0=ALU.mult, op1=ALU.add),
              reads=[t_lora, t_pb, t_rp], writes=[t_lora])
        S.act(lambda e: e.activation(out=twl[0:32, 0:n_], in_=xml[0:32, 0:n_], func=AF.Tanh), reads=[t_lora], writes=[t_sg])
        S.act(lambda e: e.activation(out=twl[64:128, 0:n_], in_=xml[64:128, 0:n_], func=AF.Sigmoid), reads=[t_lora], writes=[t_sg])
        S.pe(lambda e: e.matmul(B[4][0:n_, :], lhsT=twl[0:32, 0:n_], rhs=lw3[0:32, :], start=True, stop=False), reads=[t_sg, t_rp], writes=[bt[4]])
        S.pe(lambda e: e.matmul(B[4][0:n_, :], lhsT=ones[0:1, 0:n_], rhs=w0row[0:1, :], start=False, stop=True), reads=[t_const, t_rp], writes=[bt[4]])
        S.act(lambda e: e.activation(out=sg_tok[0:n_, :], in_=B[4][0:n_, :], func=AF.Sigmoid), reads=[bt[4]], writes=[t_sg])
        for p in range(4):
            S.pe(lambda e, p=p: e.matmul(B[5][:, p * 128:p * 128 + n_], lhsT=lw3[32:64, p * 128:(p + 1) * 128], rhs=xml[32:64, 0:n_],
                                          start=True, stop=True), reads=[t_lora, t_rp], writes=[bt[5]])
        S.dve(lambda e: e.tensor_tensor(out=aT[:, :, 0:n_], in0=B[5][:].rearrange("p (q t) -> p q t", q=4)[:, :, 0:n_],
                                        in1=bc(rwp[:, 6, :].unsqueeze(2), [128, 4, n_]), op=ALU.add), reads=[bt[5], t_rp], writes=[t_a])
        S.act(lambda e: e.activation(out=aT[:, :, 0:n_], in_=aT[:, :, 0:n_], func=AF.Sigmoid), reads=[t_a], writes=[t_a])
        S.pe(lambda e: e.matmul(B[6][0:n_, :], lhsT=twl[64:128, 0:n_], rhs=lw3[64:128, :], start=True, stop=True), reads=[t_sg, t_rp], writes=[bt[6]])
        S.act(lambda e: e.activation(out=g_tok[0:n_, :], in_=B[6][0:n_, :], func=AF.Copy), reads=[bt[6], t_y], writes=[t_g])
        S.dve(lambda e: e.tensor_tensor(out=kk[:, :, 0:n_], in0=xm[1][:, :, 0:n_], in1=bc(rwp[:, 3, :].unsqueeze(2), [128, 4, n_]), op=ALU.mult),
              reads=[t_xm, t_rp], writes=[t_kk])
        S.act(lambda e: e.activation(out=sq4[:, :, 0:n_], in_=kk[:, :, 0:n_], func=AF.Square), reads=[t_kk], writes=[t_kk])
        for p in range(4):
            S.pe(lambda e, p=p: e.matmul(B[7][:, p * 128:p * 128 + n_], lhsT=blk2[:], rhs=sq4[:, p, 0:n_], start=True, stop=True),
                 reads=[t_kk, t_const], writes=[bt[7]])
        S.act(lambda e: e.activation(out=sq4[:, :, 0:n_], in_=B[7][:].rearrange("p (q t) -> p q t", q=4)[:, :, 0:n_], func=AF.Sqrt), reads=[bt[7]], writes=[t_kk])
        S.dve(lambda e: e.tensor_scalar(out=sq4[:, :, 0:n_], in0=sq4[:, :, 0:n_], scalar1=1e-12, scalar2=None, op0=ALU.max), reads=[t_kk], writes=[t_kk])
        S.dve(lambda e: e.reciprocal(out=sq4[:, :, 0:n_], in_=sq4[:, :, 0:n_]), reads=[t_kk], writes=[t_kk])
        S.dve(lambda e: e.tensor_tensor(out=kk[:, :, 0:n_], in0=kk[:, :, 0:n_], in1=sq4[:, :, 0:n_], op=ALU.mult), reads=[t_kk], writes=[t_kk])
        S.dve(lambda e: e.tensor_tensor(out=kmod[:, :, 0:n_], in0=aT[:, :, 0:n_], in1=bc(rwp[:, 4, :].unsqueeze(2), [128, 4, n_]), op=ALU.mult),
              reads=[t_a, t_rp], writes=[t_km])
        S.dve(lambda e: e.tensor_tensor(out=kmod[:, :, 0:n_], in0=kmod[:, :, 0:n_], in1=bc(omka[:].unsqueeze(2), [128, 4, n_]), op=ALU.add),
              reads=[t_km, t_rp], writes=[t_km])
        S.dve(lambda e: e.tensor_tensor(out=kmod[:, :, 0:n_], in0=kmod[:, :, 0:n_], in1=xm[1][:, :, 0:n_], op=ALU.mult), reads=[t_km, t_xm], writes=[t_km])
        S.dve(lambda e: e.tensor_tensor(out=bb[:, :, 0:n_], in0=kk[:, :, 0:n_], in1=aT[:, :, 0:n_], op=ALU.mult), reads=[t_kk, t_a], writes=[t_b])
        S.pool(lambda e: e.tensor_tensor(out=rk[:, :, 0:n_], in0=xm[0][:, :, 0:n_], in1=kmod[:, :, 0:n_], op=ALU.mult), reads=[t_xm, t_km], writes=[t_rk])
        S.pool(lambda e: e.tensor_tensor(out=rk[:, :, 0:n_], in0=rk[:, :, 0:n_], in1=bc(rwp[:, 5, :].unsqueeze(2), [128, 4, n_]), op=ALU.mult),
               reads=[t_rk, t_rp], writes=[t_rk])
        for h in range(8):
            p, hh = h // 2, h % 2
            fp = slice(hh * 64, hh * 64 + 64)
            S.pe(lambda e, p=p, fp=fp, h=h: e.matmul(B[6][0:n_, 256 + h:256 + h + 1], lhsT=rk[fp, p, 0:n_], rhs=ones[fp, 0:1], start=True, stop=True),
                 reads=[t_rk, t_const], writes=[bt[6]])
        S.act(lambda e: e.activation(out=bsum[0:n_, :], in_=B[6][0:n_, 256:264], func=AF.Copy), reads=[bt[6], t_y], writes=[t_bs])
        tok6 = sbt(st2, "tok6", [64, 6, 512])
        t_tok6 = T_()
        S.act(lambda e: e.activation(out=tok6[:, 1, :], in_=sg_tok[0:n_, :], func=AF.Exp, scale=NEG), reads=[t_sg], writes=[t_tok6])
        for xi, (srcT, rd) in enumerate(((xm[0], t_xm), (None, None), (kmod, t_km), (xm[2], t_xm), (kk, t_kk), (bb, t_b))):
            if srcT is None:
                continue
            bk = 2 + xi % 2
            for p in range(4):
                S.pe(lambda e, p=p, srcT=srcT, bk=bk: e.transpose(B[bk][0:n_, p * 128:(p + 1) * 128], srcT[:, p, 0:n_], ident[:]),
                     reads=[rd, t_const], writes=[bt[bk]])
            S.act(lambda e, xi=xi, bk=bk: e.activation(out=tok6[:, xi, :], in_=B[bk][0:n_, :], func=AF.Copy), reads=[bt[bk]], writes=[t_tok6])
        t_rwsd = T_()
        t_rwsd_l = [T_() for i in range(24)]
        for t in range(4):
            for xi in range(6):
                S.dma(lambda e, t=t, xi=xi: e.dma_start(out=rws_d[:, :, t, xi, :], in_=tok6[16 * t:16 * t + 16, xi, :].rearrange("p (h c) -> p h c", h=8)),
                      reads=[t_tok6], writes=[t_rwsd_l[t * 6 + xi]])
        X6 = sbt(st2, "X6", [128, 4, 6, 64])
        St = sbt(st2, "St", [128, 64, 64])
        tmpS = sbt(st2, "tmpS", [128, 64, 64])
        sp_ = sbt(st2, "sp", [128, 64])
        ys = sbt(st2, "ys", [128, 4, 64])
        t_X6, t_St, t_tmpS, t_sp, t_ys = [T_() for i in range(5)]
        S.dma(lambda e: e.dma_start(out=X6[:].rearrange("p t x c -> p (t x c)"), in_=rws_d.rearrange("b h t x c -> (b h) (t x c)")), reads=t_rwsd_l, writes=[t_X6])
        S.dma(lambda e: e.dma_start(out=St[:].rearrange("p v k -> p (v k)"), in_=I["swkv"]), writes=[t_St])
        for t in range(4):
            def bk_(xi, t=t):
                return bc(X6[:, t, xi, :].unsqueeze(1), [128, 64, 64])
            def bv_(ap):
                return bc(ap.unsqueeze(2), [128, 64, 64])
            S.dve(lambda e, t=t: e.tensor_tensor(out=tmpS[:], in0=St[:], in1=bk_(4, t), op=ALU.mult), reads=[t_St, t_X6], writes=[t_tmpS])
            S.dve(lambda e: e.tensor_reduce(out=sp_[:], in_=tmpS[:], axis=AX.X, op=ALU.add), reads=[t_tmpS], writes=[t_sp])
            S.dve(lambda e, t=t: e.tensor_tensor(out=St[:], in0=St[:], in1=bk_(1, t), op=ALU.mult), reads=[t_St, t_X6, t_tmpS], writes=[t_St])
            S.pool(lambda e, t=t: e.tensor_tensor(out=tmpS[:], in0=bv_(sp_[:]), in1=bk_(5, t), op=ALU.mult), reads=[t_sp, t_X6], writes=[t_tmpS])
            S.dve(lambda e: e.tensor_tensor(out=St[:], in0=St[:], in1=tmpS[:], op=ALU.subtract), reads=[t_St, t_tmpS], writes=[t_St])
            S.pool(lambda e, t=t: e.tensor_tensor(out=tmpS[:], in0=bv_(X6[:, t, 3, :]), in1=bk_(2, t), op=ALU.mult), reads=[t_X6, t_St], writes=[t_tmpS])
            S.dve(lambda e: e.tensor_tensor(out=St[:], in0=St[:], in1=tmpS[:], op=ALU.add), reads=[t_St, t_tmpS], writes=[t_St])
            S.dve(lambda e, t=t: e.tensor_tensor(out=tmpS[:], in0=St[:], in1=bk_(0, t), op=ALU.mult), reads=[t_St, t_X6], writes=[t_tmpS])
            S.dve(lambda e, t=t: e.tensor_reduce(out=ys[:, t, :], in_=tmpS[:], axis=AX.X, op=ALU.add), reads=[t_tmpS], writes=[t_ys])
        S.dma(lambda e: e.dma_start(out=O["wkvs"], in_=St[:].rearrange("p v k -> p (v k)")), reads=[t_St])
        t_ysd = T_()
        S.dma(lambda e: e.dma_start(out=ys_d.rearrange("b h t v -> (b h) (t v)"), in_=ys[:].rearrange("p t v -> p (t v)")), reads=[t_ys], writes=[t_ysd])
        ysr = sbt(st2, "ysr", [64, 8, 64])
        t_ysr = T_()
        for t in range(4):
            S.dma(lambda e, t=t: e.dma_start(out=ysr[16 * t:16 * t + 16, :, :], in_=ys_d[:, :, t, :]), reads=[t_ysd], writes=[t_ysr])
        yps_s = ysr[:]
        Y0 = slice(0, 64)
        S.dve(lambda e: e.tensor_reduce(out=gst[Y0], in_=yps_s, axis=AX.X, op=ALU.add), reads=[t_ysr], writes=[t_y])
        S.dve(lambda e: e.tensor_scalar(out=gst[Y0], in0=gst[Y0], scalar1=1.0 / 64, scalar2=None, op0=ALU.mult), reads=[t_y], writes=[t_y])
        S.dve(lambda e: e.tensor_tensor(out=ycen[Y0], in0=yps_s, in1=bc(gst[Y0].unsqueeze(2), [64, 8, 64]), op=ALU.subtract), reads=[t_y, t_ysr], writes=[t_y])
        S.act(lambda e: e.activation(out=ysq[Y0], in_=ycen[Y0], func=AF.Square), reads=[t_y], writes=[t_y])
        S.dve(lambda e: e.tensor_reduce(out=gst2[Y0], in_=ysq[Y0], axis=AX.X, op=ALU.add), reads=[t_y], writes=[t_y])
        S.act(lambda e: e.activation(out=gst2[Y0], in_=gst2[Y0], func=AF.Sqrt, scale=1.0 / 64, bias=gneps[Y0]), reads=[t_y, t_rp], writes=[t_y])
        S.dve(lambda e: e.reciprocal(out=gst2[Y0], in_=gst2[Y0]), reads=[t_y], writes=[t_y])
        S.dve(lambda e: e.tensor_tensor(out=ycen[Y0], in0=ycen[Y0], in1=bc(gst2[Y0].unsqueeze(2), [64, 8, 64]), op=ALU.mult), reads=[t_y], writes=[t_y])
        yc2_s = ycen[Y0].rearrange("p h v -> p (h v)")
        S.dve(lambda e: e.tensor_tensor(out=yc2_s, in0=yc2_s, in1=lnxg[Y0], op=ALU.mult), reads=[t_y, t_rp], writes=[t_y])
        S.dve(lambda e: e.tensor_tensor(out=yc2_s, in0=yc2_s, in1=lnxb[Y0], op=ALU.add), reads=[t_y, t_rp], writes=[t_y])
        S.pool(lambda e: e.tensor_tensor(out=ysq[Y0], in0=tok6[:, 3, :].rearrange("p (h v) -> p h v", h=8), in1=bc(bsum[Y0].unsqueeze(2), [64, 8, 64]), op=ALU.mult),
               reads=[t_tok6, t_bs, t_y], writes=[t_y])
        S.dve(lambda e: e.tensor_tensor(out=ycen[Y0], in0=ycen[Y0], in1=ysq[Y0], op=ALU.add), reads=[t_y], writes=[t_y])
        S.dve(lambda e: e.tensor_tensor(out=yc2_s, in0=yc2_s, in1=g_tok[Y0], op=ALU.mult), reads=[t_y, t_g], writes=[t_y])
        for p in range(4):
            S.pe(lambda e, p=p: e.transpose(B[7][:, p * 128:p * 128 + 64], ycen[Y0, 2 * p:2 * p + 2, :].rearrange("p h v -> p (h v)"), ident[0:64, 0:64]),
                 reads=[t_y, t_const], writes=[bt[7]])
        S.act(lambda e: e.activation(out=mixr[:, :, 0:64].rearrange("p q (b t) -> p q t b", t=4),
                                     in_=B[7][:].rearrange("p (q x) -> p q x", q=4)[:, :, 0:64].rearrange("p q (t b) -> p q t b", t=4), func=AF.Copy),
              reads=[bt[7]], writes=[t_mixr])
        S.dma(lambda e: e.dma_start(out=mixT_d[4:8, :, T:T + NS].rearrange("q p t -> p q t"), in_=mixr[:, :, 0:64]), reads=[t_mixr])


    S.barrier()
    with contextlib.ExitStack() as st2, contextlib.suppress(_Stop):
        ck(5.0)
        wq3 = sbt(st2, "wq3", [128, 8, 1536], BF16)
        t_wq3 = Tok()
        with contextlib.ExitStack() as st3:
            wst3 = [sbt(st3, "wst3", [128, 8, 512]) for i in range(2)]
            wst3t = [Tok(), Tok()]
            for q3 in range(3):
                w_ = wst3[q3 % 2]
                S.dma(lambda e, w_=w_, q3=q3: e.dma_start(out=w_[:], in_=win_v[:, :, q3 * 512:(q3 + 1) * 512]), writes=[wst3t[q3 % 2]])
                S.pool(lambda e, w_=w_, q3=q3: e.tensor_copy(out=wq3[:, :, q3 * 512:(q3 + 1) * 512], in_=w_[:]), reads=[wst3t[q3 % 2]], writes=[t_wq3])
        S.barrier()
        B = banks
        NQ = 64
        hsp = sbt(st2, "hsp", [128, 8, 64], BF16)
        t_hsp = Tok()
        S.pool(lambda e: e.tensor_copy(out=hsp[:].rearrange("p c (s b) -> p c s b", s=4), in_=hT[:, :, T:T + NS].rearrange("p c (b s) -> p c s b", s=4)),
               reads=[t_h[8]], writes=[t_hsp])
        hs_sb = lambda c: hsp[:, c, :]
        qkvs = sbt(st2, "qkvs", [64, 3, 8, 64])
        sqs = sbt(st2, "sqs", [64, 8, 64])
        ssn = sbt(st2, "ssn", [64, 8])
        gqk = sbt(st2, "gqk", [64, 2, 64])
        t_qkv, t_sqs, t_gqk = Tok(), Tok(), Tok()
        for wi in range(2):
            S.dma(lambda e, wi=wi: e.dma_start(out=gqk[:, wi, :], in_=bass.AP(I["qkrow"].tensor, wi * 64, [[0, 64], [1, 64]])), writes=[t_gqk])
        for wi in range(3):
            for c in range(8):
                S.pe(lambda e, wi=wi, c=c: e.matmul(B[wi][0:NQ, :], lhsT=hs_sb(c), rhs=wq3[:, c, wi * 512:(wi + 1) * 512], start=(c == 0), stop=(c == 7)),
                     reads=[t_wq3, t_hsp], writes=[bt[wi]])
            pv = B[wi][0:NQ, :].rearrange("p (h c) -> p h c", h=8)
            if wi == 2:
                S.act(lambda e, pv=pv: e.activation(out=qkvs[:, 2, :, :], in_=pv, func=AF.Copy), reads=[bt[wi]], writes=[t_qkv])
            else:
                S.act(lambda e, pv=pv: e.activation(out=sqs[:], in_=pv, func=AF.Square), reads=[bt[wi]], writes=[t_sqs])
                S.dve(lambda e: e.tensor_reduce(out=ssn[:], in_=sqs[:], axis=AX.X, op=ALU.add), reads=[t_sqs], writes=[t_sqs])
                S.act(lambda e: e.activation(out=ssn[:], in_=ssn[:], func=AF.Sqrt, scale=1.0 / 64, bias=epsc[0:64]), reads=[t_sqs, t_const], writes=[t_sqs])
                S.dve(lambda e: e.reciprocal(out=ssn[:], in_=ssn[:]), reads=[t_sqs], writes=[t_sqs])
                S.dve(lambda e, pv=pv, wi=wi: e.tensor_tensor(out=qkvs[:, wi, :, :], in0=pv, in1=bc(ssn[:].unsqueeze(2), [64, 8, 64]), op=ALU.mult),
                      reads=[bt[wi], t_sqs], writes=[t_qkv])
                S.dve(lambda e, wi=wi: e.tensor_tensor(out=qkvs[:, wi, :, :], in0=qkvs[:, wi, :, :], in1=bc(gqk[:, wi, :].unsqueeze(1), [64, 8, 64]), op=ALU.mult),
                      reads=[t_qkv, t_gqk], writes=[t_qkv])
        ck(5.1)
        t_rec = Tok()
        for wi, oname, recd, cname in ((1, "kws", reck_d, "ck_s"), (2, "vws", recv_d, "cv_s")):
            for s_ in range(4):
                S.dma(lambda e, wi=wi, oname=oname, s_=s_: e.dma_start(out=O[oname].rearrange("(b s) c -> s b c", s=4)[s_],
                                                                        in_=qkvs[16 * s_:16 * s_ + 16, wi, :, :].rearrange("p h c -> p (h c)")), reads=[t_qkv])
                S.dma(lambda e, wi=wi, recd=recd, s_=s_: e.dma_start(out=recd[:, 4 + s_, :], in_=qkvs[16 * s_:16 * s_ + 16, wi, :, :].rearrange("p h c -> p (h c)")),
                      reads=[t_qkv], writes=[t_rec])
            S.dma(lambda e, recd=recd, cname=cname: e.dma_start(out=recd[:, 0:4, :], in_=I[cname][:, 2044:2048, :]), writes=[t_rec])
        ck(5.2)
        btab = sbt(st2, "btab", [64, 3, 8, 129])
        t_btab = Tok()
        with contextlib.ExitStack() as st3:
            relr = sbt(st3, "relr", [32, 8])
            ohs = sbt(st3, "ohs", [32, 3, 129])
            RHs = sbt(st3, "RHs", [32, 8, 129])
            t_r2, t_rh2 = Tok(), Tok()
            S.dma(lambda e: e.dma_start(out=relr[:], in_=I["relb"]), writes=[t_r2])
            S.dma(lambda e: e.dma_start(out=ohs[:], in_=I["ohs"]), writes=[t_r2])
            for br in range(3):
                S.dve(lambda e, br=br: e.tensor_tensor(out=RHs[:], in0=bc(relr[:].unsqueeze(2), [32, 8, 129]), in1=bc(ohs[:, br, :].unsqueeze(1), [32, 8, 129]), op=ALU.mult),
                      reads=[t_r2], writes=[t_rh2])
                for h in range(8):
                    bk = 3 + h % 2
                    S.pe(lambda e, h=h, bk=bk: e.matmul(B[bk][0:64, 0:129], lhsT=ones[0:32, 0:64], rhs=RHs[:, h, :], start=True, stop=True),
                         reads=[t_rh2, t_const], writes=[bt[bk]])
                    S.act(lambda e, h=h, bk=bk, br=br: e.activation(out=btab[:, br, h, :], in_=B[bk][0:64, 0:129], func=AF.Copy), reads=[bt[bk]], writes=[t_btab])
        S.barrier()
        ck(5.3)
        Kt = [sbt(st2, "Kt", [64, 129, 64])] * 2
        Vt = [sbt(st2, "Vt", [64, 129, 64])] * 2
        t_Kt = [[Tok() for i in range(24)]] * 2
        t_Vt = [[Tok() for i in range(24)]] * 2
        lg = sbt(st2, "lg", [64, 129])
        zz = sbt(st2, "zz", [64, 1])
        ov = sbt(st2, "ov", [64, 64])
        Oacc = sbt(st2, "Oacc", [64, 8, 64])
        Zacc = sbt(st2, "Zacc", [64, 8])
        t_lg, t_zz, t_ov, t_acc2 = Tok(), Tok(), Tok(), Tok()
        S.pool(lambda e: e.memset(Oacc[:], 0.0), writes=[t_acc2])
        S.pool(lambda e: e.memset(Zacc[:], 0.0), writes=[t_acc2])
        it = 0
        for br, dil in enumerate((1, 4, 16)):
            ncache = 125 if dil == 1 else 128
            for h in range(8):
                bi = it % 2
                it += 1
                for tl, tk, cname, recd in ((Kt[bi], t_Kt[bi], "ck_s", reck_d), (Vt[bi], t_Vt[bi], "cv_s", recv_d)):
                    for s_ in range(4):
                        ps_ = slice(16 * s_, 16 * s_ + 16)
                        for j0 in range(0, ncache, 32):
                            j1 = min(ncache, j0 + 32)
                            src = bass.AP(I[cname].tensor, (2048 + s_ - 128 * dil + j0 * dil) * 512 + h * 64, [[2048 * 512, 16], [dil * 512, j1 - j0], [1, 64]])
                            S.dma(lambda e, tl=tl, ps_=ps_, src=src, j0=j0, j1=j1: e.dma_start(out=tl[ps_, j0:j1, :], in_=src), writes=[tk[8 + 4 * s_ + j0 // 32]])
                        r0 = (1 + s_) if dil == 1 else (4 + s_)
                        src2 = bass.AP(recd.tensor, r0 * 512 + h * 64, [[8 * 512, 16], [512, 129 - ncache], [1, 64]])
                        S.dma(lambda e, tl=tl, ps_=ps_, src2=src2, ncache=ncache: e.dma_start(out=tl[ps_, ncache:129, :], in_=src2), reads=[t_rec], writes=[tk[2 * s_ + 1]])
                K_, V_ = Kt[bi], Vt[bi]
                S.dve(lambda e, K_=K_, h=h: e.tensor_tensor(out=K_[:], in0=K_[:], in1=bc(qkvs[:, 0, h, :].unsqueeze(1), [64, 129, 64]), op=ALU.mult),
                      reads=t_Kt[bi] + [t_qkv], writes=t_Kt[bi])
                S.dve(lambda e, K_=K_: e.tensor_reduce(out=lg[:], in_=K_[:], axis=AX.X, op=ALU.add), reads=t_Kt[bi], writes=[t_lg])
                S.dve(lambda e, br=br, h=h: e.scalar_tensor_tensor(out=lg[:], in0=lg[:], scalar=0.125, in1=btab[:, br, h, :], op0=ALU.mult, op1=ALU.add),
                      reads=[t_lg, t_btab], writes=[t_lg])
                S.act(lambda e: e.activation(out=lg[:], in_=lg[:], func=AF.Exp), reads=[t_lg], writes=[t_lg])
                S.dve(lambda e: e.tensor_reduce(out=zz[:], in_=lg[:], axis=AX.X, op=ALU.add), reads=[t_lg], writes=[t_zz])
                S.dve(lambda e, h=h: e.tensor_tensor(out=Zacc[:, h:h + 1], in0=Zacc[:, h:h + 1], in1=zz[:], op=ALU.add), reads=[t_zz, t_acc2], writes=[t_acc2])
                S.pool(lambda e, V_=V_: e.tensor_tensor(out=V_[:], in0=V_[:], in1=bc(lg[:].unsqueeze(2), [64, 129, 64]), op=ALU.mult),
                       reads=t_Vt[bi] + [t_lg], writes=t_Vt[bi])
                S.dve(lambda e, V_=V_: e.tensor_reduce(out=ov[:], in_=V_[:].rearrange("p j c -> p c j"), axis=AX.X, op=ALU.add), reads=t_Vt[bi], writes=[t_ov])
                S.dve(lambda e, h=h: e.tensor_tensor(out=Oacc[:, h, :], in0=Oacc[:, h, :], in1=ov[:], op=ALU.add), reads=[t_ov, t_acc2], writes=[t_acc2])
                ck(5.4 + 0.001 * it)
        S.dve(lambda e: e.reciprocal(out=Zacc[:], in_=Zacc[:]), reads=[t_acc2], writes=[t_acc2])
        S.dve(lambda e: e.tensor_tensor(out=Oacc[:], in0=Oacc[:], in1=bc(Zacc[:].unsqueeze(2), [64, 8, 64]), op=ALU.mult), reads=[t_acc2], writes=[t_acc2])
        mixs = sbt(st2, "mixs", [128, 4, 64], BF16)
        t_mixs = Tok()
        for p in range(4):
            S.pe(lambda e, p=p: e.transpose(B[5][:, p * 128:p * 128 + 64], Oacc[:, 2 * p:2 * p + 2, :].rearrange("p h c -> p (h c)"), ident[0:64, 0:64]),
                 reads=[t_acc2, t_const], writes=[bt[5]])
        S.act(lambda e: e.activation(out=mixs[:].rearrange("p q (b s) -> p q s b", s=4),
                                     in_=B[5][:].rearrange("p (q x) -> p q x", q=4)[:, :, 0:64].rearrange("p q (s b) -> p q s b", s=4), func=AF.Copy),
              reads=[bt[5]], writes=[t_mixs])
        S.dma(lambda e: e.dma_start(out=mixT_d[0:4, :, T:T + NS].rearrange("q p t -> p q t"), in_=mixs[:]), reads=[t_mixs])
    S.barrier()
    stH.close()
    if phase_limit < 6:
        S.emit()
        for sk_ in (stC_holder + [stH, st]):
            sk_.close()
        return nc
    S.nosync = True
    with contextlib.ExitStack() as st2, contextlib.suppress(_Stop):
        t_pc = Tok("peerconst")
        wout = sbt(st2, "wout", [128, 8, 1024], BF16)
        skT = sbt(st2, "skT", [128, 16, 128], BF16)
        iotaR = sbt(st2, "iotaR", [128, 128])
        with contextlib.ExitStack() as st3:
            wo_st = sbt(st3, "wo_st", [128, 8, 1024])
            sk_st = sbt(st3, "sk_st", [128, 16, 128])
            S.dma(lambda e: e.dma_start(out=wo_st[:], in_=I["w_out"].rearrange("(c p) n -> p c n", p=128)), writes=[t_pc])
            S.dma(lambda e: e.dma_start(out=sk_st[:], in_=I["skT"]), writes=[t_pc])
            S.dma(lambda e: e.dma_start(out=iotaR[:], in_=I["iotaR"]), writes=[t_pc])
            S.dve(lambda e: e.tensor_copy(out=wout[:], in_=wo_st[:]), reads=[t_pc], writes=[t_pc])
            S.dve(lambda e: e.tensor_copy(out=skT[:], in_=sk_st[:]), reads=[t_pc], writes=[t_pc])
        S.barrier()
        GS = 256
        x1g = sbt(st2, "x1g", [128, 8, GS])
        tmpg = sbt(st2, "tmpg", [128, 8, GS])
        h2g = sbt(st2, "h2g", [128, 8, GS], BF16)
        mixg = sbt(st2, "mixg", [128, 8, GS], BF16)
        rsg = sbt(st2, "rsg", [128, GS])
        qpT = sbt(st2, "qpT", [128, 16, GS], BF16)
        wpqb = sbt(st2, "wpqb", [128, 8, 512], BF16)
        s_sb = sbt(st2, "s_sb", [128, 16, 128])
        s2 = sbt(st2, "s2", [128, 256])
        vals = sbt(st2, "vals", [128, 16, 16])
        idxu = sbt(st2, "idxu", [128, 16, 16], U32)
        idxf = sbt(st2, "idxf", [128, 16, 16])
        cand = sbt(st2, "cand", [128, 8, 256])
        ts_ = sbt(st2, "ts", [128, 8, 16])
        posu = sbt(st2, "posu", [128, 8, 16], U32)
        au = sbt(st2, "au", [128, 8, 16], U32)
        bu = sbt(st2, "bu", [128, 8, 16], U32)
        a_f = sbt(st2, "a_f", [128, 8, 16])
        b_f = sbt(st2, "b_f", [128, 8, 16])
        eq = sbt(st2, "eq", [128, 8, 16, 16])
        I1 = sbt(st2, "I1", [128, 8, 16])
        I2 = sbt(st2, "I2", [128, 8, 16])
        gt_ = sbt(st2, "gt", [128, 8, 16])
        zs = sbt(st2, "zs", [128, 8])
        I1T = sbt(st2, "I1T", [128, GS], BF16)
        I2T = sbt(st2, "I2T", [128, GS], BF16)
        gT = sbt(st2, "gT", [128, GS], BF16)
        iotaRb = sbt(st2, "iotaRb", [128, 128], BF16)
        S.dve(lambda e: e.tensor_copy(out=iotaRb[:], in_=iotaR[:]), reads=[t_pc], writes=[t_pc])
        A4 = [sbt(st2, "A4", [128, 4, 128], BF16) for i in range(2)]
        B4 = [sbt(st2, "B4", [128, 4, 128], BF16) for i in range(2)]
        WT = sbt(st2, "WT", [128, GS, 128], BF16)
        eub = [sbt(st2, "eub", [128, 8, 256], BF16) for i in range(2)]
        evb = [sbt(st2, "evb", [128, 2, 1024], BF16) for i in range(2)]
        gU = [sbt(st2, "gU", [128, GS], BF16) for i in range(2)]
        Wg = [sbt(st2, "Wg", [128, GS], BF16) for i in range(2)]
        ytk = [sbt(st2, "ytk", [128, 1024])] * 2
        (t_x1, t_tmp, t_h2, t_mixg, t_rsg, t_qp, t_wpq, t_s, t_s2, t_vals, t_cand, t_ts, t_ab, t_eq, t_I, t_g, t_IT, t_WT) = [Tok() for i in range(18)]
        t_A4 = [Tok(), Tok()]
        t_B4 = [Tok(), Tok()]
        t_eub = [Tok(), Tok()]
        t_evb = [Tok(), Tok()]
        t_gU = [Tok(), Tok()]
        t_Wg = [Tok(), Tok()]
        t_ytk = [Tok()] * 2
        B = banks
        evv = ev_d.rearrange("(k p) d -> p k d", p=128)
        wpqv = wpq_d.rearrange("(c p) n -> p c n", p=128)
        iota16 = iotaR[:, 0:16]
        pgroups = [(g * GS, GS) for g in range(T // GS)] + [(T, NS)]
        x1g2 = [x1g, sbt(st2, "x1gb", [128, 8, GS])]
        h2g2 = [h2g, sbt(st2, "h2gb", [128, 8, GS], BF16)]
        t_x1s = [t_x1, Tok()]
        t_h2s = [t_h2, Tok()]

        def prep_gen(gi, t0, n):
            samp = (gi == len(pgroups) - 1)
            x1g, h2g, t_x1, t_h2 = x1g2[gi % 2], h2g2[gi % 2], t_x1s[gi % 2], t_h2s[gi % 2]
            samp = (gi == len(pgroups) - 1)
            S.dma(lambda e, t0=t0, n=n: e.dma_start(out=mixg[:, :, 0:n], in_=mixT_d[:, :, t0:t0 + n].rearrange("j p t -> p j t")), writes=[t_mixg])
            S.dma(lambda e, t0=t0, n=n: e.dma_start(out=x1g[:, :, 0:n], in_=xT_v[:, :, t0:t0 + n]), writes=[t_x1])
            for dc in range(8):
                yield
                bk = 6 + dc % 2
                for j in range(8):
                    S.pe(lambda e, dc=dc, j=j, bk=bk, n=n: e.matmul(B[bk][:, 0:n], lhsT=wout[:, j, dc * 128:(dc + 1) * 128], rhs=mixg[:, j, 0:n],
                                                                     start=(j == 0), stop=(j == 7)), reads=[t_pc, t_mixg], writes=[bt[bk]])
                if not samp:
                    S.dve(lambda e, dc=dc, bk=bk, n=n: e.scalar_tensor_tensor(out=x1g[:, dc, 0:n], in0=B[bk][:, 0:n], scalar=modT[:, 16 + dc, 0:1],
                                                                               in1=x1g[:, dc, 0:n], op0=ALU.mult, op1=ALU.add),
                          reads=[bt[bk], t_mod, t_x1], writes=[t_x1])
                else:
                    S.dve(lambda e, dc=dc, bk=bk: e.tensor_tensor(out=tmpg[:, dc, 0:NS].rearrange("p (b t) -> p b t", t=4),
                                                                  in0=B[bk][:, 0:NS].rearrange("p (b t) -> p b t", t=4),
                                                                  in1=bc(modT[:, 16 + dc, 1:17].unsqueeze(2), [128, SB, 4]), op=ALU.mult),
                          reads=[bt[bk], t_mod], writes=[t_tmp])
                    S.dve(lambda e, dc=dc: e.tensor_tensor(out=x1g[:, dc, 0:NS], in0=x1g[:, dc, 0:NS], in1=tmpg[:, dc, 0:NS], op=ALU.add),
                          reads=[t_tmp, t_x1], writes=[t_x1])
            S.act(lambda e, n=n: e.activation(out=tmpg[:, :, 0:n], in_=x1g[:, :, 0:n], func=AF.Square), reads=[t_x1], writes=[t_tmp])
            for c in range(8):
                S.pe(lambda e, c=c, n=n: e.matmul(B[6][:, 0:n], lhsT=ones[:], rhs=tmpg[:, c, 0:n], start=(c == 0), stop=(c == 7)),
                     reads=[t_tmp, t_const], writes=[bt[6]])
            S.act(lambda e, n=n: e.activation(out=rsg[:, 0:n], in_=B[6][:, 0:n], func=AF.Sqrt, scale=1.0 / D, bias=epsc[:]),
                  reads=[bt[6], t_const], writes=[t_rsg])
            S.dve(lambda e, n=n: e.reciprocal(out=rsg[:, 0:n], in_=rsg[:, 0:n]), reads=[t_rsg], writes=[t_rsg])
            S.dve(lambda e, n=n: e.tensor_tensor(out=tmpg[:, :, 0:n], in0=x1g[:, :, 0:n], in1=bc(rsg[:, 0:n].unsqueeze(1), [128, 8, n]), op=ALU.mult),
                  reads=[t_x1, t_rsg, t_tmp], writes=[t_tmp])
            if not samp:
                for c in range(8):
                    eng = S.dve if c % 2 == 0 else S.pool
                    eng(lambda e, c=c, n=n: e.tensor_scalar(out=h2g[:, c, 0:n], in0=tmpg[:, c, 0:n], scalar1=A2[:, c, 0:1], scalar2=modT[:, 24 + c, 0:1],
                                                            op0=ALU.mult, op1=ALU.add), reads=[t_tmp, t_mod], writes=[t_h2])
            else:
                tv = tmpg[:, :, 0:NS].rearrange("p c (b t) -> p c b t", t=4)
                S.dve(lambda e, tv=tv: e.tensor_tensor(out=tv, in0=tv, in1=bc(A2[:, :, 1:17].unsqueeze(3), [128, 8, SB, 4]), op=ALU.mult),
                      reads=[t_tmp, t_mod], writes=[t_tmp])
                S.dve(lambda e, tv=tv: e.tensor_tensor(out=h2g[:, :, 0:NS].rearrange("p c (b t) -> p c b t", t=4), in0=tv,
                                                       in1=bc(modT[:, 24:32, 1:17].unsqueeze(3), [128, 8, SB, 4]), op=ALU.add),
                      reads=[t_tmp, t_mod], writes=[t_h2])
            for jb in range(4):
                yield
                S.dma(lambda e, jb=jb: e.dma_start(out=wpqb[:], in_=wpqv[:, :, jb * 512:(jb + 1) * 512]), writes=[t_wpq])
                for j in range(4):
                    bk = 6 + j % 2
                    for c in range(8):
                        S.pe(lambda e, j=j, c=c, n=n, bk=bk: e.matmul(B[bk][:, 0:n], lhsT=wpqb[:, c, j * 128:(j + 1) * 128], rhs=h2g[:, c, 0:n],
                                                                      start=(c == 0), stop=(c == 7)), reads=[t_wpq, t_h2], writes=[bt[bk]])
                    S.act(lambda e, j=j, jb=jb, n=n, bk=bk: e.activation(out=qpT[:, jb * 4 + j, 0:n], in_=B[bk][:, 0:n], func=AF.Copy), reads=[bt[bk]], writes=[t_qp])
                    yield
            for tt0 in range(0, n, 128):
                m = min(128, n - tt0)
                for half in range(2):
                    for j in range(half * 8, half * 8 + 8):
                        bk = 6 + (j % 8) // 4
                        S.pe(lambda e, j=j, bk=bk, tt0=tt0, m=m: e.matmul(B[bk][0:m, (j % 4) * 128:(j % 4 + 1) * 128], lhsT=qpT[:, j, tt0:tt0 + m], rhs=skT[:, j, :],
                                                                          start=True, stop=True), reads=[t_qp, t_pc], writes=[bt[bk]])
                    for q in range(2):
                        S.act(lambda e, q=q, m=m, half=half: e.activation(out=s_sb[0:m, half * 8 + q * 4:half * 8 + (q + 1) * 4, :].rearrange("p a k -> p (a k)"),
                                                                          in_=B[6 + q][0:m, :], func=AF.Copy), reads=[bt[6 + q]], writes=[t_s])
                    yield
                for j in range(16):
                    yield
                    S.dve(lambda e, j=j, m=m: e.max(out=vals[0:m, j, 0:8], in_=s_sb[0:m, j, :]), reads=[t_s], writes=[t_vals])
                    S.dve(lambda e, j=j, m=m: e.match_replace(out=s2[0:m, 0:128], in_to_replace=vals[0:m, j, 0:8], in_values=s_sb[0:m, j, :], imm_value=-1e30),
                          reads=[t_s, t_vals], writes=[t_s2])
                    S.dve(lambda e, j=j, m=m: e.max(out=vals[0:m, j, 8:16], in_=s2[0:m, 0:128]), reads=[t_s2], writes=[t_vals])
                    S.dve(lambda e, j=j, m=m: e.max_index(out=idxu[0:m, j, 0:8], in_max=vals[0:m, j, 0:8], in_values=s_sb[0:m, j, :]), reads=[t_s, t_vals], writes=[t_vals])
                    S.dve(lambda e, j=j, m=m: e.max_index(out=idxu[0:m, j, 8:16], in_max=vals[0:m, j, 8:16], in_values=s_sb[0:m, j, :]), reads=[t_s, t_vals], writes=[t_vals])
                S.dve(lambda e, m=m: e.tensor_copy(out=idxf[0:m], in_=idxu[0:m]), reads=[t_vals], writes=[t_vals])
                v2 = vals[0:m].rearrange("p (h two) k -> p h two k", two=2)
                i2v = idxf[0:m].rearrange("p (h two) k -> p h two k", two=2)
                S.dve(lambda e, m=m, v2=v2: e.tensor_tensor(out=cand[0:m].rearrange("p h (a b) -> p h a b", a=16),
                                                            in0=bc(v2[:, :, 0, :].unsqueeze(3), [m, 8, 16, 16]),
                                                            in1=bc(v2[:, :, 1, :].unsqueeze(2), [m, 8, 16, 16]), op=ALU.add), reads=[t_vals], writes=[t_cand])
                for h in range(8):
                    yield
                    S.dve(lambda e, h=h, m=m: e.max(out=ts_[0:m, h, 0:8], in_=cand[0:m, h, :]), reads=[t_cand], writes=[t_ts])
                    S.dve(lambda e, h=h, m=m: e.match_replace(out=s2[0:m, :], in_to_replace=ts_[0:m, h, 0:8], in_values=cand[0:m, h, :], imm_value=-1e30),
                          reads=[t_cand, t_ts], writes=[t_s2])
                    S.dve(lambda e, h=h, m=m: e.max(out=ts_[0:m, h, 8:16], in_=s2[0:m, :]), reads=[t_s2], writes=[t_ts])
                    S.dve(lambda e, h=h, m=m: e.max_index(out=posu[0:m, h, 0:8], in_max=ts_[0:m, h, 0:8], in_values=cand[0:m, h, :]), reads=[t_cand, t_ts], writes=[t_ts])
                    S.dve(lambda e, h=h, m=m: e.max_index(out=posu[0:m, h, 8:16], in_max=ts_[0:m, h, 8:16], in_values=cand[0:m, h, :]), reads=[t_cand, t_ts], writes=[t_ts])
                S.dve(lambda e, m=m: e.tensor_single_scalar(out=au[0:m], in_=posu[0:m], scalar=4, op=ALU.logical_shift_right), reads=[t_ts], writes=[t_ab])
                S.dve(lambda e, m=m: e.tensor_single_scalar(out=bu[0:m], in_=posu[0:m], scalar=15, op=ALU.bitwise_and), reads=[t_ts], writes=[t_ab])
                S.dve(lambda e, m=m: e.tensor_copy(out=a_f[0:m], in_=au[0:m]), reads=[t_ab], writes=[t_ab])
                S.dve(lambda e, m=m: e.tensor_copy(out=b_f[0:m], in_=bu[0:m]), reads=[t_ab], writes=[t_ab])
                for which, sel, dstI in ((0, a_f, I1), (1, b_f, I2)):
                    yield
                    S.dve(lambda e, m=m, sel=sel: e.tensor_tensor(out=eq[0:m], in0=bc(iota16[0:m].unsqueeze(1).unsqueeze(1), [m, 8, 16, 16]),
                                                                  in1=bc(sel[0:m].unsqueeze(3), [m, 8, 16, 16]), op=ALU.is_equal),
                          reads=[t_ab, t_pc], writes=[t_eq])
                    S.dve(lambda e, m=m, which=which, i2v=i2v: e.tensor_tensor(out=eq[0:m], in0=eq[0:m], in1=bc(i2v[:, :, which, :].unsqueeze(2), [m, 8, 16, 16]), op=ALU.mult),
                          reads=[t_eq, t_vals], writes=[t_eq])
                    S.dve(lambda e, m=m, dstI=dstI: e.tensor_reduce(out=dstI[0:m], in_=eq[0:m], axis=AX.X, op=ALU.add), reads=[t_eq], writes=[t_I])
                S.dve(lambda e, m=m: e.tensor_tensor(out=gt_[0:m], in0=ts_[0:m], in1=bc(ts_[0:m, :, 0:1], [m, 8, 16]), op=ALU.subtract), reads=[t_ts], writes=[t_g])
                S.act(lambda e, m=m: e.activation(out=gt_[0:m], in_=gt_[0:m], func=AF.Exp), reads=[t_g], writes=[t_g])
                S.dve(lambda e, m=m: e.tensor_reduce(out=zs[0:m], in_=gt_[0:m], axis=AX.X, op=ALU.add), reads=[t_g], writes=[t_g])
                S.dve(lambda e, m=m: e.reciprocal(out=zs[0:m], in_=zs[0:m]), reads=[t_g], writes=[t_g])
                S.dve(lambda e, m=m: e.tensor_tensor(out=gt_[0:m], in0=gt_[0:m], in1=bc(zs[0:m].unsqueeze(2), [m, 8, 16]), op=ALU.mult), reads=[t_g], writes=[t_g])
                for srcI, dstT, rd in ((I1, I1T, t_I), (I2, I2T, t_I), (gt_, gT, t_g)):
                    yield
                    S.pe(lambda e, srcI=srcI, m=m: e.transpose(B[6][:, 0:m], srcI[0:m].rearrange("p h k -> p (h k)"), ident[0:m, 0:m]),
                         reads=[rd, t_const], writes=[bt[6]])
                    S.act(lambda e, dstT=dstT, tt0=tt0, m=m: e.activation(out=dstT[:, tt0:tt0 + m], in_=B[6][:, 0:m], func=AF.Copy), reads=[bt[6]], writes=[t_IT])
            if debug and gi == 0 and False:
                for qq, tl in enumerate((I1T, I2T, gT)):
                    S.dma(lambda e, qq=qq, tl=tl: e.dma_start(out=O["dbg_IT"][qq], in_=tl[:]), reads=[t_IT])

        def wbuild(gi, t0, n):
            for n0 in range(0, n, 4):
                bi = (n0 // 4) % 2
                S.dve(lambda e, bi=bi, n0=n0: e.tensor_tensor(out=B4[bi][:], in0=bc(iotaRb[:].unsqueeze(1), [128, 4, 128]),
                                                             in1=bc(I2T[:, n0:n0 + 4].unsqueeze(2), [128, 4, 128]), op=ALU.is_equal),
                      reads=[t_IT, t_pc], writes=[t_B4[bi]])
                S.dve(lambda e, bi=bi, n0=n0: e.tensor_tensor(out=A4[bi][:], in0=bc(iotaRb[:].unsqueeze(1), [128, 4, 128]),
                                                             in1=bc(I1T[:, n0:n0 + 4].unsqueeze(2), [128, 4, 128]), op=ALU.is_equal),
                      reads=[t_IT, t_pc], writes=[t_A4[bi]])
                S.pool(lambda e, bi=bi, n0=n0: e.tensor_tensor(out=A4[bi][:], in0=A4[bi][:],
                                                              in1=bc(gT[:, n0:n0 + 4].unsqueeze(2), [128, 4, 128]), op=ALU.mult),
                      reads=[t_IT, t_A4[bi]], writes=[t_A4[bi]])
                bk = 6 + bi
                for q in range(4):
                    S.pe(lambda e, bi=bi, q=q, bk=bk: e.matmul(B[bk][:, q * 128:(q + 1) * 128], lhsT=B4[bi][:, q, :], rhs=A4[bi][:, q, :], start=True, stop=True),
                         reads=[t_A4[bi], t_B4[bi]], writes=[bt[bk]])
                S.act(lambda e, bk=bk, n0=n0: e.activation(out=WT[:, n0:n0 + 4, :].rearrange("p n i -> p (n i)"),
                                                           in_=B[bk][:], func=AF.Copy), reads=[bt[bk]], writes=[t_WT])

        def sweep(gi, t0, n, gen):
            h2g, t_h2 = h2g2[gi % 2], t_h2s[gi % 2]
            def emit_U(i1):
                blk, k2 = i1 // 2, i1 % 2
                bi = blk % 2
                if k2 == 0:
                    S.dma(lambda e, bi=bi, blk=blk: e.dma_start(out=eub[bi][:], in_=euT_d[blk]), writes=[t_eub[bi]])
                    S.dma(lambda e, bi=bi, blk=blk: e.dma_start(out=evb[bi][:], in_=evv[:, blk * 2:(blk + 1) * 2, :]), writes=[t_evb[bi]])
                ui = i1 % 2
                ubk = 4 + ui
                for c in range(8):
                    S.pe(lambda e, bi=bi, k2=k2, c=c, ubk=ubk, n=n: e.matmul(B[ubk][:, 0:n], lhsT=eub[bi][:, c, k2 * 128:(k2 + 1) * 128], rhs=h2g[:, c, 0:n],
                                                                             start=(c == 0), stop=(c == 7)), reads=[t_eub[bi], t_h2], writes=[bt[ubk]])

            def emit_rest(i1):
                blk, k2 = i1 // 2, i1 % 2
                bi = blk % 2
                ui = i1 % 2
                ubk = 4 + ui
                S.act(lambda e, ui=ui, ubk=ubk, n=n: e.activation(out=gU[ui][:, 0:n], in_=B[ubk][:, 0:n], func=AF.Gelu), reads=[bt[ubk]], writes=[t_gU[ui]])
                S.dve(lambda e, ui=ui, i1=i1, n=n: e.tensor_tensor(out=Wg[ui][:, 0:n], in0=gU[ui][:, 0:n], in1=WT[:, 0:n, i1], op=ALU.mult),
                      reads=[t_gU[ui], t_WT], writes=[t_Wg[ui]])
                for dc in range(8):
                    abk = dc // 2
                    S.pe(lambda e, bi=bi, k2=k2, dc=dc, abk=abk, ui=ui, i1=i1, n=n: e.matmul(
                        B[abk][:, (dc % 2) * 256:(dc % 2) * 256 + n], lhsT=evb[bi][:, k2, dc * 128:(dc + 1) * 128], rhs=Wg[ui][:, 0:n],
                        start=(i1 == 0 and dc % 2 == 0), stop=(i1 == 127)), reads=[t_evb[bi], t_Wg[ui]], writes=[bt[abk]])

            for i1 in range(129):
                if i1 < 128:
                    emit_U(i1)
                if i1 >= 1:
                    emit_rest(i1 - 1)
                next(gen, None)

        def epilogue(gi, t0, n):
            samp = (gi == len(pgroups) - 1)
            x1g, t_x1 = x1g2[gi % 2], t_x1s[gi % 2]
            for dc in range(8):
                abk = dc // 2
                src = B[abk][:, (dc % 2) * 256:(dc % 2) * 256 + n]
                if not samp:
                    S.dve(lambda e, dc=dc, src=src, n=n: e.scalar_tensor_tensor(out=x1g[:, dc, 0:n], in0=src, scalar=modT[:, 40 + dc, 0:1], in1=x1g[:, dc, 0:n],
                                                                                 op0=ALU.mult, op1=ALU.add), reads=[bt[abk], t_mod, t_x1], writes=[t_x1])
                else:
                    S.dve(lambda e, dc=dc, src=src: e.tensor_tensor(out=tmpg[:, dc, 0:NS].rearrange("p (b t) -> p b t", t=4),
                                                                    in0=src.rearrange("p (b t) -> p b t", t=4),
                                                                    in1=bc(modT[:, 40 + dc, 1:17].unsqueeze(2), [128, SB, 4]), op=ALU.mult),
                          reads=[bt[abk], t_mod, t_tmp], writes=[t_tmp])
                    S.dve(lambda e, dc=dc: e.tensor_tensor(out=x1g[:, dc, 0:NS], in0=x1g[:, dc, 0:NS], in1=tmpg[:, dc, 0:NS], op=ALU.add),
                          reads=[t_tmp, t_x1], writes=[t_x1])
            for tt0 in range(0, n, 128):
                m = min(128, n - tt0)
                yi = 0
                for dc in range(8):
                    bk = 4 + dc // 4
                    S.pe(lambda e, dc=dc, bk=bk, tt0=tt0, m=m: e.transpose(B[bk][0:m, (dc % 4) * 128:(dc % 4 + 1) * 128], x1g[:, dc, tt0:tt0 + m], ident[:]),
                         reads=[t_x1, t_const], writes=[bt[bk]])
                for hf in range(2):
                    S.act(lambda e, hf=hf, yi=yi, m=m: e.activation(out=ytk[yi][0:m, hf * 512:(hf + 1) * 512], in_=B[4 + hf][0:m, :], func=AF.Copy),
                          reads=[bt[4 + hf]], writes=[t_ytk[yi]])
                S.dma(lambda e, yi=yi, t0=t0, tt0=tt0, m=m: e.dma_start(out=O["y"][t0 + tt0:t0 + tt0 + m, :], in_=ytk[yi][0:m, :]), reads=[t_ytk[yi]])

        for _ in prep_gen(0, *pgroups[0]):
            pass
        for gi, (t0, n) in enumerate(pgroups):
            wbuild(gi, t0, n)
            gen = prep_gen(gi + 1, *pgroups[gi + 1]) if gi + 1 < len(pgroups) else iter(())
            sweep(gi, t0, n, gen)
            for _ in gen:
                pass
            epilogue(gi, t0, n)
            ck(6.5 + 0.01 * gi)
    K.st = st
    S.emit()
    st.close()
    return nc


def host_prep(inp, core):
    f = np.float32
    xp = np.asarray(inp["x_prompt"], f)[core]
    xs = np.asarray(inp["x_sample"], f)[core * SB:(core + 1) * SB].reshape(NS, D)
    xT = np.ascontiguousarray(np.concatenate([xp, xs], 0).T)
    cvec = np.concatenate([np.asarray(inp["c_prompt"], f)[core:core + 1],
                           np.asarray(inp["c_sample"], f)[core * SB:(core + 1) * SB]], 0)
    cT = np.ascontiguousarray(cvec.reshape(17, 8, 128).transpose(2, 1, 0))
    m = {}
    m["xT"] = xT
    m["cT"] = cT
    bsl = slice(core * SB, (core + 1) * SB)
    m["ck_s"] = np.ascontiguousarray(np.asarray(inp["cache_k_win"], f)[0, bsl].reshape(SB, 2048, 512))
    m["cv_s"] = np.ascontiguousarray(np.asarray(inp["cache_v_win"], f)[0, bsl].reshape(SB, 2048, 512))
    m["swkv"] = np.ascontiguousarray(np.asarray(inp["state_wkv"], f)[0, bsl].reshape(128, 4096))
    sh = np.asarray(inp["state_shift"], f)[0, bsl]
    m["shT"] = np.ascontiguousarray(sh[:, 0:1536].reshape(SB, 3, 4, 128).transpose(3, 1, 2, 0))
    m["shTl"] = np.ascontiguousarray(sh[:, 1536:1664].T)
    return m


def _t5_bucket(dist):
    import math
    dist = np.asarray(dist, dtype=np.int64)
    max_exact = 16
    safe = np.maximum(dist, 1) / max_exact
    large = max_exact + (np.log(safe) / math.log(2048 / max_exact) * (32 - max_exact)).astype(np.int64)
    large = np.minimum(large, 31)
    return np.where(dist < max_exact, dist, large).astype(np.int32)


def _ohu_table():
    t = np.zeros((32, 3, 384), np.float32)
    for br, dil in enumerate((1, 4, 16)):
        j = np.arange(129)
        b = _t5_bucket(j * dil)
        t[b, br, j + 127] = 1.0
    return t


def host_shared(inp):
    f = np.float32
    m = {}
    m["ada_w"] = np.ascontiguousarray(np.asarray(inp["ada_w"], f)[0])
    m["ada_bT"] = np.ascontiguousarray(np.asarray(inp["ada_b"], f)[0].reshape(48, 128).T)
    m["n1gT"] = np.ascontiguousarray(np.asarray(inp["norm1_g"], f)[0].reshape(8, 128).T)
    m["n2gT"] = np.ascontiguousarray(np.asarray(inp["norm2_g"], f)[0].reshape(8, 128).T)
    m["w_in"] = np.ascontiguousarray(np.asarray(inp["w_in"], f)[0])
    qg = np.asarray(inp["q_norm_g"], f)[0]
    kg = np.asarray(inp["k_norm_g"], f)[0]
    m["qkg"] = np.ascontiguousarray(np.stack([np.tile(qg, 2), np.tile(kg, 2)], 1))
    m["ident"] = np.eye(128, dtype=f)
    m["ones"] = np.ones((128, 128), f)
    b2 = np.zeros((128, 128), f)
    b2[:64, :64] = 1
    b2[64:, 64:] = 1
    m["blk2"] = b2
    m["relb"] = np.ascontiguousarray(np.asarray(inp["rel_bias"], f))
    m["ohu"] = _ohu_table()

    def pf(v):
        return np.asarray(v, f).reshape(4, 128).T
    mu = np.asarray(inp["mu_shift"], f)[0]
    m["rwp"] = np.ascontiguousarray(np.stack([pf(mu[0:512]), pf(mu[512:1024]), pf(mu[1024:1536]), pf(inp["k_k"][0]), pf(inp["k_a"][0]),
                                              pf(np.asarray(inp["r_k"], f)[0].reshape(512)), pf(inp["a0"][0]), pf(inp["w0"][0])], 1))
    m["mul"] = np.ascontiguousarray(mu[1536:1664].reshape(128, 1))
    m["lw3"] = np.ascontiguousarray(np.concatenate([np.asarray(inp["w_w2"], f)[0], np.asarray(inp["w_a2"], f)[0], np.asarray(inp["w_g2"], f)[0]], 0))
    m["w0row"] = np.ascontiguousarray(np.asarray(inp["w0"], f)[0].reshape(1, 512))
    m["lnx"] = np.ascontiguousarray(np.stack([np.asarray(inp["lnx_g"], f)[0], np.asarray(inp["lnx_b"], f)[0]], 0))
    m["w_out"] = np.ascontiguousarray(np.asarray(inp["w_out"], f)[0])
    m["w_pq"] = np.ascontiguousarray(np.asarray(inp["w_peer_q"], f)[0])
    sk = np.asarray(inp["peer_sub_keys"], f)[0]
    m["skT"] = np.ascontiguousarray(sk.reshape(16, 128, 128).transpose(2, 0, 1))
    m["euT"] = np.ascontiguousarray(np.asarray(inp["expert_u"], f)[0].T)
    m["ev"] = np.ascontiguousarray(np.asarray(inp["expert_v"], f)[0])
    m["qkrow"] = np.ascontiguousarray(np.stack([qg, kg], 0))
    t_ = np.zeros((32, 3, 129), f)
    for br_, dil_ in enumerate((1, 4, 16)):
        jp = np.arange(129)
        t_[_t5_bucket((128 - jp) * dil_), br_, jp] = 1.0
    m["ohs"] = t_
    m["iotaR"] = np.ascontiguousarray(np.tile(np.arange(128, dtype=f)[None, :], (128, 1)))
    NEG = -0.6065306597126334
    i = np.arange(128)[:, None]
    t = np.arange(128)[None, :]
    same = (i // 64) == (t // 64)
    m["tri"] = np.ascontiguousarray(np.stack([np.where(same & (i <= t), NEG, 0.0), np.where(same & (i < t), NEG, 0.0)], 1).astype(f))
    ii = (np.arange(128) % 64)[:, None]
    tt = np.arange(64)[None, :]
    m["mk1"] = np.ascontiguousarray(np.stack([(tt > ii), (tt >= ii)], 1).astype(f))
    m["mk3"] = np.ascontiguousarray((tt < ii).astype(f))
    m["id2"] = np.ascontiguousarray((tt == ii).astype(f))
    return m


def kernel(**inp):
    nc = build()
    shared = host_shared(inp)
    in_maps = []
    for c in range(NCORES):
        m = dict(shared)
        m.update(host_prep(inp, c))
        in_maps.append(m)
    res = run_bass_kernel_spmd(nc, in_maps, core_ids=list(range(NCORES)))
    R = res.results
    f = np.float32
    y_p = np.stack([R[c]["y"][0:T] for c in range(NCORES)], 0).astype(f)
    y_s = np.concatenate([R[c]["y"][T:NT].reshape(SB, 4, D) for c in range(NCORES)], 0).astype(f)
    kwp = np.stack([R[c]["kwp"].reshape(2048, 8, 64) for c in range(NCORES)], 0)[None].astype(f)
    vwp = np.stack([R[c]["vwp"].reshape(2048, 8, 64) for c in range(NCORES)], 0)[None].astype(f)
    wkvp = np.stack([R[c]["wkvp"].reshape(2, 64, 4, 64).transpose(2, 0, 3, 1).reshape(8, 64, 64) for c in range(NCORES)], 0)[None].astype(f)
    shp = np.stack([R[c]["shp"].reshape(CR) for c in range(NCORES)], 0)[None].astype(f)
    kws = np.concatenate([R[c]["kws"].reshape(SB, 4, 8, 64) for c in range(NCORES)], 0)[None].astype(f)
    vws = np.concatenate([R[c]["vws"].reshape(SB, 4, 8, 64) for c in range(NCORES)], 0)[None].astype(f)
    wkvs = np.concatenate([R[c]["wkvs"].reshape(SB, 8, 64, 64) for c in range(NCORES)], 0)[None].astype(f)
    shs = np.concatenate([R[c]["shs"].reshape(SB, CR) for c in range(NCORES)], 0)[None].astype(f)
    return (y_p, y_s, kwp, vwp, wkvp, shp, kws, vws, wkvs, shs)
```
